# Optimizing a Trainium2 kernel written in Bass

```python
import jax, jax.numpy as jnp
from jax import lax
import numpy as np

D_MODEL = 1024
BATCH = 8
SEQ = 4096
DEPTH = 2

CTX_LEN = 256
GRID_W = 64
HEAD_DIM = 64
N_Q_HEADS = 8
N_KV_HEADS = 2
GQA_GROUP = N_Q_HEADS // N_KV_HEADS
Q_WIDTH = N_Q_HEADS * HEAD_DIM
KV_WIDTH = N_KV_HEADS * HEAD_DIM
CONV_WIDTH = D_MODEL - Q_WIDTH
CONV_KERNEL = 31
IN_COLS = Q_WIDTH + 2 * KV_WIDTH + 2 * CONV_WIDTH
Q_BLOCK = 128
ROPE_THETA = 10000.0
ATTN_SCALE = HEAD_DIM ** -0.5
PEER_HEADS = 8
PEER_KEYS = 128
PEER_N = PEER_KEYS * PEER_KEYS
PEER_QDIM = 128
PEER_HALF = PEER_QDIM // 2
PEER_TOPK = 16
PEER_CHUNK = 128
EPS = 1e-6

kernel_name = "hybrid_attn_conformer_peer_dit"


def rms(x):
    xf = x.astype(jnp.float32)
    return (xf * lax.rsqrt(jnp.mean(xf * xf, axis=-1, keepdims=True) + EPS)).astype(x.dtype)


def layer_norm(x, g, b):
    xf = x.astype(jnp.float32)
    mu = jnp.mean(xf, axis=-1, keepdims=True)
    var = jnp.mean(jnp.square(xf - mu), axis=-1, keepdims=True)
    return ((xf - mu) * lax.rsqrt(var + 1e-5)).astype(x.dtype) * g + b


def modulate(x, shift, scale):
    return rms(x) * (1 + scale) + shift


def rope_tables(row, col):
    n = HEAD_DIM // 4
    inv = ROPE_THETA ** (-jnp.arange(n, dtype=jnp.float32) / n)
    ang = jnp.concatenate([row.astype(jnp.float32)[:, None] * inv,
                           col.astype(jnp.float32)[:, None] * inv], axis=-1)
    return jnp.cos(ang), jnp.sin(ang)


def apply_rope(x, cos, sin):
    half = HEAD_DIM // 2
    x1, x2 = x[..., :half], x[..., half:]
    c = cos[None, :, None, :].astype(x.dtype)
    s = sin[None, :, None, :].astype(x.dtype)
    return jnp.concatenate([x1 * c - x2 * s, x1 * s + x2 * c], axis=-1)


def split_cols(p):
    return jnp.split(p, [Q_WIDTH, Q_WIDTH + KV_WIDTH, Q_WIDTH + 2 * KV_WIDTH], axis=-1)


def gqa_core(q, k, v):
    s = jnp.einsum('bqhgd,bkhd->bhgqk', q, k).astype(jnp.float32) * ATTN_SCALE
    p = jax.nn.softmax(s, axis=-1).astype(v.dtype)
    return jnp.einsum('bhgqk,bkhd->bqhgd', p, v)


def attend_latent(q, k_all, v_all):
    B, S = q.shape[:2]
    nb = S // Q_BLOCK
    qb = q.reshape(B, nb, Q_BLOCK, N_KV_HEADS, GQA_GROUP, HEAD_DIM).transpose(1, 0, 2, 3, 4, 5)
    o = lax.map(lambda qi: gqa_core(qi, k_all, v_all), qb)
    return o.transpose(1, 0, 2, 3, 4, 5).reshape(B, S, Q_WIDTH)


def attend_context(q, k, v):
    B, L = q.shape[:2]
    o = gqa_core(q.reshape(B, L, N_KV_HEADS, GQA_GROUP, HEAD_DIM), k, v)
    return o.reshape(B, L, Q_WIDTH)


def conformer_conv(u, conv_w, conv_b, ln_g, ln_b):
    a, gate = jnp.split(u, 2, axis=-1)
    g = a * jax.nn.sigmoid(gate)
    y = lax.conv_general_dilated(
        g, conv_w[:, None, :], window_strides=(1,),
        padding=[(CONV_KERNEL // 2, CONV_KERNEL // 2)],
        dimension_numbers=('NWC', 'WIO', 'NWC'),
        feature_group_count=CONV_WIDTH) + conv_b
    return jax.nn.silu(layer_norm(y, ln_g, ln_b))


def merge_groups(o_attn, o_conv, beta_attn, beta_conv, w_o):
    o = jnp.concatenate([rms(o_attn) * beta_attn, rms(o_conv) * beta_conv], axis=-1)
    return o @ w_o


def peer(h, w_pq, sub_keys, u_tab, v_tab):
    B, T, D = h.shape
    q = (h @ w_pq).reshape(B, T, PEER_HEADS, 2, PEER_HALF)
    s = jnp.einsum('bthcd,hckd->bthck', q, sub_keys).astype(jnp.float32)
    top_s, top_i = lax.top_k(s, PEER_TOPK)
    cand = top_s[..., 0, :, None] + top_s[..., 1, None, :]
    best_s, best_j = lax.top_k(cand.reshape(B, T, PEER_HEADS, PEER_TOPK * PEER_TOPK), PEER_TOPK)
    i1 = jnp.take_along_axis(top_i[..., 0, :], best_j // PEER_TOPK, axis=-1)
    i2 = jnp.take_along_axis(top_i[..., 1, :], best_j % PEER_TOPK, axis=-1)
    expert = i1 * PEER_KEYS + i2
    w = jax.nn.softmax(best_s, axis=-1)
    n_chunks = (B * T) // PEER_CHUNK
    hx = h.reshape(n_chunks, PEER_CHUNK, D)
    ex = expert.reshape(n_chunks, PEER_CHUNK, PEER_HEADS * PEER_TOPK)
    wx = w.reshape(n_chunks, PEER_CHUNK, PEER_HEADS * PEER_TOPK)

    def chunk(args):
        hc, ec, wc = args
        u = jnp.take(u_tab, ec, axis=0)
        act = jax.nn.gelu(jnp.einsum('cd,ced->ce', hc, u), approximate=False)
        v = jnp.take(v_tab, ec, axis=0)
        return jnp.einsum('ce,ced->cd', (act * wc).astype(h.dtype), v)

    return lax.map(chunk, (hx, ex, wx)).reshape(B, T, D)


def setup_inputs(seed: int = 0) -> dict:
    key = jax.random.key(seed)
    ks = jax.random.split(key, 22)
    D = D_MODEL

    def nrm(k, shape, s):
        return jax.random.normal(k, shape, jnp.float32) * s

    return {
        "x": nrm(ks[0], (BATCH, SEQ, D), 1.0),
        "c": nrm(ks[1], (BATCH, D), 1.0),
        "ctx": nrm(ks[2], (BATCH, CTX_LEN, D), 1.0),
        "c_ctx": nrm(ks[3], (D,), 1.0),
        "w_mod": nrm(ks[4], (DEPTH, D, 6 * D), 0.5 * D ** -0.5),
        "b_mod": nrm(ks[5], (DEPTH, 6 * D), 0.02),
        "w_in": nrm(ks[6], (DEPTH, D, IN_COLS), D ** -0.5),
        "q_gain": 1.0 + nrm(ks[7], (DEPTH, HEAD_DIM), 0.02),
        "k_gain": 1.0 + nrm(ks[8], (DEPTH, HEAD_DIM), 0.02),
        "conv_w": nrm(ks[9], (DEPTH, CONV_KERNEL, CONV_WIDTH), CONV_KERNEL ** -0.5),
        "conv_b": nrm(ks[10], (DEPTH, CONV_WIDTH), 0.02),
        "ln_g": 1.0 + nrm(ks[11], (DEPTH, CONV_WIDTH), 0.02),
        "ln_b": nrm(ks[12], (DEPTH, CONV_WIDTH), 0.02),
        "beta_attn": 1.0 + nrm(ks[13], (DEPTH, Q_WIDTH), 0.02),
        "beta_conv": 1.0 + nrm(ks[14], (DEPTH, CONV_WIDTH), 0.02),
        "w_o": nrm(ks[15], (DEPTH, D, D), D ** -0.5),
        "peer_wq": nrm(ks[16], (DEPTH, D, PEER_HEADS * PEER_QDIM), D ** -0.5),
        "peer_keys": nrm(ks[17], (DEPTH, PEER_HEADS, 2, PEER_KEYS, PEER_HALF), PEER_HALF ** -0.5),
        "peer_u": nrm(ks[18], (DEPTH, PEER_N, D), D ** -0.5),
        "peer_v": nrm(ks[19], (DEPTH, PEER_N, D), 0.5),
        "final_g": 1.0 + nrm(ks[20], (D,), 0.02),
    }


def reference(x, c, ctx, c_ctx, w_mod, b_mod, w_in, q_gain, k_gain, conv_w, conv_b, ln_g, ln_b,
              beta_attn, beta_conv, w_o, peer_wq, peer_keys, peer_u, peer_v, final_g):
    B, S, D = x.shape
    L = ctx.shape[1]
    ROWS = S // GRID_W
    row = jnp.repeat(jnp.arange(ROWS), GRID_W)
    col = jnp.tile(jnp.arange(GRID_W), ROWS)
    cos, sin = rope_tables(row, col)
    sc = jax.nn.silu(c)
    sc_ctx = jax.nn.silu(c_ctx)
    h_ctx = ctx
    for l in range(DEPTH):
        last = l == DEPTH - 1
        sh1, s1, g1, sh2, s2, g2 = jnp.split((sc @ w_mod[l] + b_mod[l])[:, None, :], 6, axis=-1)
        csh1, cs1, cg1, csh2, cs2, cg2 = jnp.split(sc_ctx @ w_mod[l] + b_mod[l], 6, axis=-1)

        hc = modulate(h_ctx, csh1, cs1)
        if last:
            kc, vc = jnp.split(hc @ w_in[l][:, Q_WIDTH:Q_WIDTH + 2 * KV_WIDTH], 2, axis=-1)
        else:
            qc, kc, vc, uc = split_cols(hc @ w_in[l])
            qc = rms(qc.reshape(B, L, N_Q_HEADS, HEAD_DIM)) * q_gain[l]
        kc = rms(kc.reshape(B, L, N_KV_HEADS, HEAD_DIM)) * k_gain[l]
        vc = vc.reshape(B, L, N_KV_HEADS, HEAD_DIM)

        hx = modulate(x, sh1, s1)
        q, k, v, u = split_cols(hx @ w_in[l])
        q = apply_rope(rms(q.reshape(B, S, N_Q_HEADS, HEAD_DIM)) * q_gain[l], cos, sin)
        k = apply_rope(rms(k.reshape(B, S, N_KV_HEADS, HEAD_DIM)) * k_gain[l], cos, sin)
        v = v.reshape(B, S, N_KV_HEADS, HEAD_DIM)
        o_attn = attend_latent(q, jnp.concatenate([kc, k], axis=1), jnp.concatenate([vc, v], axis=1))
        o_conv = conformer_conv(u, conv_w[l], conv_b[l], ln_g[l], ln_b[l])
        x = x + g1 * merge_groups(o_attn, o_conv, beta_attn[l], beta_conv[l], w_o[l])

        x = x + g2 * peer(modulate(x, sh2, s2), peer_wq[l], peer_keys[l], peer_u[l], peer_v[l])

        if not last:
            oc_attn = attend_context(qc, kc, vc)
            oc_conv = conformer_conv(uc, conv_w[l], conv_b[l], ln_g[l], ln_b[l])
            h_ctx = h_ctx + cg1 * merge_groups(oc_attn, oc_conv, beta_attn[l], beta_conv[l], w_o[l])
            h_ctx = h_ctx + cg2 * peer(modulate(h_ctx, csh2, cs2), peer_wq[l], peer_keys[l], peer_u[l], peer_v[l])

    return rms(x) * final_g
```

```python
import numpy as np
from contextlib import ExitStack
import concourse.bass as bass
import concourse.mybir as mybir
from concourse.bass_utils import run_bass_kernel_spmd

F32 = mybir.dt.float32
BF16 = mybir.dt.bfloat16
I32 = mybir.dt.int32
U32 = mybir.dt.uint32
ALU = mybir.AluOpType
AF = mybir.ActivationFunctionType
AX = mybir.AxisListType

import os
CUT = int(os.environ.get("KCUT", "99"))
SEM_LIMIT = 30000


class Buf:
    __slots__ = ("name", "w", "r")

    def __init__(self, name):
        self.name = name
        self.w = None
        self.r = {}


class Eng:
    def __init__(self, sync, name, eng):
        self.sync = sync
        self.name = name
        self.eng = eng
        self.sem = None
        self.count = 0
        self.known = {}
        self.last = None

    def new_event(self):
        if self.sem is None or self.count >= SEM_LIMIT:
            self.sem = self.sync.new_sem(self.name)
            self.count = 0
        self.count += 1
        self.last = [self.sem, self.count, self.name]
        return self.last


def DmaSlot(sync, name, group=False):
    if name not in sync.slot_by_name:
        sync.slot_by_name[name] = _DmaSlot(sync, name, group)
    return sync.slot_by_name[name]


class _DmaSlot:
    def __init__(self, sync, name, group=False):
        self.name = name
        self.sem = None
        self.val = 0
        self.group = group
        self.pending = []
        sync.slots.append(self)

    def bump(self, sync):
        if self.sem is None or (self.val >= SEM_LIMIT and not self.pending):
            self.sem = sync.new_sem("d" + self.name)
            self.val = 0
        self.val += 16
        ev = [self.sem, self.val, "dma"]
        if self.group:
            self.pending.append(ev)
        return ev

    def close(self):
        for ev in self.pending:
            ev[1] = self.val
        self.pending = []


class Sync:
    def __init__(self, nc, stack):
        self.nc = nc
        self.stack = stack
        self.nsem = 0
        self.slots = []
        self.slot_by_name = {}
        self.E = {
            "pe": Eng(self, "pe", nc.tensor),
            "dve": Eng(self, "dve", nc.vector),
            "act": Eng(self, "act", nc.scalar),
            "pool": Eng(self, "pool", nc.gpsimd),
            "sp": Eng(self, "sp", nc.sync),
        }
        self.ninstr = 0
        self.nwait = 0

    def new_sem(self, name):
        self.nsem += 1
        return self.stack.enter_context(self.nc.semaphore(f"s{self.nsem}_{name}"))

    def _wait(self, E, evs):
        best = {}
        for ev in evs:
            if ev is None:
                continue
            if E.name == "pe" and ev[2] == "pe":
                continue
            k = id(ev[0])
            if k not in best or best[k][1] < ev[1]:
                best[k] = ev
        for ev in best.values():
            sem, val, src = ev
            if E.known.get(id(sem), 0) >= val:
                continue
            E.eng.wait_ge(sem, val)
            self.nwait += 1
            E.known[id(sem)] = val

    def _deps(self, E, reads, writes):
        evs = []
        for b in reads:
            evs.append(b.w)
        for b in writes:
            if b.w is not None and b.w[2] != E.name:
                evs.append(b.w)
            for ev in b.r.values():
                if ev[2] == E.name:
                    continue
                evs.append(ev)
        return evs

    @staticmethod
    def _bufs(lst):
        return [t.b if hasattr(t, "b") else t for t in lst]

    def op(self, engname, fn, r=(), w=()):
        E = self.E[engname]
        r = self._bufs(r)
        w = self._bufs(w)
        self._wait(E, self._deps(E, r, w))
        ins = fn(E.eng)
        ev = E.new_event()
        ins.then_inc(ev[0], 1)
        self.ninstr += 1
        key = ev[2] if ev[2] != "dma" else id(ev[0])
        for b in r:
            b.r[key] = ev
        for b in w:
            b.w = ev
            b.r = {}
        return ev

    def dma(self, qname, fn, slot, r=(), w=()):
        E = self.E[qname]
        r = self._bufs(r)
        w = self._bufs(w)
        self._wait(E, self._deps(E, r, w))
        ins = fn(E.eng)
        ev = slot.bump(self)
        ins.then_inc(ev[0], 16)
        self.ninstr += 1
        key = ev[2] if ev[2] != "dma" else id(ev[0])
        for b in r:
            b.r[key] = ev
        for b in w:
            b.w = ev
            b.r = {}
        return ev

    def barrier(self):
        for s in self.slots:
            if s.group:
                s.close()
        evs = []
        for E in self.E.values():
            if E.last is not None:
                evs.append(E.last)
        for s in self.slots:
            if s.sem is not None:
                evs.append([s.sem, s.val, "dma"])
        for E in self.E.values():
            self._wait(E, [ev for ev in evs if ev[2] != E.name or ev[2] == "dma"])


class Tile:
    def __init__(self, h, name):
        self.h = h
        self.b = Buf(name)

    def __getitem__(self, k):
        return self.h[k]


P = 128
D = 1024
S_LAT = 4096
S_CTX = 256
NTL = 32
NTC = 2
NT = NTL + NTC
DEPTH = 2
INC = 1792
EPS = 1e-6
NPEER = 16384
GPAD = 16
NSLOT = 8


def build(depth=DEPTH, dbg=False, stop=None):
    nc = bass.Bass("TRN2", target_bir_lowering=False)

    def din(name, shape, dt=F32):
        return nc.dram_tensor(name, shape, dt, kind="ExternalInput").ap()

    x_d = din("x", [S_LAT, D])
    ctx_d = din("ctx", [S_CTX, D])
    cT_d = din("cT", [P, 8])
    cctxT_d = din("cctxT", [P, 8])
    w_mod_d = din("w_mod", [DEPTH, D, 6 * D])
    b_mod_d = din("b_mod", [DEPTH, 6 * D])
    w_in_d = din("w_in", [DEPTH, D, INC])
    q_gain_d = din("q_gain", [DEPTH, 64])
    k_gain_d = din("k_gain", [DEPTH, 64])
    conv_w_d = din("conv_w", [DEPTH, 31, 512])
    conv_b_d = din("conv_b", [DEPTH, 512])
    ln_g_d = din("ln_g", [DEPTH, 512])
    ln_b_d = din("ln_b", [DEPTH, 512])
    beta_attn_d = din("beta_attn", [DEPTH, 512])
    beta_conv_d = din("beta_conv", [DEPTH, 512])
    w_o_d = din("w_o", [DEPTH, D, D])
    peer_wq_d = din("peer_wq", [DEPTH, D, D])
    peer_keys_d = din("peer_keys", [DEPTH, 8, 2, 128, 64])
    peer_u_d = din("peer_u", [DEPTH, NPEER, D])
    peer_v_d = din("peer_v", [DEPTH, NPEER, D])
    final_g_d = din("final_g", [1, D])
    cos2_d = din("cos2", [S_LAT, 64])
    sin2_d = din("sin2", [S_LAT, 64])
    peer_u_flat = peer_u_d.rearrange("l n d -> (l n) d")
    peer_v_flat = peer_v_d.rearrange("l n d -> (l n) d")
    out_d = nc.dram_tensor("out", [S_LAT, D], F32, kind="ExternalOutput").ap()
    xs_d = nc.dram_tensor("xs", [S_LAT + S_CTX, D], F32).ap()
    dbg_d = {}
    if dbg:
        for nm, shp in [("d_qkvu", [NT * P, INC]), ("d_x1", [NT * P, D]), ("d_oT", [P, 4 * S_LAT]),
                        ("d_pre", [NT * P, 128]), ("d_eidx", [NT * P, 128]), ("d_w", [NT * P, 128]),
                        ("d_qT", [P, 4 * S_LAT]), ("d_x2", [NT * P, D]), ("d_mod", [P, 4096])]:
            dbg_d[nm] = nc.dram_tensor(nm, shp, F32, kind="ExternalOutput").ap()

    top = ExitStack()
    with top:
        S = Sync(nc, top)

        uid = [0]

        def sbt(stack, name, shape, dt):
            uid[0] += 1
            name = f"{name}_u{uid[0]}"
            return Tile(stack.enter_context(nc.sbuf_tensor(name, shape, dt)), name)

        def pst(stack, name, shape, dt):
            uid[0] += 1
            name = f"{name}_u{uid[0]}"
            return Tile(stack.enter_context(nc.psum_tensor(name, shape, dt)), name)

        out_slot = DmaSlot(S, "out", group=True)
        dbg_slots = {}
        xs_buf = [Buf(f"xs{i}") for i in range(NT)]

        ident_f = sbt(top, "ident_f", [P, P], F32)
        ident_b = sbt(top, "ident_b", [P, P], BF16)
        ones_f = sbt(top, "ones_f", [P, P], F32)
        io_t = sbt(top, "io_t", [P, P], F32)
        pid_t = sbt(top, "pid_t", [P, 1], F32)
        S.op("pool", lambda e: e.iota(io_t[:], pattern=[[1, P]], base=0, channel_multiplier=0,
                                      allow_small_or_imprecise_dtypes=True), w=[io_t])
        S.op("pool", lambda e: e.iota(pid_t[:], pattern=[[0, 1]], base=0, channel_multiplier=1,
                                      allow_small_or_imprecise_dtypes=True), w=[pid_t])
        S.op("dve", lambda e: e.tensor_scalar(ident_f[:], io_t[:], pid_t[:, 0:1], None, op0=ALU.is_equal),
             r=[io_t, pid_t], w=[ident_f])
        S.op("dve", lambda e: e.tensor_copy(ident_b[:], ident_f[:]), r=[ident_f], w=[ident_b])
        S.op("pool", lambda e: e.memset(ones_f[:], 1.0), w=[ones_f])

        cslot = DmaSlot(S, "const", group=True)
        craw = sbt(top, "craw", [P, 2, 8], F32)
        scT = sbt(top, "scT", [P, 8, 2], F32)
        S.dma("sp", lambda e: e.dma_start(out=craw[:, 0, :], in_=cT_d), cslot, w=[craw])
        S.dma("sp", lambda e: e.dma_start(out=craw[:, 1, :], in_=cctxT_d), cslot, w=[craw])
        cslot.close()
        S.op("act", lambda e: e.activation(scT[:].rearrange("p j r -> p r j"), craw[:], AF.Silu), r=[craw], w=[scT])
        modcol = sbt(top, "modcol", [P, 2, 16], F32)
        eps6 = sbt(top, "eps6", [P, 1], F32)
        eps5 = sbt(top, "eps5", [P, 1], F32)
        S.op("pool", lambda e: e.memset(eps6[:], 1e-6), w=[eps6])
        S.op("pool", lambda e: e.memset(eps5[:], 1e-5), w=[eps5])


        def rstd_from_ss(ss, rs, scale, eps_t):
            S.op("act", lambda e: e.activation(rs[:], ss[:], AF.Sqrt, scale=scale, bias=eps_t[:, 0:1]), r=[ss, eps_t], w=[rs])
            S.op("dve", lambda e: e.reciprocal(rs[:], rs[:]), r=[rs], w=[rs])

        def dbg_store(name, rows, tile_ap, rtiles):
            if dbg and name in dbg_d:
                if name not in dbg_slots:
                    dbg_slots[name] = (DmaSlot(S, name), Buf(name))
                S.dma("sp", lambda e: e.dma_start(out=dbg_d[name][rows], in_=tile_ap), dbg_slots[name][0], r=rtiles, w=[dbg_slots[name][1]])

        def mod_cols(l):
            with ExitStack() as ph:
                wm = [sbt(ph, f"wm{k}", [P, 8, P], F32) for k in range(2)]
                wslot = [DmaSlot(S, f"wm{k}") for k in range(2)]
                bcol = sbt(ph, "bcol", [P, 16], F32)
                pcol = pst(ph, "pcol", [P, 16, 2], F32)
                S.dma("sp", lambda e: e.dma_start(out=bcol[:], in_=b_mod_d[l, 0:2048].rearrange("(c p) -> p c", p=P),
                                                  allow_slow_non_contiguous=True), DmaSlot(S, "bcol"), w=[bcol])
                for cc in range(16):
                    t = wm[cc % 2]
                    S.dma("sp", lambda e, t=t, cc=cc: e.dma_start(
                        out=t[:], in_=w_mod_d[l, :, cc * P:(cc + 1) * P].rearrange("(j p) m -> p j m", p=P)),
                          wslot[cc % 2], w=[t])
                    for j in range(8):
                        S.op("pe", lambda e, t=t, j=j, cc=cc: e.matmul(pcol[:, cc, :], lhsT=t[:, j, :],
                                                                     rhs=scT[:, j, :], start=(j == 0), stop=(j == 7)),
                             r=[t, scT], w=[pcol])
                S.op("dve", lambda e: e.tensor_tensor(modcol[:].rearrange("p r c -> p c r"), pcol[:],
                                                      bcol[:].unsqueeze(2).to_broadcast([P, 16, 2]), op=ALU.add),
                     r=[pcol, bcol], w=[modcol])
                S.op("dve", lambda e: e.tensor_scalar(modcol[:, :, 8:16], modcol[:, :, 8:16], 1.0, None, op0=ALU.add),
                     r=[modcol], w=[modcol])
                S.barrier()

        def mod_bc(l, r, modbc):
            with ExitStack() as ph:
                NW = 4
                wm = [sbt(ph, f"wmb{k}", [P, 512], F32) for k in range(NW)]
                wslot = [DmaSlot(S, f"wmb{k}") for k in range(NW)]
                bbc = [sbt(ph, f"bbc{k}", [P, 512], F32) for k in range(2)]
                bslot = [DmaSlot(S, f"bbc{k}") for k in range(2)]
                screp = sbt(ph, "screp", [P, 8, P], F32)
                pb = pst(ph, "pbm", [P, 512], F32)
                S.op("dve", lambda e: e.tensor_copy(screp[:], scT[:, :, r].unsqueeze(2).to_broadcast([P, 8, P])),
                     r=[scT], w=[screp])
                n = 0
                for cc in range(8):
                    c0 = 2048 + cc * 512
                    S.dma("sp", lambda e, cc=cc, c0=c0: e.dma_start(out=bbc[cc % 2][:], in_=b_mod_d[l, c0:c0 + 512].partition_broadcast(P)),
                          bslot[cc % 2], w=[bbc[cc % 2]])
                    for j in range(8):
                        t = wm[n % NW]
                        S.dma("sp", lambda e, t=t, j=j, c0=c0: e.dma_start(out=t[:], in_=w_mod_d[l, j * P:(j + 1) * P, c0:c0 + 512]),
                              wslot[n % NW], w=[t])
                        n += 1
                        S.op("pe", lambda e, t=t, j=j: e.matmul(pb[:], lhsT=screp[:, j, :], rhs=t[:], start=(j == 0), stop=(j == 7)),
                             r=[t, screp], w=[pb])
                    S.op("dve", lambda e, cc=cc: e.tensor_tensor(modbc[:, cc * 512:(cc + 1) * 512], pb[:], bbc[cc % 2][:], op=ALU.add),
                         r=[pb, bbc[cc % 2]], w=[modbc])
                S.op("dve", lambda e: e.tensor_scalar(modbc[:, 2048:3072], modbc[:, 2048:3072], 1.0, None, op0=ALU.add),
                     r=[modbc], w=[modbc])
                S.barrier()

        def phase_a(l, M, src_aps, last):
            with ExitStack() as ph:
                win = sbt(ph, "win", [P, 8, INC], BF16)
                stg = sbt(ph, "stg", [P, INC], F32)
                stg_slot = DmaSlot(S, "stg")
                bias_bc = sbt(ph, "bias_bc", [P, INC], F32)
                sh1rep = sbt(ph, "sh1rep", [P, 2, 8, P], BF16)
                gain = sbt(ph, "gain", [P, 640], F32)
                graw = sbt(ph, "graw", [P, 128], F32)
                cos2 = sbt(ph, "cos2", [P, NTL, 64], F32)
                sin2 = sbt(ph, "sin2", [P, NTL, 64], F32)
                xt = [sbt(ph, f"xt{k}", [P, D], F32) for k in range(2)]
                xt_slot = [DmaSlot(S, f"xt{k}") for k in range(2)]
                xb = [sbt(ph, f"xb{k}", [P, D], BF16) for k in range(2)]
                xT = [sbt(ph, f"xT{k}", [P, 8, P], BF16) for k in range(2)]
                junk = sbt(ph, "junkA", [P, D], BF16)
                ss = [sbt(ph, f"ssA{k}", [P, 1], F32) for k in range(2)]
                rstd = [sbt(ph, f"rstdA{k}", [P, 1], F32) for k in range(2)]
                qkvu = sbt(ph, "qkvu", [P, INC], F32)
                sq = sbt(ph, "sq", [P, 640], F32)
                ssq = sbt(ph, "ssq", [P, 10], F32)
                rsq = sbt(ph, "rsq", [P, 10], F32)
                qn = sbt(ph, "qn", [P, 640], F32)
                rb = sbt(ph, "rb", [P, 640], F32)
                qr = sbt(ph, "qr", [P, 4, 2, 64], BF16)
                kr = sbt(ph, "kr", [P, 128], BF16)
                sig = sbt(ph, "sig", [P, 512], F32)
                gg = sbt(ph, "gg", [P, 512], BF16)
                ptb = pst(ph, "ptb", [P, 8, P], BF16)
                pq = [pst(ph, f"pq{k}", [P, 512], F32) for k in range(4)]
                ptq = pst(ph, "ptq", [P, 5, P], BF16)
                ptg = pst(ph, "ptg", [P, 4, P], BF16)
                ra = sq
                qT, qTc, kT, Vp, gT, gTc = M["qT"], M["qTc"], M["kT"], M["Vp"], M["gT"], M["gTc"]

                cslot2 = DmaSlot(S, "rope", group=True)
                S.dma("sp", lambda e: e.dma_start(out=cos2[:], in_=cos2_d.rearrange("(n p) f -> p n f", p=P)), cslot2, w=[cos2])
                S.dma("sp", lambda e: e.dma_start(out=sin2[:], in_=sin2_d.rearrange("(n p) f -> p n f", p=P)), cslot2, w=[sin2])
                S.dma("sp", lambda e: e.dma_start(out=graw[:, 0:64], in_=q_gain_d[l].partition_broadcast(P)), cslot2, w=[graw])
                S.dma("sp", lambda e: e.dma_start(out=graw[:, 64:128], in_=k_gain_d[l].partition_broadcast(P)), cslot2, w=[graw])
                cslot2.close()
                for j in range(8):
                    S.dma("sp", lambda e, j=j: e.dma_start(out=stg[:], in_=w_in_d[l, j * P:(j + 1) * P, :]), stg_slot, w=[stg])
                    S.op("act", lambda e, j=j: e.activation(win[:, j, :], stg[:], AF.Copy), r=[stg], w=[win])
                S.op("dve", lambda e: e.tensor_copy(gain[:, 0:512].rearrange("p (h d) -> p h d", h=8),
                                                    graw[:, 0:64].unsqueeze(1).to_broadcast([P, 8, 64])), r=[graw], w=[gain])
                S.op("dve", lambda e: e.tensor_copy(gain[:, 512:640].rearrange("p (h d) -> p h d", h=2),
                                                    graw[:, 64:128].unsqueeze(1).to_broadcast([P, 2, 64])), r=[graw], w=[gain])
                S.op("dve", lambda e: e.tensor_copy(sh1rep[:], modcol[:, :, 0:8].unsqueeze(3).to_broadcast([P, 2, 8, P])),
                     r=[modcol], w=[sh1rep])

                def make_bias(r_):
                    for cc in range(4):
                        w_ = min(512, INC - cc * 512)
                        for j in range(8):
                            S.op("pe", lambda e, cc=cc, j=j, w_=w_: e.matmul(
                                pq[cc][:, 0:w_], lhsT=sh1rep[:, r_, j, :], rhs=win[:, j, cc * 512:cc * 512 + w_],
                                start=(j == 0), stop=(j == 7)), r=[sh1rep, win], w=[pq[cc]])
                        S.op("act", lambda e, cc=cc, w_=w_: e.activation(
                            bias_bc[:, cc * 512:cc * 512 + w_], pq[cc][:, 0:w_], AF.Copy), r=[pq[cc]], w=[bias_bc])

                def load(i):
                    k = i % 2
                    S.dma("sp", lambda e: e.dma_start(out=xt[k][:], in_=src_aps[i]), xt_slot[k], r=[xs_buf[i]], w=[xt[k]])

                make_bias(0)
                load(0)
                for i in range(NT if stop not in ("a0", "a1") else (0 if stop == "a0" else 1)):
                    k = i % 2
                    r_ = 0 if i < NTL else 1
                    is_ctx = i >= NTL
                    if i == NTL:
                        make_bias(1)
                    if i + 1 < NT:
                        load(i + 1)
                    S.op("act", lambda e: e.activation(junk[:], xt[k][:], AF.Square, accum_out=ss[k][:]), r=[xt[k]], w=[junk, ss[k]])
                    S.op("act", lambda e: e.activation(xb[k][:], xt[k][:], AF.Copy), r=[xt[k]], w=[xb[k]])
                    rstd_from_ss(ss[k], rstd[k], 1.0 / D, eps6)
                    for j in range(8):
                        S.op("pe", lambda e, j=j: e.transpose(ptb[:, j, :], xb[k][:, j * P:(j + 1) * P], ident_b[:]),
                             r=[xb[k], ident_b], w=[ptb])
                    S.op("dve", lambda e: e.tensor_tensor(xT[k][:], ptb[:],
                                                          modcol[:, r_, 8:16].unsqueeze(2).to_broadcast([P, 8, P]), op=ALU.mult),
                         r=[ptb, modcol], w=[xT[k]])
                    if CUT <= 1:
                        continue
                    for cc in range(4):
                        w_ = min(512, INC - cc * 512)
                        for j in range(8):
                            S.op("pe", lambda e, cc=cc, j=j, w_=w_: e.matmul(
                                pq[cc][:, 0:w_], lhsT=xT[k][:, j, :], rhs=win[:, j, cc * 512:cc * 512 + w_],
                                start=(j == 0), stop=(j == 7)), r=[xT[k], win], w=[pq[cc]])
                        S.op("dve", lambda e, cc=cc, w_=w_: e.scalar_tensor_tensor(
                            out=qkvu[:, cc * 512:cc * 512 + w_], in0=pq[cc][:, 0:w_], scalar=rstd[k][:, 0:1],
                            in1=bias_bc[:, cc * 512:cc * 512 + w_], op0=ALU.mult, op1=ALU.add),
                             r=[pq[cc], rstd[k], bias_bc], w=[qkvu])
                    if l == 0:
                        dbg_store("d_qkvu", slice(i * P, (i + 1) * P), qkvu[:], [qkvu])
                    if CUT <= 2:
                        continue
                    S.op("pool", lambda e: e.tensor_tensor(sq[:], qkvu[:, 0:640], qkvu[:, 0:640], op=ALU.mult), r=[qkvu], w=[sq])
                    S.op("dve", lambda e: e.tensor_reduce(out=ssq[:], in_=sq[:].rearrange("p (h d) -> p h d", h=10),
                                                          axis=AX.X, op=ALU.add), r=[sq], w=[ssq])
                    rstd_from_ss(ssq, rsq, 1.0 / 64, eps6)
                    S.op("dve", lambda e: e.tensor_tensor(qn[:].rearrange("p (h d) -> p h d", h=10),
                                                          qkvu[:, 0:640].rearrange("p (h d) -> p h d", h=10),
                                                          rsq[:].unsqueeze(2).to_broadcast([P, 10, 64]), op=ALU.mult),
                         r=[qkvu, rsq], w=[qn])
                    S.op("pool", lambda e: e.tensor_tensor(qn[:], qn[:], gain[:], op=ALU.mult), r=[qn, gain], w=[qn])
                    if CUT <= 3:
                        continue
                    if not is_ctx:
                        qn3 = qn[:].rearrange("p (h d) -> p h d", h=10)
                        rb3 = rb[:].rearrange("p (h d) -> p h d", h=10)
                        S.op("dve", lambda e: e.tensor_tensor(ra[:].rearrange("p (h d) -> p h d", h=10), qn3,
                                                              cos2[:, i, :].unsqueeze(1).to_broadcast([P, 10, 64]), op=ALU.mult),
                             r=[qn, cos2], w=[ra])
                        S.op("pool", lambda e: e.tensor_tensor(rb3[:, :, 0:32], qn3[:, :, 32:64],
                                                               sin2[:, i, 0:32].unsqueeze(1).to_broadcast([P, 10, 32]), op=ALU.mult),
                             r=[qn, sin2], w=[rb])
                        S.op("pool", lambda e: e.tensor_tensor(rb3[:, :, 32:64], qn3[:, :, 0:32],
                                                               sin2[:, i, 32:64].unsqueeze(1).to_broadcast([P, 10, 32]), op=ALU.mult),
                             r=[qn, sin2], w=[rb])
                        S.op("dve", lambda e: e.tensor_tensor(qr[:].rearrange("p pr hh d -> p hh pr d"),
                                                              ra[:, 0:512].rearrange("p (hh pr d) -> p hh pr d", hh=2, pr=4),
                                                              rb[:, 0:512].rearrange("p (hh pr d) -> p hh pr d", hh=2, pr=4), op=ALU.add),
                             r=[ra, rb], w=[qr])
                        S.op("pool", lambda e: e.tensor_tensor(kr[:], ra[:, 512:640], rb[:, 512:640], op=ALU.add), r=[ra, rb], w=[kr])
                    else:
                        S.op("dve", lambda e: e.tensor_copy(qr[:].rearrange("p pr hh d -> p hh pr d"),
                                                            qn[:, 0:512].rearrange("p (hh pr d) -> p hh pr d", hh=2, pr=4)),
                             r=[qn], w=[qr])
                        S.op("pool", lambda e: e.tensor_copy(kr[:], qn[:, 512:640]), r=[qn], w=[kr])
                    if CUT <= 4:
                        continue
                    need_q = (not is_ctx) or (not last)
                    if need_q:
                        for pr in range(4):
                            S.op("pe", lambda e, pr=pr: e.transpose(ptq[:, pr, :], qr[:, pr, :, :].rearrange("p hh d -> p (hh d)"), ident_b[:]),
                                 r=[qr, ident_b], w=[ptq])
                    S.op("pe", lambda e: e.transpose(ptq[:, 4, :], kr[:], ident_b[:]), r=[kr, ident_b], w=[ptq])
                    if need_q:
                        if not is_ctx:
                            S.op("act", lambda e: e.activation(qT[:, :, i * P:(i + 1) * P], ptq[:, 0:4, :], AF.Copy),
                                 r=[ptq], w=[M["qT_b"][pr][i // 4] for pr in range(4)])
                        else:
                            S.op("act", lambda e: e.activation(qTc[:, :, (i - NTL) * P:(i - NTL + 1) * P], ptq[:, 0:4, :], AF.Copy),
                                 r=[ptq], w=[qTc])
                    S.op("act", lambda e: e.activation(kT[:, i * P:(i + 1) * P], ptq[:, 4, :], AF.Copy), r=[ptq], w=[M["kT_b"][i]])
                    if CUT <= 5:
                        continue
                    S.op("pool", lambda e: e.tensor_copy(Vp[:, i, 0:64], qkvu[:, 640:704]), r=[qkvu], w=[M["Vp_b"][i]])
                    S.op("pool", lambda e: e.tensor_copy(Vp[:, i, 128:192], qkvu[:, 704:768]), r=[qkvu], w=[M["Vp_b"][i]])
                    if CUT <= 6:
                        continue
                    if need_q:
                        S.op("act", lambda e: e.activation(sig[:], qkvu[:, 1280:1792], AF.Sigmoid), r=[qkvu], w=[sig])
                        S.op("dve", lambda e: e.tensor_tensor(gg[:], qkvu[:, 768:1280], sig[:], op=ALU.mult), r=[qkvu, sig], w=[gg])
                        for c in range(4):
                            S.op("pe", lambda e, c=c: e.transpose(ptg[:, c, :], gg[:, c * P:(c + 1) * P], ident_b[:]),
                                 r=[gg, ident_b], w=[ptg])
                        if not is_ctx:
                            S.op("act", lambda e: e.activation(gT[:, :, GPAD + i * P:GPAD + (i + 1) * P], ptg[:], AF.Copy),
                                 r=[ptg], w=[M["gT_b"][i]])
                        else:
                            S.op("act", lambda e: e.activation(gTc[:, :, GPAD + (i - NTL) * P:GPAD + (i - NTL + 1) * P], ptg[:], AF.Copy),
                                 r=[ptg], w=[M["gTc_b"][i - NTL]])
                S.barrier()

        def phase_b(l, M, last):
            with ExitStack() as ph:
                NPT = 4
                pT = [sbt(ph, f"pT{k}", [P, 512], BF16) for k in range(NPT)]
                rd = sbt(ph, "rd", [P, 512], F32)
                bcs = sbt(ph, "bcs", [P, 512], F32)
                ps_s = [pst(ph, f"ps_s{k}", [P, 512], F32) for k in range(4)]
                po = [pst(ph, f"po{k}", [P, 512], F32) for k in range(2)]
                pbc = pst(ph, "pbc", [P, 512], F32)
                qT, qTc, kT, Vp = M["qT"], M["qTc"], M["kT"], M["Vp"]
                cnt = [0]

                def attend(qsrc_fn, qbuf, n, ktiles):
                    for kt_i, kt in enumerate(ktiles):
                        for hh in range(2):
                            k = cnt[0] % 4
                            kp = cnt[0] % NPT
                            cnt[0] += 1
                            S.op("pe", lambda e, hh=hh, kt=kt, k=k: e.matmul(
                                ps_s[k][:, 0:n], lhsT=kT[hh * 64:(hh + 1) * 64, kt * P:(kt + 1) * P], rhs=qsrc_fn(hh),
                                start=True, stop=True), r=[M["kT_b"][kt], qbuf], w=[ps_s[k]])
                            S.op("act", lambda e, k=k, kp=kp: e.activation(pT[kp][:, 0:n], ps_s[k][:, 0:n], AF.Exp, scale=0.125),
                                 r=[ps_s[k]], w=[pT[kp]])
                            S.op("pe", lambda e, hh=hh, kt=kt, kp=kp, kt_i=kt_i: e.matmul(
                                po[hh][:, 0:n], lhsT=Vp[:, kt, hh * 64:hh * 64 + 128], rhs=pT[kp][:, 0:n],
                                start=(kt_i == 0), stop=(kt_i == len(ktiles) - 1)), r=[M["Vp_b"][kt], pT[kp]], w=[po[hh]])
                    for hh in range(2):
                        dp = 64 if hh == 0 else 0
                        S.op("dve", lambda e, hh=hh, dp=dp: e.reciprocal(rd[dp:dp + 1, 0:n], po[hh][dp:dp + 1, 0:n]),
                             r=[po[hh]], w=[rd])
                        S.op("pe", lambda e, dp=dp: e.matmul(pbc[0:64, 0:n], lhsT=ones_f[dp:dp + 1, 0:64], rhs=rd[dp:dp + 1, 0:n],
                                                            start=True, stop=True), r=[ones_f, rd], w=[pbc])
                        S.op("act", lambda e, hh=hh: e.activation(bcs[hh * 64:(hh + 1) * 64, 0:n], pbc[0:64, 0:n], AF.Copy),
                             r=[pbc], w=[bcs])
                        S.op("dve", lambda e, hh=hh: e.tensor_tensor(qsrc_fn(hh), po[hh][hh * 64:(hh + 1) * 64, 0:n],
                                                                     bcs[hh * 64:(hh + 1) * 64, 0:n], op=ALU.mult),
                             r=[po[hh], bcs], w=[qbuf])

                for c in range(8):
                    for pr in range(4):
                        attend(lambda hh, c=c, pr=pr: qT[hh * 64:(hh + 1) * 64, pr, c * 512:(c + 1) * 512],
                               M["qT_b"][pr][c], 512, list(range(NT)))
                if not last:
                    for pr in range(4):
                        attend(lambda hh, pr=pr: qTc[hh * 64:(hh + 1) * 64, pr, :], qTc.b, S_CTX, [NTL, NTL + 1])
                S.barrier()

        def phase_c(l, M, tiles, src_aps, r_, modbc):
            with ExitStack() as ph:
                wcol = sbt(ph, "wcol", [P, 4, 31], F32)
                DG = sbt(ph, "DG", [P, 4, 31, P], BF16)
                wo = sbt(ph, "wo", [P, 8, D], BF16)
                stg = [sbt(ph, f"stgC{k}", [P, D], F32) for k in range(2)]
                stg_slot = [DmaSlot(S, f"stgC{k}") for k in range(2)]
                betaA = sbt(ph, "betaA", [P, 8], F32)
                betac = sbt(ph, "betac", [P, 8], F32)
                cb_bc = sbt(ph, "cb_bc", [P, 512], F32)
                lg_bc = sbt(ph, "lg_bc", [P, 512], F32)
                lb_bc = sbt(ph, "lb_bc", [P, 512], F32)
                xt = [sbt(ph, f"xtC{k}", [P, D], F32) for k in range(2)]
                xt_slot = [DmaSlot(S, f"xtC{k}") for k in range(2)]
                y = sbt(ph, "yC", [P, 512], F32)
                junk = sbt(ph, "junkC", [P, 512], F32)
                st4 = sbt(ph, "st4", [P, 4], F32)
                rln = sbt(ph, "rln", [P, 1], F32)
                msq = sbt(ph, "msq", [P, 1], F32)
                z = sbt(ph, "zC", [P, 512], F32)
                oc = sbt(ph, "oc", [P, 512], F32)
                ocb = sbt(ph, "ocb", [P, 512], BF16)
                ocT = sbt(ph, "ocT", [P, 4, P], BF16)
                sqo = sbt(ph, "sqo", [P, 4, P], F32)
                ssc = sbt(ph, "ssc", [P, 2], F32)
                rsc = sbt(ph, "rsc", [P, 2], F32)
                t1 = sbt(ph, "t1", [P, D], F32)
                x1 = [sbt(ph, f"x1_{k}", [P, D], F32) for k in range(2)]
                x1_slot = [DmaSlot(S, f"x1s{k}") for k in range(2)]
                py = pst(ph, "py", [P, 512], F32)
                ptc = pst(ph, "ptc", [P, 4, P], BF16)
                pss = pst(ph, "pss", [P, 16], F32)
                pa = [pst(ph, f"pa{k}", [P, 512], F32) for k in range(2)]
                pc = [pst(ph, f"pc{k}", [P, 512], F32) for k in range(2)]
                qT, qTc, gT, gTc = M["qT"], M["qTc"], M["gT"], M["gTc"]

                vs = DmaSlot(S, "vecC", group=True)
                for c in range(4):
                    S.dma("sp", lambda e, c=c: e.dma_start(out=wcol[:, c, :], in_=conv_w_d[l, :, c * P:(c + 1) * P].rearrange("j p -> p j"),
                                                           allow_slow_non_contiguous=True), vs, w=[wcol])
                for half in range(2):
                    S.dma("sp", lambda e, half=half: e.dma_start(out=betaA[half * 64:(half + 1) * 64, :],
                                                                 in_=beta_attn_d[l].rearrange("(h d) -> d h", d=64),
                                                                 allow_slow_non_contiguous=True), vs, w=[betaA])
                S.dma("sp", lambda e: e.dma_start(out=betac[:, 4:8], in_=beta_conv_d[l].rearrange("(c p) -> p c", p=P),
                                                  allow_slow_non_contiguous=True), vs, w=[betac])
                S.dma("sp", lambda e: e.dma_start(out=cb_bc[:], in_=conv_b_d[l].partition_broadcast(P)), vs, w=[cb_bc])
                S.dma("sp", lambda e: e.dma_start(out=lg_bc[:], in_=ln_g_d[l].partition_broadcast(P)), vs, w=[lg_bc])
                S.dma("sp", lambda e: e.dma_start(out=lb_bc[:], in_=ln_b_d[l].partition_broadcast(P)), vs, w=[lb_bc])
                vs.close()
                S.op("dve", lambda e: e.tensor_copy(betac[0:64, 0:4], betaA[0:64, 0:4]), r=[betaA], w=[betac])
                S.op("dve", lambda e: e.tensor_copy(betac[64:128, 0:4], betaA[64:128, 4:8]), r=[betaA], w=[betac])
                for kk in range(8):
                    t = stg[kk % 2]
                    sl = stg_slot[kk % 2]
                    if kk < 4:
                        S.dma("sp", lambda e, t=t, kk=kk: e.dma_start(out=t[0:64, :], in_=w_o_d[l, kk * 64:(kk + 1) * 64, :]), sl, w=[t])
                        S.dma("sp", lambda e, t=t, kk=kk: e.dma_start(out=t[64:128, :], in_=w_o_d[l, (kk + 4) * 64:(kk + 5) * 64, :]), sl, w=[t])
                    else:
                        S.dma("sp", lambda e, t=t, kk=kk: e.dma_start(out=t[:], in_=w_o_d[l, 512 + (kk - 4) * P:512 + (kk - 3) * P, :]), sl, w=[t])
                    S.op("act", lambda e, t=t, kk=kk: e.activation(wo[:, kk, :], t[:], AF.Identity, scale=betac[:, kk:kk + 1]),
                         r=[t, betac], w=[wo])
                for c in range(4):
                    for j in range(31):
                        S.op("pool", lambda e, c=c, j=j: e.tensor_scalar(DG[:, c, j, :], ident_f[:], wcol[:, c, j:j + 1], None, op0=ALU.mult),
                             r=[ident_f, wcol], w=[DG])

                def load(n_):
                    i = tiles[n_]
                    k = n_ % 2
                    S.dma("sp", lambda e: e.dma_start(out=xt[k][:], in_=src_aps[i]), xt_slot[k], r=[xs_buf[i]], w=[xt[k]])

                load(0)
                for n_, i in enumerate(tiles):
                    k = n_ % 2
                    is_ctx = i >= NTL
                    if n_ + 1 < len(tiles):
                        load(n_ + 1)
                    if not is_ctx:
                        gsrc, gb, t0 = gT, M["gT_b"], i * P
                        lo, hi = max(0, i - 1), min(NTL - 1, i + 1)
                        osrc = lambda pr: qT[:, pr, i * P:(i + 1) * P]
                        obufs = [M["qT_b"][pr][i // 4] for pr in range(4)]
                    else:
                        gsrc, gb, t0 = gTc, M["gTc_b"], (i - NTL) * P
                        lo, hi = max(0, i - NTL - 1), min(NTC - 1, i - NTL + 1)
                        osrc = lambda pr: qTc[:, pr, (i - NTL) * P:(i - NTL + 1) * P]
                        obufs = [qTc.b]
                    for c in range(4):
                        for j in range(31):
                            S.op("pe", lambda e, c=c, j=j: e.matmul(py[:, c * P:(c + 1) * P], lhsT=gsrc[:, c, t0 + j + GPAD - 15:t0 + j + GPAD - 15 + P],
                                                                  rhs=DG[:, c, j, :], start=(j == 0), stop=(j == 30)),
                                 r=[gb[q] for q in range(lo, hi + 1)] + [DG], w=[py])
                    S.op("dve", lambda e: e.tensor_tensor(y[:], py[:], cb_bc[:], op=ALU.add), r=[py, cb_bc], w=[y])
                    S.op("act", lambda e: e.activation(junk[:], y[:], AF.Identity, accum_out=st4[:, 0:1]), r=[y], w=[junk, st4])
                    S.op("act", lambda e: e.activation(junk[:], y[:], AF.Square, accum_out=st4[:, 1:2]), r=[y], w=[junk, st4])
                    S.op("dve", lambda e: e.tensor_scalar(st4[:, 2:4], st4[:, 0:2], 1.0 / 512, None, op0=ALU.mult), r=[st4], w=[st4])
                    S.op("dve", lambda e: e.tensor_tensor(msq[:], st4[:, 2:3], st4[:, 2:3], op=ALU.mult), r=[st4], w=[msq])
                    S.op("dve", lambda e: e.tensor_tensor(msq[:], st4[:, 3:4], msq[:], op=ALU.subtract), r=[st4, msq], w=[msq])
                    rstd_from_ss(msq, rln, 1.0, eps5)
                    S.op("dve", lambda e: e.tensor_scalar(z[:], y[:], st4[:, 2:3], rln[:, 0:1], op0=ALU.subtract, op1=ALU.mult),
                         r=[y, st4, rln], w=[z])
                    S.op("pool", lambda e: e.tensor_tensor(z[:], z[:], lg_bc[:], op=ALU.mult), r=[z, lg_bc], w=[z])
                    S.op("pool", lambda e: e.tensor_tensor(z[:], z[:], lb_bc[:], op=ALU.add), r=[z, lb_bc], w=[z])
                    S.op("act", lambda e: e.activation(oc[:], z[:], AF.Silu), r=[z], w=[oc])
                    S.op("act", lambda e: e.activation(junk[:], oc[:], AF.Square, accum_out=ssc[:, 0:1]), r=[oc], w=[junk, ssc])
                    S.op("pool", lambda e: e.tensor_copy(ocb[:], oc[:]), r=[oc], w=[ocb])
                    for c in range(4):
                        S.op("pe", lambda e, c=c: e.transpose(ptc[:, c, :], ocb[:, c * P:(c + 1) * P], ident_b[:]),
                             r=[ocb, ident_b], w=[ptc])
                    S.op("act", lambda e: e.activation(ocT[:], ptc[:], AF.Copy), r=[ptc], w=[ocT])
                    for pr in range(4):
                        S.op("pool", lambda e, pr=pr: e.tensor_tensor(sqo[:, pr, :], osrc(pr), osrc(pr), op=ALU.mult), r=obufs, w=[sqo])
                    for pr in range(4):
                        S.op("pe", lambda e, pr=pr: e.matmul(pss[:, 0:2], lhsT=sqo[:, pr, :], rhs=ones_f[:, 0:2],
                                                            start=(pr == 0), stop=(pr == 3)), r=[sqo, ones_f], w=[pss])
                    S.op("dve", lambda e: e.tensor_copy(ssc[:, 1:2], pss[:, 0:1]), r=[pss], w=[ssc])
                    rstd_from_ss(ssc, rsc, 1.0 / 512, eps6)
                    for cc in range(2):
                        for pr in range(4):
                            S.op("pe", lambda e, cc=cc, pr=pr: e.matmul(pa[cc][:], lhsT=osrc(pr), rhs=wo[:, pr, cc * 512:(cc + 1) * 512],
                                                                      start=(pr == 0), stop=(pr == 3)), r=obufs + [wo], w=[pa[cc]])
                        for c in range(4):
                            S.op("pe", lambda e, cc=cc, c=c: e.matmul(pc[cc][:], lhsT=ocT[:, c, :], rhs=wo[:, 4 + c, cc * 512:(cc + 1) * 512],
                                                                    start=(c == 0), stop=(c == 3)), r=[ocT, wo], w=[pc[cc]])
                        sl = slice(cc * 512, (cc + 1) * 512)
                        S.op("dve", lambda e, cc=cc, sl=sl: e.tensor_scalar(t1[:, sl], pa[cc][:], rsc[:, 1:2], None, op0=ALU.mult),
                             r=[pa[cc], rsc], w=[t1])
                        S.op("dve", lambda e, cc=cc, sl=sl: e.scalar_tensor_tensor(out=t1[:, sl], in0=pc[cc][:], scalar=rsc[:, 0:1],
                                                                                   in1=t1[:, sl], op0=ALU.mult, op1=ALU.add),
                             r=[pc[cc], rsc, t1], w=[t1])
                    S.op("pool", lambda e: e.tensor_tensor(t1[:], t1[:], modbc[:, 0:1024], op=ALU.mult), r=[t1, modbc], w=[t1])
                    S.op("pool", lambda e: e.tensor_tensor(x1[k][:], t1[:], xt[k][:], op=ALU.add), r=[t1, xt[k]], w=[x1[k]])
                    S.dma("sp", lambda e: e.dma_start(out=xs_d[i * P:(i + 1) * P, :], in_=x1[k][:]), x1_slot[k], r=[x1[k]], w=[xs_buf[i]])
                    if l == 0:
                        dbg_store("d_x1", slice(i * P, (i + 1) * P), x1[k][:], [x1[k]])
                S.barrier()

        def phase_p(l, tiles, modbc, last):
            with ExitStack() as ph:
                wq = sbt(ph, "wq", [P, 8, D], F32)
                keysT = sbt(ph, "keysT", [P, 8, P], F32)
                kraw = sbt(ph, "kraw", [P, 16, 64], F32)
                keysTz = sbt(ph, "keysTz", [P, 8, 2, P], F32)
                iota16 = sbt(ph, "iota16", [P, 128, 16], F32)
                xt = [sbt(ph, f"xtP{k}", [P, D], F32) for k in range(2)]
                xt_slot = [DmaSlot(S, f"xtP{k}") for k in range(2)]
                junk = sbt(ph, "junkP", [P, D], F32)
                ss = sbt(ph, "ssP", [P, 1], F32)
                rstd = sbt(ph, "rstdP", [P, 1], F32)
                h = sbt(ph, "hP", [P, D], F32)
                hT = sbt(ph, "hT", [P, 8, P], F32)
                qp = sbt(ph, "qp", [P, D], F32)
                qpT = sbt(ph, "qpT", [P, 8, P], F32)
                sc = sbt(ph, "scP", [P, 16, 128], F32)
                wk = sbt(ph, "wkP", [P, 16, 128], F32)
                topv = sbt(ph, "topv", [P, 16, 16], F32)
                idx = sbt(ph, "idxP", [P, 16, 16], U32)
                idxf = sbt(ph, "idxf", [P, 16, 16], F32)
                cand = sbt(ph, "cand", [P, 8, 256], F32)
                wk2 = sbt(ph, "wk2", [P, 8, 256], F32)
                best = sbt(ph, "best", [P, 8, 16], F32)
                pos = sbt(ph, "pos", [P, 8, 16], U32)
                pab = sbt(ph, "pab", [P, 2, 128], U32)
                abf = sbt(ph, "abf", [P, 2, 128], F32)
                eq = sbt(ph, "eq", [P, 128, 16], F32)
                isel = sbt(ph, "isel", [P, 2, 128], F32)
                ef = sbt(ph, "ef", [P, 128], F32)
                eidx = sbt(ph, "eidx", [P, 128], I32)
                ew = sbt(ph, "ew", [P, 128], F32)
                se = sbt(ph, "se", [P, 8], F32)
                wgt = sbt(ph, "wgt", [P, 128], F32)
                pre = sbt(ph, "pre", [P, 128], F32)
                aw = sbt(ph, "aw", [P, 128], F32)
                gbuf = [sbt(ph, f"gbuf{k}", [P, D], F32) for k in range(NSLOT)]
                gslot = [DmaSlot(S, f"g{k}") for k in range(NSLOT)]
                acc = sbt(ph, "accP", [P, D], F32)
                x2 = [sbt(ph, f"x2_{k}", [P, D], F32) for k in range(2)]
                x2_slot = [DmaSlot(S, f"x2s{k}") for k in range(2)]
                fg_bc = sbt(ph, "fg_bc", [P, D], F32)
                pT2 = pst(ph, "pT2", [P, 8, P], F32)
                pqp = [pst(ph, f"pqp{k}", [P, 512], F32) for k in range(2)]
                psc = [pst(ph, f"psc{k}", [P, 4, P], F32) for k in range(4)]

                S.op("pool", lambda e: e.iota(iota16[:], pattern=[[0, 128], [1, 16]], base=0, channel_multiplier=0,
                                              allow_small_or_imprecise_dtypes=True), w=[iota16])
                ws = DmaSlot(S, "wqload", group=True)
                for j in range(8):
                    S.dma("sp", lambda e, j=j: e.dma_start(out=wq[:, j, :], in_=peer_wq_d[l, j * P:(j + 1) * P, :]), ws, w=[wq])
                S.dma("sp", lambda e: e.dma_start(out=kraw[:], in_=peer_keys_d[l].rearrange("h c k d -> k (h c) d")), ws, w=[kraw])
                if last:
                    S.dma("sp", lambda e: e.dma_start(out=fg_bc[:], in_=final_g_d[0].partition_broadcast(P)), ws, w=[fg_bc])
                ws.close()
                for hh in range(8):
                    S.op("pe", lambda e, hh=hh: e.transpose(pT2[:, hh, :], kraw[:, 2 * hh:2 * hh + 2, :].rearrange("p c d -> p (c d)"), ident_f[:]),
                         r=[kraw, ident_f], w=[pT2])
                S.op("act", lambda e: e.activation(keysT[:], pT2[:], AF.Copy), r=[pT2], w=[keysT])
                S.op("pool", lambda e: e.memset(keysTz[:], 0.0), w=[keysTz])
                S.op("dve", lambda e: e.tensor_copy(keysTz[0:64, :, 0, :], keysT[0:64, :, :]), r=[keysT], w=[keysTz])
                S.op("dve", lambda e: e.tensor_copy(keysTz[64:128, :, 1, :], keysT[64:128, :, :]), r=[keysT], w=[keysTz])

                gi = [0]

                def load(n_):
                    i = tiles[n_]
                    k = n_ % 2
                    S.dma("sp", lambda e: e.dma_start(out=xt[k][:], in_=xs_d[i * P:(i + 1) * P, :]), xt_slot[k], r=[xs_buf[i]], w=[xt[k]])

                load(0)
                for n_, i in enumerate(tiles):
                    k = n_ % 2
                    if n_ + 1 < len(tiles):
                        load(n_ + 1)
                    S.op("act", lambda e: e.activation(junk[:], xt[k][:], AF.Square, accum_out=ss[:]), r=[xt[k]], w=[junk, ss])
                    rstd_from_ss(ss, rstd, 1.0 / D, eps6)
                    S.op("dve", lambda e: e.scalar_tensor_tensor(out=h[:], in0=xt[k][:], scalar=rstd[:, 0:1], in1=modbc[:, 2048:3072],
                                                                 op0=ALU.mult, op1=ALU.mult), r=[xt[k], rstd, modbc], w=[h])
                    S.op("dve", lambda e: e.tensor_tensor(h[:], h[:], modbc[:, 1024:2048], op=ALU.add), r=[h, modbc], w=[h])
                    if CUT <= 11:
                        continue
                    for j in range(8):
                        S.op("pe", lambda e, j=j: e.transpose(pT2[:, j, :], h[:, j * P:(j + 1) * P], ident_f[:]), r=[h, ident_f], w=[pT2])
                    S.op("act", lambda e: e.activation(hT[:], pT2[:], AF.Copy), r=[pT2], w=[hT])
                    for cc in range(2):
                        for j in range(8):
                            S.op("pe", lambda e, cc=cc, j=j: e.matmul(pqp[cc][:], lhsT=hT[:, j, :], rhs=wq[:, j, cc * 512:(cc + 1) * 512],
                                                                    start=(j == 0), stop=(j == 7)), r=[hT, wq], w=[pqp[cc]])
                        S.op("act", lambda e, cc=cc: e.activation(qp[:, cc * 512:(cc + 1) * 512], pqp[cc][:], AF.Copy), r=[pqp[cc]], w=[qp])
                    if CUT <= 12:
                        continue
                    for hh in range(8):
                        S.op("pe", lambda e, hh=hh: e.transpose(pT2[:, hh, :], qp[:, hh * P:(hh + 1) * P], ident_f[:]), r=[qp, ident_f], w=[pT2])
                    S.op("act", lambda e: e.activation(qpT[:], pT2[:], AF.Copy), r=[pT2], w=[qpT])
                    if CUT <= 13:
                        continue
                    for hh in range(8):
                        S.op("pe", lambda e, hh=hh: e.matmul(psc[hh // 2][:, (hh % 2) * 2:(hh % 2) * 2 + 2, :], lhsT=qpT[:, hh, :],
                                                            rhs=keysTz[:, hh, :, :], start=True, stop=True),
                             r=[qpT, keysTz], w=[psc[hh // 2]])
                    for q in range(4):
                        S.op("act", lambda e, q=q: e.activation(sc[:, 4 * q:4 * q + 4, :], psc[q][:], AF.Copy), r=[psc[q]], w=[sc])
                    if CUT <= 14:
                        continue
                    for g in range(16):
                        S.op("dve", lambda e, g=g: e.max(out=topv[:, g, 0:8], in_=sc[:, g, :]), r=[sc], w=[topv])
                        S.op("dve", lambda e, g=g: e.max_index(out=idx[:, g, 0:8], in_max=topv[:, g, 0:8], in_values=sc[:, g, :]),
                             r=[sc, topv], w=[idx])
                        S.op("dve", lambda e, g=g: e.match_replace(out=wk[:, g, :], in_to_replace=topv[:, g, 0:8], in_values=sc[:, g, :],
                                                                   imm_value=-1e30), r=[sc, topv], w=[wk])
                        S.op("dve", lambda e, g=g: e.max(out=topv[:, g, 8:16], in_=wk[:, g, :]), r=[wk], w=[topv])
                        S.op("dve", lambda e, g=g: e.max_index(out=idx[:, g, 8:16], in_max=topv[:, g, 8:16], in_values=wk[:, g, :]),
                             r=[wk, topv], w=[idx])
                    S.op("dve", lambda e: e.tensor_copy(idxf[:], idx[:]), r=[idx], w=[idxf])
                    if CUT <= 15:
                        continue
                    tv4 = topv[:].rearrange("p (h c) a -> p h c a", c=2)
                    if4 = idxf[:].rearrange("p (h c) a -> p h c a", c=2)
                    S.op("dve", lambda e: e.tensor_tensor(cand[:].rearrange("p h (a b) -> p h a b", a=16),
                                                          tv4[:, :, 0, :].unsqueeze(3).to_broadcast([P, 8, 16, 16]),
                                                          tv4[:, :, 1, :].unsqueeze(2).to_broadcast([P, 8, 16, 16]), op=ALU.add),
                         r=[topv], w=[cand])
                    for hh in range(8):
                        S.op("dve", lambda e, hh=hh: e.max(out=best[:, hh, 0:8], in_=cand[:, hh, :]), r=[cand], w=[best])
                        S.op("dve", lambda e, hh=hh: e.max_index(out=pos[:, hh, 0:8], in_max=best[:, hh, 0:8], in_values=cand[:, hh, :]),
                             r=[cand, best], w=[pos])
                        S.op("dve", lambda e, hh=hh: e.match_replace(out=wk2[:, hh, :], in_to_replace=best[:, hh, 0:8],
                                                                     in_values=cand[:, hh, :], imm_value=-1e30), r=[cand, best], w=[wk2])
                        S.op("dve", lambda e, hh=hh: e.max(out=best[:, hh, 8:16], in_=wk2[:, hh, :]), r=[wk2], w=[best])
                        S.op("dve", lambda e, hh=hh: e.max_index(out=pos[:, hh, 8:16], in_max=best[:, hh, 8:16], in_values=wk2[:, hh, :]),
                             r=[wk2, best], w=[pos])
                    if CUT <= 16:
                        continue
                    posf = pos[:].rearrange("p h s -> p (h s)")
                    S.op("dve", lambda e: e.tensor_single_scalar(pab[:, 0, :], posf, 4, op=ALU.logical_shift_right), r=[pos], w=[pab])
                    S.op("dve", lambda e: e.tensor_single_scalar(pab[:, 1, :], posf, 15, op=ALU.bitwise_and), r=[pos], w=[pab])
                    S.op("dve", lambda e: e.tensor_copy(abf[:], pab[:]), r=[pab], w=[abf])
                    for c in range(2):
                        S.op("dve", lambda e, c=c: e.tensor_tensor(eq[:], iota16[:], abf[:, c, :].unsqueeze(2).to_broadcast([P, 128, 16]),
                                                                   op=ALU.is_equal), r=[iota16, abf], w=[eq])
                        S.op("dve", lambda e, c=c: e.tensor_tensor(eq[:].rearrange("p (h s) a -> p h s a", h=8),
                                                                   eq[:].rearrange("p (h s) a -> p h s a", h=8),
                                                                   if4[:, :, c, :].unsqueeze(2).to_broadcast([P, 8, 16, 16]), op=ALU.mult),
                             r=[eq, idxf], w=[eq])
                        S.op("dve", lambda e, c=c: e.tensor_reduce(out=isel[:, c, :], in_=eq[:], axis=AX.X, op=ALU.add), r=[eq], w=[isel])
                    S.op("dve", lambda e: e.scalar_tensor_tensor(out=ef[:], in0=isel[:, 0, :], scalar=128.0, in1=isel[:, 1, :],
                                                                 op0=ALU.mult, op1=ALU.add), r=[isel], w=[ef])
                    S.op("dve", lambda e: e.tensor_scalar(ef[:], ef[:], float(l * NPEER), None, op0=ALU.add), r=[ef], w=[ef])
                    S.op("dve", lambda e: e.tensor_copy(eidx[:], ef[:]), r=[ef], w=[eidx])
                    if CUT <= 17:
                        continue
                    S.op("dve", lambda e: e.tensor_tensor(ew[:].rearrange("p (h s) -> p h s", h=8), best[:],
                                                          best[:, :, 0:1].to_broadcast([P, 8, 16]), op=ALU.subtract), r=[best], w=[ew])
                    S.op("act", lambda e: e.activation(ew[:], ew[:], AF.Exp), r=[ew], w=[ew])
                    S.op("dve", lambda e: e.tensor_reduce(out=se[:], in_=ew[:].rearrange("p (h s) -> p h s", h=8), axis=AX.X, op=ALU.add),
                         r=[ew], w=[se])
                    S.op("dve", lambda e: e.reciprocal(se[:], se[:]), r=[se], w=[se])
                    S.op("dve", lambda e: e.tensor_tensor(wgt[:].rearrange("p (h s) -> p h s", h=8), ew[:].rearrange("p (h s) -> p h s", h=8),
                                                          se[:].unsqueeze(2).to_broadcast([P, 8, 16]), op=ALU.mult), r=[ew, se], w=[wgt])
                    if stop == "p0":
                        dbg_store("d_eidx", slice(i * P, (i + 1) * P), ef[:], [ef])
                        dbg_store("d_w", slice(i * P, (i + 1) * P), wgt[:], [wgt])
                        dbg_store("d_pre", slice(i * P, (i + 1) * P), best[:].rearrange("p h s -> p (h s)"), [best])
                        continue
                    for s_ in range(128):
                        b_ = gi[0] % NSLOT
                        gi[0] += 1
                        S.dma("pool", lambda e, s_=s_, b_=b_: e.indirect_dma_start(
                            out=gbuf[b_][:], out_offset=None, in_=peer_u_flat,
                            in_offset=bass.IndirectOffsetOnAxis(ap=eidx[:, s_:s_ + 1], axis=0)), gslot[b_], r=[eidx], w=[gbuf[b_]])
                        S.op("dve", lambda e, s_=s_, b_=b_: e.scalar_tensor_tensor(
                            out=junk[:], in0=h[:], scalar=1.0, in1=gbuf[b_][:], op0=ALU.mult, op1=ALU.mult,
                            accum_out=pre[:, s_:s_ + 1]), r=[h, gbuf[b_]], w=[junk, pre])
                    S.op("act", lambda e: e.activation(aw[:], pre[:], AF.Gelu), r=[pre], w=[aw])
                    S.op("dve", lambda e: e.tensor_tensor(aw[:], aw[:], wgt[:], op=ALU.mult), r=[aw, wgt], w=[aw])
                    if l == 0:
                        dbg_store("d_pre", slice(i * P, (i + 1) * P), pre[:], [pre])
                        dbg_store("d_eidx", slice(i * P, (i + 1) * P), ef[:], [ef])
                        dbg_store("d_w", slice(i * P, (i + 1) * P), wgt[:], [wgt])
                    for s_ in range(128):
                        b_ = gi[0] % NSLOT
                        gi[0] += 1
                        S.dma("pool", lambda e, s_=s_, b_=b_: e.indirect_dma_start(
                            out=gbuf[b_][:], out_offset=None, in_=peer_v_flat,
                            in_offset=bass.IndirectOffsetOnAxis(ap=eidx[:, s_:s_ + 1], axis=0)), gslot[b_], r=[eidx], w=[gbuf[b_]])
                        if s_ == 0:
                            S.op("dve", lambda e, b_=b_: e.tensor_scalar(acc[:], gbuf[b_][:], aw[:, 0:1], None, op0=ALU.mult),
                                 r=[gbuf[b_], aw], w=[acc])
                        else:
                            S.op("dve", lambda e, s_=s_, b_=b_: e.scalar_tensor_tensor(
                                out=acc[:], in0=gbuf[b_][:], scalar=aw[:, s_:s_ + 1], in1=acc[:], op0=ALU.mult, op1=ALU.add),
                                 r=[gbuf[b_], aw, acc], w=[acc])
                    S.op("dve", lambda e: e.tensor_tensor(acc[:], acc[:], modbc[:, 3072:4096], op=ALU.mult), r=[acc, modbc], w=[acc])
                    S.op("dve", lambda e: e.tensor_tensor(x2[k][:], acc[:], xt[k][:], op=ALU.add), r=[acc, xt[k]], w=[x2[k]])
                    if l == 0:
                        dbg_store("d_x2", slice(i * P, (i + 1) * P), x2[k][:], [x2[k]])
                    if last:
                        S.op("act", lambda e: e.activation(junk[:], x2[k][:], AF.Square, accum_out=ss[:]), r=[x2[k]], w=[junk, ss])
                        rstd_from_ss(ss, rstd, 1.0 / D, eps6)
                        S.op("dve", lambda e: e.scalar_tensor_tensor(out=x2[k][:], in0=x2[k][:], scalar=rstd[:, 0:1], in1=fg_bc[:],
                                                                     op0=ALU.mult, op1=ALU.mult), r=[x2[k], rstd, fg_bc], w=[x2[k]])
                        S.dma("sp", lambda e: e.dma_start(out=out_d[i * P:(i + 1) * P, :], in_=x2[k][:]), x2_slot[k], r=[x2[k]])
                    else:
                        S.dma("sp", lambda e: e.dma_start(out=xs_d[i * P:(i + 1) * P, :], in_=x2[k][:]), x2_slot[k], r=[x2[k]], w=[xs_buf[i]])
                S.barrier()

        lat_tiles = list(range(NTL))
        ctx_tiles = list(range(NTL, NT))
        for l in range(depth):
            last = (l == DEPTH - 1)
            if l == 0:
                src_aps = [x_d[i * P:(i + 1) * P, :] for i in range(NTL)] + [ctx_d[i * P:(i + 1) * P, :] for i in range(NTC)]
            else:
                src_aps = [xs_d[i * P:(i + 1) * P, :] for i in range(NT)]
            mod_cols(l)
            if l == 0:
                dbg_store("d_mod", (slice(0, P), slice(0, 32)), modcol[:].rearrange("p r c -> p (r c)"), [modcol])
                dbg_store("d_mod", (slice(0, P), slice(32, 48)), scT[:].rearrange("p j r -> p (j r)"), [scT])
            if stop == "m":
                break
            with ExitStack() as mix:
                M = {}
                M["qT"] = sbt(mix, "qT", [P, 4, S_LAT], BF16)
                M["qT_b"] = [[Buf(f"qT{pr}_{c}") for c in range(8)] for pr in range(4)]
                M["qTc"] = sbt(mix, "qTc", [P, 4, S_CTX], BF16)
                M["gT"] = sbt(mix, "gT", [P, 4, S_LAT + 2 * GPAD], BF16)
                M["gT_b"] = [Buf(f"gT{i}") for i in range(NTL)]
                M["gTc"] = sbt(mix, "gTc", [P, 4, S_CTX + 2 * GPAD], BF16)
                M["gTc_b"] = [Buf(f"gTc{i}") for i in range(NTC)]
                S.op("pool", lambda e: e.memset(M["gT"][:], 0.0), w=M["gT_b"])
                S.op("pool", lambda e: e.memset(M["gTc"][:], 0.0), w=M["gTc_b"])
                with ExitStack() as ab:
                    M["kT"] = sbt(ab, "kT", [P, NT * P], BF16)
                    M["kT_b"] = [Buf(f"kT{i}") for i in range(NT)]
                    M["Vp"] = sbt(ab, "Vp", [P, NT, 192], BF16)
                    M["Vp_b"] = [Buf(f"Vp{i}") for i in range(NT)]
                    S.op("pool", lambda e: e.memset(M["Vp"][:], 1.0), w=M["Vp_b"])
                    phase_a(l, M, src_aps, last)
                    if stop in ("a", "a0", "a1"):
                        break
                    phase_b(l, M, last)
                if stop == "b":
                    break
                with ExitStack() as cs:
                    modbc = sbt(cs, "modbcC", [P, 4096], F32)
                    mod_bc(l, 0, modbc)
                    phase_c(l, M, lat_tiles, src_aps, 0, modbc)
                    if not last:
                        mod_bc(l, 1, modbc)
                        phase_c(l, M, ctx_tiles, src_aps, 1, modbc)
            if stop == "c":
                break
            with ExitStack() as pp:
                modbc = sbt(pp, "modbcP", [P, 4096], F32)
                mod_bc(l, 0, modbc)
                phase_p(l, lat_tiles if stop not in ("p1", "p0") else lat_tiles[:1], modbc, last)
                if not last and stop not in ("p1", "p0"):
                    mod_bc(l, 1, modbc)
                    phase_p(l, ctx_tiles, modbc, last)
            if stop in ("p1", "p0"):
                break
        out_slot.close()
        if out_slot.sem is not None:
            for en in ("sp", "act", "pool"):
                S.E[en].eng.wait_ge(out_slot.sem, out_slot.val)
        S.barrier()
        print(f"[build] instr={S.ninstr} waits={S.nwait} sems={S.nsem}")
    return nc


_NC_CACHE = {}


def _rope_tables():
    n = 16
    inv = (10000.0 ** (-np.arange(n, dtype=np.float32) / n)).astype(np.float32)
    row = np.repeat(np.arange(64), 64).astype(np.float32)
    col = np.tile(np.arange(64), 64).astype(np.float32)
    ang = np.concatenate([row[:, None] * inv, col[:, None] * inv], axis=-1).astype(np.float32)
    cos = np.cos(ang).astype(np.float32)
    sin = np.sin(ang).astype(np.float32)
    cos2 = np.concatenate([cos, cos], axis=-1)
    sin2 = np.concatenate([-sin, sin], axis=-1)
    return np.ascontiguousarray(cos2), np.ascontiguousarray(sin2)


def make_in_maps(inputs, cores):
    f = lambda a: np.ascontiguousarray(np.asarray(a, dtype=np.float32))
    cos2, sin2 = _rope_tables()
    shared = {k: f(inputs[k]) for k in ["w_mod", "b_mod", "w_in", "q_gain", "k_gain", "conv_w", "conv_b", "ln_g", "ln_b",
                                        "beta_attn", "beta_conv", "w_o", "peer_wq", "peer_keys", "peer_u", "peer_v"]}
    shared["final_g"] = f(inputs["final_g"]).reshape(1, D)
    shared["cos2"] = cos2
    shared["sin2"] = sin2
    shared["cctxT"] = np.ascontiguousarray(f(inputs["c_ctx"]).reshape(8, P).T)
    x = f(inputs["x"])
    ctx = f(inputs["ctx"])
    c = f(inputs["c"])
    maps = []
    for b in cores:
        m = dict(shared)
        m["x"] = x[b]
        m["ctx"] = ctx[b]
        m["cT"] = np.ascontiguousarray(c[b].reshape(8, P).T)
        maps.append(m)
    return maps


def kernel(**inputs):
    if "nc" not in _NC_CACHE:
        _NC_CACHE["nc"] = build()
    nc = _NC_CACHE["nc"]
    B = np.asarray(inputs["x"]).shape[0]
    maps = make_in_maps(inputs, list(range(B)))
    res = run_bass_kernel_spmd(nc, maps, core_ids=list(range(B)))
    out = np.stack([np.asarray(r["out"], dtype=np.float32) for r in res.results], axis=0)
    return out
```

```python
import numpy as np
from contextlib import ExitStack
import concourse.bass as bass
import concourse.mybir as mybir
from concourse.bass_utils import run_bass_kernel_spmd

F32 = mybir.dt.float32
BF16 = mybir.dt.bfloat16
I32 = mybir.dt.int32
U32 = mybir.dt.uint32
ALU = mybir.AluOpType
AF = mybir.ActivationFunctionType
AX = mybir.AxisListType

import os
CUT = int(os.environ.get("KCUT", "99"))
SEM_LIMIT = 30000


class Buf:
    __slots__ = ("name", "w", "r")

    def __init__(self, name):
        self.name = name
        self.w = None
        self.r = {}


class Eng:
    def __init__(self, sync, name, eng):
        self.sync = sync
        self.name = name
        self.eng = eng
        self.sem = None
        self.count = 0
        self.known = {}
        self.last = None

    def new_event(self):
        if self.sem is None or self.count >= SEM_LIMIT:
            self.sem = self.sync.new_sem(self.name)
            self.count = 0
        self.count += 1
        self.last = [self.sem, self.count, self.name]
        return self.last


def DmaSlot(sync, name, group=False):
    if name not in sync.slot_by_name:
        sync.slot_by_name[name] = _DmaSlot(sync, name, group)
    return sync.slot_by_name[name]


class _DmaSlot:
    def __init__(self, sync, name, group=False):
        self.name = name
        self.sem = None
        self.val = 0
        self.group = group
        self.pending = []
        sync.slots.append(self)

    def bump(self, sync):
        if self.sem is None or (self.val >= SEM_LIMIT and not self.pending):
            self.sem = sync.new_sem("d" + self.name)
            self.val = 0
        self.val += 16
        ev = [self.sem, self.val, "dma"]
        if self.group:
            self.pending.append(ev)
        return ev

    def close(self):
        for ev in self.pending:
            ev[1] = self.val
        self.pending = []


class Sync:
    def __init__(self, nc, stack):
        self.nc = nc
        self.stack = stack
        self.nsem = 0
        self.slots = []
        self.slot_by_name = {}
        self.E = {
            "pe": Eng(self, "pe", nc.tensor),
            "dve": Eng(self, "dve", nc.vector),
            "act": Eng(self, "act", nc.scalar),
            "pool": Eng(self, "pool", nc.gpsimd),
            "sp": Eng(self, "sp", nc.sync),
        }
        self.ninstr = 0
        self.nwait = 0

    def new_sem(self, name):
        self.nsem += 1
        return self.stack.enter_context(self.nc.semaphore(f"s{self.nsem}_{name}"))

    def _wait(self, E, evs):
        best = {}
        for ev in evs:
            if ev is None:
                continue
            if E.name == "pe" and ev[2] == "pe":
                continue
            k = id(ev[0])
            if k not in best or best[k][1] < ev[1]:
                best[k] = ev
        for ev in best.values():
            sem, val, src = ev
            if E.known.get(id(sem), 0) >= val:
                continue
            E.eng.wait_ge(sem, val)
            self.nwait += 1
            E.known[id(sem)] = val

    def _deps(self, E, reads, writes):
        evs = []
        for b in reads:
            evs.append(b.w)
        for b in writes:
            if b.w is not None and b.w[2] != E.name:
                evs.append(b.w)
            for ev in b.r.values():
                if ev[2] == E.name:
                    continue
                evs.append(ev)
        return evs

    @staticmethod
    def _bufs(lst):
        return [t.b if hasattr(t, "b") else t for t in lst]

    def op(self, engname, fn, r=(), w=()):
        E = self.E[engname]
        r = self._bufs(r)
        w = self._bufs(w)
        self._wait(E, self._deps(E, r, w))
        ins = fn(E.eng)
        ev = E.new_event()
        ins.then_inc(ev[0], 1)
        self.ninstr += 1
        key = ev[2] if ev[2] != "dma" else id(ev[0])
        for b in r:
            b.r[key] = ev
        for b in w:
            b.w = ev
            b.r = {}
        return ev

    def dma(self, qname, fn, slot, r=(), w=()):
        E = self.E[qname]
        r = self._bufs(r)
        w = self._bufs(w)
        self._wait(E, self._deps(E, r, w))
        ins = fn(E.eng)
        ev = slot.bump(self)
        ins.then_inc(ev[0], 16)
        self.ninstr += 1
        key = ev[2] if ev[2] != "dma" else id(ev[0])
        for b in r:
            b.r[key] = ev
        for b in w:
            b.w = ev
            b.r = {}
        return ev

    def barrier(self):
        for s in self.slots:
            if s.group:
                s.close()
        evs = []
        for E in self.E.values():
            if E.last is not None:
                evs.append(E.last)
        for s in self.slots:
            if s.sem is not None:
                evs.append([s.sem, s.val, "dma"])
        for E in self.E.values():
            self._wait(E, [ev for ev in evs if ev[2] != E.name or ev[2] == "dma"])


class Tile:
    def __init__(self, h, name):
        self.h = h
        self.b = Buf(name)

    def __getitem__(self, k):
        return self.h[k]


P = 128
D = 1024
S_LAT = 4096
S_CTX = 256
NTL = 32
NTC = 2
NT = NTL + NTC
DEPTH = 2
INC = 1792
EPS = 1e-6
NPEER = 16384
GPAD = 16
NSLOT = 14
GRP = 8


def build(depth=DEPTH, dbg=False, stop=None):
    nc = bass.Bass("TRN2", target_bir_lowering=False)

    def din(name, shape, dt=F32):
        return nc.dram_tensor(name, shape, dt, kind="ExternalInput").ap()

    x_d = din("x", [S_LAT, D])
    ctx_d = din("ctx", [S_CTX, D])
    cT_d = din("cT", [P, 8])
    cctxT_d = din("cctxT", [P, 8])
    w_mod_d = din("w_mod", [DEPTH, D, 6 * D])
    b_mod_d = din("b_mod", [DEPTH, 6 * D])
    w_in_d = din("w_in", [DEPTH, D, INC])
    q_gain_d = din("q_gain", [DEPTH, 64])
    k_gain_d = din("k_gain", [DEPTH, 64])
    conv_w_d = din("conv_w", [DEPTH, 31, 512])
    conv_b_d = din("conv_b", [DEPTH, 512])
    ln_g_d = din("ln_g", [DEPTH, 512])
    ln_b_d = din("ln_b", [DEPTH, 512])
    beta_attn_d = din("beta_attn", [DEPTH, 512])
    beta_conv_d = din("beta_conv", [DEPTH, 512])
    w_o_d = din("w_o", [DEPTH, D, D])
    peer_wq_d = din("peer_wq", [DEPTH, D, D])
    peer_keys_d = din("peer_keys", [DEPTH, 8, 2, 128, 64])
    peer_u_d = din("peer_u", [DEPTH, NPEER, D])
    peer_v_d = din("peer_v", [DEPTH, NPEER, D])
    final_g_d = din("final_g", [1, D])
    cos2_d = din("cos2", [S_LAT, 64])
    sin2_d = din("sin2", [S_LAT, 64])
    peer_u_flat = peer_u_d.rearrange("l n d -> (l n) d")
    peer_v_flat = peer_v_d.rearrange("l n d -> (l n) d")
    out_d = nc.dram_tensor("out", [S_LAT, D], F32, kind="ExternalOutput").ap()
    xs_d = nc.dram_tensor("xs", [S_LAT + S_CTX, D], F32).ap()
    uv_d = nc.dram_tensor("uv", [DEPTH * NPEER, 2 * D], BF16).ap()
    dbg_d = {}
    if dbg:
        for nm, shp in [("d_qkvu", [NT * P, INC]), ("d_x1", [NT * P, D]), ("d_oT", [P, 4 * S_LAT]),
                        ("d_pre", [NT * P, 128]), ("d_eidx", [NT * P, 128]), ("d_w", [NT * P, 128]),
                        ("d_qT", [P, 4 * S_LAT]), ("d_x2", [NT * P, D]), ("d_mod", [P, 4096])]:
            dbg_d[nm] = nc.dram_tensor(nm, shp, F32, kind="ExternalOutput").ap()

    top = ExitStack()
    with top:
        S = Sync(nc, top)

        uid = [0]

        def sbt(stack, name, shape, dt):
            uid[0] += 1
            name = f"{name}_u{uid[0]}"
            return Tile(stack.enter_context(nc.sbuf_tensor(name, shape, dt)), name)

        def pst(stack, name, shape, dt):
            uid[0] += 1
            name = f"{name}_u{uid[0]}"
            return Tile(stack.enter_context(nc.psum_tensor(name, shape, dt)), name)

        out_slot = DmaSlot(S, "out", group=True)
        dbg_slots = {}
        xs_buf = [Buf(f"xs{i}") for i in range(NT)]

        ident_f = sbt(top, "ident_f", [P, P], F32)
        ident_b = sbt(top, "ident_b", [P, P], BF16)
        ones_f = sbt(top, "ones_f", [P, P], F32)
        io_t = sbt(top, "io_t", [P, P], F32)
        pid_t = sbt(top, "pid_t", [P, 1], F32)
        S.op("pool", lambda e: e.iota(io_t[:], pattern=[[1, P]], base=0, channel_multiplier=0,
                                      allow_small_or_imprecise_dtypes=True), w=[io_t])
        S.op("pool", lambda e: e.iota(pid_t[:], pattern=[[0, 1]], base=0, channel_multiplier=1,
                                      allow_small_or_imprecise_dtypes=True), w=[pid_t])
        S.op("dve", lambda e: e.tensor_scalar(ident_f[:], io_t[:], pid_t[:, 0:1], None, op0=ALU.is_equal),
             r=[io_t, pid_t], w=[ident_f])
        S.op("dve", lambda e: e.tensor_copy(ident_b[:], ident_f[:]), r=[ident_f], w=[ident_b])
        S.op("pool", lambda e: e.memset(ones_f[:], 1.0), w=[ones_f])

        cslot = DmaSlot(S, "const", group=True)
        craw = sbt(top, "craw", [P, 2, 8], F32)
        scT = sbt(top, "scT", [P, 8, 2], F32)
        S.dma("sp", lambda e: e.dma_start(out=craw[:, 0, :], in_=cT_d), cslot, w=[craw])
        S.dma("sp", lambda e: e.dma_start(out=craw[:, 1, :], in_=cctxT_d), cslot, w=[craw])
        cslot.close()
        S.op("act", lambda e: e.activation(scT[:].rearrange("p j r -> p r j"), craw[:], AF.Silu), r=[craw], w=[scT])
        modcol = sbt(top, "modcol", [P, 2, 16], F32)
        eps6 = sbt(top, "eps6", [P, 1], F32)
        eps5 = sbt(top, "eps5", [P, 1], F32)
        S.op("pool", lambda e: e.memset(eps6[:], 1e-6), w=[eps6])
        S.op("pool", lambda e: e.memset(eps5[:], 1e-5), w=[eps5])


        def rstd_from_ss(ss, rs, scale, eps_t):
            S.op("act", lambda e: e.activation(rs[:], ss[:], AF.Sqrt, scale=scale, bias=eps_t[:, 0:1]), r=[ss, eps_t], w=[rs])
            S.op("dve", lambda e: e.reciprocal(rs[:], rs[:]), r=[rs], w=[rs])

        def dbg_store(name, rows, tile_ap, rtiles):
            if dbg and name in dbg_d:
                if name not in dbg_slots:
                    dbg_slots[name] = (DmaSlot(S, name), Buf(name))
                S.dma("sp", lambda e: e.dma_start(out=dbg_d[name][rows], in_=tile_ap), dbg_slots[name][0], r=rtiles, w=[dbg_slots[name][1]])


        def convert_tables():
            with ExitStack() as ph:
                NB = 4
                stg = [sbt(ph, f"cvi{k}", [P, 4, D], F32) for k in range(NB)]
                obf = [sbt(ph, f"cvo{k}", [P, 4, D], BF16) for k in range(NB)]
                islot = [DmaSlot(S, f"cvi{k}") for k in range(NB)]
                oslot = [DmaSlot(S, f"cvo{k}") for k in range(NB)]
                n = 0
                for l in range(DEPTH):
                    for tab, col0 in ((peer_u_d, 0), (peer_v_d, D)):
                        for t in range(NPEER // 512):
                            k = n % NB
                            src = tab[l, t * 512:(t + 1) * 512, :].rearrange("(p r) d -> p r d", r=4)
                            dst = uv_d[l * NPEER + t * 512:l * NPEER + (t + 1) * 512, col0:col0 + D].rearrange("(p r) d -> p r d", r=4)
                            S.dma("sp", lambda e, k=k, src=src: e.dma_start(out=stg[k][:], in_=src), islot[k], w=[stg[k]])
                            if n % 2 == 0:
                                S.op("act", lambda e, k=k: e.activation(obf[k][:], stg[k][:], AF.Copy), r=[stg[k]], w=[obf[k]])
                            else:
                                S.op("dve", lambda e, k=k: e.tensor_copy(obf[k][:], stg[k][:]), r=[stg[k]], w=[obf[k]])
                            S.dma("pool", lambda e, k=k, dst=dst: e.dma_start(out=dst, in_=obf[k][:]), oslot[k], r=[obf[k]])
                            n += 1
                S.barrier()

        def mod_cols(l):
            with ExitStack() as ph:
                wm = [sbt(ph, f"wm{k}", [P, 8, P], F32) for k in range(2)]
                wslot = [DmaSlot(S, f"wm{k}") for k in range(2)]
                bcol = sbt(ph, "bcol", [P, 16], F32)
                pcol = pst(ph, "pcol", [P, 16, 2], F32)
                S.dma("sp", lambda e: e.dma_start(out=bcol[:], in_=b_mod_d[l, 0:2048].rearrange("(c p) -> p c", p=P),
                                                  allow_slow_non_contiguous=True), DmaSlot(S, "bcol"), w=[bcol])
                for cc in range(16):
                    t = wm[cc % 2]
                    S.dma("sp", lambda e, t=t, cc=cc: e.dma_start(
                        out=t[:], in_=w_mod_d[l, :, cc * P:(cc + 1) * P].rearrange("(j p) m -> p j m", p=P)),
                          wslot[cc % 2], w=[t])
                    for j in range(8):
                        S.op("pe", lambda e, t=t, j=j, cc=cc: e.matmul(pcol[:, cc, :], lhsT=t[:, j, :],
                                                                     rhs=scT[:, j, :], start=(j == 0), stop=(j == 7)),
                             r=[t, scT], w=[pcol])
                S.op("dve", lambda e: e.tensor_tensor(modcol[:].rearrange("p r c -> p c r"), pcol[:],
                                                      bcol[:].unsqueeze(2).to_broadcast([P, 16, 2]), op=ALU.add),
                     r=[pcol, bcol], w=[modcol])
                S.op("dve", lambda e: e.tensor_scalar(modcol[:, :, 8:16], modcol[:, :, 8:16], 1.0, None, op0=ALU.add),
                     r=[modcol], w=[modcol])
                S.barrier()

        def mod_bc(l, r, modbc):
            with ExitStack() as ph:
                NW = 4
                wm = [sbt(ph, f"wmb{k}", [P, 512], F32) for k in range(NW)]
                wslot = [DmaSlot(S, f"wmb{k}") for k in range(NW)]
                bbc = [sbt(ph, f"bbc{k}", [P, 512], F32) for k in range(2)]
                bslot = [DmaSlot(S, f"bbc{k}") for k in range(2)]
                screp = sbt(ph, "screp", [P, 8, P], F32)
                pb = pst(ph, "pbm", [P, 512], F32)
                S.op("dve", lambda e: e.tensor_copy(screp[:], scT[:, :, r].unsqueeze(2).to_broadcast([P, 8, P])),
                     r=[scT], w=[screp])
                n = 0
                for cc in range(8):
                    c0 = 2048 + cc * 512
                    S.dma("sp", lambda e, cc=cc, c0=c0: e.dma_start(out=bbc[cc % 2][:], in_=b_mod_d[l, c0:c0 + 512].partition_broadcast(P)),
                          bslot[cc % 2], w=[bbc[cc % 2]])
                    for j in range(8):
                        t = wm[n % NW]
                        S.dma("sp", lambda e, t=t, j=j, c0=c0: e.dma_start(out=t[:], in_=w_mod_d[l, j * P:(j + 1) * P, c0:c0 + 512]),
                              wslot[n % NW], w=[t])
                        n += 1
                        S.op("pe", lambda e, t=t, j=j: e.matmul(pb[:], lhsT=screp[:, j, :], rhs=t[:], start=(j == 0), stop=(j == 7)),
                             r=[t, screp], w=[pb])
                    S.op("dve", lambda e, cc=cc: e.tensor_tensor(modbc[:, cc * 512:(cc + 1) * 512], pb[:], bbc[cc % 2][:], op=ALU.add),
                         r=[pb, bbc[cc % 2]], w=[modbc])
                S.op("dve", lambda e: e.tensor_scalar(modbc[:, 2048:3072], modbc[:, 2048:3072], 1.0, None, op0=ALU.add),
                     r=[modbc], w=[modbc])
                S.barrier()

        def phase_a(l, M, src_aps, last):
            with ExitStack() as ph:
                win = sbt(ph, "win", [P, 8, INC], BF16)
                stg = sbt(ph, "stg", [P, INC], F32)
                stg_slot = DmaSlot(S, "stg")
                bias_bc = sbt(ph, "bias_bc", [P, INC], F32)
                sh1rep = sbt(ph, "sh1rep", [P, 2, 8, P], BF16)
                gain = sbt(ph, "gain", [P, 640], F32)
                graw = sbt(ph, "graw", [P, 128], F32)
                cos2 = sbt(ph, "cos2", [P, NTL, 64], F32)
                sin2 = sbt(ph, "sin2", [P, NTL, 64], F32)
                xt = [sbt(ph, f"xt{k}", [P, D], F32) for k in range(2)]
                xt_slot = [DmaSlot(S, f"xt{k}") for k in range(2)]
                xb = [sbt(ph, f"xb{k}", [P, D], BF16) for k in range(2)]
                xT = [sbt(ph, f"xT{k}", [P, 8, P], BF16) for k in range(2)]
                junk = sbt(ph, "junkA", [P, D], BF16)
                ss = [sbt(ph, f"ssA{k}", [P, 1], F32) for k in range(2)]
                rstd = [sbt(ph, f"rstdA{k}", [P, 1], F32) for k in range(2)]
                qkvu = sbt(ph, "qkvu", [P, INC], F32)
                sq = sbt(ph, "sq", [P, 640], F32)
                ssq = sbt(ph, "ssq", [P, 10], F32)
                rsq = sbt(ph, "rsq", [P, 10], F32)
                qn = sbt(ph, "qn", [P, 640], F32)
                rb = sbt(ph, "rb", [P, 640], F32)
                qr = sbt(ph, "qr", [P, 4, 2, 64], BF16)
                kr = sbt(ph, "kr", [P, 128], BF16)
                sig = sbt(ph, "sig", [P, 512], F32)
                gg = sbt(ph, "gg", [P, 512], BF16)
                ptb = pst(ph, "ptb", [P, 8, P], BF16)
                pq = [pst(ph, f"pq{k}", [P, 512], F32) for k in range(4)]
                ptq = pst(ph, "ptq", [P, 5, P], BF16)
                ptg = pst(ph, "ptg", [P, 4, P], BF16)
                ra = sq
                qT, qTc, kT, Vp, gT, gTc = M["qT"], M["qTc"], M["kT"], M["Vp"], M["gT"], M["gTc"]

                cslot2 = DmaSlot(S, "rope", group=True)
                S.dma("sp", lambda e: e.dma_start(out=cos2[:], in_=cos2_d.rearrange("(n p) f -> p n f", p=P)), cslot2, w=[cos2])
                S.dma("sp", lambda e: e.dma_start(out=sin2[:], in_=sin2_d.rearrange("(n p) f -> p n f", p=P)), cslot2, w=[sin2])
                S.dma("sp", lambda e: e.dma_start(out=graw[:, 0:64], in_=q_gain_d[l].partition_broadcast(P)), cslot2, w=[graw])
                S.dma("sp", lambda e: e.dma_start(out=graw[:, 64:128], in_=k_gain_d[l].partition_broadcast(P)), cslot2, w=[graw])
                cslot2.close()
                for j in range(8):
                    S.dma("sp", lambda e, j=j: e.dma_start(out=stg[:], in_=w_in_d[l, j * P:(j + 1) * P, :]), stg_slot, w=[stg])
                    S.op("act", lambda e, j=j: e.activation(win[:, j, :], stg[:], AF.Copy), r=[stg], w=[win])
                S.op("dve", lambda e: e.tensor_copy(gain[:, 0:512].rearrange("p (h d) -> p h d", h=8),
                                                    graw[:, 0:64].unsqueeze(1).to_broadcast([P, 8, 64])), r=[graw], w=[gain])
                S.op("dve", lambda e: e.tensor_copy(gain[:, 512:640].rearrange("p (h d) -> p h d", h=2),
                                                    graw[:, 64:128].unsqueeze(1).to_broadcast([P, 2, 64])), r=[graw], w=[gain])
                S.op("dve", lambda e: e.tensor_copy(sh1rep[:], modcol[:, :, 0:8].unsqueeze(3).to_broadcast([P, 2, 8, P])),
                     r=[modcol], w=[sh1rep])

                def make_bias(r_):
                    for cc in range(4):
                        w_ = min(512, INC - cc * 512)
                        for j in range(8):
                            S.op("pe", lambda e, cc=cc, j=j, w_=w_: e.matmul(
                                pq[cc][:, 0:w_], lhsT=sh1rep[:, r_, j, :], rhs=win[:, j, cc * 512:cc * 512 + w_],
                                start=(j == 0), stop=(j == 7)), r=[sh1rep, win], w=[pq[cc]])
                        S.op("act", lambda e, cc=cc, w_=w_: e.activation(
                            bias_bc[:, cc * 512:cc * 512 + w_], pq[cc][:, 0:w_], AF.Copy), r=[pq[cc]], w=[bias_bc])

                def load(i):
                    k = i % 2
                    S.dma("sp", lambda e: e.dma_start(out=xt[k][:], in_=src_aps[i]), xt_slot[k], r=[xs_buf[i]], w=[xt[k]])

                make_bias(0)
                load(0)
                for i in range(NT if stop not in ("a0", "a1") else (0 if stop == "a0" else 1)):
                    k = i % 2
                    r_ = 0 if i < NTL else 1
                    is_ctx = i >= NTL
                    if i == NTL:
                        make_bias(1)
                    if i + 1 < NT:
                        load(i + 1)
                    S.op("act", lambda e: e.activation(junk[:], xt[k][:], AF.Square, accum_out=ss[k][:]), r=[xt[k]], w=[junk, ss[k]])
                    S.op("act", lambda e: e.activation(xb[k][:], xt[k][:], AF.Copy), r=[xt[k]], w=[xb[k]])
                    rstd_from_ss(ss[k], rstd[k], 1.0 / D, eps6)
                    for j in range(8):
                        S.op("pe", lambda e, j=j: e.transpose(ptb[:, j, :], xb[k][:, j * P:(j + 1) * P], ident_b[:]),
                             r=[xb[k], ident_b], w=[ptb])
                    S.op("dve", lambda e: e.tensor_tensor(xT[k][:], ptb[:],
                                                          modcol[:, r_, 8:16].unsqueeze(2).to_broadcast([P, 8, P]), op=ALU.mult),
                         r=[ptb, modcol], w=[xT[k]])
                    if CUT <= 1:
                        continue
                    for cc in range(4):
                        w_ = min(512, INC - cc * 512)
                        for j in range(8):
                            S.op("pe", lambda e, cc=cc, j=j, w_=w_: e.matmul(
                                pq[cc][:, 0:w_], lhsT=xT[k][:, j, :], rhs=win[:, j, cc * 512:cc * 512 + w_],
                                start=(j == 0), stop=(j == 7)), r=[xT[k], win], w=[pq[cc]])
                        S.op("dve", lambda e, cc=cc, w_=w_: e.scalar_tensor_tensor(
                            out=qkvu[:, cc * 512:cc * 512 + w_], in0=pq[cc][:, 0:w_], scalar=rstd[k][:, 0:1],
                            in1=bias_bc[:, cc * 512:cc * 512 + w_], op0=ALU.mult, op1=ALU.add),
                             r=[pq[cc], rstd[k], bias_bc], w=[qkvu])
                    if l == 0:
                        dbg_store("d_qkvu", slice(i * P, (i + 1) * P), qkvu[:], [qkvu])
                    if CUT <= 2:
                        continue
                    S.op("pool", lambda e: e.tensor_tensor(sq[:], qkvu[:, 0:640], qkvu[:, 0:640], op=ALU.mult), r=[qkvu], w=[sq])
                    S.op("dve", lambda e: e.tensor_reduce(out=ssq[:], in_=sq[:].rearrange("p (h d) -> p h d", h=10),
                                                          axis=AX.X, op=ALU.add), r=[sq], w=[ssq])
                    rstd_from_ss(ssq, rsq, 1.0 / 64, eps6)
                    S.op("dve", lambda e: e.tensor_tensor(qn[:].rearrange("p (h d) -> p h d", h=10),
                                                          qkvu[:, 0:640].rearrange("p (h d) -> p h d", h=10),
                                                          rsq[:].unsqueeze(2).to_broadcast([P, 10, 64]), op=ALU.mult),
                         r=[qkvu, rsq], w=[qn])
                    S.op("pool", lambda e: e.tensor_tensor(qn[:], qn[:], gain[:], op=ALU.mult), r=[qn, gain], w=[qn])
                    if CUT <= 3:
                        continue
                    if not is_ctx:
                        qn3 = qn[:].rearrange("p (h d) -> p h d", h=10)
                        rb3 = rb[:].rearrange("p (h d) -> p h d", h=10)
                        S.op("dve", lambda e: e.tensor_tensor(ra[:].rearrange("p (h d) -> p h d", h=10), qn3,
                                                              cos2[:, i, :].unsqueeze(1).to_broadcast([P, 10, 64]), op=ALU.mult),
                             r=[qn, cos2], w=[ra])
                        S.op("pool", lambda e: e.tensor_tensor(rb3[:, :, 0:32], qn3[:, :, 32:64],
                                                               sin2[:, i, 0:32].unsqueeze(1).to_broadcast([P, 10, 32]), op=ALU.mult),
                             r=[qn, sin2], w=[rb])
                        S.op("pool", lambda e: e.tensor_tensor(rb3[:, :, 32:64], qn3[:, :, 0:32],
                                                               sin2[:, i, 32:64].unsqueeze(1).to_broadcast([P, 10, 32]), op=ALU.mult),
                             r=[qn, sin2], w=[rb])
                        S.op("dve", lambda e: e.tensor_tensor(qr[:].rearrange("p pr hh d -> p hh pr d"),
                                                              ra[:, 0:512].rearrange("p (hh pr d) -> p hh pr d", hh=2, pr=4),
                                                              rb[:, 0:512].rearrange("p (hh pr d) -> p hh pr d", hh=2, pr=4), op=ALU.add),
                             r=[ra, rb], w=[qr])
                        S.op("pool", lambda e: e.tensor_tensor(kr[:], ra[:, 512:640], rb[:, 512:640], op=ALU.add), r=[ra, rb], w=[kr])
                    else:
                        S.op("dve", lambda e: e.tensor_copy(qr[:].rearrange("p pr hh d -> p hh pr d"),
                                                            qn[:, 0:512].rearrange("p (hh pr d) -> p hh pr d", hh=2, pr=4)),
                             r=[qn], w=[qr])
                        S.op("pool", lambda e: e.tensor_copy(kr[:], qn[:, 512:640]), r=[qn], w=[kr])
                    if CUT <= 4:
                        continue
                    need_q = (not is_ctx) or (not last)
                    if need_q:
                        for pr in range(4):
                            S.op("pe", lambda e, pr=pr: e.transpose(ptq[:, pr, :], qr[:, pr, :, :].rearrange("p hh d -> p (hh d)"), ident_b[:]),
                                 r=[qr, ident_b], w=[ptq])
                    S.op("pe", lambda e: e.transpose(ptq[:, 4, :], kr[:], ident_b[:]), r=[kr, ident_b], w=[ptq])
                    if need_q:
                        if not is_ctx:
                            S.op("act", lambda e: e.activation(qT[:, :, i * P:(i + 1) * P], ptq[:, 0:4, :], AF.Copy),
                                 r=[ptq], w=[M["qT_b"][pr][i // 4] for pr in range(4)])
                        else:
                            S.op("act", lambda e: e.activation(qTc[:, :, (i - NTL) * P:(i - NTL + 1) * P], ptq[:, 0:4, :], AF.Copy),
                                 r=[ptq], w=[qTc])
                    S.op("act", lambda e: e.activation(kT[:, i * P:(i + 1) * P], ptq[:, 4, :], AF.Copy), r=[ptq], w=[M["kT_b"][i]])
                    if CUT <= 5:
                        continue
                    S.op("pool", lambda e: e.tensor_copy(Vp[:, i, 0:64], qkvu[:, 640:704]), r=[qkvu], w=[M["Vp_b"][i]])
                    S.op("pool", lambda e: e.tensor_copy(Vp[:, i, 128:192], qkvu[:, 704:768]), r=[qkvu], w=[M["Vp_b"][i]])
                    if CUT <= 6:
                        continue
                    if need_q:
                        S.op("act", lambda e: e.activation(sig[:], qkvu[:, 1280:1792], AF.Sigmoid), r=[qkvu], w=[sig])
                        S.op("dve", lambda e: e.tensor_tensor(gg[:], qkvu[:, 768:1280], sig[:], op=ALU.mult), r=[qkvu, sig], w=[gg])
                        for c in range(4):
                            S.op("pe", lambda e, c=c: e.transpose(ptg[:, c, :], gg[:, c * P:(c + 1) * P], ident_b[:]),
                                 r=[gg, ident_b], w=[ptg])
                        if not is_ctx:
                            S.op("act", lambda e: e.activation(gT[:, :, GPAD + i * P:GPAD + (i + 1) * P], ptg[:], AF.Copy),
                                 r=[ptg], w=[M["gT_b"][i]])
                        else:
                            S.op("act", lambda e: e.activation(gTc[:, :, GPAD + (i - NTL) * P:GPAD + (i - NTL + 1) * P], ptg[:], AF.Copy),
                                 r=[ptg], w=[M["gTc_b"][i - NTL]])
                S.barrier()

        def phase_b(l, M, last):
            with ExitStack() as ph:
                NPT = 4
                pT = [sbt(ph, f"pT{k}", [P, 512], BF16) for k in range(NPT)]
                rd = sbt(ph, "rd", [P, 512], F32)
                bcs = sbt(ph, "bcs", [P, 512], F32)
                ps_s = [pst(ph, f"ps_s{k}", [P, 512], F32) for k in range(4)]
                po = [pst(ph, f"po{k}", [P, 512], F32) for k in range(2)]
                pbc = pst(ph, "pbc", [P, 512], F32)
                qT, qTc, kT, Vp = M["qT"], M["qTc"], M["kT"], M["Vp"]
                cnt = [0]

                def attend(qsrc_fn, qbuf, n, ktiles):
                    for kt_i, kt in enumerate(ktiles):
                        for hh in range(2):
                            k = cnt[0] % 4
                            kp = cnt[0] % NPT
                            cnt[0] += 1
                            S.op("pe", lambda e, hh=hh, kt=kt, k=k: e.matmul(
                                ps_s[k][:, 0:n], lhsT=kT[hh * 64:(hh + 1) * 64, kt * P:(kt + 1) * P], rhs=qsrc_fn(hh),
                                start=True, stop=True), r=[M["kT_b"][kt], qbuf], w=[ps_s[k]])
                            S.op("act", lambda e, k=k, kp=kp: e.activation(pT[kp][:, 0:n], ps_s[k][:, 0:n], AF.Exp, scale=0.125),
                                 r=[ps_s[k]], w=[pT[kp]])
                            S.op("pe", lambda e, hh=hh, kt=kt, kp=kp, kt_i=kt_i: e.matmul(
                                po[hh][:, 0:n], lhsT=Vp[:, kt, hh * 64:hh * 64 + 128], rhs=pT[kp][:, 0:n],
                                start=(kt_i == 0), stop=(kt_i == len(ktiles) - 1)), r=[M["Vp_b"][kt], pT[kp]], w=[po[hh]])
                    for hh in range(2):
                        dp = 64 if hh == 0 else 0
                        S.op("dve", lambda e, hh=hh, dp=dp: e.reciprocal(rd[dp:dp + 1, 0:n], po[hh][dp:dp + 1, 0:n]),
                             r=[po[hh]], w=[rd])
                        S.op("pe", lambda e, dp=dp: e.matmul(pbc[0:64, 0:n], lhsT=ones_f[dp:dp + 1, 0:64], rhs=rd[dp:dp + 1, 0:n],
                                                            start=True, stop=True), r=[ones_f, rd], w=[pbc])
                        S.op("act", lambda e, hh=hh: e.activation(bcs[hh * 64:(hh + 1) * 64, 0:n], pbc[0:64, 0:n], AF.Copy),
                             r=[pbc], w=[bcs])
                        S.op("dve", lambda e, hh=hh: e.tensor_tensor(qsrc_fn(hh), po[hh][hh * 64:(hh + 1) * 64, 0:n],
                                                                     bcs[hh * 64:(hh + 1) * 64, 0:n], op=ALU.mult),
                             r=[po[hh], bcs], w=[qbuf])

                for c in range(8):
                    for pr in range(4):
                        attend(lambda hh, c=c, pr=pr: qT[hh * 64:(hh + 1) * 64, pr, c * 512:(c + 1) * 512],
                               M["qT_b"][pr][c], 512, list(range(NT)))
                if not last:
                    for pr in range(4):
                        attend(lambda hh, pr=pr: qTc[hh * 64:(hh + 1) * 64, pr, :], qTc.b, S_CTX, [NTL, NTL + 1])
                S.barrier()

        def phase_c(l, M, tiles, src_aps, r_, modbc):
            with ExitStack() as ph:
                wcol = sbt(ph, "wcol", [P, 4, 31], F32)
                DG = sbt(ph, "DG", [P, 4, 31, P], BF16)
                wo = sbt(ph, "wo", [P, 8, D], BF16)
                stg = [sbt(ph, f"stgC{k}", [P, D], F32) for k in range(2)]
                stg_slot = [DmaSlot(S, f"stgC{k}") for k in range(2)]
                betaA = sbt(ph, "betaA", [P, 8], F32)
                betac = sbt(ph, "betac", [P, 8], F32)
                cb_bc = sbt(ph, "cb_bc", [P, 512], F32)
                lg_bc = sbt(ph, "lg_bc", [P, 512], F32)
                lb_bc = sbt(ph, "lb_bc", [P, 512], F32)
                xt = [sbt(ph, f"xtC{k}", [P, D], F32) for k in range(2)]
                xt_slot = [DmaSlot(S, f"xtC{k}") for k in range(2)]
                y = sbt(ph, "yC", [P, 512], F32)
                junk = sbt(ph, "junkC", [P, 512], F32)
                st4 = sbt(ph, "st4", [P, 4], F32)
                rln = sbt(ph, "rln", [P, 1], F32)
                msq = sbt(ph, "msq", [P, 1], F32)
                z = sbt(ph, "zC", [P, 512], F32)
                oc = sbt(ph, "oc", [P, 512], F32)
                ocb = sbt(ph, "ocb", [P, 512], BF16)
                ocT = sbt(ph, "ocT", [P, 4, P], BF16)
                sqo = sbt(ph, "sqo", [P, 4, P], F32)
                ssc = sbt(ph, "ssc", [P, 2], F32)
                rsc = sbt(ph, "rsc", [P, 2], F32)
                t1 = sbt(ph, "t1", [P, D], F32)
                x1 = [sbt(ph, f"x1_{k}", [P, D], F32) for k in range(2)]
                x1_slot = [DmaSlot(S, f"x1s{k}") for k in range(2)]
                py = pst(ph, "py", [P, 512], F32)
                ptc = pst(ph, "ptc", [P, 4, P], BF16)
                pss = pst(ph, "pss", [P, 16], F32)
                pa = [pst(ph, f"pa{k}", [P, 512], F32) for k in range(2)]
                pc = [pst(ph, f"pc{k}", [P, 512], F32) for k in range(2)]
                qT, qTc, gT, gTc = M["qT"], M["qTc"], M["gT"], M["gTc"]

                vs = DmaSlot(S, "vecC", group=True)
                for c in range(4):
                    S.dma("sp", lambda e, c=c: e.dma_start(out=wcol[:, c, :], in_=conv_w_d[l, :, c * P:(c + 1) * P].rearrange("j p -> p j"),
                                                           allow_slow_non_contiguous=True), vs, w=[wcol])
                for half in range(2):
                    S.dma("sp", lambda e, half=half: e.dma_start(out=betaA[half * 64:(half + 1) * 64, :],
                                                                 in_=beta_attn_d[l].rearrange("(h d) -> d h", d=64),
                                                                 allow_slow_non_contiguous=True), vs, w=[betaA])
                S.dma("sp", lambda e: e.dma_start(out=betac[:, 4:8], in_=beta_conv_d[l].rearrange("(c p) -> p c", p=P),
                                                  allow_slow_non_contiguous=True), vs, w=[betac])
                S.dma("sp", lambda e: e.dma_start(out=cb_bc[:], in_=conv_b_d[l].partition_broadcast(P)), vs, w=[cb_bc])
                S.dma("sp", lambda e: e.dma_start(out=lg_bc[:], in_=ln_g_d[l].partition_broadcast(P)), vs, w=[lg_bc])
                S.dma("sp", lambda e: e.dma_start(out=lb_bc[:], in_=ln_b_d[l].partition_broadcast(P)), vs, w=[lb_bc])
                vs.close()
                S.op("dve", lambda e: e.tensor_copy(betac[0:64, 0:4], betaA[0:64, 0:4]), r=[betaA], w=[betac])
                S.op("dve", lambda e: e.tensor_copy(betac[64:128, 0:4], betaA[64:128, 4:8]), r=[betaA], w=[betac])
                for kk in range(8):
                    t = stg[kk % 2]
                    sl = stg_slot[kk % 2]
                    if kk < 4:
                        S.dma("sp", lambda e, t=t, kk=kk: e.dma_start(out=t[0:64, :], in_=w_o_d[l, kk * 64:(kk + 1) * 64, :]), sl, w=[t])
                        S.dma("sp", lambda e, t=t, kk=kk: e.dma_start(out=t[64:128, :], in_=w_o_d[l, (kk + 4) * 64:(kk + 5) * 64, :]), sl, w=[t])
                    else:
                        S.dma("sp", lambda e, t=t, kk=kk: e.dma_start(out=t[:], in_=w_o_d[l, 512 + (kk - 4) * P:512 + (kk - 3) * P, :]), sl, w=[t])
                    S.op("act", lambda e, t=t, kk=kk: e.activation(wo[:, kk, :], t[:], AF.Identity, scale=betac[:, kk:kk + 1]),
                         r=[t, betac], w=[wo])
                for c in range(4):
                    for j in range(31):
                        S.op("pool", lambda e, c=c, j=j: e.tensor_scalar(DG[:, c, j, :], ident_f[:], wcol[:, c, j:j + 1], None, op0=ALU.mult),
                             r=[ident_f, wcol], w=[DG])

                def load(n_):
                    i = tiles[n_]
                    k = n_ % 2
                    S.dma("sp", lambda e: e.dma_start(out=xt[k][:], in_=src_aps[i]), xt_slot[k], r=[xs_buf[i]], w=[xt[k]])

                load(0)
                for n_, i in enumerate(tiles):
                    k = n_ % 2
                    is_ctx = i >= NTL
                    if n_ + 1 < len(tiles):
                        load(n_ + 1)
                    if not is_ctx:
                        gsrc, gb, t0 = gT, M["gT_b"], i * P
                        lo, hi = max(0, i - 1), min(NTL - 1, i + 1)
                        osrc = lambda pr: qT[:, pr, i * P:(i + 1) * P]
                        obufs = [M["qT_b"][pr][i // 4] for pr in range(4)]
                    else:
                        gsrc, gb, t0 = gTc, M["gTc_b"], (i - NTL) * P
                        lo, hi = max(0, i - NTL - 1), min(NTC - 1, i - NTL + 1)
                        osrc = lambda pr: qTc[:, pr, (i - NTL) * P:(i - NTL + 1) * P]
                        obufs = [qTc.b]
                    for c in range(4):
                        for j in range(31):
                            S.op("pe", lambda e, c=c, j=j: e.matmul(py[:, c * P:(c + 1) * P], lhsT=gsrc[:, c, t0 + j + GPAD - 15:t0 + j + GPAD - 15 + P],
                                                                  rhs=DG[:, c, j, :], start=(j == 0), stop=(j == 30)),
                                 r=[gb[q] for q in range(lo, hi + 1)] + [DG], w=[py])
                    S.op("dve", lambda e: e.tensor_tensor(y[:], py[:], cb_bc[:], op=ALU.add), r=[py, cb_bc], w=[y])
                    S.op("act", lambda e: e.activation(junk[:], y[:], AF.Identity, accum_out=st4[:, 0:1]), r=[y], w=[junk, st4])
                    S.op("act", lambda e: e.activation(junk[:], y[:], AF.Square, accum_out=st4[:, 1:2]), r=[y], w=[junk, st4])
                    S.op("dve", lambda e: e.tensor_scalar(st4[:, 2:4], st4[:, 0:2], 1.0 / 512, None, op0=ALU.mult), r=[st4], w=[st4])
                    S.op("dve", lambda e: e.tensor_tensor(msq[:], st4[:, 2:3], st4[:, 2:3], op=ALU.mult), r=[st4], w=[msq])
                    S.op("dve", lambda e: e.tensor_tensor(msq[:], st4[:, 3:4], msq[:], op=ALU.subtract), r=[st4, msq], w=[msq])
                    rstd_from_ss(msq, rln, 1.0, eps5)
                    S.op("dve", lambda e: e.tensor_scalar(z[:], y[:], st4[:, 2:3], rln[:, 0:1], op0=ALU.subtract, op1=ALU.mult),
                         r=[y, st4, rln], w=[z])
                    S.op("pool", lambda e: e.tensor_tensor(z[:], z[:], lg_bc[:], op=ALU.mult), r=[z, lg_bc], w=[z])
                    S.op("pool", lambda e: e.tensor_tensor(z[:], z[:], lb_bc[:], op=ALU.add), r=[z, lb_bc], w=[z])
                    S.op("act", lambda e: e.activation(oc[:], z[:], AF.Silu), r=[z], w=[oc])
                    S.op("act", lambda e: e.activation(junk[:], oc[:], AF.Square, accum_out=ssc[:, 0:1]), r=[oc], w=[junk, ssc])
                    S.op("pool", lambda e: e.tensor_copy(ocb[:], oc[:]), r=[oc], w=[ocb])
                    for c in range(4):
                        S.op("pe", lambda e, c=c: e.transpose(ptc[:, c, :], ocb[:, c * P:(c + 1) * P], ident_b[:]),
                             r=[ocb, ident_b], w=[ptc])
                    S.op("act", lambda e: e.activation(ocT[:], ptc[:], AF.Copy), r=[ptc], w=[ocT])
                    for pr in range(4):
                        S.op("pool", lambda e, pr=pr: e.tensor_tensor(sqo[:, pr, :], osrc(pr), osrc(pr), op=ALU.mult), r=obufs, w=[sqo])
                    for pr in range(4):
                        S.op("pe", lambda e, pr=pr: e.matmul(pss[:, 0:2], lhsT=sqo[:, pr, :], rhs=ones_f[:, 0:2],
                                                            start=(pr == 0), stop=(pr == 3)), r=[sqo, ones_f], w=[pss])
                    S.op("dve", lambda e: e.tensor_copy(ssc[:, 1:2], pss[:, 0:1]), r=[pss], w=[ssc])
                    rstd_from_ss(ssc, rsc, 1.0 / 512, eps6)
                    for cc in range(2):
                        for pr in range(4):
                            S.op("pe", lambda e, cc=cc, pr=pr: e.matmul(pa[cc][:], lhsT=osrc(pr), rhs=wo[:, pr, cc * 512:(cc + 1) * 512],
                                                                      start=(pr == 0), stop=(pr == 3)), r=obufs + [wo], w=[pa[cc]])
                        for c in range(4):
                            S.op("pe", lambda e, cc=cc, c=c: e.matmul(pc[cc][:], lhsT=ocT[:, c, :], rhs=wo[:, 4 + c, cc * 512:(cc + 1) * 512],
                                                                    start=(c == 0), stop=(c == 3)), r=[ocT, wo], w=[pc[cc]])
                        sl = slice(cc * 512, (cc + 1) * 512)
                        S.op("dve", lambda e, cc=cc, sl=sl: e.tensor_scalar(t1[:, sl], pa[cc][:], rsc[:, 1:2], None, op0=ALU.mult),
                             r=[pa[cc], rsc], w=[t1])
                        S.op("dve", lambda e, cc=cc, sl=sl: e.scalar_tensor_tensor(out=t1[:, sl], in0=pc[cc][:], scalar=rsc[:, 0:1],
                                                                                   in1=t1[:, sl], op0=ALU.mult, op1=ALU.add),
                             r=[pc[cc], rsc, t1], w=[t1])
                    S.op("pool", lambda e: e.tensor_tensor(t1[:], t1[:], modbc[:, 0:1024], op=ALU.mult), r=[t1, modbc], w=[t1])
                    S.op("pool", lambda e: e.tensor_tensor(x1[k][:], t1[:], xt[k][:], op=ALU.add), r=[t1, xt[k]], w=[x1[k]])
                    S.dma("sp", lambda e: e.dma_start(out=xs_d[i * P:(i + 1) * P, :], in_=x1[k][:]), x1_slot[k], r=[x1[k]], w=[xs_buf[i]])
                    if l == 0:
                        dbg_store("d_x1", slice(i * P, (i + 1) * P), x1[k][:], [x1[k]])
                S.barrier()

        def phase_p(l, tiles, modbc, last):
            with ExitStack() as ph:
                wq = sbt(ph, "wq", [P, 8, D], F32)
                keysTz = sbt(ph, "keysTz", [P, 8, 2, P], F32)
                iota16 = sbt(ph, "iota16", [P, 128, 16], F32)
                xt = [sbt(ph, f"xtP{k}", [P, D], F32) for k in range(2)]
                xt_slot = [DmaSlot(S, f"xtP{k}") for k in range(2)]
                junk = sbt(ph, "junkP", [P, D], BF16)
                ss = sbt(ph, "ssP", [P, 1], F32)
                rstd = sbt(ph, "rstdP", [P, 1], F32)
                h = sbt(ph, "hP", [P, D], F32)
                hT = sbt(ph, "hT", [P, 8, P], F32)
                qpT = hT
                qp = sbt(ph, "qp", [P, D], F32)
                sc = sbt(ph, "scP", [P, 16, 128], F32)
                wk = sbt(ph, "wkP", [P, 16, 128], F32)
                eq_ap = wk[:].rearrange("p a b -> p (a b)").rearrange("p (x c) -> p x c", c=16)
                topv = sbt(ph, "topv", [P, 16, 16], F32)
                idx = sbt(ph, "idxP", [P, 16, 16], U32)
                idxf = sbt(ph, "idxf", [P, 16, 16], F32)
                cand = sbt(ph, "cand", [P, 8, 256], F32)
                wk2 = sbt(ph, "wk2", [P, 8, 256], F32)
                best = sbt(ph, "best", [P, 8, 16], F32)
                pos = sbt(ph, "pos", [P, 8, 16], U32)
                pab = sbt(ph, "pab", [P, 2, 128], U32)
                abf = sbt(ph, "abf", [P, 2, 128], F32)
                isel = sbt(ph, "isel", [P, 2, 128], F32)
                ef = sbt(ph, "ef", [P, 128], F32)
                eidx = sbt(ph, "eidx", [P, 128], I32)
                ew = sbt(ph, "ew", [P, 128], F32)
                se = sbt(ph, "se", [P, 8], F32)
                wgt = sbt(ph, "wgt", [P, 128], F32)
                pre = sbt(ph, "pre", [P, 128], F32)
                aw = sbt(ph, "aw", [P, 128], F32)
                gbuf = [sbt(ph, f"gbuf{k}", [P, 2 * D], BF16) for k in range(NSLOT)]
                diag = [sbt(ph, f"diag{k}", [P, P], BF16) for k in range(4)]
                pre_b = [Buf(f"pre_b{k}") for k in range(4)]
                di = [0]
                gslot = [DmaSlot(S, f"g{k}") for k in range(NSLOT)]
                x2 = [sbt(ph, f"x2_{k}", [P, D], F32) for k in range(2)]
                x2_slot = [DmaSlot(S, f"x2s{k}") for k in range(2)]
                fg_bc = sbt(ph, "fg_bc", [P, D], F32)
                pT2 = pst(ph, "pT2", [P, 8, P], F32)
                pqp = [pst(ph, f"pqp{k}", [P, 512], F32) for k in range(2)]
                psc = [pst(ph, f"psc{k}", [P, 4, P], F32) for k in range(4)]

                S.op("pool", lambda e: e.iota(iota16[:], pattern=[[0, 128], [1, 16]], base=0, channel_multiplier=0,
                                              allow_small_or_imprecise_dtypes=True), w=[iota16])
                ws = DmaSlot(S, "wqload", group=True)
                for j in range(8):
                    S.dma("sp", lambda e, j=j: e.dma_start(out=wq[:, j, :], in_=peer_wq_d[l, j * P:(j + 1) * P, :]), ws, w=[wq])
                kraw_ap = sc[:].rearrange("p a b -> p (a b)")[:, 0:1024].rearrange("p (a d) -> p a d", d=64)
                keysT_ap = cand[:].rearrange("p a b -> p (a b)")[:, 0:1024].rearrange("p (a k) -> p a k", k=P)
                S.dma("sp", lambda e: e.dma_start(out=kraw_ap, in_=peer_keys_d[l].rearrange("h c k d -> k (h c) d")), ws, w=[sc])
                if last:
                    S.dma("sp", lambda e: e.dma_start(out=fg_bc[:], in_=final_g_d[0].partition_broadcast(P)), ws, w=[fg_bc])
                ws.close()
                for hh in range(8):
                    S.op("pe", lambda e, hh=hh: e.transpose(pT2[:, hh, :], kraw_ap[:, 2 * hh:2 * hh + 2, :].rearrange("p c d -> p (c d)"), ident_f[:]),
                         r=[sc, ident_f], w=[pT2])
                S.op("act", lambda e: e.activation(keysT_ap, pT2[:], AF.Copy), r=[pT2], w=[cand])
                S.op("pool", lambda e: e.memset(keysTz[:], 0.0), w=[keysTz])
                S.op("dve", lambda e: e.tensor_copy(keysTz[0:64, :, 0, :], keysT_ap[0:64, :, :]), r=[cand], w=[keysTz])
                S.op("dve", lambda e: e.tensor_copy(keysTz[64:128, :, 1, :], keysT_ap[64:128, :, :]), r=[cand], w=[keysTz])

                gi = [0]

                def load(n_):
                    i = tiles[n_]
                    k = n_ % 2
                    S.dma("sp", lambda e: e.dma_start(out=xt[k][:], in_=xs_d[i * P:(i + 1) * P, :]), xt_slot[k], r=[xs_buf[i]], w=[xt[k]])

                load(0)
                for n_, i in enumerate(tiles):
                    k = n_ % 2
                    if n_ + 1 < len(tiles):
                        load(n_ + 1)
                    S.op("act", lambda e: e.activation(junk[:], xt[k][:], AF.Square, accum_out=ss[:]), r=[xt[k]], w=[junk, ss])
                    rstd_from_ss(ss, rstd, 1.0 / D, eps6)
                    S.op("dve", lambda e: e.scalar_tensor_tensor(out=h[:], in0=xt[k][:], scalar=rstd[:, 0:1], in1=modbc[:, 2048:3072],
                                                                 op0=ALU.mult, op1=ALU.mult), r=[xt[k], rstd, modbc], w=[h])
                    S.op("dve", lambda e: e.tensor_tensor(h[:], h[:], modbc[:, 1024:2048], op=ALU.add), r=[h, modbc], w=[h])
                    if CUT <= 11:
                        continue
                    for j in range(8):
                        S.op("pe", lambda e, j=j: e.transpose(pT2[:, j, :], h[:, j * P:(j + 1) * P], ident_f[:]), r=[h, ident_f], w=[pT2])
                    S.op("act", lambda e: e.activation(hT[:], pT2[:], AF.Copy), r=[pT2], w=[hT])
                    for cc in range(2):
                        for j in range(8):
                            S.op("pe", lambda e, cc=cc, j=j: e.matmul(pqp[cc][:], lhsT=hT[:, j, :], rhs=wq[:, j, cc * 512:(cc + 1) * 512],
                                                                    start=(j == 0), stop=(j == 7)), r=[hT, wq], w=[pqp[cc]])
                        S.op("act", lambda e, cc=cc: e.activation(qp[:, cc * 512:(cc + 1) * 512], pqp[cc][:], AF.Copy), r=[pqp[cc]], w=[qp])
                    if CUT <= 12:
                        continue
                    for hh in range(8):
                        S.op("pe", lambda e, hh=hh: e.transpose(pT2[:, hh, :], qp[:, hh * P:(hh + 1) * P], ident_f[:]), r=[qp, ident_f], w=[pT2])
                    S.op("act", lambda e: e.activation(qpT[:], pT2[:], AF.Copy), r=[pT2], w=[qpT])
                    if CUT <= 13:
                        continue
                    for hh in range(8):
                        S.op("pe", lambda e, hh=hh: e.matmul(psc[hh // 2][:, (hh % 2) * 2:(hh % 2) * 2 + 2, :], lhsT=qpT[:, hh, :],
                                                            rhs=keysTz[:, hh, :, :], start=True, stop=True),
                             r=[qpT, keysTz], w=[psc[hh // 2]])
                    for q in range(4):
                        S.op("act", lambda e, q=q: e.activation(sc[:, 4 * q:4 * q + 4, :], psc[q][:], AF.Copy), r=[psc[q]], w=[sc])
                    if CUT <= 14:
                        continue
                    for g in range(16):
                        S.op("dve", lambda e, g=g: e.max(out=topv[:, g, 0:8], in_=sc[:, g, :]), r=[sc], w=[topv])
                        S.op("dve", lambda e, g=g: e.max_index(out=idx[:, g, 0:8], in_max=topv[:, g, 0:8], in_values=sc[:, g, :]),
                             r=[sc, topv], w=[idx])
                        S.op("dve", lambda e, g=g: e.match_replace(out=wk[:, g, :], in_to_replace=topv[:, g, 0:8], in_values=sc[:, g, :],
                                                                   imm_value=-1e30), r=[sc, topv], w=[wk])
                        S.op("dve", lambda e, g=g: e.max(out=topv[:, g, 8:16], in_=wk[:, g, :]), r=[wk], w=[topv])
                        S.op("dve", lambda e, g=g: e.max_index(out=idx[:, g, 8:16], in_max=topv[:, g, 8:16], in_values=wk[:, g, :]),
                             r=[wk, topv], w=[idx])
                    S.op("dve", lambda e: e.tensor_copy(idxf[:], idx[:]), r=[idx], w=[idxf])
                    if CUT <= 15:
                        continue
                    tv4 = topv[:].rearrange("p (h c) a -> p h c a", c=2)
                    if4 = idxf[:].rearrange("p (h c) a -> p h c a", c=2)
                    S.op("dve", lambda e: e.tensor_tensor(cand[:].rearrange("p h (a b) -> p h a b", a=16),
                                                          tv4[:, :, 0, :].unsqueeze(3).to_broadcast([P, 8, 16, 16]),
                                                          tv4[:, :, 1, :].unsqueeze(2).to_broadcast([P, 8, 16, 16]), op=ALU.add),
                         r=[topv], w=[cand])
                    for hh in range(8):
                        S.op("dve", lambda e, hh=hh: e.max(out=best[:, hh, 0:8], in_=cand[:, hh, :]), r=[cand], w=[best])
                        S.op("dve", lambda e, hh=hh: e.max_index(out=pos[:, hh, 0:8], in_max=best[:, hh, 0:8], in_values=cand[:, hh, :]),
                             r=[cand, best], w=[pos])
                        S.op("dve", lambda e, hh=hh: e.match_replace(out=wk2[:, hh, :], in_to_replace=best[:, hh, 0:8],
                                                                     in_values=cand[:, hh, :], imm_value=-1e30), r=[cand, best], w=[wk2])
                        S.op("dve", lambda e, hh=hh: e.max(out=best[:, hh, 8:16], in_=wk2[:, hh, :]), r=[wk2], w=[best])
                        S.op("dve", lambda e, hh=hh: e.max_index(out=pos[:, hh, 8:16], in_max=best[:, hh, 8:16], in_values=wk2[:, hh, :]),
                             r=[wk2, best], w=[pos])
                    if CUT <= 16:
                        continue
                    posf = pos[:].rearrange("p h s -> p (h s)")
                    S.op("dve", lambda e: e.tensor_single_scalar(pab[:, 0, :], posf, 4, op=ALU.logical_shift_right), r=[pos], w=[pab])
                    S.op("dve", lambda e: e.tensor_single_scalar(pab[:, 1, :], posf, 15, op=ALU.bitwise_and), r=[pos], w=[pab])
                    S.op("dve", lambda e: e.tensor_copy(abf[:], pab[:]), r=[pab], w=[abf])
                    for c in range(2):
                        S.op("dve", lambda e, c=c: e.tensor_tensor(eq_ap, iota16[:], abf[:, c, :].unsqueeze(2).to_broadcast([P, 128, 16]),
                                                                   op=ALU.is_equal), r=[iota16, abf], w=[wk])
                        S.op("dve", lambda e, c=c: e.tensor_tensor(eq_ap.rearrange("p (h s) a -> p h s a", h=8),
                                                                   eq_ap.rearrange("p (h s) a -> p h s a", h=8),
                                                                   if4[:, :, c, :].unsqueeze(2).to_broadcast([P, 8, 16, 16]), op=ALU.mult),
                             r=[wk, idxf], w=[wk])
                        S.op("dve", lambda e, c=c: e.tensor_reduce(out=isel[:, c, :], in_=eq_ap, axis=AX.X, op=ALU.add), r=[wk], w=[isel])
                    S.op("dve", lambda e: e.scalar_tensor_tensor(out=ef[:], in0=isel[:, 0, :], scalar=128.0, in1=isel[:, 1, :],
                                                                 op0=ALU.mult, op1=ALU.add), r=[isel], w=[ef])
                    S.op("dve", lambda e: e.tensor_scalar(ef[:], ef[:], float(l * NPEER), None, op0=ALU.add), r=[ef], w=[ef])
                    S.op("dve", lambda e: e.tensor_copy(eidx[:], ef[:]), r=[ef], w=[eidx])
                    if CUT <= 17:
                        continue
                    S.op("dve", lambda e: e.tensor_tensor(ew[:].rearrange("p (h s) -> p h s", h=8), best[:],
                                                          best[:, :, 0:1].to_broadcast([P, 8, 16]), op=ALU.subtract), r=[best], w=[ew])
                    S.op("act", lambda e: e.activation(ew[:], ew[:], AF.Exp), r=[ew], w=[ew])
                    S.op("dve", lambda e: e.tensor_reduce(out=se[:], in_=ew[:].rearrange("p (h s) -> p h s", h=8), axis=AX.X, op=ALU.add),
                         r=[ew], w=[se])
                    S.op("dve", lambda e: e.reciprocal(se[:], se[:]), r=[se], w=[se])
                    S.op("dve", lambda e: e.tensor_tensor(wgt[:].rearrange("p (h s) -> p h s", h=8), ew[:].rearrange("p (h s) -> p h s", h=8),
                                                          se[:].unsqueeze(2).to_broadcast([P, 8, 16]), op=ALU.mult), r=[ew, se], w=[wgt])
                    if stop == "p0":
                        dbg_store("d_eidx", slice(i * P, (i + 1) * P), ef[:], [ef])
                        dbg_store("d_w", slice(i * P, (i + 1) * P), wgt[:], [wgt])
                        dbg_store("d_pre", slice(i * P, (i + 1) * P), best[:].rearrange("p h s -> p (h s)"), [best])
                        continue
                    for g_ in range(128 // GRP):
                        pb_ = pre_b[g_ % 4]
                        for s_ in range(g_ * GRP, (g_ + 1) * GRP):
                            b_ = gi[0] % NSLOT
                            gi[0] += 1
                            S.dma("pool", lambda e, s_=s_, b_=b_: e.indirect_dma_start(
                                out=gbuf[b_][:], out_offset=None, in_=uv_d,
                                in_offset=bass.IndirectOffsetOnAxis(ap=eidx[:, s_:s_ + 1], axis=0)), gslot[b_], r=[eidx], w=[gbuf[b_]])
                            S.op("dve", lambda e, s_=s_, b_=b_: e.scalar_tensor_tensor(
                                out=junk[:], in0=h[:], scalar=1.0, in1=gbuf[b_][:, 0:D], op0=ALU.mult, op1=ALU.mult,
                                accum_out=pre[:, s_:s_ + 1]), r=[h, gbuf[b_]], w=[junk, pb_])
                        gs = slice(g_ * GRP, (g_ + 1) * GRP)
                        S.op("act", lambda e, gs=gs: e.activation(aw[:, gs], pre[:, gs], AF.Gelu), r=[pb_], w=[pb_])
                        S.op("dve", lambda e, gs=gs: e.tensor_tensor(aw[:, gs], aw[:, gs], wgt[:, gs], op=ALU.mult), r=[pb_, wgt], w=[pb_])
                        for s_ in range(g_ * GRP, (g_ + 1) * GRP):
                            b_ = (gi[0] - (g_ + 1) * GRP + s_) % NSLOT
                            dg = diag[di[0] % len(diag)]
                            di[0] += 1
                            S.op("act", lambda e, s_=s_, dg=dg: e.activation(dg[:], ident_b[:], AF.Identity, scale=aw[:, s_:s_ + 1]),
                                 r=[ident_b, pb_], w=[dg])
                            for cc in range(2):
                                S.op("pe", lambda e, s_=s_, b_=b_, dg=dg, cc=cc: e.matmul(
                                    pqp[cc][:], lhsT=dg[:], rhs=gbuf[b_][:, D + cc * 512:D + (cc + 1) * 512],
                                    start=(s_ == 0), stop=(s_ == 127)), r=[dg, gbuf[b_]], w=[pqp[cc]])
                    if l == 0:
                        dbg_store("d_pre", slice(i * P, (i + 1) * P), pre[:], pre_b)
                        dbg_store("d_eidx", slice(i * P, (i + 1) * P), ef[:], [ef])
                        dbg_store("d_w", slice(i * P, (i + 1) * P), wgt[:], [wgt])
                    for cc in range(2):
                        sl = slice(cc * 512, (cc + 1) * 512)
                        S.op("dve", lambda e, cc=cc, sl=sl: e.tensor_tensor(x2[k][:, sl], pqp[cc][:], modbc[:, 3072 + cc * 512:3072 + (cc + 1) * 512],
                                                                           op=ALU.mult), r=[pqp[cc], modbc], w=[x2[k]])
                    S.op("dve", lambda e: e.tensor_tensor(x2[k][:], x2[k][:], xt[k][:], op=ALU.add), r=[x2[k], xt[k]], w=[x2[k]])
                    if l == 0:
                        dbg_store("d_x2", slice(i * P, (i + 1) * P), x2[k][:], [x2[k]])
                    if last:
                        S.op("act", lambda e: e.activation(junk[:], x2[k][:], AF.Square, accum_out=ss[:]), r=[x2[k]], w=[junk, ss])
                        rstd_from_ss(ss, rstd, 1.0 / D, eps6)
                        S.op("dve", lambda e: e.scalar_tensor_tensor(out=x2[k][:], in0=x2[k][:], scalar=rstd[:, 0:1], in1=fg_bc[:],
                                                                     op0=ALU.mult, op1=ALU.mult), r=[x2[k], rstd, fg_bc], w=[x2[k]])
                        S.dma("sp", lambda e: e.dma_start(out=out_d[i * P:(i + 1) * P, :], in_=x2[k][:]), x2_slot[k], r=[x2[k]])
                    else:
                        S.dma("sp", lambda e: e.dma_start(out=xs_d[i * P:(i + 1) * P, :], in_=x2[k][:]), x2_slot[k], r=[x2[k]], w=[xs_buf[i]])
                S.barrier()

        lat_tiles = list(range(NTL))
        ctx_tiles = list(range(NTL, NT))
        if stop in (None, "p1"):
            convert_tables()
        for l in range(depth):
            last = (l == DEPTH - 1)
            if l == 0:
                src_aps = [x_d[i * P:(i + 1) * P, :] for i in range(NTL)] + [ctx_d[i * P:(i + 1) * P, :] for i in range(NTC)]
            else:
                src_aps = [xs_d[i * P:(i + 1) * P, :] for i in range(NT)]
            mod_cols(l)
            if l == 0:
                dbg_store("d_mod", (slice(0, P), slice(0, 32)), modcol[:].rearrange("p r c -> p (r c)"), [modcol])
                dbg_store("d_mod", (slice(0, P), slice(32, 48)), scT[:].rearrange("p j r -> p (j r)"), [scT])
            if stop == "m":
                break
            with ExitStack() as mix:
                M = {}
                M["qT"] = sbt(mix, "qT", [P, 4, S_LAT], BF16)
                M["qT_b"] = [[Buf(f"qT{pr}_{c}") for c in range(8)] for pr in range(4)]
                M["qTc"] = sbt(mix, "qTc", [P, 4, S_CTX], BF16)
                M["gT"] = sbt(mix, "gT", [P, 4, S_LAT + 2 * GPAD], BF16)
                M["gT_b"] = [Buf(f"gT{i}") for i in range(NTL)]
                M["gTc"] = sbt(mix, "gTc", [P, 4, S_CTX + 2 * GPAD], BF16)
                M["gTc_b"] = [Buf(f"gTc{i}") for i in range(NTC)]
                S.op("pool", lambda e: e.memset(M["gT"][:], 0.0), w=M["gT_b"])
                S.op("pool", lambda e: e.memset(M["gTc"][:], 0.0), w=M["gTc_b"])
                with ExitStack() as ab:
                    M["kT"] = sbt(ab, "kT", [P, NT * P], BF16)
                    M["kT_b"] = [Buf(f"kT{i}") for i in range(NT)]
                    M["Vp"] = sbt(ab, "Vp", [P, NT, 192], BF16)
                    M["Vp_b"] = [Buf(f"Vp{i}") for i in range(NT)]
                    S.op("pool", lambda e: e.memset(M["Vp"][:], 1.0), w=M["Vp_b"])
                    phase_a(l, M, src_aps, last)
                    if stop in ("a", "a0", "a1"):
                        break
                    phase_b(l, M, last)
                if stop == "b":
                    break
                with ExitStack() as cs:
                    modbc = sbt(cs, "modbcC", [P, 4096], F32)
                    mod_bc(l, 0, modbc)
                    phase_c(l, M, lat_tiles, src_aps, 0, modbc)
                    if not last:
                        mod_bc(l, 1, modbc)
                        phase_c(l, M, ctx_tiles, src_aps, 1, modbc)
            if stop == "c":
                break
            with ExitStack() as pp:
                modbc = sbt(pp, "modbcP", [P, 4096], F32)
                mod_bc(l, 0, modbc)
                phase_p(l, lat_tiles if stop not in ("p1", "p0") else lat_tiles[:1], modbc, last)
                if not last and stop not in ("p1", "p0"):
                    mod_bc(l, 1, modbc)
                    phase_p(l, ctx_tiles, modbc, last)
            if stop in ("p1", "p0"):
                break
        out_slot.close()
        if out_slot.sem is not None:
            for en in ("sp", "act", "pool"):
                S.E[en].eng.wait_ge(out_slot.sem, out_slot.val)
        S.barrier()
        print(f"[build] instr={S.ninstr} waits={S.nwait} sems={S.nsem}")
    return nc


_NC_CACHE = {}


def _rope_tables():
    n = 16
    inv = (10000.0 ** (-np.arange(n, dtype=np.float32) / n)).astype(np.float32)
    row = np.repeat(np.arange(64), 64).astype(np.float32)
    col = np.tile(np.arange(64), 64).astype(np.float32)
    ang = np.concatenate([row[:, None] * inv, col[:, None] * inv], axis=-1).astype(np.float32)
    cos = np.cos(ang).astype(np.float32)
    sin = np.sin(ang).astype(np.float32)
    cos2 = np.concatenate([cos, cos], axis=-1)
    sin2 = np.concatenate([-sin, sin], axis=-1)
    return np.ascontiguousarray(cos2), np.ascontiguousarray(sin2)


def make_in_maps(inputs, cores):
    f = lambda a: np.ascontiguousarray(np.asarray(a, dtype=np.float32))
    cos2, sin2 = _rope_tables()
    shared = {k: f(inputs[k]) for k in ["w_mod", "b_mod", "w_in", "q_gain", "k_gain", "conv_w", "conv_b", "ln_g", "ln_b",
                                        "beta_attn", "beta_conv", "w_o", "peer_wq", "peer_keys", "peer_u", "peer_v"]}
    shared["final_g"] = f(inputs["final_g"]).reshape(1, D)
    shared["cos2"] = cos2
    shared["sin2"] = sin2
    shared["cctxT"] = np.ascontiguousarray(f(inputs["c_ctx"]).reshape(8, P).T)
    x = f(inputs["x"])
    ctx = f(inputs["ctx"])
    c = f(inputs["c"])
    maps = []
    for b in cores:
        m = dict(shared)
        m["x"] = x[b]
        m["ctx"] = ctx[b]
        m["cT"] = np.ascontiguousarray(c[b].reshape(8, P).T)
        maps.append(m)
    return maps


def kernel(**inputs):
    if "nc" not in _NC_CACHE:
        _NC_CACHE["nc"] = build()
    nc = _NC_CACHE["nc"]
    B = np.asarray(inputs["x"]).shape[0]
    maps = make_in_maps(inputs, list(range(B)))
    res = run_bass_kernel_spmd(nc, maps, core_ids=list(range(B)))
    out = np.stack([np.asarray(r["out"], dtype=np.float32) for r in res.results], axis=0)
    return out
```

```python
import numpy as np
from contextlib import ExitStack
import concourse.bass as bass
import concourse.mybir as mybir
from concourse.bass_utils import run_bass_kernel_spmd

F32 = mybir.dt.float32
BF16 = mybir.dt.bfloat16
I32 = mybir.dt.int32
U32 = mybir.dt.uint32
ALU = mybir.AluOpType
AF = mybir.ActivationFunctionType
AX = mybir.AxisListType

import os
CUT = int(os.environ.get("KCUT", "99"))
SEM_LIMIT = 30000


class Buf:
    __slots__ = ("name", "w", "r")

    def __init__(self, name):
        self.name = name
        self.w = None
        self.r = {}


class Eng:
    def __init__(self, sync, name, eng):
        self.sync = sync
        self.name = name
        self.eng = eng
        self.sem = None
        self.count = 0
        self.known = {}
        self.last = None

    def new_event(self):
        if self.sem is None or self.count >= SEM_LIMIT:
            self.sem = self.sync.new_sem(self.name)
            self.count = 0
        self.count += 1
        self.last = [self.sem, self.count, self.name]
        return self.last


def DmaSlot(sync, name, group=False):
    if name not in sync.slot_by_name:
        sync.slot_by_name[name] = _DmaSlot(sync, name, group)
    return sync.slot_by_name[name]


class _DmaSlot:
    def __init__(self, sync, name, group=False):
        self.name = name
        self.sem = None
        self.val = 0
        self.group = group
        self.pending = []
        sync.slots.append(self)

    def bump(self, sync):
        if self.sem is None or (self.val >= SEM_LIMIT and not self.pending):
            self.sem = sync.new_sem("d" + self.name)
            self.val = 0
        self.val += 16
        ev = [self.sem, self.val, "dma"]
        if self.group:
            self.pending.append(ev)
        return ev

    def close(self):
        for ev in self.pending:
            ev[1] = self.val
        self.pending = []


class Sync:
    def __init__(self, nc, stack):
        self.nc = nc
        self.stack = stack
        self.nsem = 0
        self.slots = []
        self.slot_by_name = {}
        self.E = {
            "pe": Eng(self, "pe", nc.tensor),
            "dve": Eng(self, "dve", nc.vector),
            "act": Eng(self, "act", nc.scalar),
            "pool": Eng(self, "pool", nc.gpsimd),
            "sp": Eng(self, "sp", nc.sync),
        }
        self.ninstr = 0
        self.nwait = 0

    def new_sem(self, name):
        self.nsem += 1
        return self.stack.enter_context(self.nc.semaphore(f"s{self.nsem}_{name}"))

    def _wait(self, E, evs):
        best = {}
        for ev in evs:
            if ev is None:
                continue
            if E.name == "pe" and ev[2] == "pe":
                continue
            k = id(ev[0])
            if k not in best or best[k][1] < ev[1]:
                best[k] = ev
        for ev in best.values():
            sem, val, src = ev
            if E.known.get(id(sem), 0) >= val:
                continue
            E.eng.wait_ge(sem, val)
            self.nwait += 1
            E.known[id(sem)] = val

    def _deps(self, E, reads, writes):
        evs = []
        for b in reads:
            evs.append(b.w)
        for b in writes:
            if b.w is not None and b.w[2] != E.name:
                evs.append(b.w)
            for ev in b.r.values():
                if ev[2] == E.name:
                    continue
                evs.append(ev)
        return evs

    @staticmethod
    def _bufs(lst):
        return [t.b if hasattr(t, "b") else t for t in lst]

    def op(self, engname, fn, r=(), w=()):
        E = self.E[engname]
        r = self._bufs(r)
        w = self._bufs(w)
        self._wait(E, self._deps(E, r, w))
        ins = fn(E.eng)
        ev = E.new_event()
        ins.then_inc(ev[0], 1)
        self.ninstr += 1
        key = ev[2] if ev[2] != "dma" else id(ev[0])
        for b in r:
            b.r[key] = ev
        for b in w:
            b.w = ev
            b.r = {}
        return ev

    def dma(self, qname, fn, slot, r=(), w=()):
        E = self.E[qname]
        r = self._bufs(r)
        w = self._bufs(w)
        self._wait(E, self._deps(E, r, w))
        ins = fn(E.eng)
        ev = slot.bump(self)
        ins.then_inc(ev[0], 16)
        self.ninstr += 1
        key = ev[2] if ev[2] != "dma" else id(ev[0])
        for b in r:
            b.r[key] = ev
        for b in w:
            b.w = ev
            b.r = {}
        return ev

    def barrier(self):
        for s in self.slots:
            if s.group:
                s.close()
        evs = []
        for E in self.E.values():
            if E.last is not None:
                evs.append(E.last)
        for s in self.slots:
            if s.sem is not None:
                evs.append([s.sem, s.val, "dma"])
        for E in self.E.values():
            self._wait(E, [ev for ev in evs if not (E.name == "pe" and ev[2] == "pe")])


class Tile:
    def __init__(self, h, name):
        self.h = h
        self.b = Buf(name)

    def __getitem__(self, k):
        return self.h[k]


P = 128
D = 1024
S_LAT = 4096
S_CTX = 256
NTL = 32
NTC = 2
NT = NTL + NTC
DEPTH = 2
INC = 1792
EPS = 1e-6
NPEER = 16384
GPAD = 16
NSLOT = 14
GRP = 8


def build(depth=DEPTH, dbg=False, stop=None):
    nc = bass.Bass("TRN2", target_bir_lowering=False)

    def din(name, shape, dt=F32):
        return nc.dram_tensor(name, shape, dt, kind="ExternalInput").ap()

    x_d = din("x", [S_LAT, D])
    ctx_d = din("ctx", [S_CTX, D])
    cT_d = din("cT", [P, 8])
    cctxT_d = din("cctxT", [P, 8])
    w_mod_d = din("w_mod", [DEPTH, D, 6 * D])
    b_mod_d = din("b_mod", [DEPTH, 6 * D])
    w_in_d = din("w_in", [DEPTH, D, INC])
    q_gain_d = din("q_gain", [DEPTH, 64])
    k_gain_d = din("k_gain", [DEPTH, 64])
    conv_w_d = din("conv_w", [DEPTH, 31, 512])
    conv_b_d = din("conv_b", [DEPTH, 512])
    ln_g_d = din("ln_g", [DEPTH, 512])
    ln_b_d = din("ln_b", [DEPTH, 512])
    beta_attn_d = din("beta_attn", [DEPTH, 512])
    beta_conv_d = din("beta_conv", [DEPTH, 512])
    w_o_d = din("w_o", [DEPTH, D, D])
    peer_wq_d = din("peer_wq", [DEPTH, D, D])
    peer_keys_d = din("peer_keys", [DEPTH, 8, 2, 128, 64])
    peer_u_d = din("peer_u", [DEPTH, NPEER, D])
    peer_v_d = din("peer_v", [DEPTH, NPEER, D])
    final_g_d = din("final_g", [1, D])
    cos2_d = din("cos2", [S_LAT, 64])
    sin2_d = din("sin2", [S_LAT, 64])
    peer_u_flat = peer_u_d.rearrange("l n d -> (l n) d")
    peer_v_flat = peer_v_d.rearrange("l n d -> (l n) d")
    out_d = nc.dram_tensor("out", [S_LAT, D], F32, kind="ExternalOutput").ap()
    xs_d = nc.dram_tensor("xs", [S_LAT + S_CTX, D], F32).ap()
    uv_d = nc.dram_tensor("uv", [DEPTH * NPEER, 2 * D], BF16).ap()
    dbg_d = {}
    if dbg:
        for nm, shp in [("d_qkvu", [NT * P, INC]), ("d_x1", [NT * P, D]), ("d_oT", [P, 4 * S_LAT]),
                        ("d_pre", [NT * P, 128]), ("d_eidx", [NT * P, 128]), ("d_w", [NT * P, 128]),
                        ("d_qT", [P, 4 * S_LAT]), ("d_x2", [NT * P, D]), ("d_mod", [P, 4096])]:
            dbg_d[nm] = nc.dram_tensor(nm, shp, F32, kind="ExternalOutput").ap()

    top = ExitStack()
    with top:
        S = Sync(nc, top)

        uid = [0]

        def sbt(stack, name, shape, dt):
            uid[0] += 1
            name = f"{name}_u{uid[0]}"
            return Tile(stack.enter_context(nc.sbuf_tensor(name, shape, dt)), name)

        def pst(stack, name, shape, dt):
            uid[0] += 1
            name = f"{name}_u{uid[0]}"
            return Tile(stack.enter_context(nc.psum_tensor(name, shape, dt)), name)

        out_slot = DmaSlot(S, "out", group=True)
        dbg_slots = {}
        xs_buf = [Buf(f"xs{i}") for i in range(NT)]

        ident_f = sbt(top, "ident_f", [P, P], F32)
        ident_b = sbt(top, "ident_b", [P, P], BF16)
        ones_f = sbt(top, "ones_f", [P, P], F32)
        io_t = sbt(top, "io_t", [P, P], F32)
        pid_t = sbt(top, "pid_t", [P, 1], F32)
        S.op("pool", lambda e: e.iota(io_t[:], pattern=[[1, P]], base=0, channel_multiplier=0,
                                      allow_small_or_imprecise_dtypes=True), w=[io_t])
        S.op("pool", lambda e: e.iota(pid_t[:], pattern=[[0, 1]], base=0, channel_multiplier=1,
                                      allow_small_or_imprecise_dtypes=True), w=[pid_t])
        S.op("dve", lambda e: e.tensor_scalar(ident_f[:], io_t[:], pid_t[:, 0:1], None, op0=ALU.is_equal),
             r=[io_t, pid_t], w=[ident_f])
        S.op("dve", lambda e: e.tensor_copy(ident_b[:], ident_f[:]), r=[ident_f], w=[ident_b])
        S.op("pool", lambda e: e.memset(ones_f[:], 1.0), w=[ones_f])

        cslot = DmaSlot(S, "const", group=True)
        craw = sbt(top, "craw", [P, 2, 8], F32)
        scT = sbt(top, "scT", [P, 8, 2], F32)
        S.dma("sp", lambda e: e.dma_start(out=craw[:, 0, :], in_=cT_d), cslot, w=[craw])
        S.dma("sp", lambda e: e.dma_start(out=craw[:, 1, :], in_=cctxT_d), cslot, w=[craw])
        cslot.close()
        S.op("act", lambda e: e.activation(scT[:].rearrange("p j r -> p r j"), craw[:], AF.Silu), r=[craw], w=[scT])
        modcol = sbt(top, "modcol", [P, 2, 16], F32)
        eps6 = sbt(top, "eps6", [P, 1], F32)
        eps5 = sbt(top, "eps5", [P, 1], F32)
        S.op("pool", lambda e: e.memset(eps6[:], 1e-6), w=[eps6])
        S.op("pool", lambda e: e.memset(eps5[:], 1e-5), w=[eps5])


        def rstd_from_ss(ss, rs, scale, eps_t):
            S.op("act", lambda e: e.activation(rs[:], ss[:], AF.Sqrt, scale=scale, bias=eps_t[:, 0:1]), r=[ss, eps_t], w=[rs])
            S.op("dve", lambda e: e.reciprocal(rs[:], rs[:]), r=[rs], w=[rs])

        def dbg_store(name, rows, tile_ap, rtiles):
            if dbg and name in dbg_d:
                if name not in dbg_slots:
                    dbg_slots[name] = (DmaSlot(S, name), Buf(name))
                S.dma("sp", lambda e: e.dma_start(out=dbg_d[name][rows], in_=tile_ap), dbg_slots[name][0], r=rtiles, w=[dbg_slots[name][1]])


        def convert_tables():
            with ExitStack() as ph:
                NB = 4
                stg = [sbt(ph, f"cvi{k}", [P, 4, D], F32) for k in range(NB)]
                obf = [sbt(ph, f"cvo{k}", [P, 4, D], BF16) for k in range(NB)]
                islot = [DmaSlot(S, f"cvi{k}") for k in range(NB)]
                oslot = [DmaSlot(S, f"cvo{k}") for k in range(NB)]
                n = 0
                for l in range(DEPTH):
                    for tab, col0 in ((peer_u_d, 0), (peer_v_d, D)):
                        for t in range(NPEER // 512):
                            k = n % NB
                            src = tab[l, t * 512:(t + 1) * 512, :].rearrange("(p r) d -> p r d", r=4)
                            dst = uv_d[l * NPEER + t * 512:l * NPEER + (t + 1) * 512, col0:col0 + D].rearrange("(p r) d -> p r d", r=4)
                            S.dma("sp", lambda e, k=k, src=src: e.dma_start(out=stg[k][:], in_=src), islot[k], w=[stg[k]])
                            if n % 2 == 0:
                                S.op("act", lambda e, k=k: e.activation(obf[k][:], stg[k][:], AF.Copy), r=[stg[k]], w=[obf[k]])
                            else:
                                S.op("dve", lambda e, k=k: e.tensor_copy(obf[k][:], stg[k][:]), r=[stg[k]], w=[obf[k]])
                            S.dma("pool", lambda e, k=k, dst=dst: e.dma_start(out=dst, in_=obf[k][:]), oslot[k], r=[obf[k]])
                            n += 1
                S.barrier()

        def mod_cols(l):
            with ExitStack() as ph:
                wm = [sbt(ph, f"wm{k}", [P, 8, P], F32) for k in range(2)]
                wslot = [DmaSlot(S, f"wm{k}") for k in range(2)]
                bcol = sbt(ph, "bcol", [P, 16], F32)
                pcol = pst(ph, "pcol", [P, 16, 2], F32)
                S.dma("sp", lambda e: e.dma_start(out=bcol[:], in_=b_mod_d[l, 0:2048].rearrange("(c p) -> p c", p=P),
                                                  allow_slow_non_contiguous=True), DmaSlot(S, "bcol"), w=[bcol])
                for cc in range(16):
                    t = wm[cc % 2]
                    S.dma("sp", lambda e, t=t, cc=cc: e.dma_start(
                        out=t[:], in_=w_mod_d[l, :, cc * P:(cc + 1) * P].rearrange("(j p) m -> p j m", p=P)),
                          wslot[cc % 2], w=[t])
                    for j in range(8):
                        S.op("pe", lambda e, t=t, j=j, cc=cc: e.matmul(pcol[:, cc, :], lhsT=t[:, j, :],
                                                                     rhs=scT[:, j, :], start=(j == 0), stop=(j == 7)),
                             r=[t, scT], w=[pcol])
                S.op("dve", lambda e: e.tensor_tensor(modcol[:].rearrange("p r c -> p c r"), pcol[:],
                                                      bcol[:].unsqueeze(2).to_broadcast([P, 16, 2]), op=ALU.add),
                     r=[pcol, bcol], w=[modcol])
                S.op("dve", lambda e: e.tensor_scalar(modcol[:, :, 8:16], modcol[:, :, 8:16], 1.0, None, op0=ALU.add),
                     r=[modcol], w=[modcol])
                S.barrier()

        def mod_bc(l, r, modbc):
            with ExitStack() as ph:
                NW = 4
                wm = [sbt(ph, f"wmb{k}", [P, 512], F32) for k in range(NW)]
                wslot = [DmaSlot(S, f"wmb{k}") for k in range(NW)]
                bbc = [sbt(ph, f"bbc{k}", [P, 512], F32) for k in range(2)]
                bslot = [DmaSlot(S, f"bbc{k}") for k in range(2)]
                screp = sbt(ph, "screp", [P, 8, P], F32)
                pb = pst(ph, "pbm", [P, 512], F32)
                S.op("dve", lambda e: e.tensor_copy(screp[:], scT[:, :, r].unsqueeze(2).to_broadcast([P, 8, P])),
                     r=[scT], w=[screp])
                n = 0
                for cc in range(8):
                    c0 = 2048 + cc * 512
                    S.dma("sp", lambda e, cc=cc, c0=c0: e.dma_start(out=bbc[cc % 2][:], in_=b_mod_d[l, c0:c0 + 512].partition_broadcast(P)),
                          bslot[cc % 2], w=[bbc[cc % 2]])
                    for j in range(8):
                        t = wm[n % NW]
                        S.dma("sp", lambda e, t=t, j=j, c0=c0: e.dma_start(out=t[:], in_=w_mod_d[l, j * P:(j + 1) * P, c0:c0 + 512]),
                              wslot[n % NW], w=[t])
                        n += 1
                        S.op("pe", lambda e, t=t, j=j: e.matmul(pb[:], lhsT=screp[:, j, :], rhs=t[:], start=(j == 0), stop=(j == 7)),
                             r=[t, screp], w=[pb])
                    S.op("dve", lambda e, cc=cc: e.tensor_tensor(modbc[:, cc * 512:(cc + 1) * 512], pb[:], bbc[cc % 2][:], op=ALU.add),
                         r=[pb, bbc[cc % 2]], w=[modbc])
                S.op("dve", lambda e: e.tensor_scalar(modbc[:, 2048:3072], modbc[:, 2048:3072], 1.0, None, op0=ALU.add),
                     r=[modbc], w=[modbc])
                S.barrier()

        def phase_a(l, M, src_aps, last):
            with ExitStack() as ph:
                win = sbt(ph, "win", [P, 8, INC], BF16)
                stg = sbt(ph, "stg", [P, INC], F32)
                stg_slot = DmaSlot(S, "stg")
                bias_bc = sbt(ph, "bias_bc", [P, INC], F32)
                sh1rep = sbt(ph, "sh1rep", [P, 2, 8, P], BF16)
                gain = sbt(ph, "gain", [P, 640], F32)
                graw = sbt(ph, "graw", [P, 128], F32)
                cos2 = sbt(ph, "cos2", [P, NTL, 64], F32)
                sin2 = sbt(ph, "sin2", [P, NTL, 64], F32)
                xt = [sbt(ph, f"xt{k}", [P, D], F32) for k in range(2)]
                xt_slot = [DmaSlot(S, f"xt{k}") for k in range(2)]
                xb = [sbt(ph, f"xb{k}", [P, D], BF16) for k in range(2)]
                xT = [sbt(ph, f"xT{k}", [P, 8, P], BF16) for k in range(2)]
                junk = sbt(ph, "junkA", [P, D], BF16)
                ss = [sbt(ph, f"ssA{k}", [P, 1], F32) for k in range(2)]
                rstd = [sbt(ph, f"rstdA{k}", [P, 1], F32) for k in range(2)]
                qkvu = sbt(ph, "qkvu", [P, INC], F32)
                sq = sbt(ph, "sq", [P, 640], F32)
                ssq = sbt(ph, "ssq", [P, 10], F32)
                rsq = sbt(ph, "rsq", [P, 10], F32)
                qn = sbt(ph, "qn", [P, 640], F32)
                rb = sbt(ph, "rb", [P, 640], F32)
                qr = sbt(ph, "qr", [P, 4, 2, 64], BF16)
                kr = sbt(ph, "kr", [P, 128], BF16)
                sig = sbt(ph, "sig", [P, 512], F32)
                gg = sbt(ph, "gg", [P, 512], BF16)
                ptb = pst(ph, "ptb", [P, 8, P], BF16)
                pq = [pst(ph, f"pq{k}", [P, 512], F32) for k in range(4)]
                ptq = pst(ph, "ptq", [P, 5, P], BF16)
                ptg = pst(ph, "ptg", [P, 4, P], BF16)
                ra = sq
                qT, qTc, kT, Vp, gT, gTc = M["qT"], M["qTc"], M["kT"], M["Vp"], M["gT"], M["gTc"]

                cslot2 = DmaSlot(S, "rope", group=True)
                S.dma("sp", lambda e: e.dma_start(out=cos2[:], in_=cos2_d.rearrange("(n p) f -> p n f", p=P)), cslot2, w=[cos2])
                S.dma("sp", lambda e: e.dma_start(out=sin2[:], in_=sin2_d.rearrange("(n p) f -> p n f", p=P)), cslot2, w=[sin2])
                S.dma("sp", lambda e: e.dma_start(out=graw[:, 0:64], in_=q_gain_d[l].partition_broadcast(P)), cslot2, w=[graw])
                S.dma("sp", lambda e: e.dma_start(out=graw[:, 64:128], in_=k_gain_d[l].partition_broadcast(P)), cslot2, w=[graw])
                cslot2.close()
                for j in range(8):
                    S.dma("sp", lambda e, j=j: e.dma_start(out=stg[:], in_=w_in_d[l, j * P:(j + 1) * P, :]), stg_slot, w=[stg])
                    S.op("act", lambda e, j=j: e.activation(win[:, j, :], stg[:], AF.Copy), r=[stg], w=[win])
                S.op("dve", lambda e: e.tensor_copy(gain[:, 0:512].rearrange("p (h d) -> p h d", h=8),
                                                    graw[:, 0:64].unsqueeze(1).to_broadcast([P, 8, 64])), r=[graw], w=[gain])
                S.op("dve", lambda e: e.tensor_copy(gain[:, 512:640].rearrange("p (h d) -> p h d", h=2),
                                                    graw[:, 64:128].unsqueeze(1).to_broadcast([P, 2, 64])), r=[graw], w=[gain])
                S.op("dve", lambda e: e.tensor_copy(sh1rep[:], modcol[:, :, 0:8].unsqueeze(3).to_broadcast([P, 2, 8, P])),
                     r=[modcol], w=[sh1rep])

                def make_bias(r_):
                    for cc in range(4):
                        w_ = min(512, INC - cc * 512)
                        for j in range(8):
                            S.op("pe", lambda e, cc=cc, j=j, w_=w_: e.matmul(
                                pq[cc][:, 0:w_], lhsT=sh1rep[:, r_, j, :], rhs=win[:, j, cc * 512:cc * 512 + w_],
                                start=(j == 0), stop=(j == 7)), r=[sh1rep, win], w=[pq[cc]])
                        S.op("act", lambda e, cc=cc, w_=w_: e.activation(
                            bias_bc[:, cc * 512:cc * 512 + w_], pq[cc][:, 0:w_], AF.Copy), r=[pq[cc]], w=[bias_bc])

                def load(i):
                    k = i % 2
                    S.dma("sp", lambda e: e.dma_start(out=xt[k][:], in_=src_aps[i]), xt_slot[k], r=[xs_buf[i]], w=[xt[k]])

                make_bias(0)
                load(0)
                for i in range(NT if stop not in ("a0", "a1") else (0 if stop == "a0" else 1)):
                    k = i % 2
                    r_ = 0 if i < NTL else 1
                    is_ctx = i >= NTL
                    if i == NTL:
                        make_bias(1)
                    if i + 1 < NT:
                        load(i + 1)
                    S.op("act", lambda e: e.activation(junk[:], xt[k][:], AF.Square, accum_out=ss[k][:]), r=[xt[k]], w=[junk, ss[k]])
                    S.op("act", lambda e: e.activation(xb[k][:], xt[k][:], AF.Copy), r=[xt[k]], w=[xb[k]])
                    rstd_from_ss(ss[k], rstd[k], 1.0 / D, eps6)
                    for j in range(8):
                        S.op("pe", lambda e, j=j: e.transpose(ptb[:, j, :], xb[k][:, j * P:(j + 1) * P], ident_b[:]),
                             r=[xb[k], ident_b], w=[ptb])
                    S.op("dve", lambda e: e.tensor_tensor(xT[k][:], ptb[:],
                                                          modcol[:, r_, 8:16].unsqueeze(2).to_broadcast([P, 8, P]), op=ALU.mult),
                         r=[ptb, modcol], w=[xT[k]])
                    if CUT <= 1:
                        continue
                    for cc in range(4):
                        w_ = min(512, INC - cc * 512)
                        for j in range(8):
                            S.op("pe", lambda e, cc=cc, j=j, w_=w_: e.matmul(
                                pq[cc][:, 0:w_], lhsT=xT[k][:, j, :], rhs=win[:, j, cc * 512:cc * 512 + w_],
                                start=(j == 0), stop=(j == 7)), r=[xT[k], win], w=[pq[cc]])
                        S.op("dve", lambda e, cc=cc, w_=w_: e.scalar_tensor_tensor(
                            out=qkvu[:, cc * 512:cc * 512 + w_], in0=pq[cc][:, 0:w_], scalar=rstd[k][:, 0:1],
                            in1=bias_bc[:, cc * 512:cc * 512 + w_], op0=ALU.mult, op1=ALU.add),
                             r=[pq[cc], rstd[k], bias_bc], w=[qkvu])
                    if l == 0:
                        dbg_store("d_qkvu", slice(i * P, (i + 1) * P), qkvu[:], [qkvu])
                    if CUT <= 2:
                        continue
                    S.op("pool", lambda e: e.tensor_tensor(sq[:], qkvu[:, 0:640], qkvu[:, 0:640], op=ALU.mult), r=[qkvu], w=[sq])
                    S.op("dve", lambda e: e.tensor_reduce(out=ssq[:], in_=sq[:].rearrange("p (h d) -> p h d", h=10),
                                                          axis=AX.X, op=ALU.add), r=[sq], w=[ssq])
                    rstd_from_ss(ssq, rsq, 1.0 / 64, eps6)
                    S.op("dve", lambda e: e.tensor_tensor(qn[:].rearrange("p (h d) -> p h d", h=10),
                                                          qkvu[:, 0:640].rearrange("p (h d) -> p h d", h=10),
                                                          rsq[:].unsqueeze(2).to_broadcast([P, 10, 64]), op=ALU.mult),
                         r=[qkvu, rsq], w=[qn])
                    S.op("pool", lambda e: e.tensor_tensor(qn[:], qn[:], gain[:], op=ALU.mult), r=[qn, gain], w=[qn])
                    if CUT <= 3:
                        continue
                    if not is_ctx:
                        qn3 = qn[:].rearrange("p (h d) -> p h d", h=10)
                        rb3 = rb[:].rearrange("p (h d) -> p h d", h=10)
                        S.op("dve", lambda e: e.tensor_tensor(ra[:].rearrange("p (h d) -> p h d", h=10), qn3,
                                                              cos2[:, i, :].unsqueeze(1).to_broadcast([P, 10, 64]), op=ALU.mult),
                             r=[qn, cos2], w=[ra])
                        S.op("pool", lambda e: e.tensor_tensor(rb3[:, :, 0:32], qn3[:, :, 32:64],
                                                               sin2[:, i, 0:32].unsqueeze(1).to_broadcast([P, 10, 32]), op=ALU.mult),
                             r=[qn, sin2], w=[rb])
                        S.op("pool", lambda e: e.tensor_tensor(rb3[:, :, 32:64], qn3[:, :, 0:32],
                                                               sin2[:, i, 32:64].unsqueeze(1).to_broadcast([P, 10, 32]), op=ALU.mult),
                             r=[qn, sin2], w=[rb])
                        S.op("dve", lambda e: e.tensor_tensor(qr[:].rearrange("p pr hh d -> p hh pr d"),
                                                              ra[:, 0:512].rearrange("p (hh pr d) -> p hh pr d", hh=2, pr=4),
                                                              rb[:, 0:512].rearrange("p (hh pr d) -> p hh pr d", hh=2, pr=4), op=ALU.add),
                             r=[ra, rb], w=[qr])
                        S.op("pool", lambda e: e.tensor_tensor(kr[:], ra[:, 512:640], rb[:, 512:640], op=ALU.add), r=[ra, rb], w=[kr])
                    else:
                        S.op("dve", lambda e: e.tensor_copy(qr[:].rearrange("p pr hh d -> p hh pr d"),
                                                            qn[:, 0:512].rearrange("p (hh pr d) -> p hh pr d", hh=2, pr=4)),
                             r=[qn], w=[qr])
                        S.op("pool", lambda e: e.tensor_copy(kr[:], qn[:, 512:640]), r=[qn], w=[kr])
                    if CUT <= 4:
                        continue
                    need_q = (not is_ctx) or (not last)
                    if need_q:
                        for pr in range(4):
                            S.op("pe", lambda e, pr=pr: e.transpose(ptq[:, pr, :], qr[:, pr, :, :].rearrange("p hh d -> p (hh d)"), ident_b[:]),
                                 r=[qr, ident_b], w=[ptq])
                    S.op("pe", lambda e: e.transpose(ptq[:, 4, :], kr[:], ident_b[:]), r=[kr, ident_b], w=[ptq])
                    if need_q:
                        if not is_ctx:
                            S.op("act", lambda e: e.activation(qT[:, :, i * P:(i + 1) * P], ptq[:, 0:4, :], AF.Copy),
                                 r=[ptq], w=[M["qT_b"][pr][i // 4] for pr in range(4)])
                        else:
                            S.op("act", lambda e: e.activation(qTc[:, :, (i - NTL) * P:(i - NTL + 1) * P], ptq[:, 0:4, :], AF.Copy),
                                 r=[ptq], w=[qTc])
                    S.op("act", lambda e: e.activation(kT[:, i * P:(i + 1) * P], ptq[:, 4, :], AF.Copy), r=[ptq], w=[M["kT_b"][i]])
                    if CUT <= 5:
                        continue
                    S.op("pool", lambda e: e.tensor_copy(Vp[:, i, 0:64], qkvu[:, 640:704]), r=[qkvu], w=[M["Vp_b"][i]])
                    S.op("pool", lambda e: e.tensor_copy(Vp[:, i, 128:192], qkvu[:, 704:768]), r=[qkvu], w=[M["Vp_b"][i]])
                    if CUT <= 6:
                        continue
                    if need_q:
                        S.op("act", lambda e: e.activation(sig[:], qkvu[:, 1280:1792], AF.Sigmoid), r=[qkvu], w=[sig])
                        S.op("dve", lambda e: e.tensor_tensor(gg[:], qkvu[:, 768:1280], sig[:], op=ALU.mult), r=[qkvu, sig], w=[gg])
                        for c in range(4):
                            S.op("pe", lambda e, c=c: e.transpose(ptg[:, c, :], gg[:, c * P:(c + 1) * P], ident_b[:]),
                                 r=[gg, ident_b], w=[ptg])
                        if not is_ctx:
                            S.op("act", lambda e: e.activation(gT[:, :, GPAD + i * P:GPAD + (i + 1) * P], ptg[:], AF.Copy),
                                 r=[ptg], w=[M["gT_b"][i]])
                        else:
                            S.op("act", lambda e: e.activation(gTc[:, :, GPAD + (i - NTL) * P:GPAD + (i - NTL + 1) * P], ptg[:], AF.Copy),
                                 r=[ptg], w=[M["gTc_b"][i - NTL]])
                S.barrier()

        def phase_b(l, M, last):
            with ExitStack() as ph:
                NPT = 4
                NPS = 3
                LA = 2
                pT = [sbt(ph, f"pT{k}", [P, 512], BF16) for k in range(NPT)]
                rd = sbt(ph, "rd", [P, 512], F32)
                bcs = sbt(ph, "bcs", [P, 512], F32)
                ps_s = [pst(ph, f"ps_s{k}", [P, 512], F32) for k in range(NPS)]
                po = [pst(ph, f"po{k}", [P, 512], F32) for k in range(4)]
                pbc = pst(ph, "pbc", [P, 512], F32)
                qT, qTc, kT, Vp = M["qT"], M["qTc"], M["kT"], M["Vp"]
                cnt = [0]
                grp = [0]

                def attend(qsrc_fn, qbuf, n, ktiles):
                    its = [(kt_i, kt, hh) for kt_i, kt in enumerate(ktiles) for hh in range(2)]
                    base = cnt[0]
                    cnt[0] += len(its)
                    pg = po[2 * (grp[0] % 2):2 * (grp[0] % 2) + 2]
                    grp[0] += 1

                    def emit_s(m):
                        kt_i, kt, hh = its[m]
                        k = (base + m) % NPS
                        kp = (base + m) % NPT
                        S.op("pe", lambda e: e.matmul(ps_s[k][:, 0:n], lhsT=kT[hh * 64:(hh + 1) * 64, kt * P:(kt + 1) * P], rhs=qsrc_fn(hh),
                                                      start=True, stop=True), r=[M["kT_b"][kt], qbuf], w=[ps_s[k]])
                        S.op("act", lambda e: e.activation(pT[kp][:, 0:n], ps_s[k][:, 0:n], AF.Exp, scale=0.125),
                             r=[ps_s[k]], w=[pT[kp]])

                    def emit_pv(m):
                        kt_i, kt, hh = its[m]
                        kp = (base + m) % NPT
                        S.op("pe", lambda e: e.matmul(pg[hh][:, 0:n], lhsT=Vp[:, kt, hh * 64:hh * 64 + 128], rhs=pT[kp][:, 0:n],
                                                      start=(kt_i == 0), stop=(kt_i == len(ktiles) - 1)), r=[M["Vp_b"][kt], pT[kp]], w=[pg[hh]])

                    for m in range(len(its) + LA):
                        if m < len(its):
                            emit_s(m)
                        if m - LA >= 0:
                            emit_pv(m - LA)
                    for hh in range(2):
                        dp = 64 if hh == 0 else 0
                        S.op("dve", lambda e, hh=hh, dp=dp: e.reciprocal(rd[dp:dp + 1, 0:n], pg[hh][dp:dp + 1, 0:n]),
                             r=[pg[hh]], w=[rd])
                        S.op("pe", lambda e, dp=dp: e.matmul(pbc[0:64, 0:n], lhsT=ones_f[dp:dp + 1, 0:64], rhs=rd[dp:dp + 1, 0:n],
                                                            start=True, stop=True), r=[ones_f, rd], w=[pbc])
                        S.op("act", lambda e, hh=hh: e.activation(bcs[hh * 64:(hh + 1) * 64, 0:n], pbc[0:64, 0:n], AF.Copy),
                             r=[pbc], w=[bcs])
                        S.op("dve", lambda e, hh=hh: e.tensor_tensor(qsrc_fn(hh), pg[hh][hh * 64:(hh + 1) * 64, 0:n],
                                                                     bcs[hh * 64:(hh + 1) * 64, 0:n], op=ALU.mult),
                             r=[pg[hh], bcs], w=[qbuf])

                for c in range(8):
                    for pr in range(4):
                        attend(lambda hh, c=c, pr=pr: qT[hh * 64:(hh + 1) * 64, pr, c * 512:(c + 1) * 512],
                               M["qT_b"][pr][c], 512, list(range(NT)))
                if not last:
                    for pr in range(4):
                        attend(lambda hh, pr=pr: qTc[hh * 64:(hh + 1) * 64, pr, :], qTc.b, S_CTX, [NTL, NTL + 1])
                S.barrier()

        def phase_c(l, M, tiles, src_aps, r_, modbc):
            with ExitStack() as ph:
                wcol = sbt(ph, "wcol", [P, 4, 31], F32)
                DG = sbt(ph, "DG", [P, 4, 31, P], BF16)
                wo = sbt(ph, "wo", [P, 8, D], BF16)
                stg = [sbt(ph, f"stgC{k}", [P, D], F32) for k in range(2)]
                stg_slot = [DmaSlot(S, f"stgC{k}") for k in range(2)]
                betaA = sbt(ph, "betaA", [P, 8], F32)
                betac = sbt(ph, "betac", [P, 8], F32)
                cb_bc = sbt(ph, "cb_bc", [P, 512], F32)
                lg_bc = sbt(ph, "lg_bc", [P, 512], F32)
                lb_bc = sbt(ph, "lb_bc", [P, 512], F32)
                xt = [sbt(ph, f"xtC{k}", [P, D], F32) for k in range(2)]
                xt_slot = [DmaSlot(S, f"xtC{k}") for k in range(2)]
                y = sbt(ph, "yC", [P, 512], F32)
                junk = sbt(ph, "junkC", [P, 512], F32)
                st4 = sbt(ph, "st4", [P, 4], F32)
                rln = sbt(ph, "rln", [P, 1], F32)
                msq = sbt(ph, "msq", [P, 1], F32)
                z = sbt(ph, "zC", [P, 512], F32)
                oc = sbt(ph, "oc", [P, 512], F32)
                ocb = sbt(ph, "ocb", [P, 512], BF16)
                ocT = sbt(ph, "ocT", [P, 4, P], BF16)
                sqo = sbt(ph, "sqo", [P, 4, P], F32)
                ssc = sbt(ph, "ssc", [P, 2], F32)
                rsc = sbt(ph, "rsc", [P, 2], F32)
                t1 = sbt(ph, "t1", [P, D], F32)
                x1 = [sbt(ph, f"x1_{k}", [P, D], F32) for k in range(2)]
                x1_slot = [DmaSlot(S, f"x1s{k}") for k in range(2)]
                py = pst(ph, "py", [P, 512], F32)
                ptc = pst(ph, "ptc", [P, 4, P], BF16)
                pss = pst(ph, "pss", [P, 16], F32)
                pa = [pst(ph, f"pa{k}", [P, 512], F32) for k in range(2)]
                pc = [pst(ph, f"pc{k}", [P, 512], F32) for k in range(2)]
                qT, qTc, gT, gTc = M["qT"], M["qTc"], M["gT"], M["gTc"]

                vs = DmaSlot(S, "vecC", group=True)
                for c in range(4):
                    S.dma("sp", lambda e, c=c: e.dma_start(out=wcol[:, c, :], in_=conv_w_d[l, :, c * P:(c + 1) * P].rearrange("j p -> p j"),
                                                           allow_slow_non_contiguous=True), vs, w=[wcol])
                for half in range(2):
                    S.dma("sp", lambda e, half=half: e.dma_start(out=betaA[half * 64:(half + 1) * 64, :],
                                                                 in_=beta_attn_d[l].rearrange("(h d) -> d h", d=64),
                                                                 allow_slow_non_contiguous=True), vs, w=[betaA])
                S.dma("sp", lambda e: e.dma_start(out=betac[:, 4:8], in_=beta_conv_d[l].rearrange("(c p) -> p c", p=P),
                                                  allow_slow_non_contiguous=True), vs, w=[betac])
                S.dma("sp", lambda e: e.dma_start(out=cb_bc[:], in_=conv_b_d[l].partition_broadcast(P)), vs, w=[cb_bc])
                S.dma("sp", lambda e: e.dma_start(out=lg_bc[:], in_=ln_g_d[l].partition_broadcast(P)), vs, w=[lg_bc])
                S.dma("sp", lambda e: e.dma_start(out=lb_bc[:], in_=ln_b_d[l].partition_broadcast(P)), vs, w=[lb_bc])
                vs.close()
                S.op("dve", lambda e: e.tensor_copy(betac[0:64, 0:4], betaA[0:64, 0:4]), r=[betaA], w=[betac])
                S.op("dve", lambda e: e.tensor_copy(betac[64:128, 0:4], betaA[64:128, 4:8]), r=[betaA], w=[betac])
                for kk in range(8):
                    t = stg[kk % 2]
                    sl = stg_slot[kk % 2]
                    if kk < 4:
                        S.dma("sp", lambda e, t=t, kk=kk: e.dma_start(out=t[0:64, :], in_=w_o_d[l, kk * 64:(kk + 1) * 64, :]), sl, w=[t])
                        S.dma("sp", lambda e, t=t, kk=kk: e.dma_start(out=t[64:128, :], in_=w_o_d[l, (kk + 4) * 64:(kk + 5) * 64, :]), sl, w=[t])
                    else:
                        S.dma("sp", lambda e, t=t, kk=kk: e.dma_start(out=t[:], in_=w_o_d[l, 512 + (kk - 4) * P:512 + (kk - 3) * P, :]), sl, w=[t])
                    S.op("act", lambda e, t=t, kk=kk: e.activation(wo[:, kk, :], t[:], AF.Identity, scale=betac[:, kk:kk + 1]),
                         r=[t, betac], w=[wo])
                for c in range(4):
                    for j in range(31):
                        S.op("pool", lambda e, c=c, j=j: e.tensor_scalar(DG[:, c, j, :], ident_f[:], wcol[:, c, j:j + 1], None, op0=ALU.mult),
                             r=[ident_f, wcol], w=[DG])

                def load(n_):
                    i = tiles[n_]
                    k = n_ % 2
                    S.dma("sp", lambda e: e.dma_start(out=xt[k][:], in_=src_aps[i]), xt_slot[k], r=[xs_buf[i]], w=[xt[k]])

                load(0)
                for n_, i in enumerate(tiles):
                    k = n_ % 2
                    is_ctx = i >= NTL
                    if n_ + 1 < len(tiles):
                        load(n_ + 1)
                    if not is_ctx:
                        gsrc, gb, t0 = gT, M["gT_b"], i * P
                        lo, hi = max(0, i - 1), min(NTL - 1, i + 1)
                        osrc = lambda pr: qT[:, pr, i * P:(i + 1) * P]
                        obufs = [M["qT_b"][pr][i // 4] for pr in range(4)]
                    else:
                        gsrc, gb, t0 = gTc, M["gTc_b"], (i - NTL) * P
                        lo, hi = max(0, i - NTL - 1), min(NTC - 1, i - NTL + 1)
                        osrc = lambda pr: qTc[:, pr, (i - NTL) * P:(i - NTL + 1) * P]
                        obufs = [qTc.b]
                    for c in range(4):
                        for j in range(31):
                            S.op("pe", lambda e, c=c, j=j: e.matmul(py[:, c * P:(c + 1) * P], lhsT=gsrc[:, c, t0 + j + GPAD - 15:t0 + j + GPAD - 15 + P],
                                                                  rhs=DG[:, c, j, :], start=(j == 0), stop=(j == 30)),
                                 r=[gb[q] for q in range(lo, hi + 1)] + [DG], w=[py])
                    S.op("dve", lambda e: e.tensor_tensor(y[:], py[:], cb_bc[:], op=ALU.add), r=[py, cb_bc], w=[y])
                    S.op("act", lambda e: e.activation(junk[:], y[:], AF.Identity, accum_out=st4[:, 0:1]), r=[y], w=[junk, st4])
                    S.op("act", lambda e: e.activation(junk[:], y[:], AF.Square, accum_out=st4[:, 1:2]), r=[y], w=[junk, st4])
                    S.op("dve", lambda e: e.tensor_scalar(st4[:, 2:4], st4[:, 0:2], 1.0 / 512, None, op0=ALU.mult), r=[st4], w=[st4])
                    S.op("dve", lambda e: e.tensor_tensor(msq[:], st4[:, 2:3], st4[:, 2:3], op=ALU.mult), r=[st4], w=[msq])
                    S.op("dve", lambda e: e.tensor_tensor(msq[:], st4[:, 3:4], msq[:], op=ALU.subtract), r=[st4, msq], w=[msq])
                    rstd_from_ss(msq, rln, 1.0, eps5)
                    S.op("dve", lambda e: e.tensor_scalar(z[:], y[:], st4[:, 2:3], rln[:, 0:1], op0=ALU.subtract, op1=ALU.mult),
                         r=[y, st4, rln], w=[z])
                    S.op("pool", lambda e: e.tensor_tensor(z[:], z[:], lg_bc[:], op=ALU.mult), r=[z, lg_bc], w=[z])
                    S.op("pool", lambda e: e.tensor_tensor(z[:], z[:], lb_bc[:], op=ALU.add), r=[z, lb_bc], w=[z])
                    S.op("act", lambda e: e.activation(oc[:], z[:], AF.Silu), r=[z], w=[oc])
                    S.op("act", lambda e: e.activation(junk[:], oc[:], AF.Square, accum_out=ssc[:, 0:1]), r=[oc], w=[junk, ssc])
                    S.op("pool", lambda e: e.tensor_copy(ocb[:], oc[:]), r=[oc], w=[ocb])
                    for c in range(4):
                        S.op("pe", lambda e, c=c: e.transpose(ptc[:, c, :], ocb[:, c * P:(c + 1) * P], ident_b[:]),
                             r=[ocb, ident_b], w=[ptc])
                    S.op("act", lambda e: e.activation(ocT[:], ptc[:], AF.Copy), r=[ptc], w=[ocT])
                    for pr in range(4):
                        S.op("pool", lambda e, pr=pr: e.tensor_tensor(sqo[:, pr, :], osrc(pr), osrc(pr), op=ALU.mult), r=obufs, w=[sqo])
                    for pr in range(4):
                        S.op("pe", lambda e, pr=pr: e.matmul(pss[:, 0:2], lhsT=sqo[:, pr, :], rhs=ones_f[:, 0:2],
                                                            start=(pr == 0), stop=(pr == 3)), r=[sqo, ones_f], w=[pss])
                    S.op("dve", lambda e: e.tensor_copy(ssc[:, 1:2], pss[:, 0:1]), r=[pss], w=[ssc])
                    rstd_from_ss(ssc, rsc, 1.0 / 512, eps6)
                    for cc in range(2):
                        for pr in range(4):
                            S.op("pe", lambda e, cc=cc, pr=pr: e.matmul(pa[cc][:], lhsT=osrc(pr), rhs=wo[:, pr, cc * 512:(cc + 1) * 512],
                                                                      start=(pr == 0), stop=(pr == 3)), r=obufs + [wo], w=[pa[cc]])
                        for c in range(4):
                            S.op("pe", lambda e, cc=cc, c=c: e.matmul(pc[cc][:], lhsT=ocT[:, c, :], rhs=wo[:, 4 + c, cc * 512:(cc + 1) * 512],
                                                                    start=(c == 0), stop=(c == 3)), r=[ocT, wo], w=[pc[cc]])
                        sl = slice(cc * 512, (cc + 1) * 512)
                        S.op("dve", lambda e, cc=cc, sl=sl: e.tensor_scalar(t1[:, sl], pa[cc][:], rsc[:, 1:2], None, op0=ALU.mult),
                             r=[pa[cc], rsc], w=[t1])
                        S.op("dve", lambda e, cc=cc, sl=sl: e.scalar_tensor_tensor(out=t1[:, sl], in0=pc[cc][:], scalar=rsc[:, 0:1],
                                                                                   in1=t1[:, sl], op0=ALU.mult, op1=ALU.add),
                             r=[pc[cc], rsc, t1], w=[t1])
                    S.op("pool", lambda e: e.tensor_tensor(t1[:], t1[:], modbc[:, 0:1024], op=ALU.mult), r=[t1, modbc], w=[t1])
                    S.op("pool", lambda e: e.tensor_tensor(x1[k][:], t1[:], xt[k][:], op=ALU.add), r=[t1, xt[k]], w=[x1[k]])
                    S.dma("sp", lambda e: e.dma_start(out=xs_d[i * P:(i + 1) * P, :], in_=x1[k][:]), x1_slot[k], r=[x1[k]], w=[xs_buf[i]])
                    if l == 0:
                        dbg_store("d_x1", slice(i * P, (i + 1) * P), x1[k][:], [x1[k]])
                S.barrier()

        def phase_p(l, tiles, modbc, last):
            with ExitStack() as ph:
                wq = sbt(ph, "wq", [P, 8, D], F32)
                keysTz = sbt(ph, "keysTz", [P, 8, 2, P], F32)
                iota16 = sbt(ph, "iota16", [P, 128, 16], F32)
                xt = [sbt(ph, f"xtP{k}", [P, D], F32) for k in range(2)]
                xt_slot = [DmaSlot(S, f"xtP{k}") for k in range(2)]
                junk = sbt(ph, "junkP", [P, D], BF16)
                junkr = sbt(ph, "junkR", [P, D], BF16)
                ss = sbt(ph, "ssP", [P, 1], F32)
                rstd = sbt(ph, "rstdP", [P, 1], F32)
                ssf = sbt(ph, "ssF", [P, 1], F32)
                rstdf = sbt(ph, "rstdF", [P, 1], F32)
                h = [sbt(ph, f"hP{k}", [P, D], F32) for k in range(2)]
                hT = sbt(ph, "hT", [P, 8, P], F32)
                qpT = hT
                qp = sbt(ph, "qp", [P, D], F32)
                sc = sbt(ph, "scP", [P, 16, 128], F32)
                wk = sbt(ph, "wkP", [P, 16, 128], F32)
                eq_ap = wk[:].rearrange("p a b -> p (a b)").rearrange("p (x c) -> p x c", c=16)
                topv = sbt(ph, "topv", [P, 16, 16], F32)
                idx = sbt(ph, "idxP", [P, 16, 16], U32)
                idxf = sbt(ph, "idxf", [P, 16, 16], F32)
                cand = sbt(ph, "cand", [P, 8, 256], F32)
                wk2 = sbt(ph, "wk2", [P, 8, 256], F32)
                best = sbt(ph, "best", [P, 8, 16], F32)
                pos = sbt(ph, "pos", [P, 8, 16], U32)
                pab = sbt(ph, "pab", [P, 2, 128], U32)
                abf = sbt(ph, "abf", [P, 2, 128], F32)
                isel = sbt(ph, "isel", [P, 2, 128], F32)
                ef = [sbt(ph, f"ef{k}", [P, 128], F32) for k in range(2)]
                eidx = [sbt(ph, f"eidx{k}", [P, 128], I32) for k in range(2)]
                ew = sbt(ph, "ew", [P, 128], F32)
                se = sbt(ph, "se", [P, 8], F32)
                wgt = [sbt(ph, f"wgt{k}", [P, 128], F32) for k in range(2)]
                pre = sbt(ph, "pre", [P, 128], F32)
                aw = sbt(ph, "aw", [P, 128], F32)
                gbuf = [sbt(ph, f"gbuf{k}", [P, 2 * D], BF16) for k in range(NSLOT)]
                diag = [sbt(ph, f"diag{k}", [P, P], BF16) for k in range(4)]
                pre_b = [Buf(f"pre_b{k}") for k in range(4)]
                di = [0]
                gslot = [DmaSlot(S, f"g{k}") for k in range(NSLOT)]
                x2 = [sbt(ph, f"x2_{k}", [P, D], F32) for k in range(2)]
                x2_slot = [DmaSlot(S, f"x2s{k}") for k in range(2)]
                fg_bc = sbt(ph, "fg_bc", [P, D], F32)
                pT2 = pst(ph, "pT2", [P, 8, P], F32)
                pqp = [pst(ph, f"pqp{k}", [P, 512], F32) for k in range(2)]
                pcs = [pst(ph, f"pcs{k}", [P, 512], F32) for k in range(2)]
                pva = [pst(ph, f"pva{k}", [P, 512], F32) for k in range(2)]
                psc = pqp + pcs
                print(f"[build] phase_p sbuf remaining {nc.sbuf_bytes_remaining}")

                S.op("pool", lambda e: e.iota(iota16[:], pattern=[[0, 128], [1, 16]], base=0, channel_multiplier=0,
                                              allow_small_or_imprecise_dtypes=True), w=[iota16])
                ws = DmaSlot(S, "wqload", group=True)
                for j in range(8):
                    S.dma("sp", lambda e, j=j: e.dma_start(out=wq[:, j, :], in_=peer_wq_d[l, j * P:(j + 1) * P, :]), ws, w=[wq])
                kraw_ap = sc[:].rearrange("p a b -> p (a b)")[:, 0:1024].rearrange("p (a d) -> p a d", d=64)
                keysT_ap = cand[:].rearrange("p a b -> p (a b)")[:, 0:1024].rearrange("p (a k) -> p a k", k=P)
                S.dma("sp", lambda e: e.dma_start(out=kraw_ap, in_=peer_keys_d[l].rearrange("h c k d -> k (h c) d")), ws, w=[sc])
                if last:
                    S.dma("sp", lambda e: e.dma_start(out=fg_bc[:], in_=final_g_d[0].partition_broadcast(P)), ws, w=[fg_bc])
                ws.close()
                for hh in range(8):
                    S.op("pe", lambda e, hh=hh: e.transpose(pT2[:, hh, :], kraw_ap[:, 2 * hh:2 * hh + 2, :].rearrange("p c d -> p (c d)"), ident_f[:]),
                         r=[sc, ident_f], w=[pT2])
                S.op("act", lambda e: e.activation(keysT_ap, pT2[:], AF.Copy), r=[pT2], w=[cand])
                S.op("pool", lambda e: e.memset(keysTz[:], 0.0), w=[keysTz])
                S.op("dve", lambda e: e.tensor_copy(keysTz[0:64, :, 0, :], keysT_ap[0:64, :, :]), r=[cand], w=[keysTz])
                S.op("dve", lambda e: e.tensor_copy(keysTz[64:128, :, 1, :], keysT_ap[64:128, :, :]), r=[cand], w=[keysTz])

                gi = [0]

                def routing(n_):
                    rq = []
                    i = tiles[n_]
                    k = n_ % 2
                    xk, hk, efk, eik, wgk = xt[k], h[k], ef[k], eidx[k], wgt[k]

                    def op(eng, fn, r=(), w=()):
                        rq.append(("op", eng, fn, r, w))

                    rq.append(("dma", "sp", lambda e: e.dma_start(out=xk[:], in_=xs_d[i * P:(i + 1) * P, :]), xt_slot[k], [xs_buf[i]], [xk]))
                    op("act", lambda e: e.activation(junkr[:], xk[:], AF.Square, accum_out=ss[:]), r=[xk], w=[junkr, ss])
                    op("act", lambda e: e.activation(rstd[:], ss[:], AF.Sqrt, scale=1.0 / D, bias=eps6[:, 0:1]), r=[ss, eps6], w=[rstd])
                    op("dve", lambda e: e.reciprocal(rstd[:], rstd[:]), r=[rstd], w=[rstd])
                    op("dve", lambda e: e.scalar_tensor_tensor(out=hk[:], in0=xk[:], scalar=rstd[:, 0:1], in1=modbc[:, 2048:3072],
                                                               op0=ALU.mult, op1=ALU.mult), r=[xk, rstd, modbc], w=[hk])
                    op("dve", lambda e: e.tensor_tensor(hk[:], hk[:], modbc[:, 1024:2048], op=ALU.add), r=[hk, modbc], w=[hk])
                    for j in range(8):
                        op("pe", lambda e, j=j: e.transpose(pT2[:, j, :], hk[:, j * P:(j + 1) * P], ident_f[:]), r=[hk, ident_f], w=[pT2])
                    op("act", lambda e: e.activation(hT[:], pT2[:], AF.Copy), r=[pT2], w=[hT])
                    for cc in range(2):
                        for j in range(8):
                            op("pe", lambda e, cc=cc, j=j: e.matmul(pqp[cc][:], lhsT=hT[:, j, :], rhs=wq[:, j, cc * 512:(cc + 1) * 512],
                                                                  start=(j == 0), stop=(j == 7)), r=[hT, wq], w=[pqp[cc]])
                        op("act", lambda e, cc=cc: e.activation(qp[:, cc * 512:(cc + 1) * 512], pqp[cc][:], AF.Copy), r=[pqp[cc]], w=[qp])
                    for hh in range(8):
                        op("pe", lambda e, hh=hh: e.transpose(pT2[:, hh, :], qp[:, hh * P:(hh + 1) * P], ident_f[:]), r=[qp, ident_f], w=[pT2])
                    op("act", lambda e: e.activation(qpT[:], pT2[:], AF.Copy), r=[pT2], w=[qpT])
                    for hh in range(8):
                        pv = psc[hh // 2][:].rearrange("p (a k) -> p a k", k=P)
                        op("pe", lambda e, hh=hh, pv=pv: e.matmul(pv[:, (hh % 2) * 2:(hh % 2) * 2 + 2, :], lhsT=qpT[:, hh, :],
                                                                 rhs=keysTz[:, hh, :, :], start=True, stop=True),
                           r=[qpT, keysTz], w=[psc[hh // 2]])
                    for q in range(4):
                        op("act", lambda e, q=q: e.activation(sc[:, 4 * q:4 * q + 4, :], psc[q][:].rearrange("p (a k) -> p a k", k=P), AF.Copy),
                           r=[psc[q]], w=[sc])
                    for g in range(16):
                        op("dve", lambda e, g=g: e.max(out=topv[:, g, 0:8], in_=sc[:, g, :]), r=[sc], w=[topv])
                        op("dve", lambda e, g=g: e.max_index(out=idx[:, g, 0:8], in_max=topv[:, g, 0:8], in_values=sc[:, g, :]),
                           r=[sc, topv], w=[idx])
                        op("dve", lambda e, g=g: e.match_replace(out=wk[:, g, :], in_to_replace=topv[:, g, 0:8], in_values=sc[:, g, :],
                                                                 imm_value=-1e30), r=[sc, topv], w=[wk])
                        op("dve", lambda e, g=g: e.max(out=topv[:, g, 8:16], in_=wk[:, g, :]), r=[wk], w=[topv])
                        op("dve", lambda e, g=g: e.max_index(out=idx[:, g, 8:16], in_max=topv[:, g, 8:16], in_values=wk[:, g, :]),
                           r=[wk, topv], w=[idx])
                    op("dve", lambda e: e.tensor_copy(idxf[:], idx[:]), r=[idx], w=[idxf])
                    tv4 = topv[:].rearrange("p (h c) a -> p h c a", c=2)
                    if4 = idxf[:].rearrange("p (h c) a -> p h c a", c=2)
                    op("dve", lambda e: e.tensor_tensor(cand[:].rearrange("p h (a b) -> p h a b", a=16),
                                                        tv4[:, :, 0, :].unsqueeze(3).to_broadcast([P, 8, 16, 16]),
                                                        tv4[:, :, 1, :].unsqueeze(2).to_broadcast([P, 8, 16, 16]), op=ALU.add),
                       r=[topv], w=[cand])
                    for hh in range(8):
                        op("dve", lambda e, hh=hh: e.max(out=best[:, hh, 0:8], in_=cand[:, hh, :]), r=[cand], w=[best])
                        op("dve", lambda e, hh=hh: e.max_index(out=pos[:, hh, 0:8], in_max=best[:, hh, 0:8], in_values=cand[:, hh, :]),
                           r=[cand, best], w=[pos])
                        op("dve", lambda e, hh=hh: e.match_replace(out=wk2[:, hh, :], in_to_replace=best[:, hh, 0:8],
                                                                   in_values=cand[:, hh, :], imm_value=-1e30), r=[cand, best], w=[wk2])
                        op("dve", lambda e, hh=hh: e.max(out=best[:, hh, 8:16], in_=wk2[:, hh, :]), r=[wk2], w=[best])
                        op("dve", lambda e, hh=hh: e.max_index(out=pos[:, hh, 8:16], in_max=best[:, hh, 8:16], in_values=wk2[:, hh, :]),
                           r=[wk2, best], w=[pos])
                    posf = pos[:].rearrange("p h s -> p (h s)")
                    op("dve", lambda e: e.tensor_single_scalar(pab[:, 0, :], posf, 4, op=ALU.logical_shift_right), r=[pos], w=[pab])
                    op("dve", lambda e: e.tensor_single_scalar(pab[:, 1, :], posf, 15, op=ALU.bitwise_and), r=[pos], w=[pab])
                    op("dve", lambda e: e.tensor_copy(abf[:], pab[:]), r=[pab], w=[abf])
                    for c in range(2):
                        op("dve", lambda e, c=c: e.tensor_tensor(eq_ap, iota16[:], abf[:, c, :].unsqueeze(2).to_broadcast([P, 128, 16]),
                                                                 op=ALU.is_equal), r=[iota16, abf], w=[wk])
                        op("dve", lambda e, c=c: e.tensor_tensor(eq_ap.rearrange("p (h s) a -> p h s a", h=8),
                                                                 eq_ap.rearrange("p (h s) a -> p h s a", h=8),
                                                                 if4[:, :, c, :].unsqueeze(2).to_broadcast([P, 8, 16, 16]), op=ALU.mult),
                           r=[wk, idxf], w=[wk])
                        op("dve", lambda e, c=c: e.tensor_reduce(out=isel[:, c, :], in_=eq_ap, axis=AX.X, op=ALU.add), r=[wk], w=[isel])
                    op("dve", lambda e: e.scalar_tensor_tensor(out=efk[:], in0=isel[:, 0, :], scalar=128.0, in1=isel[:, 1, :],
                                                               op0=ALU.mult, op1=ALU.add), r=[isel], w=[efk])
                    op("dve", lambda e: e.tensor_scalar(efk[:], efk[:], float(l * NPEER), None, op0=ALU.add), r=[efk], w=[efk])
                    op("dve", lambda e: e.tensor_copy(eik[:], efk[:]), r=[efk], w=[eik])
                    op("dve", lambda e: e.tensor_tensor(ew[:].rearrange("p (h s) -> p h s", h=8), best[:],
                                                        best[:, :, 0:1].to_broadcast([P, 8, 16]), op=ALU.subtract), r=[best], w=[ew])
                    op("act", lambda e: e.activation(ew[:], ew[:], AF.Exp), r=[ew], w=[ew])
                    op("dve", lambda e: e.tensor_reduce(out=se[:], in_=ew[:].rearrange("p (h s) -> p h s", h=8), axis=AX.X, op=ALU.add),
                       r=[ew], w=[se])
                    op("dve", lambda e: e.reciprocal(se[:], se[:]), r=[se], w=[se])
                    op("dve", lambda e: e.tensor_tensor(wgk[:].rearrange("p (h s) -> p h s", h=8), ew[:].rearrange("p (h s) -> p h s", h=8),
                                                        se[:].unsqueeze(2).to_broadcast([P, 8, 16]), op=ALU.mult), r=[ew, se], w=[wgk])
                    return rq

                def emit(rq, n):
                    for _ in range(n):
                        if not rq:
                            return
                        t = rq.pop(0)
                        if t[0] == "op":
                            S.op(t[1], t[2], t[3], t[4])
                        else:
                            S.dma(t[1], t[2], t[3], t[4], t[5])

                rq = routing(0)
                emit(rq, len(rq))
                for n_, i in enumerate(tiles):
                    k = n_ % 2
                    hk, efk, eik, wgk = h[k], ef[k], eidx[k], wgt[k]
                    rq = routing(n_ + 1) if n_ + 1 < len(tiles) else []
                    per_slot = -(-len(rq) // 112)
                    for g_ in range(128 // GRP):
                        pb_ = pre_b[g_ % 4]
                        for s_ in range(g_ * GRP, (g_ + 1) * GRP):
                            b_ = gi[0] % NSLOT
                            gi[0] += 1
                            S.dma("pool", lambda e, s_=s_, b_=b_: e.indirect_dma_start(
                                out=gbuf[b_][:], out_offset=None, in_=uv_d,
                                in_offset=bass.IndirectOffsetOnAxis(ap=eik[:, s_:s_ + 1], axis=0)), gslot[b_], r=[eik], w=[gbuf[b_]])
                            S.op("dve", lambda e, s_=s_, b_=b_: e.scalar_tensor_tensor(
                                out=junk[:], in0=hk[:], scalar=1.0, in1=gbuf[b_][:, 0:D], op0=ALU.mult, op1=ALU.mult,
                                accum_out=pre[:, s_:s_ + 1]), r=[hk, gbuf[b_]], w=[junk, pb_])
                            emit(rq, per_slot)
                        gs = slice(g_ * GRP, (g_ + 1) * GRP)
                        S.op("act", lambda e, gs=gs: e.activation(aw[:, gs], pre[:, gs], AF.Gelu), r=[pb_], w=[pb_])
                        S.op("dve", lambda e, gs=gs: e.tensor_tensor(aw[:, gs], aw[:, gs], wgk[:, gs], op=ALU.mult), r=[pb_, wgk], w=[pb_])
                        for s_ in range(g_ * GRP, (g_ + 1) * GRP):
                            b_ = (gi[0] - (g_ + 1) * GRP + s_) % NSLOT
                            dg = diag[di[0] % len(diag)]
                            di[0] += 1
                            S.op("act", lambda e, s_=s_, dg=dg: e.activation(dg[:], ident_b[:], AF.Identity, scale=aw[:, s_:s_ + 1]),
                                 r=[ident_b, pb_], w=[dg])
                            for cc in range(2):
                                S.op("pe", lambda e, s_=s_, b_=b_, dg=dg, cc=cc: e.matmul(
                                    pva[cc][:], lhsT=dg[:], rhs=gbuf[b_][:, D + cc * 512:D + (cc + 1) * 512],
                                    start=(s_ == 0), stop=(s_ == 127)), r=[dg, gbuf[b_]], w=[pva[cc]])
                    if l == 0:
                        dbg_store("d_pre", slice(i * P, (i + 1) * P), pre[:], pre_b)
                        dbg_store("d_eidx", slice(i * P, (i + 1) * P), efk[:], [efk])
                        dbg_store("d_w", slice(i * P, (i + 1) * P), wgk[:], [wgk])
                    for cc in range(2):
                        sl = slice(cc * 512, (cc + 1) * 512)
                        S.op("dve", lambda e, cc=cc, sl=sl: e.tensor_tensor(x2[k][:, sl], pva[cc][:], modbc[:, 3072 + cc * 512:3072 + (cc + 1) * 512],
                                                                           op=ALU.mult), r=[pva[cc], modbc], w=[x2[k]])
                    S.op("dve", lambda e: e.tensor_tensor(x2[k][:], x2[k][:], xt[k][:], op=ALU.add), r=[x2[k], xt[k]], w=[x2[k]])
                    if l == 0:
                        dbg_store("d_x2", slice(i * P, (i + 1) * P), x2[k][:], [x2[k]])
                    if last:
                        S.op("act", lambda e: e.activation(junk[:], x2[k][:], AF.Square, accum_out=ssf[:]), r=[x2[k]], w=[junk, ssf])
                        rstd_from_ss(ssf, rstdf, 1.0 / D, eps6)
                        S.op("dve", lambda e: e.scalar_tensor_tensor(out=x2[k][:], in0=x2[k][:], scalar=rstdf[:, 0:1], in1=fg_bc[:],
                                                                     op0=ALU.mult, op1=ALU.mult), r=[x2[k], rstdf, fg_bc], w=[x2[k]])
                        S.dma("sp", lambda e: e.dma_start(out=out_d[i * P:(i + 1) * P, :], in_=x2[k][:]), x2_slot[k], r=[x2[k]])
                    else:
                        S.dma("sp", lambda e: e.dma_start(out=xs_d[i * P:(i + 1) * P, :], in_=x2[k][:]), x2_slot[k], r=[x2[k]], w=[xs_buf[i]])
                    emit(rq, len(rq))
                S.barrier()

        lat_tiles = list(range(NTL))
        ctx_tiles = list(range(NTL, NT))
        if stop in (None, "p1"):
            convert_tables()
        for l in range(depth):
            last = (l == DEPTH - 1)
            if l == 0:
                src_aps = [x_d[i * P:(i + 1) * P, :] for i in range(NTL)] + [ctx_d[i * P:(i + 1) * P, :] for i in range(NTC)]
            else:
                src_aps = [xs_d[i * P:(i + 1) * P, :] for i in range(NT)]
            mod_cols(l)
            if l == 0:
                dbg_store("d_mod", (slice(0, P), slice(0, 32)), modcol[:].rearrange("p r c -> p (r c)"), [modcol])
                dbg_store("d_mod", (slice(0, P), slice(32, 48)), scT[:].rearrange("p j r -> p (j r)"), [scT])
            if stop == "m":
                break
            with ExitStack() as mix:
                M = {}
                M["qT"] = sbt(mix, "qT", [P, 4, S_LAT], BF16)
                M["qT_b"] = [[Buf(f"qT{pr}_{c}") for c in range(8)] for pr in range(4)]
                M["qTc"] = sbt(mix, "qTc", [P, 4, S_CTX], BF16)
                M["gT"] = sbt(mix, "gT", [P, 4, S_LAT + 2 * GPAD], BF16)
                M["gT_b"] = [Buf(f"gT{i}") for i in range(NTL)]
                M["gTc"] = sbt(mix, "gTc", [P, 4, S_CTX + 2 * GPAD], BF16)
                M["gTc_b"] = [Buf(f"gTc{i}") for i in range(NTC)]
                S.op("pool", lambda e: e.memset(M["gT"][:], 0.0), w=M["gT_b"])
                S.op("pool", lambda e: e.memset(M["gTc"][:], 0.0), w=M["gTc_b"])
                with ExitStack() as ab:
                    M["kT"] = sbt(ab, "kT", [P, NT * P], BF16)
                    M["kT_b"] = [Buf(f"kT{i}") for i in range(NT)]
                    M["Vp"] = sbt(ab, "Vp", [P, NT, 192], BF16)
                    M["Vp_b"] = [Buf(f"Vp{i}") for i in range(NT)]
                    S.op("pool", lambda e: e.memset(M["Vp"][:], 1.0), w=M["Vp_b"])
                    phase_a(l, M, src_aps, last)
                    if stop in ("a", "a0", "a1"):
                        break
                    phase_b(l, M, last)
                if stop == "b":
                    break
                with ExitStack() as cs:
                    modbc = sbt(cs, "modbcC", [P, 4096], F32)
                    mod_bc(l, 0, modbc)
                    phase_c(l, M, lat_tiles, src_aps, 0, modbc)
                    if not last:
                        mod_bc(l, 1, modbc)
                        phase_c(l, M, ctx_tiles, src_aps, 1, modbc)
            if stop == "c":
                break
            with ExitStack() as pp:
                modbc = sbt(pp, "modbcP", [P, 4096], F32)
                mod_bc(l, 0, modbc)
                phase_p(l, lat_tiles if stop not in ("p1", "p0") else lat_tiles[:2], modbc, last)
                if not last and stop not in ("p1", "p0"):
                    mod_bc(l, 1, modbc)
                    phase_p(l, ctx_tiles, modbc, last)
            if stop in ("p1", "p0"):
                break
        out_slot.close()
        if out_slot.sem is not None:
            for en in ("sp", "act", "pool"):
                S.E[en].eng.wait_ge(out_slot.sem, out_slot.val)
        S.barrier()
        print(f"[build] instr={S.ninstr} waits={S.nwait} sems={S.nsem}")
    return nc


_NC_CACHE = {}


def _rope_tables():
    n = 16
    inv = (10000.0 ** (-np.arange(n, dtype=np.float32) / n)).astype(np.float32)
    row = np.repeat(np.arange(64), 64).astype(np.float32)
    col = np.tile(np.arange(64), 64).astype(np.float32)
    ang = np.concatenate([row[:, None] * inv, col[:, None] * inv], axis=-1).astype(np.float32)
    cos = np.cos(ang).astype(np.float32)
    sin = np.sin(ang).astype(np.float32)
    cos2 = np.concatenate([cos, cos], axis=-1)
    sin2 = np.concatenate([-sin, sin], axis=-1)
    return np.ascontiguousarray(cos2), np.ascontiguousarray(sin2)


def make_in_maps(inputs, cores):
    f = lambda a: np.ascontiguousarray(np.asarray(a, dtype=np.float32))
    cos2, sin2 = _rope_tables()
    shared = {k: f(inputs[k]) for k in ["w_mod", "b_mod", "w_in", "q_gain", "k_gain", "conv_w", "conv_b", "ln_g", "ln_b",
                                        "beta_attn", "beta_conv", "w_o", "peer_wq", "peer_keys", "peer_u", "peer_v"]}
    shared["final_g"] = f(inputs["final_g"]).reshape(1, D)
    shared["cos2"] = cos2
    shared["sin2"] = sin2
    shared["cctxT"] = np.ascontiguousarray(f(inputs["c_ctx"]).reshape(8, P).T)
    x = f(inputs["x"])
    ctx = f(inputs["ctx"])
    c = f(inputs["c"])
    maps = []
    for b in cores:
        m = dict(shared)
        m["x"] = x[b]
        m["ctx"] = ctx[b]
        m["cT"] = np.ascontiguousarray(c[b].reshape(8, P).T)
        maps.append(m)
    return maps


def kernel(**inputs):
    if "nc" not in _NC_CACHE:
        _NC_CACHE["nc"] = build()
    nc = _NC_CACHE["nc"]
    B = np.asarray(inputs["x"]).shape[0]
    maps = make_in_maps(inputs, list(range(B)))
    res = run_bass_kernel_spmd(nc, maps, core_ids=list(range(B)))
    out = np.stack([np.asarray(r["out"], dtype=np.float32) for r in res.results], axis=0)
    return out
```

```python
import numpy as np
from contextlib import ExitStack
import concourse.bass as bass
import concourse.mybir as mybir
from concourse.bass_utils import run_bass_kernel_spmd

F32 = mybir.dt.float32
BF16 = mybir.dt.bfloat16
I32 = mybir.dt.int32
U32 = mybir.dt.uint32
ALU = mybir.AluOpType
AF = mybir.ActivationFunctionType
AX = mybir.AxisListType

import os
CUT = int(os.environ.get("KCUT", "99"))
SEM_LIMIT = 30000


class Buf:
    __slots__ = ("name", "w", "r")

    def __init__(self, name):
        self.name = name
        self.w = None
        self.r = {}


class Eng:
    def __init__(self, sync, name, eng):
        self.sync = sync
        self.name = name
        self.eng = eng
        self.sem = None
        self.count = 0
        self.known = {}
        self.last = None

    def new_event(self):
        if self.sem is None or self.count >= SEM_LIMIT:
            self.sem = self.sync.new_sem(self.name)
            self.count = 0
        self.count += 1
        self.last = [self.sem, self.count, self.name]
        return self.last


def DmaSlot(sync, name, group=False):
    if name not in sync.slot_by_name:
        sync.slot_by_name[name] = _DmaSlot(sync, name, group)
    return sync.slot_by_name[name]


class _DmaSlot:
    def __init__(self, sync, name, group=False):
        self.name = name
        self.sem = None
        self.val = 0
        self.group = group
        self.pending = []
        sync.slots.append(self)

    def bump(self, sync):
        if self.sem is None or (self.val >= SEM_LIMIT and not self.pending):
            self.sem = sync.new_sem("d" + self.name)
            self.val = 0
        self.val += 16
        ev = [self.sem, self.val, "dma"]
        if self.group:
            self.pending.append(ev)
        return ev

    def close(self):
        for ev in self.pending:
            ev[1] = self.val
        self.pending = []


class Sync:
    def __init__(self, nc, stack):
        self.nc = nc
        self.stack = stack
        self.nsem = 0
        self.slots = []
        self.slot_by_name = {}
        self.E = {
            "pe": Eng(self, "pe", nc.tensor),
            "dve": Eng(self, "dve", nc.vector),
            "act": Eng(self, "act", nc.scalar),
            "pool": Eng(self, "pool", nc.gpsimd),
            "sp": Eng(self, "sp", nc.sync),
        }
        self.ninstr = 0
        self.nwait = 0

    def new_sem(self, name):
        self.nsem += 1
        return self.stack.enter_context(self.nc.semaphore(f"s{self.nsem}_{name}"))

    def _wait(self, E, evs):
        best = {}
        for ev in evs:
            if ev is None:
                continue
            if E.name == "pe" and ev[2] == "pe":
                continue
            k = id(ev[0])
            if k not in best or best[k][1] < ev[1]:
                best[k] = ev
        for ev in best.values():
            sem, val, src = ev
            if E.known.get(id(sem), 0) >= val:
                continue
            E.eng.wait_ge(sem, val)
            self.nwait += 1
            E.known[id(sem)] = val

    def _deps(self, E, reads, writes):
        evs = []
        for b in reads:
            evs.append(b.w)
        for b in writes:
            if b.w is not None and b.w[2] != E.name:
                evs.append(b.w)
            for ev in b.r.values():
                if ev[2] == E.name:
                    continue
                evs.append(ev)
        return evs

    @staticmethod
    def _bufs(lst):
        return [t.b if hasattr(t, "b") else t for t in lst]

    def op(self, engname, fn, r=(), w=()):
        E = self.E[engname]
        r = self._bufs(r)
        w = self._bufs(w)
        self._wait(E, self._deps(E, r, w))
        ins = fn(E.eng)
        ev = E.new_event()
        ins.then_inc(ev[0], 1)
        self.ninstr += 1
        key = ev[2] if ev[2] != "dma" else id(ev[0])
        for b in r:
            b.r[key] = ev
        for b in w:
            b.w = ev
            b.r = {}
        return ev

    def dma(self, qname, fn, slot, r=(), w=()):
        E = self.E[qname]
        r = self._bufs(r)
        w = self._bufs(w)
        self._wait(E, self._deps(E, r, w))
        ins = fn(E.eng)
        ev = slot.bump(self)
        ins.then_inc(ev[0], 16)
        self.ninstr += 1
        key = ev[2] if ev[2] != "dma" else id(ev[0])
        for b in r:
            b.r[key] = ev
        for b in w:
            b.w = ev
            b.r = {}
        return ev

    def barrier(self):
        for s in self.slots:
            if s.group:
                s.close()
        evs = []
        for E in self.E.values():
            if E.last is not None:
                evs.append(E.last)
        for s in self.slots:
            if s.sem is not None:
                evs.append([s.sem, s.val, "dma"])
        for E in self.E.values():
            self._wait(E, [ev for ev in evs if not (E.name == "pe" and ev[2] == "pe")])


class Tile:
    def __init__(self, h, name):
        self.h = h
        self.b = Buf(name)

    def __getitem__(self, k):
        return self.h[k]


P = 128
D = 1024
S_LAT = 4096
S_CTX = 256
NTL = 32
NTC = 2
NT = NTL + NTC
DEPTH = 2
INC = 1792
EPS = 1e-6
NPEER = 16384
GPAD = 16
NSLOT = 12
GRP = 8


def build(depth=DEPTH, dbg=False, stop=None):
    nc = bass.Bass("TRN2", target_bir_lowering=False)

    def din(name, shape, dt=F32):
        return nc.dram_tensor(name, shape, dt, kind="ExternalInput").ap()

    x_d = din("x", [S_LAT, D])
    ctx_d = din("ctx", [S_CTX, D])
    cT_d = din("cT", [P, 8])
    cctxT_d = din("cctxT", [P, 8])
    w_mod_d = din("w_mod", [DEPTH, D, 6 * D])
    b_mod_d = din("b_mod", [DEPTH, 6 * D])
    w_in_d = din("w_in", [DEPTH, D, INC])
    q_gain_d = din("q_gain", [DEPTH, 64])
    k_gain_d = din("k_gain", [DEPTH, 64])
    conv_w_d = din("conv_w", [DEPTH, 31, 512])
    conv_b_d = din("conv_b", [DEPTH, 512])
    ln_g_d = din("ln_g", [DEPTH, 512])
    ln_b_d = din("ln_b", [DEPTH, 512])
    beta_attn_d = din("beta_attn", [DEPTH, 512])
    beta_conv_d = din("beta_conv", [DEPTH, 512])
    w_o_d = din("w_o", [DEPTH, D, D])
    peer_wq_d = din("peer_wq", [DEPTH, D, D])
    peer_keys_d = din("peer_keys", [DEPTH, 8, 2, 128, 64])
    peer_u_d = din("peer_u", [DEPTH, NPEER, D])
    peer_v_d = din("peer_v", [DEPTH, NPEER, D])
    final_g_d = din("final_g", [1, D])
    cos2_d = din("cos2", [S_LAT, 64])
    sin2_d = din("sin2", [S_LAT, 64])
    peer_u_flat = peer_u_d.rearrange("l n d -> (l n) d")
    peer_v_flat = peer_v_d.rearrange("l n d -> (l n) d")
    out_d = nc.dram_tensor("out", [S_LAT, D], F32, kind="ExternalOutput").ap()
    xs_d = nc.dram_tensor("xs", [S_LAT + S_CTX, D], F32).ap()
    uv_d = nc.dram_tensor("uv", [DEPTH * NPEER, 2 * D], BF16).ap()
    dbg_d = {}
    if dbg:
        for nm, shp in [("d_qkvu", [NT * P, INC]), ("d_x1", [NT * P, D]), ("d_oT", [P, 4 * S_LAT]),
                        ("d_pre", [NT * P, 128]), ("d_eidx", [NT * P, 128]), ("d_w", [NT * P, 128]),
                        ("d_qT", [P, 4 * S_LAT]), ("d_x2", [NT * P, D]), ("d_mod", [P, 4096])]:
            dbg_d[nm] = nc.dram_tensor(nm, shp, F32, kind="ExternalOutput").ap()

    top = ExitStack()
    with top:
        S = Sync(nc, top)

        uid = [0]

        def sbt(stack, name, shape, dt):
            uid[0] += 1
            name = f"{name}_u{uid[0]}"
            return Tile(stack.enter_context(nc.sbuf_tensor(name, shape, dt)), name)

        def pst(stack, name, shape, dt):
            uid[0] += 1
            name = f"{name}_u{uid[0]}"
            return Tile(stack.enter_context(nc.psum_tensor(name, shape, dt)), name)

        out_slot = DmaSlot(S, "out", group=True)
        dbg_slots = {}
        xs_buf = [Buf(f"xs{i}") for i in range(NT)]

        ident_f = sbt(top, "ident_f", [P, P], F32)
        ident_b = sbt(top, "ident_b", [P, P], BF16)
        ones_f = sbt(top, "ones_f", [P, P], F32)
        io_t = sbt(top, "io_t", [P, P], F32)
        pid_t = sbt(top, "pid_t", [P, 1], F32)
        S.op("pool", lambda e: e.iota(io_t[:], pattern=[[1, P]], base=0, channel_multiplier=0,
                                      allow_small_or_imprecise_dtypes=True), w=[io_t])
        S.op("pool", lambda e: e.iota(pid_t[:], pattern=[[0, 1]], base=0, channel_multiplier=1,
                                      allow_small_or_imprecise_dtypes=True), w=[pid_t])
        S.op("dve", lambda e: e.tensor_scalar(ident_f[:], io_t[:], pid_t[:, 0:1], None, op0=ALU.is_equal),
             r=[io_t, pid_t], w=[ident_f])
        S.op("dve", lambda e: e.tensor_copy(ident_b[:], ident_f[:]), r=[ident_f], w=[ident_b])
        S.op("pool", lambda e: e.memset(ones_f[:], 1.0), w=[ones_f])

        cslot = DmaSlot(S, "const", group=True)
        craw = sbt(top, "craw", [P, 2, 8], F32)
        scT = sbt(top, "scT", [P, 8, 2], F32)
        S.dma("sp", lambda e: e.dma_start(out=craw[:, 0, :], in_=cT_d), cslot, w=[craw])
        S.dma("sp", lambda e: e.dma_start(out=craw[:, 1, :], in_=cctxT_d), cslot, w=[craw])
        cslot.close()
        S.op("act", lambda e: e.activation(scT[:].rearrange("p j r -> p r j"), craw[:], AF.Silu), r=[craw], w=[scT])
        modcol = sbt(top, "modcol", [P, 2, 16], F32)
        eps6 = sbt(top, "eps6", [P, 1], F32)
        eps5 = sbt(top, "eps5", [P, 1], F32)
        S.op("pool", lambda e: e.memset(eps6[:], 1e-6), w=[eps6])
        S.op("pool", lambda e: e.memset(eps5[:], 1e-5), w=[eps5])


        def rstd_from_ss(ss, rs, scale, eps_t):
            S.op("act", lambda e: e.activation(rs[:], ss[:], AF.Sqrt, scale=scale, bias=eps_t[:, 0:1]), r=[ss, eps_t], w=[rs])
            S.op("dve", lambda e: e.reciprocal(rs[:], rs[:]), r=[rs], w=[rs])

        def dbg_store(name, rows, tile_ap, rtiles):
            if dbg and name in dbg_d:
                if name not in dbg_slots:
                    dbg_slots[name] = (DmaSlot(S, name), Buf(name))
                S.dma("sp", lambda e: e.dma_start(out=dbg_d[name][rows], in_=tile_ap), dbg_slots[name][0], r=rtiles, w=[dbg_slots[name][1]])


        def convert_tables():
            with ExitStack() as ph:
                NB = 4
                stg = [sbt(ph, f"cvi{k}", [P, 4, D], F32) for k in range(NB)]
                obf = [sbt(ph, f"cvo{k}", [P, 4, D], BF16) for k in range(NB)]
                islot = [DmaSlot(S, f"cvi{k}") for k in range(NB)]
                oslot = [DmaSlot(S, f"cvo{k}") for k in range(NB)]
                n = 0
                for l in range(DEPTH):
                    for tab, col0 in ((peer_u_d, 0), (peer_v_d, D)):
                        for t in range(NPEER // 512):
                            k = n % NB
                            src = tab[l, t * 512:(t + 1) * 512, :].rearrange("(p r) d -> p r d", r=4)
                            dst = uv_d[l * NPEER + t * 512:l * NPEER + (t + 1) * 512, col0:col0 + D].rearrange("(p r) d -> p r d", r=4)
                            S.dma("sp", lambda e, k=k, src=src: e.dma_start(out=stg[k][:], in_=src), islot[k], w=[stg[k]])
                            if n % 2 == 0:
                                S.op("act", lambda e, k=k: e.activation(obf[k][:], stg[k][:], AF.Copy), r=[stg[k]], w=[obf[k]])
                            else:
                                S.op("dve", lambda e, k=k: e.tensor_copy(obf[k][:], stg[k][:]), r=[stg[k]], w=[obf[k]])
                            S.dma("pool", lambda e, k=k, dst=dst: e.dma_start(out=dst, in_=obf[k][:]), oslot[k], r=[obf[k]])
                            n += 1
                S.barrier()

        def mod_cols(l):
            with ExitStack() as ph:
                wm = [sbt(ph, f"wm{k}", [P, 8, P], F32) for k in range(2)]
                wslot = [DmaSlot(S, f"wm{k}") for k in range(2)]
                bcol = sbt(ph, "bcol", [P, 16], F32)
                pcol = pst(ph, "pcol", [P, 16, 2], F32)
                S.dma("sp", lambda e: e.dma_start(out=bcol[:], in_=b_mod_d[l, 0:2048].rearrange("(c p) -> p c", p=P),
                                                  allow_slow_non_contiguous=True), DmaSlot(S, "bcol"), w=[bcol])
                for cc in range(16):
                    t = wm[cc % 2]
                    S.dma("sp", lambda e, t=t, cc=cc: e.dma_start(
                        out=t[:], in_=w_mod_d[l, :, cc * P:(cc + 1) * P].rearrange("(j p) m -> p j m", p=P)),
                          wslot[cc % 2], w=[t])
                    for j in range(8):
                        S.op("pe", lambda e, t=t, j=j, cc=cc: e.matmul(pcol[:, cc, :], lhsT=t[:, j, :],
                                                                     rhs=scT[:, j, :], start=(j == 0), stop=(j == 7)),
                             r=[t, scT], w=[pcol])
                S.op("dve", lambda e: e.tensor_tensor(modcol[:].rearrange("p r c -> p c r"), pcol[:],
                                                      bcol[:].unsqueeze(2).to_broadcast([P, 16, 2]), op=ALU.add),
                     r=[pcol, bcol], w=[modcol])
                S.op("dve", lambda e: e.tensor_scalar(modcol[:, :, 8:16], modcol[:, :, 8:16], 1.0, None, op0=ALU.add),
                     r=[modcol], w=[modcol])
                S.barrier()

        def mod_bc(l, r, modbc):
            with ExitStack() as ph:
                NW = 4
                wm = [sbt(ph, f"wmb{k}", [P, 512], F32) for k in range(NW)]
                wslot = [DmaSlot(S, f"wmb{k}") for k in range(NW)]
                bbc = [sbt(ph, f"bbc{k}", [P, 512], F32) for k in range(2)]
                bslot = [DmaSlot(S, f"bbc{k}") for k in range(2)]
                screp = sbt(ph, "screp", [P, 8, P], F32)
                pb = pst(ph, "pbm", [P, 512], F32)
                S.op("dve", lambda e: e.tensor_copy(screp[:], scT[:, :, r].unsqueeze(2).to_broadcast([P, 8, P])),
                     r=[scT], w=[screp])
                n = 0
                for cc in range(8):
                    c0 = 2048 + cc * 512
                    S.dma("sp", lambda e, cc=cc, c0=c0: e.dma_start(out=bbc[cc % 2][:], in_=b_mod_d[l, c0:c0 + 512].partition_broadcast(P)),
                          bslot[cc % 2], w=[bbc[cc % 2]])
                    for j in range(8):
                        t = wm[n % NW]
                        S.dma("sp", lambda e, t=t, j=j, c0=c0: e.dma_start(out=t[:], in_=w_mod_d[l, j * P:(j + 1) * P, c0:c0 + 512]),
                              wslot[n % NW], w=[t])
                        n += 1
                        S.op("pe", lambda e, t=t, j=j: e.matmul(pb[:], lhsT=screp[:, j, :], rhs=t[:], start=(j == 0), stop=(j == 7)),
                             r=[t, screp], w=[pb])
                    S.op("dve", lambda e, cc=cc: e.tensor_tensor(modbc[:, cc * 512:(cc + 1) * 512], pb[:], bbc[cc % 2][:], op=ALU.add),
                         r=[pb, bbc[cc % 2]], w=[modbc])
                S.op("dve", lambda e: e.tensor_scalar(modbc[:, 2048:3072], modbc[:, 2048:3072], 1.0, None, op0=ALU.add),
                     r=[modbc], w=[modbc])
                S.barrier()

        def phase_a(l, M, src_aps, last):
            with ExitStack() as ph:
                win = sbt(ph, "win", [P, 8, INC], BF16)
                stg = sbt(ph, "stg", [P, INC], F32)
                stg_slot = DmaSlot(S, "stg")
                bias_bc = sbt(ph, "bias_bc", [P, INC], F32)
                sh1rep = sbt(ph, "sh1rep", [P, 2, 8, P], BF16)
                gain = sbt(ph, "gain", [P, 640], F32)
                graw = sbt(ph, "graw", [P, 128], F32)
                cos2 = sbt(ph, "cos2", [P, NTL, 64], F32)
                sin2 = sbt(ph, "sin2", [P, NTL, 64], F32)
                xt = [sbt(ph, f"xt{k}", [P, D], F32) for k in range(2)]
                xt_slot = [DmaSlot(S, f"xt{k}") for k in range(2)]
                xb = [sbt(ph, f"xb{k}", [P, D], BF16) for k in range(2)]
                xT = [sbt(ph, f"xT{k}", [P, 8, P], BF16) for k in range(2)]
                junk = sbt(ph, "junkA", [P, D], BF16)
                ss = [sbt(ph, f"ssA{k}", [P, 1], F32) for k in range(2)]
                rstd = [sbt(ph, f"rstdA{k}", [P, 1], F32) for k in range(2)]
                qkvu = sbt(ph, "qkvu", [P, INC], F32)
                sq = sbt(ph, "sq", [P, 640], F32)
                ssq = sbt(ph, "ssq", [P, 10], F32)
                rsq = sbt(ph, "rsq", [P, 10], F32)
                qn = sbt(ph, "qn", [P, 640], F32)
                rb = sbt(ph, "rb", [P, 640], F32)
                qr = sbt(ph, "qr", [P, 4, 2, 64], BF16)
                kr = sbt(ph, "kr", [P, 128], BF16)
                sig = sbt(ph, "sig", [P, 512], F32)
                gg = sbt(ph, "gg", [P, 512], BF16)
                ptb = pst(ph, "ptb", [P, 8, P], BF16)
                pq = [pst(ph, f"pq{k}", [P, 512], F32) for k in range(4)]
                ptq = pst(ph, "ptq", [P, 5, P], BF16)
                ptg = pst(ph, "ptg", [P, 4, P], BF16)
                ra = sq
                qT, qTc, kT, Vp, gT, gTc = M["qT"], M["qTc"], M["kT"], M["Vp"], M["gT"], M["gTc"]

                cslot2 = DmaSlot(S, "rope", group=True)
                S.dma("sp", lambda e: e.dma_start(out=cos2[:], in_=cos2_d.rearrange("(n p) f -> p n f", p=P)), cslot2, w=[cos2])
                S.dma("sp", lambda e: e.dma_start(out=sin2[:], in_=sin2_d.rearrange("(n p) f -> p n f", p=P)), cslot2, w=[sin2])
                S.dma("sp", lambda e: e.dma_start(out=graw[:, 0:64], in_=q_gain_d[l].partition_broadcast(P)), cslot2, w=[graw])
                S.dma("sp", lambda e: e.dma_start(out=graw[:, 64:128], in_=k_gain_d[l].partition_broadcast(P)), cslot2, w=[graw])
                cslot2.close()
                for j in range(8):
                    S.dma("sp", lambda e, j=j: e.dma_start(out=stg[:], in_=w_in_d[l, j * P:(j + 1) * P, :]), stg_slot, w=[stg])
                    S.op("act", lambda e, j=j: e.activation(win[:, j, :], stg[:], AF.Copy), r=[stg], w=[win])
                S.op("dve", lambda e: e.tensor_copy(gain[:, 0:512].rearrange("p (h d) -> p h d", h=8),
                                                    graw[:, 0:64].unsqueeze(1).to_broadcast([P, 8, 64])), r=[graw], w=[gain])
                S.op("dve", lambda e: e.tensor_copy(gain[:, 512:640].rearrange("p (h d) -> p h d", h=2),
                                                    graw[:, 64:128].unsqueeze(1).to_broadcast([P, 2, 64])), r=[graw], w=[gain])
                S.op("dve", lambda e: e.tensor_copy(sh1rep[:], modcol[:, :, 0:8].unsqueeze(3).to_broadcast([P, 2, 8, P])),
                     r=[modcol], w=[sh1rep])

                def make_bias(r_):
                    for cc in range(4):
                        w_ = min(512, INC - cc * 512)
                        for j in range(8):
                            S.op("pe", lambda e, cc=cc, j=j, w_=w_: e.matmul(
                                pq[cc][:, 0:w_], lhsT=sh1rep[:, r_, j, :], rhs=win[:, j, cc * 512:cc * 512 + w_],
                                start=(j == 0), stop=(j == 7)), r=[sh1rep, win], w=[pq[cc]])
                        S.op("act", lambda e, cc=cc, w_=w_: e.activation(
                            bias_bc[:, cc * 512:cc * 512 + w_], pq[cc][:, 0:w_], AF.Copy), r=[pq[cc]], w=[bias_bc])

                def load(i):
                    k = i % 2
                    S.dma("sp", lambda e: e.dma_start(out=xt[k][:], in_=src_aps[i]), xt_slot[k], r=[xs_buf[i]], w=[xt[k]])

                make_bias(0)
                load(0)
                for i in range(NT if stop not in ("a0", "a1") else (0 if stop == "a0" else 1)):
                    k = i % 2
                    r_ = 0 if i < NTL else 1
                    is_ctx = i >= NTL
                    if i == NTL:
                        make_bias(1)
                    if i + 1 < NT:
                        load(i + 1)
                    S.op("act", lambda e: e.activation(junk[:], xt[k][:], AF.Square, accum_out=ss[k][:]), r=[xt[k]], w=[junk, ss[k]])
                    S.op("act", lambda e: e.activation(xb[k][:], xt[k][:], AF.Copy), r=[xt[k]], w=[xb[k]])
                    rstd_from_ss(ss[k], rstd[k], 1.0 / D, eps6)
                    for j in range(8):
                        S.op("pe", lambda e, j=j: e.transpose(ptb[:, j, :], xb[k][:, j * P:(j + 1) * P], ident_b[:]),
                             r=[xb[k], ident_b], w=[ptb])
                    S.op("dve", lambda e: e.tensor_tensor(xT[k][:], ptb[:],
                                                          modcol[:, r_, 8:16].unsqueeze(2).to_broadcast([P, 8, P]), op=ALU.mult),
                         r=[ptb, modcol], w=[xT[k]])
                    if CUT <= 1:
                        continue
                    for cc in range(4):
                        w_ = min(512, INC - cc * 512)
                        for j in range(8):
                            S.op("pe", lambda e, cc=cc, j=j, w_=w_: e.matmul(
                                pq[cc][:, 0:w_], lhsT=xT[k][:, j, :], rhs=win[:, j, cc * 512:cc * 512 + w_],
                                start=(j == 0), stop=(j == 7)), r=[xT[k], win], w=[pq[cc]])
                        S.op("dve", lambda e, cc=cc, w_=w_: e.scalar_tensor_tensor(
                            out=qkvu[:, cc * 512:cc * 512 + w_], in0=pq[cc][:, 0:w_], scalar=rstd[k][:, 0:1],
                            in1=bias_bc[:, cc * 512:cc * 512 + w_], op0=ALU.mult, op1=ALU.add),
                             r=[pq[cc], rstd[k], bias_bc], w=[qkvu])
                    if l == 0:
                        dbg_store("d_qkvu", slice(i * P, (i + 1) * P), qkvu[:], [qkvu])
                    if CUT <= 2:
                        continue
                    S.op("pool", lambda e: e.tensor_tensor(sq[:], qkvu[:, 0:640], qkvu[:, 0:640], op=ALU.mult), r=[qkvu], w=[sq])
                    S.op("dve", lambda e: e.tensor_reduce(out=ssq[:], in_=sq[:].rearrange("p (h d) -> p h d", h=10),
                                                          axis=AX.X, op=ALU.add), r=[sq], w=[ssq])
                    rstd_from_ss(ssq, rsq, 1.0 / 64, eps6)
                    S.op("dve", lambda e: e.tensor_tensor(qn[:].rearrange("p (h d) -> p h d", h=10),
                                                          qkvu[:, 0:640].rearrange("p (h d) -> p h d", h=10),
                                                          rsq[:].unsqueeze(2).to_broadcast([P, 10, 64]), op=ALU.mult),
                         r=[qkvu, rsq], w=[qn])
                    S.op("pool", lambda e: e.tensor_tensor(qn[:], qn[:], gain[:], op=ALU.mult), r=[qn, gain], w=[qn])
                    if CUT <= 3:
                        continue
                    if not is_ctx:
                        qn3 = qn[:].rearrange("p (h d) -> p h d", h=10)
                        rb3 = rb[:].rearrange("p (h d) -> p h d", h=10)
                        S.op("dve", lambda e: e.tensor_tensor(ra[:].rearrange("p (h d) -> p h d", h=10), qn3,
                                                              cos2[:, i, :].unsqueeze(1).to_broadcast([P, 10, 64]), op=ALU.mult),
                             r=[qn, cos2], w=[ra])
                        S.op("pool", lambda e: e.tensor_tensor(rb3[:, :, 0:32], qn3[:, :, 32:64],
                                                               sin2[:, i, 0:32].unsqueeze(1).to_broadcast([P, 10, 32]), op=ALU.mult),
                             r=[qn, sin2], w=[rb])
                        S.op("pool", lambda e: e.tensor_tensor(rb3[:, :, 32:64], qn3[:, :, 0:32],
                                                               sin2[:, i, 32:64].unsqueeze(1).to_broadcast([P, 10, 32]), op=ALU.mult),
                             r=[qn, sin2], w=[rb])
                        S.op("dve", lambda e: e.tensor_tensor(qr[:].rearrange("p pr hh d -> p hh pr d"),
                                                              ra[:, 0:512].rearrange("p (hh pr d) -> p hh pr d", hh=2, pr=4),
                                                              rb[:, 0:512].rearrange("p (hh pr d) -> p hh pr d", hh=2, pr=4), op=ALU.add),
                             r=[ra, rb], w=[qr])
                        S.op("pool", lambda e: e.tensor_tensor(kr[:], ra[:, 512:640], rb[:, 512:640], op=ALU.add), r=[ra, rb], w=[kr])
                    else:
                        S.op("dve", lambda e: e.tensor_copy(qr[:].rearrange("p pr hh d -> p hh pr d"),
                                                            qn[:, 0:512].rearrange("p (hh pr d) -> p hh pr d", hh=2, pr=4)),
                             r=[qn], w=[qr])
                        S.op("pool", lambda e: e.tensor_copy(kr[:], qn[:, 512:640]), r=[qn], w=[kr])
                    if CUT <= 4:
                        continue
                    need_q = (not is_ctx) or (not last)
                    if need_q:
                        for pr in range(4):
                            S.op("pe", lambda e, pr=pr: e.transpose(ptq[:, pr, :], qr[:, pr, :, :].rearrange("p hh d -> p (hh d)"), ident_b[:]),
                                 r=[qr, ident_b], w=[ptq])
                    S.op("pe", lambda e: e.transpose(ptq[:, 4, :], kr[:], ident_b[:]), r=[kr, ident_b], w=[ptq])
                    if need_q:
                        if not is_ctx:
                            S.op("act", lambda e: e.activation(qT[:, :, i * P:(i + 1) * P], ptq[:, 0:4, :], AF.Copy),
                                 r=[ptq], w=[M["qT_b"][pr][i // 4] for pr in range(4)])
                        else:
                            S.op("act", lambda e: e.activation(qTc[:, :, (i - NTL) * P:(i - NTL + 1) * P], ptq[:, 0:4, :], AF.Copy),
                                 r=[ptq], w=[qTc])
                    S.op("act", lambda e: e.activation(kT[:, i * P:(i + 1) * P], ptq[:, 4, :], AF.Copy), r=[ptq], w=[M["kT_b"][i]])
                    if CUT <= 5:
                        continue
                    S.op("pool", lambda e: e.tensor_copy(Vp[:, i, 0:64], qkvu[:, 640:704]), r=[qkvu], w=[M["Vp_b"][i]])
                    S.op("pool", lambda e: e.tensor_copy(Vp[:, i, 128:192], qkvu[:, 704:768]), r=[qkvu], w=[M["Vp_b"][i]])
                    if CUT <= 6:
                        continue
                    if need_q:
                        S.op("act", lambda e: e.activation(sig[:], qkvu[:, 1280:1792], AF.Sigmoid), r=[qkvu], w=[sig])
                        S.op("dve", lambda e: e.tensor_tensor(gg[:], qkvu[:, 768:1280], sig[:], op=ALU.mult), r=[qkvu, sig], w=[gg])
                        for c in range(4):
                            S.op("pe", lambda e, c=c: e.transpose(ptg[:, c, :], gg[:, c * P:(c + 1) * P], ident_b[:]),
                                 r=[gg, ident_b], w=[ptg])
                        if not is_ctx:
                            S.op("act", lambda e: e.activation(gT[:, :, GPAD + i * P:GPAD + (i + 1) * P], ptg[:], AF.Copy),
                                 r=[ptg], w=[M["gT_b"][i]])
                        else:
                            S.op("act", lambda e: e.activation(gTc[:, :, GPAD + (i - NTL) * P:GPAD + (i - NTL + 1) * P], ptg[:], AF.Copy),
                                 r=[ptg], w=[M["gTc_b"][i - NTL]])
                S.barrier()

        def phase_b(l, M, last):
            with ExitStack() as ph:
                NPT = 4
                NPS = 4
                LA = 1
                pT = [sbt(ph, f"pT{k}", [P, 512], BF16) for k in range(NPT)]
                rd = sbt(ph, "rd", [P, 512], F32)
                bcs = sbt(ph, "bcs", [P, 512], F32)
                ps_s = [pst(ph, f"ps_s{k}", [P, 512], F32) for k in range(NPS)]
                po = [pst(ph, f"po{k}", [P, 512], F32) for k in range(3)]
                pbc = pst(ph, "pbc", [P, 512], F32)
                qT, qTc, kT, Vp = M["qT"], M["qTc"], M["kT"], M["Vp"]
                cnt = [0]
                grp = [0]

                def attend(qsrc_fn, qbuf, n, ktiles):
                    its = [(kt_i, kt, hh) for kt_i, kt in enumerate(ktiles) for hh in range(2)]
                    base = cnt[0]
                    cnt[0] += len(its)
                    pg = [po[(2 * grp[0]) % 3], po[(2 * grp[0] + 1) % 3]]
                    grp[0] += 1

                    def emit_s(m):
                        kt_i, kt, hh = its[m]
                        k = (base + m) % NPS
                        kp = (base + m) % NPT
                        S.op("pe", lambda e: e.matmul(ps_s[k][:, 0:n], lhsT=kT[hh * 64:(hh + 1) * 64, kt * P:(kt + 1) * P], rhs=qsrc_fn(hh),
                                                      start=True, stop=True), r=[M["kT_b"][kt], qbuf], w=[ps_s[k]])
                        S.op("act", lambda e: e.activation(pT[kp][:, 0:n], ps_s[k][:, 0:n], AF.Exp, scale=0.125),
                             r=[ps_s[k]], w=[pT[kp]])

                    def emit_pv(m):
                        kt_i, kt, hh = its[m]
                        kp = (base + m) % NPT
                        S.op("pe", lambda e: e.matmul(pg[hh][:, 0:n], lhsT=Vp[:, kt, hh * 64:hh * 64 + 128], rhs=pT[kp][:, 0:n],
                                                      start=(kt_i == 0), stop=(kt_i == len(ktiles) - 1)), r=[M["Vp_b"][kt], pT[kp]], w=[pg[hh]])

                    npair = len(its) // 2
                    for j in range(npair + LA):
                        if j < npair:
                            emit_s(2 * j)
                            emit_s(2 * j + 1)
                        if j - LA >= 0:
                            emit_pv(2 * (j - LA))
                            emit_pv(2 * (j - LA) + 1)
                    for hh in range(2):
                        dp = 64 if hh == 0 else 0
                        S.op("dve", lambda e, hh=hh, dp=dp: e.reciprocal(rd[dp:dp + 1, 0:n], pg[hh][dp:dp + 1, 0:n]),
                             r=[pg[hh]], w=[rd])
                        S.op("pe", lambda e, dp=dp: e.matmul(pbc[0:64, 0:n], lhsT=ones_f[dp:dp + 1, 0:64], rhs=rd[dp:dp + 1, 0:n],
                                                            start=True, stop=True), r=[ones_f, rd], w=[pbc])
                        S.op("act", lambda e, hh=hh: e.activation(bcs[hh * 64:(hh + 1) * 64, 0:n], pbc[0:64, 0:n], AF.Copy),
                             r=[pbc], w=[bcs])
                        S.op("dve", lambda e, hh=hh: e.tensor_tensor(qsrc_fn(hh), pg[hh][hh * 64:(hh + 1) * 64, 0:n],
                                                                     bcs[hh * 64:(hh + 1) * 64, 0:n], op=ALU.mult),
                             r=[pg[hh], bcs], w=[qbuf])

                for c in range(8):
                    for pr in range(4):
                        attend(lambda hh, c=c, pr=pr: qT[hh * 64:(hh + 1) * 64, pr, c * 512:(c + 1) * 512],
                               M["qT_b"][pr][c], 512, list(range(NT)))
                if not last:
                    for pr in range(4):
                        attend(lambda hh, pr=pr: qTc[hh * 64:(hh + 1) * 64, pr, :], qTc.b, S_CTX, [NTL, NTL + 1])
                S.barrier()

        def phase_c(l, M, tiles, src_aps, r_, modbc):
            with ExitStack() as ph:
                wcol = sbt(ph, "wcol", [P, 4, 31], F32)
                DG = sbt(ph, "DG", [P, 4, 31, P], BF16)
                wo = sbt(ph, "wo", [P, 8, D], BF16)
                stg = [sbt(ph, f"stgC{k}", [P, D], F32) for k in range(2)]
                stg_slot = [DmaSlot(S, f"stgC{k}") for k in range(2)]
                betaA = sbt(ph, "betaA", [P, 8], F32)
                betac = sbt(ph, "betac", [P, 8], F32)
                cb_bc = sbt(ph, "cb_bc", [P, 512], F32)
                lg_bc = sbt(ph, "lg_bc", [P, 512], F32)
                lb_bc = sbt(ph, "lb_bc", [P, 512], F32)
                xt = [sbt(ph, f"xtC{k}", [P, D], F32) for k in range(2)]
                xt_slot = [DmaSlot(S, f"xtC{k}") for k in range(2)]
                y = sbt(ph, "yC", [P, 512], F32)
                junk = sbt(ph, "junkC", [P, 512], F32)
                st4 = sbt(ph, "st4", [P, 4], F32)
                rln = sbt(ph, "rln", [P, 1], F32)
                msq = sbt(ph, "msq", [P, 1], F32)
                z = sbt(ph, "zC", [P, 512], F32)
                oc = sbt(ph, "oc", [P, 512], F32)
                ocb = sbt(ph, "ocb", [P, 512], BF16)
                ocT = sbt(ph, "ocT", [P, 4, P], BF16)
                sqo = sbt(ph, "sqo", [P, 4, P], F32)
                ssc = sbt(ph, "ssc", [P, 2], F32)
                rsc = sbt(ph, "rsc", [P, 2], F32)
                t1 = sbt(ph, "t1", [P, D], F32)
                x1 = [sbt(ph, f"x1_{k}", [P, D], F32) for k in range(2)]
                x1_slot = [DmaSlot(S, f"x1s{k}") for k in range(2)]
                py = pst(ph, "py", [P, 512], F32)
                ptc = pst(ph, "ptc", [P, 4, P], BF16)
                pss = pst(ph, "pss", [P, 16], F32)
                pa = [pst(ph, f"pa{k}", [P, 512], F32) for k in range(2)]
                pc = [pst(ph, f"pc{k}", [P, 512], F32) for k in range(2)]
                qT, qTc, gT, gTc = M["qT"], M["qTc"], M["gT"], M["gTc"]

                vs = DmaSlot(S, "vecC", group=True)
                for c in range(4):
                    S.dma("sp", lambda e, c=c: e.dma_start(out=wcol[:, c, :], in_=conv_w_d[l, :, c * P:(c + 1) * P].rearrange("j p -> p j"),
                                                           allow_slow_non_contiguous=True), vs, w=[wcol])
                for half in range(2):
                    S.dma("sp", lambda e, half=half: e.dma_start(out=betaA[half * 64:(half + 1) * 64, :],
                                                                 in_=beta_attn_d[l].rearrange("(h d) -> d h", d=64),
                                                                 allow_slow_non_contiguous=True), vs, w=[betaA])
                S.dma("sp", lambda e: e.dma_start(out=betac[:, 4:8], in_=beta_conv_d[l].rearrange("(c p) -> p c", p=P),
                                                  allow_slow_non_contiguous=True), vs, w=[betac])
                S.dma("sp", lambda e: e.dma_start(out=cb_bc[:], in_=conv_b_d[l].partition_broadcast(P)), vs, w=[cb_bc])
                S.dma("sp", lambda e: e.dma_start(out=lg_bc[:], in_=ln_g_d[l].partition_broadcast(P)), vs, w=[lg_bc])
                S.dma("sp", lambda e: e.dma_start(out=lb_bc[:], in_=ln_b_d[l].partition_broadcast(P)), vs, w=[lb_bc])
                vs.close()
                S.op("dve", lambda e: e.tensor_copy(betac[0:64, 0:4], betaA[0:64, 0:4]), r=[betaA], w=[betac])
                S.op("dve", lambda e: e.tensor_copy(betac[64:128, 0:4], betaA[64:128, 4:8]), r=[betaA], w=[betac])
                for kk in range(8):
                    t = stg[kk % 2]
                    sl = stg_slot[kk % 2]
                    if kk < 4:
                        S.dma("sp", lambda e, t=t, kk=kk: e.dma_start(out=t[0:64, :], in_=w_o_d[l, kk * 64:(kk + 1) * 64, :]), sl, w=[t])
                        S.dma("sp", lambda e, t=t, kk=kk: e.dma_start(out=t[64:128, :], in_=w_o_d[l, (kk + 4) * 64:(kk + 5) * 64, :]), sl, w=[t])
                    else:
                        S.dma("sp", lambda e, t=t, kk=kk: e.dma_start(out=t[:], in_=w_o_d[l, 512 + (kk - 4) * P:512 + (kk - 3) * P, :]), sl, w=[t])
                    S.op("act", lambda e, t=t, kk=kk: e.activation(wo[:, kk, :], t[:], AF.Identity, scale=betac[:, kk:kk + 1]),
                         r=[t, betac], w=[wo])
                for c in range(4):
                    for j in range(31):
                        S.op("pool", lambda e, c=c, j=j: e.tensor_scalar(DG[:, c, j, :], ident_f[:], wcol[:, c, j:j + 1], None, op0=ALU.mult),
                             r=[ident_f, wcol], w=[DG])

                def load(n_):
                    i = tiles[n_]
                    k = n_ % 2
                    S.dma("sp", lambda e: e.dma_start(out=xt[k][:], in_=src_aps[i]), xt_slot[k], r=[xs_buf[i]], w=[xt[k]])

                load(0)
                for n_, i in enumerate(tiles):
                    k = n_ % 2
                    is_ctx = i >= NTL
                    if n_ + 1 < len(tiles):
                        load(n_ + 1)
                    if not is_ctx:
                        gsrc, gb, t0 = gT, M["gT_b"], i * P
                        lo, hi = max(0, i - 1), min(NTL - 1, i + 1)
                        osrc = lambda pr: qT[:, pr, i * P:(i + 1) * P]
                        obufs = [M["qT_b"][pr][i // 4] for pr in range(4)]
                    else:
                        gsrc, gb, t0 = gTc, M["gTc_b"], (i - NTL) * P
                        lo, hi = max(0, i - NTL - 1), min(NTC - 1, i - NTL + 1)
                        osrc = lambda pr: qTc[:, pr, (i - NTL) * P:(i - NTL + 1) * P]
                        obufs = [qTc.b]
                    for c in range(4):
                        for j in range(31):
                            S.op("pe", lambda e, c=c, j=j: e.matmul(py[:, c * P:(c + 1) * P], lhsT=gsrc[:, c, t0 + j + GPAD - 15:t0 + j + GPAD - 15 + P],
                                                                  rhs=DG[:, c, j, :], start=(j == 0), stop=(j == 30)),
                                 r=[gb[q] for q in range(lo, hi + 1)] + [DG], w=[py])
                    S.op("dve", lambda e: e.tensor_tensor(y[:], py[:], cb_bc[:], op=ALU.add), r=[py, cb_bc], w=[y])
                    S.op("act", lambda e: e.activation(junk[:], y[:], AF.Identity, accum_out=st4[:, 0:1]), r=[y], w=[junk, st4])
                    S.op("act", lambda e: e.activation(junk[:], y[:], AF.Square, accum_out=st4[:, 1:2]), r=[y], w=[junk, st4])
                    S.op("dve", lambda e: e.tensor_scalar(st4[:, 2:4], st4[:, 0:2], 1.0 / 512, None, op0=ALU.mult), r=[st4], w=[st4])
                    S.op("dve", lambda e: e.tensor_tensor(msq[:], st4[:, 2:3], st4[:, 2:3], op=ALU.mult), r=[st4], w=[msq])
                    S.op("dve", lambda e: e.tensor_tensor(msq[:], st4[:, 3:4], msq[:], op=ALU.subtract), r=[st4, msq], w=[msq])
                    rstd_from_ss(msq, rln, 1.0, eps5)
                    S.op("dve", lambda e: e.tensor_scalar(z[:], y[:], st4[:, 2:3], rln[:, 0:1], op0=ALU.subtract, op1=ALU.mult),
                         r=[y, st4, rln], w=[z])
                    S.op("pool", lambda e: e.tensor_tensor(z[:], z[:], lg_bc[:], op=ALU.mult), r=[z, lg_bc], w=[z])
                    S.op("pool", lambda e: e.tensor_tensor(z[:], z[:], lb_bc[:], op=ALU.add), r=[z, lb_bc], w=[z])
                    S.op("act", lambda e: e.activation(oc[:], z[:], AF.Silu), r=[z], w=[oc])
                    S.op("act", lambda e: e.activation(junk[:], oc[:], AF.Square, accum_out=ssc[:, 0:1]), r=[oc], w=[junk, ssc])
                    S.op("pool", lambda e: e.tensor_copy(ocb[:], oc[:]), r=[oc], w=[ocb])
                    for c in range(4):
                        S.op("pe", lambda e, c=c: e.transpose(ptc[:, c, :], ocb[:, c * P:(c + 1) * P], ident_b[:]),
                             r=[ocb, ident_b], w=[ptc])
                    S.op("act", lambda e: e.activation(ocT[:], ptc[:], AF.Copy), r=[ptc], w=[ocT])
                    for pr in range(4):
                        S.op("pool", lambda e, pr=pr: e.tensor_tensor(sqo[:, pr, :], osrc(pr), osrc(pr), op=ALU.mult), r=obufs, w=[sqo])
                    for pr in range(4):
                        S.op("pe", lambda e, pr=pr: e.matmul(pss[:, 0:2], lhsT=sqo[:, pr, :], rhs=ones_f[:, 0:2],
                                                            start=(pr == 0), stop=(pr == 3)), r=[sqo, ones_f], w=[pss])
                    S.op("dve", lambda e: e.tensor_copy(ssc[:, 1:2], pss[:, 0:1]), r=[pss], w=[ssc])
                    rstd_from_ss(ssc, rsc, 1.0 / 512, eps6)
                    for cc in range(2):
                        for pr in range(4):
                            S.op("pe", lambda e, cc=cc, pr=pr: e.matmul(pa[cc][:], lhsT=osrc(pr), rhs=wo[:, pr, cc * 512:(cc + 1) * 512],
                                                                      start=(pr == 0), stop=(pr == 3)), r=obufs + [wo], w=[pa[cc]])
                        for c in range(4):
                            S.op("pe", lambda e, cc=cc, c=c: e.matmul(pc[cc][:], lhsT=ocT[:, c, :], rhs=wo[:, 4 + c, cc * 512:(cc + 1) * 512],
                                                                    start=(c == 0), stop=(c == 3)), r=[ocT, wo], w=[pc[cc]])
                        sl = slice(cc * 512, (cc + 1) * 512)
                        S.op("dve", lambda e, cc=cc, sl=sl: e.tensor_scalar(t1[:, sl], pa[cc][:], rsc[:, 1:2], None, op0=ALU.mult),
                             r=[pa[cc], rsc], w=[t1])
                        S.op("dve", lambda e, cc=cc, sl=sl: e.scalar_tensor_tensor(out=t1[:, sl], in0=pc[cc][:], scalar=rsc[:, 0:1],
                                                                                   in1=t1[:, sl], op0=ALU.mult, op1=ALU.add),
                             r=[pc[cc], rsc, t1], w=[t1])
                    S.op("pool", lambda e: e.tensor_tensor(t1[:], t1[:], modbc[:, 0:1024], op=ALU.mult), r=[t1, modbc], w=[t1])
                    S.op("pool", lambda e: e.tensor_tensor(x1[k][:], t1[:], xt[k][:], op=ALU.add), r=[t1, xt[k]], w=[x1[k]])
                    S.dma("sp", lambda e: e.dma_start(out=xs_d[i * P:(i + 1) * P, :], in_=x1[k][:]), x1_slot[k], r=[x1[k]], w=[xs_buf[i]])
                    if l == 0:
                        dbg_store("d_x1", slice(i * P, (i + 1) * P), x1[k][:], [x1[k]])
                S.barrier()

        def phase_p(l, tiles, modbc, last):
            with ExitStack() as ph:
                wq = sbt(ph, "wq", [P, 8, D], F32)
                keysTz = sbt(ph, "keysTz", [P, 8, 2, P], F32)
                iota16 = sbt(ph, "iota16", [P, 128, 16], F32)
                xt = [sbt(ph, f"xtP{k}", [P, D], F32) for k in range(2)]
                xt_slot = [DmaSlot(S, f"xtP{k}") for k in range(2)]
                junk = sbt(ph, "junkP", [P, D], BF16)
                junkr = sbt(ph, "junkR", [P, D], BF16)
                prod = [sbt(ph, f"prod{k}", [P, D], BF16) for k in range(3)]
                hb = [sbt(ph, f"hb{k}", [P, D], BF16) for k in range(2)]
                pi = [0]
                ss = sbt(ph, "ssP", [P, 1], F32)
                rstd = sbt(ph, "rstdP", [P, 1], F32)
                ssf = sbt(ph, "ssF", [P, 1], F32)
                rstdf = sbt(ph, "rstdF", [P, 1], F32)
                h = [sbt(ph, "hP", [P, D], F32)] * 2
                hT = sbt(ph, "hT", [P, 8, P], F32)
                qpT = hT
                qp = sbt(ph, "qp", [P, D], F32)
                sc = sbt(ph, "scP", [P, 16, 128], F32)
                wk = sbt(ph, "wkP", [P, 16, 128], F32)
                eq_ap = wk[:].rearrange("p a b -> p (a b)").rearrange("p (x c) -> p x c", c=16)
                topv = sbt(ph, "topv", [P, 16, 16], F32)
                idx = sbt(ph, "idxP", [P, 16, 16], U32)
                idxf = sbt(ph, "idxf", [P, 16, 16], F32)
                cand = sbt(ph, "cand", [P, 8, 256], F32)
                wk2 = sbt(ph, "wk2", [P, 8, 256], F32)
                best = sbt(ph, "best", [P, 8, 16], F32)
                pos = sbt(ph, "pos", [P, 8, 16], U32)
                pab = sbt(ph, "pab", [P, 2, 128], U32)
                abf = sbt(ph, "abf", [P, 2, 128], F32)
                isel = sbt(ph, "isel", [P, 2, 128], F32)
                ef = [sbt(ph, f"ef{k}", [P, 128], F32) for k in range(2)]
                eidx = [sbt(ph, f"eidx{k}", [P, 128], I32) for k in range(2)]
                ew = sbt(ph, "ew", [P, 128], F32)
                se = sbt(ph, "se", [P, 8], F32)
                wgt = [sbt(ph, f"wgt{k}", [P, 128], F32) for k in range(2)]
                pre = sbt(ph, "pre", [P, 128], F32)
                aw = sbt(ph, "aw", [P, 128], F32)
                gbuf = [sbt(ph, f"gbuf{k}", [P, 2 * D], BF16) for k in range(NSLOT)]
                diag = [sbt(ph, f"diag{k}", [P, P], BF16) for k in range(4)]
                pre_b = [Buf(f"pre_b{k}") for k in range(4)]
                pre_d = [Buf(f"pre_d{k}") for k in range(4)]
                di = [0]
                gslot = [DmaSlot(S, f"g{k}") for k in range(NSLOT)]
                x2 = [sbt(ph, f"x2_{k}", [P, D], F32) for k in range(2)]
                x2_slot = [DmaSlot(S, f"x2s{k}") for k in range(2)]
                fg_bc = sbt(ph, "fg_bc", [P, D], F32)
                pT2 = pst(ph, "pT2", [P, 8, P], F32)
                pqp = [pst(ph, f"pqp{k}", [P, 512], F32) for k in range(2)]
                pcs = [pst(ph, f"pcs{k}", [P, 512], F32) for k in range(2)]
                pva = [pst(ph, f"pva{k}", [P, 512], F32) for k in range(2)]
                psc = pqp + pcs
                print(f"[build] phase_p sbuf remaining {nc.sbuf_bytes_remaining}")

                S.op("pool", lambda e: e.iota(iota16[:], pattern=[[0, 128], [1, 16]], base=0, channel_multiplier=0,
                                              allow_small_or_imprecise_dtypes=True), w=[iota16])
                ws = DmaSlot(S, "wqload", group=True)
                for j in range(8):
                    S.dma("sp", lambda e, j=j: e.dma_start(out=wq[:, j, :], in_=peer_wq_d[l, j * P:(j + 1) * P, :]), ws, w=[wq])
                kraw_ap = sc[:].rearrange("p a b -> p (a b)")[:, 0:1024].rearrange("p (a d) -> p a d", d=64)
                keysT_ap = cand[:].rearrange("p a b -> p (a b)")[:, 0:1024].rearrange("p (a k) -> p a k", k=P)
                S.dma("sp", lambda e: e.dma_start(out=kraw_ap, in_=peer_keys_d[l].rearrange("h c k d -> k (h c) d")), ws, w=[sc])
                if last:
                    S.dma("sp", lambda e: e.dma_start(out=fg_bc[:], in_=final_g_d[0].partition_broadcast(P)), ws, w=[fg_bc])
                ws.close()
                for hh in range(8):
                    S.op("pe", lambda e, hh=hh: e.transpose(pT2[:, hh, :], kraw_ap[:, 2 * hh:2 * hh + 2, :].rearrange("p c d -> p (c d)"), ident_f[:]),
                         r=[sc, ident_f], w=[pT2])
                S.op("act", lambda e: e.activation(keysT_ap, pT2[:], AF.Copy), r=[pT2], w=[cand])
                S.op("pool", lambda e: e.memset(keysTz[:], 0.0), w=[keysTz])
                S.op("dve", lambda e: e.tensor_copy(keysTz[0:64, :, 0, :], keysT_ap[0:64, :, :]), r=[cand], w=[keysTz])
                S.op("dve", lambda e: e.tensor_copy(keysTz[64:128, :, 1, :], keysT_ap[64:128, :, :]), r=[cand], w=[keysTz])

                gi = [0]

                def routing(n_):
                    rq = []
                    i = tiles[n_]
                    k = n_ % 2
                    xk, hk, efk, eik, wgk = xt[k], h[k], ef[k], eidx[k], wgt[k]
                    hbk = hb[k]

                    def op(eng, fn, r=(), w=()):
                        rq.append(("op", eng, fn, r, w))

                    rq.append(("dma", "sp", lambda e: e.dma_start(out=xk[:], in_=xs_d[i * P:(i + 1) * P, :]), xt_slot[k], [xs_buf[i]], [xk]))
                    op("act", lambda e: e.activation(junkr[:], xk[:], AF.Square, accum_out=ss[:]), r=[xk], w=[junkr, ss])
                    op("act", lambda e: e.activation(rstd[:], ss[:], AF.Sqrt, scale=1.0 / D, bias=eps6[:, 0:1]), r=[ss, eps6], w=[rstd])
                    op("dve", lambda e: e.reciprocal(rstd[:], rstd[:]), r=[rstd], w=[rstd])
                    op("dve", lambda e: e.scalar_tensor_tensor(out=hk[:], in0=xk[:], scalar=rstd[:, 0:1], in1=modbc[:, 2048:3072],
                                                               op0=ALU.mult, op1=ALU.mult), r=[xk, rstd, modbc], w=[hk])
                    op("dve", lambda e: e.tensor_tensor(hk[:], hk[:], modbc[:, 1024:2048], op=ALU.add), r=[hk, modbc], w=[hk])
                    op("act", lambda e: e.activation(hbk[:], hk[:], AF.Copy), r=[hk], w=[hbk])
                    for j in range(8):
                        op("pe", lambda e, j=j: e.transpose(pT2[:, j, :], hk[:, j * P:(j + 1) * P], ident_f[:]), r=[hk, ident_f], w=[pT2])
                    op("act", lambda e: e.activation(hT[:], pT2[:], AF.Copy), r=[pT2], w=[hT])
                    for cc in range(2):
                        for j in range(8):
                            op("pe", lambda e, cc=cc, j=j: e.matmul(pqp[cc][:], lhsT=hT[:, j, :], rhs=wq[:, j, cc * 512:(cc + 1) * 512],
                                                                  start=(j == 0), stop=(j == 7)), r=[hT, wq], w=[pqp[cc]])
                        op("act", lambda e, cc=cc: e.activation(qp[:, cc * 512:(cc + 1) * 512], pqp[cc][:], AF.Copy), r=[pqp[cc]], w=[qp])
                    for hh in range(8):
                        op("pe", lambda e, hh=hh: e.transpose(pT2[:, hh, :], qp[:, hh * P:(hh + 1) * P], ident_f[:]), r=[qp, ident_f], w=[pT2])
                    op("act", lambda e: e.activation(qpT[:], pT2[:], AF.Copy), r=[pT2], w=[qpT])
                    for hh in range(8):
                        pv = psc[hh // 2][:].rearrange("p (a k) -> p a k", k=P)
                        op("pe", lambda e, hh=hh, pv=pv: e.matmul(pv[:, (hh % 2) * 2:(hh % 2) * 2 + 2, :], lhsT=qpT[:, hh, :],
                                                                 rhs=keysTz[:, hh, :, :], start=True, stop=True),
                           r=[qpT, keysTz], w=[psc[hh // 2]])
                    for q in range(4):
                        op("act", lambda e, q=q: e.activation(sc[:, 4 * q:4 * q + 4, :], psc[q][:].rearrange("p (a k) -> p a k", k=P), AF.Copy),
                           r=[psc[q]], w=[sc])
                    for g in range(16):
                        op("dve", lambda e, g=g: e.max(out=topv[:, g, 0:8], in_=sc[:, g, :]), r=[sc], w=[topv])
                        op("dve", lambda e, g=g: e.max_index(out=idx[:, g, 0:8], in_max=topv[:, g, 0:8], in_values=sc[:, g, :]),
                           r=[sc, topv], w=[idx])
                        op("dve", lambda e, g=g: e.match_replace(out=wk[:, g, :], in_to_replace=topv[:, g, 0:8], in_values=sc[:, g, :],
                                                                 imm_value=-1e30), r=[sc, topv], w=[wk])
                        op("dve", lambda e, g=g: e.max(out=topv[:, g, 8:16], in_=wk[:, g, :]), r=[wk], w=[topv])
                        op("dve", lambda e, g=g: e.max_index(out=idx[:, g, 8:16], in_max=topv[:, g, 8:16], in_values=wk[:, g, :]),
                           r=[wk, topv], w=[idx])
                    op("dve", lambda e: e.tensor_copy(idxf[:], idx[:]), r=[idx], w=[idxf])
                    tv4 = topv[:].rearrange("p (h c) a -> p h c a", c=2)
                    if4 = idxf[:].rearrange("p (h c) a -> p h c a", c=2)
                    op("dve", lambda e: e.tensor_tensor(cand[:].rearrange("p h (a b) -> p h a b", a=16),
                                                        tv4[:, :, 0, :].unsqueeze(3).to_broadcast([P, 8, 16, 16]),
                                                        tv4[:, :, 1, :].unsqueeze(2).to_broadcast([P, 8, 16, 16]), op=ALU.add),
                       r=[topv], w=[cand])
                    for hh in range(8):
                        op("dve", lambda e, hh=hh: e.max(out=best[:, hh, 0:8], in_=cand[:, hh, :]), r=[cand], w=[best])
                        op("dve", lambda e, hh=hh: e.max_index(out=pos[:, hh, 0:8], in_max=best[:, hh, 0:8], in_values=cand[:, hh, :]),
                           r=[cand, best], w=[pos])
                        op("dve", lambda e, hh=hh: e.match_replace(out=wk2[:, hh, :], in_to_replace=best[:, hh, 0:8],
                                                                   in_values=cand[:, hh, :], imm_value=-1e30), r=[cand, best], w=[wk2])
                        op("dve", lambda e, hh=hh: e.max(out=best[:, hh, 8:16], in_=wk2[:, hh, :]), r=[wk2], w=[best])
                        op("dve", lambda e, hh=hh: e.max_index(out=pos[:, hh, 8:16], in_max=best[:, hh, 8:16], in_values=wk2[:, hh, :]),
                           r=[wk2, best], w=[pos])
                    posf = pos[:].rearrange("p h s -> p (h s)")
                    op("dve", lambda e: e.tensor_single_scalar(pab[:, 0, :], posf, 4, op=ALU.logical_shift_right), r=[pos], w=[pab])
                    op("dve", lambda e: e.tensor_single_scalar(pab[:, 1, :], posf, 15, op=ALU.bitwise_and), r=[pos], w=[pab])
                    op("dve", lambda e: e.tensor_copy(abf[:], pab[:]), r=[pab], w=[abf])
                    for c in range(2):
                        op("dve", lambda e, c=c: e.tensor_tensor(eq_ap, iota16[:], abf[:, c, :].unsqueeze(2).to_broadcast([P, 128, 16]),
                                                                 op=ALU.is_equal), r=[iota16, abf], w=[wk])
                        op("dve", lambda e, c=c: e.tensor_tensor(eq_ap.rearrange("p (h s) a -> p h s a", h=8),
                                                                 eq_ap.rearrange("p (h s) a -> p h s a", h=8),
                                                                 if4[:, :, c, :].unsqueeze(2).to_broadcast([P, 8, 16, 16]), op=ALU.mult),
                           r=[wk, idxf], w=[wk])
                        op("dve", lambda e, c=c: e.tensor_reduce(out=isel[:, c, :], in_=eq_ap, axis=AX.X, op=ALU.add), r=[wk], w=[isel])
                    op("dve", lambda e: e.scalar_tensor_tensor(out=efk[:], in0=isel[:, 0, :], scalar=128.0, in1=isel[:, 1, :],
                                                               op0=ALU.mult, op1=ALU.add), r=[isel], w=[efk])
                    op("dve", lambda e: e.tensor_scalar(efk[:], efk[:], float(l * NPEER), None, op0=ALU.add), r=[efk], w=[efk])
                    op("dve", lambda e: e.tensor_copy(eik[:], efk[:]), r=[efk], w=[eik])
                    op("dve", lambda e: e.tensor_tensor(ew[:].rearrange("p (h s) -> p h s", h=8), best[:],
                                                        best[:, :, 0:1].to_broadcast([P, 8, 16]), op=ALU.subtract), r=[best], w=[ew])
                    op("act", lambda e: e.activation(ew[:], ew[:], AF.Exp), r=[ew], w=[ew])
                    op("dve", lambda e: e.tensor_reduce(out=se[:], in_=ew[:].rearrange("p (h s) -> p h s", h=8), axis=AX.X, op=ALU.add),
                       r=[ew], w=[se])
                    op("dve", lambda e: e.reciprocal(se[:], se[:]), r=[se], w=[se])
                    op("dve", lambda e: e.tensor_tensor(wgk[:].rearrange("p (h s) -> p h s", h=8), ew[:].rearrange("p (h s) -> p h s", h=8),
                                                        se[:].unsqueeze(2).to_broadcast([P, 8, 16]), op=ALU.mult), r=[ew, se], w=[wgk])
                    return rq

                def emit(rq, n):
                    for _ in range(n):
                        if not rq:
                            return
                        t = rq.pop(0)
                        if t[0] == "op":
                            S.op(t[1], t[2], t[3], t[4])
                        else:
                            S.dma(t[1], t[2], t[3], t[4], t[5])

                rq = routing(0)
                emit(rq, len(rq))
                for n_, i in enumerate(tiles):
                    k = n_ % 2
                    hbk, efk, eik, wgk = hb[k], ef[k], eidx[k], wgt[k]
                    rq = routing(n_ + 1) if n_ + 1 < len(tiles) else []
                    per_slot = -(-len(rq) // 112)
                    for g_ in range(128 // GRP):
                        pb_ = pre_b[g_ % 4]
                        pd_ = pre_d[g_ % 4]
                        for s_ in range(g_ * GRP, (g_ + 1) * GRP):
                            b_ = gi[0] % NSLOT
                            gi[0] += 1
                            S.dma("pool", lambda e, s_=s_, b_=b_: e.indirect_dma_start(
                                out=gbuf[b_][:], out_offset=None, in_=uv_d,
                                in_offset=bass.IndirectOffsetOnAxis(ap=eik[:, s_:s_ + 1], axis=0)), gslot[b_], r=[eik], w=[gbuf[b_]])
                            if s_ % 4 == 3:
                                S.op("dve", lambda e, s_=s_, b_=b_: e.scalar_tensor_tensor(
                                    out=junk[:], in0=hbk[:], scalar=1.0, in1=gbuf[b_][:, 0:D], op0=ALU.mult, op1=ALU.mult,
                                    accum_out=pre[:, s_:s_ + 1]), r=[hbk, gbuf[b_]], w=[junk, pd_])
                            else:
                                pr_ = prod[pi[0] % len(prod)]
                                pi[0] += 1
                                S.op("dve", lambda e, b_=b_, pr_=pr_: e.tensor_tensor(pr_[:], hbk[:], gbuf[b_][:, 0:D], op=ALU.mult),
                                     r=[hbk, gbuf[b_]], w=[pr_])
                                S.op("act", lambda e, s_=s_, pr_=pr_: e.activation(junkr[:], pr_[:], AF.Identity, accum_out=pre[:, s_:s_ + 1]),
                                     r=[pr_], w=[junkr, pb_])
                            emit(rq, per_slot)
                        gs = slice(g_ * GRP, (g_ + 1) * GRP)
                        S.op("act", lambda e, gs=gs: e.activation(aw[:, gs], pre[:, gs], AF.Gelu), r=[pb_, pd_], w=[pb_])
                        S.op("dve", lambda e, gs=gs: e.tensor_tensor(aw[:, gs], aw[:, gs], wgk[:, gs], op=ALU.mult), r=[pb_, wgk], w=[pb_])
                        for s_ in range(g_ * GRP, (g_ + 1) * GRP):
                            b_ = (gi[0] - (g_ + 1) * GRP + s_) % NSLOT
                            dg = diag[di[0] % len(diag)]
                            di[0] += 1
                            S.op("act", lambda e, s_=s_, dg=dg: e.activation(dg[:], ident_b[:], AF.Identity, scale=aw[:, s_:s_ + 1]),
                                 r=[ident_b, pb_], w=[dg])
                            for cc in range(2):
                                S.op("pe", lambda e, s_=s_, b_=b_, dg=dg, cc=cc: e.matmul(
                                    pva[cc][:], lhsT=dg[:], rhs=gbuf[b_][:, D + cc * 512:D + (cc + 1) * 512],
                                    start=(s_ == 0), stop=(s_ == 127)), r=[dg, gbuf[b_]], w=[pva[cc]])
                    if l == 0:
                        dbg_store("d_pre", slice(i * P, (i + 1) * P), pre[:], pre_b + pre_d)
                        dbg_store("d_eidx", slice(i * P, (i + 1) * P), efk[:], [efk])
                        dbg_store("d_w", slice(i * P, (i + 1) * P), wgk[:], [wgk])
                    for cc in range(2):
                        sl = slice(cc * 512, (cc + 1) * 512)
                        S.op("dve", lambda e, cc=cc, sl=sl: e.tensor_tensor(x2[k][:, sl], pva[cc][:], modbc[:, 3072 + cc * 512:3072 + (cc + 1) * 512],
                                                                           op=ALU.mult), r=[pva[cc], modbc], w=[x2[k]])
                    S.op("dve", lambda e: e.tensor_tensor(x2[k][:], x2[k][:], xt[k][:], op=ALU.add), r=[x2[k], xt[k]], w=[x2[k]])
                    if l == 0:
                        dbg_store("d_x2", slice(i * P, (i + 1) * P), x2[k][:], [x2[k]])
                    if last:
                        S.op("act", lambda e: e.activation(junk[:], x2[k][:], AF.Square, accum_out=ssf[:]), r=[x2[k]], w=[junk, ssf])
                        rstd_from_ss(ssf, rstdf, 1.0 / D, eps6)
                        S.op("dve", lambda e: e.scalar_tensor_tensor(out=x2[k][:], in0=x2[k][:], scalar=rstdf[:, 0:1], in1=fg_bc[:],
                                                                     op0=ALU.mult, op1=ALU.mult), r=[x2[k], rstdf, fg_bc], w=[x2[k]])
                        S.dma("sp", lambda e: e.dma_start(out=out_d[i * P:(i + 1) * P, :], in_=x2[k][:]), x2_slot[k], r=[x2[k]])
                    else:
                        S.dma("sp", lambda e: e.dma_start(out=xs_d[i * P:(i + 1) * P, :], in_=x2[k][:]), x2_slot[k], r=[x2[k]], w=[xs_buf[i]])
                    emit(rq, len(rq))
                S.barrier()

        lat_tiles = list(range(NTL))
        ctx_tiles = list(range(NTL, NT))
        if stop in (None, "p1"):
            convert_tables()
        for l in range(depth):
            last = (l == DEPTH - 1)
            if l == 0:
                src_aps = [x_d[i * P:(i + 1) * P, :] for i in range(NTL)] + [ctx_d[i * P:(i + 1) * P, :] for i in range(NTC)]
            else:
                src_aps = [xs_d[i * P:(i + 1) * P, :] for i in range(NT)]
            mod_cols(l)
            if l == 0:
                dbg_store("d_mod", (slice(0, P), slice(0, 32)), modcol[:].rearrange("p r c -> p (r c)"), [modcol])
                dbg_store("d_mod", (slice(0, P), slice(32, 48)), scT[:].rearrange("p j r -> p (j r)"), [scT])
            if stop == "m":
                break
            with ExitStack() as mix:
                M = {}
                M["qT"] = sbt(mix, "qT", [P, 4, S_LAT], BF16)
                M["qT_b"] = [[Buf(f"qT{pr}_{c}") for c in range(8)] for pr in range(4)]
                M["qTc"] = sbt(mix, "qTc", [P, 4, S_CTX], BF16)
                M["gT"] = sbt(mix, "gT", [P, 4, S_LAT + 2 * GPAD], BF16)
                M["gT_b"] = [Buf(f"gT{i}") for i in range(NTL)]
                M["gTc"] = sbt(mix, "gTc", [P, 4, S_CTX + 2 * GPAD], BF16)
                M["gTc_b"] = [Buf(f"gTc{i}") for i in range(NTC)]
                S.op("pool", lambda e: e.memset(M["gT"][:], 0.0), w=M["gT_b"])
                S.op("pool", lambda e: e.memset(M["gTc"][:], 0.0), w=M["gTc_b"])
                with ExitStack() as ab:
                    M["kT"] = sbt(ab, "kT", [P, NT * P], BF16)
                    M["kT_b"] = [Buf(f"kT{i}") for i in range(NT)]
                    M["Vp"] = sbt(ab, "Vp", [P, NT, 192], BF16)
                    M["Vp_b"] = [Buf(f"Vp{i}") for i in range(NT)]
                    S.op("pool", lambda e: e.memset(M["Vp"][:], 1.0), w=M["Vp_b"])
                    phase_a(l, M, src_aps, last)
                    if stop in ("a", "a0", "a1"):
                        break
                    phase_b(l, M, last)
                if stop == "b":
                    break
                with ExitStack() as cs:
                    modbc = sbt(cs, "modbcC", [P, 4096], F32)
                    mod_bc(l, 0, modbc)
                    phase_c(l, M, lat_tiles, src_aps, 0, modbc)
                    if not last:
                        mod_bc(l, 1, modbc)
                        phase_c(l, M, ctx_tiles, src_aps, 1, modbc)
            if stop == "c":
                break
            with ExitStack() as pp:
                modbc = sbt(pp, "modbcP", [P, 4096], F32)
                mod_bc(l, 0, modbc)
                phase_p(l, lat_tiles if stop not in ("p1", "p0") else lat_tiles[:2], modbc, last)
                if not last and stop not in ("p1", "p0"):
                    mod_bc(l, 1, modbc)
                    phase_p(l, ctx_tiles, modbc, last)
            if stop in ("p1", "p0"):
                break
        out_slot.close()
        if out_slot.sem is not None:
            for en in ("sp", "act", "pool"):
                S.E[en].eng.wait_ge(out_slot.sem, out_slot.val)
        S.barrier()
        print(f"[build] instr={S.ninstr} waits={S.nwait} sems={S.nsem}")
    return nc


_NC_CACHE = {}


def _rope_tables():
    n = 16
    inv = (10000.0 ** (-np.arange(n, dtype=np.float32) / n)).astype(np.float32)
    row = np.repeat(np.arange(64), 64).astype(np.float32)
    col = np.tile(np.arange(64), 64).astype(np.float32)
    ang = np.concatenate([row[:, None] * inv, col[:, None] * inv], axis=-1).astype(np.float32)
    cos = np.cos(ang).astype(np.float32)
    sin = np.sin(ang).astype(np.float32)
    cos2 = np.concatenate([cos, cos], axis=-1)
    sin2 = np.concatenate([-sin, sin], axis=-1)
    return np.ascontiguousarray(cos2), np.ascontiguousarray(sin2)


def make_in_maps(inputs, cores):
    f = lambda a: np.ascontiguousarray(np.asarray(a, dtype=np.float32))
    cos2, sin2 = _rope_tables()
    shared = {k: f(inputs[k]) for k in ["w_mod", "b_mod", "w_in", "q_gain", "k_gain", "conv_w", "conv_b", "ln_g", "ln_b",
                                        "beta_attn", "beta_conv", "w_o", "peer_wq", "peer_keys", "peer_u", "peer_v"]}
    shared["final_g"] = f(inputs["final_g"]).reshape(1, D)
    shared["cos2"] = cos2
    shared["sin2"] = sin2
    shared["cctxT"] = np.ascontiguousarray(f(inputs["c_ctx"]).reshape(8, P).T)
    x = f(inputs["x"])
    ctx = f(inputs["ctx"])
    c = f(inputs["c"])
    maps = []
    for b in cores:
        m = dict(shared)
        m["x"] = x[b]
        m["ctx"] = ctx[b]
        m["cT"] = np.ascontiguousarray(c[b].reshape(8, P).T)
        maps.append(m)
    return maps


def kernel(**inputs):
    if "nc" not in _NC_CACHE:
        _NC_CACHE["nc"] = build()
    nc = _NC_CACHE["nc"]
    B = np.asarray(inputs["x"]).shape[0]
    maps = make_in_maps(inputs, list(range(B)))
    res = run_bass_kernel_spmd(nc, maps, core_ids=list(range(B)))
    out = np.stack([np.asarray(r["out"], dtype=np.float32) for r in res.results], axis=0)
    return out
```

```python
import numpy as np
from contextlib import ExitStack
import concourse.bass as bass
import concourse.mybir as mybir
from concourse.bass_utils import run_bass_kernel_spmd

F32 = mybir.dt.float32
BF16 = mybir.dt.bfloat16
I32 = mybir.dt.int32
U32 = mybir.dt.uint32
ALU = mybir.AluOpType
AF = mybir.ActivationFunctionType
AX = mybir.AxisListType

import os
CUT = int(os.environ.get("KCUT", "99"))
SEM_LIMIT = 30000


class Buf:
    __slots__ = ("name", "w", "r")

    def __init__(self, name):
        self.name = name
        self.w = None
        self.r = {}


class Eng:
    def __init__(self, sync, name, eng):
        self.sync = sync
        self.name = name
        self.eng = eng
        self.sem = None
        self.count = 0
        self.known = {}
        self.last = None

    def new_event(self):
        if self.sem is None or self.count >= SEM_LIMIT:
            self.sem = self.sync.new_sem(self.name)
            self.count = 0
        self.count += 1
        self.last = [self.sem, self.count, self.name]
        return self.last


def DmaSlot(sync, name, group=False):
    if name not in sync.slot_by_name:
        sync.slot_by_name[name] = _DmaSlot(sync, name, group)
    return sync.slot_by_name[name]


class _DmaSlot:
    def __init__(self, sync, name, group=False):
        self.name = name
        self.sem = None
        self.val = 0
        self.group = group
        self.pending = []
        sync.slots.append(self)

    def bump(self, sync):
        if self.sem is None or (self.val >= SEM_LIMIT and not self.pending):
            self.sem = sync.new_sem("d" + self.name)
            self.val = 0
        self.val += 16
        ev = [self.sem, self.val, "dma"]
        if self.group:
            self.pending.append(ev)
        return ev

    def close(self):
        for ev in self.pending:
            ev[1] = self.val
        self.pending = []


class Sync:
    def __init__(self, nc, stack):
        self.nc = nc
        self.stack = stack
        self.nsem = 0
        self.slots = []
        self.slot_by_name = {}
        self.E = {
            "pe": Eng(self, "pe", nc.tensor),
            "dve": Eng(self, "dve", nc.vector),
            "act": Eng(self, "act", nc.scalar),
            "pool": Eng(self, "pool", nc.gpsimd),
            "sp": Eng(self, "sp", nc.sync),
        }
        self.ninstr = 0
        self.nwait = 0

    def new_sem(self, name):
        self.nsem += 1
        return self.stack.enter_context(self.nc.semaphore(f"s{self.nsem}_{name}"))

    def _wait(self, E, evs):
        best = {}
        for ev in evs:
            if ev is None:
                continue
            if E.name == "pe" and ev[2] == "pe":
                continue
            k = id(ev[0])
            if k not in best or best[k][1] < ev[1]:
                best[k] = ev
        for ev in best.values():
            sem, val, src = ev
            if E.known.get(id(sem), 0) >= val:
                continue
            E.eng.wait_ge(sem, val)
            self.nwait += 1
            E.known[id(sem)] = val

    def _deps(self, E, reads, writes):
        evs = []
        for b in reads:
            evs.append(b.w)
        for b in writes:
            if b.w is not None and b.w[2] != E.name:
                evs.append(b.w)
            for ev in b.r.values():
                if ev[2] == E.name:
                    continue
                evs.append(ev)
        return evs

    @staticmethod
    def _bufs(lst):
        return [t.b if hasattr(t, "b") else t for t in lst]

    def op(self, engname, fn, r=(), w=()):
        E = self.E[engname]
        r = self._bufs(r)
        w = self._bufs(w)
        self._wait(E, self._deps(E, r, w))
        ins = fn(E.eng)
        ev = E.new_event()
        ins.then_inc(ev[0], 1)
        self.ninstr += 1
        key = ev[2] if ev[2] != "dma" else id(ev[0])
        for b in r:
            b.r[key] = ev
        for b in w:
            b.w = ev
            b.r = {}
        return ev

    def dma(self, qname, fn, slot, r=(), w=()):
        E = self.E[qname]
        r = self._bufs(r)
        w = self._bufs(w)
        self._wait(E, self._deps(E, r, w))
        ins = fn(E.eng)
        ev = slot.bump(self)
        ins.then_inc(ev[0], 16)
        self.ninstr += 1
        key = ev[2] if ev[2] != "dma" else id(ev[0])
        for b in r:
            b.r[key] = ev
        for b in w:
            b.w = ev
            b.r = {}
        return ev

    def barrier(self):
        for s in self.slots:
            if s.group:
                s.close()
        evs = []
        for E in self.E.values():
            if E.last is not None:
                evs.append(E.last)
        for s in self.slots:
            if s.sem is not None:
                evs.append([s.sem, s.val, "dma"])
        for E in self.E.values():
            self._wait(E, [ev for ev in evs if not (E.name == "pe" and ev[2] == "pe")])


class Tile:
    def __init__(self, h, name):
        self.h = h
        self.b = Buf(name)

    def __getitem__(self, k):
        return self.h[k]


P = 128
D = 1024
S_LAT = 4096
S_CTX = 256
NTL = 32
NTC = 2
NT = NTL + NTC
DEPTH = 2
INC = 1792
EPS = 1e-6
NPEER = 16384
GPAD = 16
NSLOT = 17
GRP = 4


def build(depth=DEPTH, dbg=False, stop=None):
    nc = bass.Bass("TRN2", target_bir_lowering=False)

    def din(name, shape, dt=F32):
        return nc.dram_tensor(name, shape, dt, kind="ExternalInput").ap()

    x_d = din("x", [S_LAT, D])
    ctx_d = din("ctx", [S_CTX, D])
    cT_d = din("cT", [P, 8])
    cctxT_d = din("cctxT", [P, 8])
    w_mod_d = din("w_mod", [DEPTH, D, 6 * D])
    b_mod_d = din("b_mod", [DEPTH, 6 * D])
    w_in_d = din("w_in", [DEPTH, D, INC])
    q_gain_d = din("q_gain", [DEPTH, 64])
    k_gain_d = din("k_gain", [DEPTH, 64])
    conv_w_d = din("conv_w", [DEPTH, 31, 512])
    conv_b_d = din("conv_b", [DEPTH, 512])
    ln_g_d = din("ln_g", [DEPTH, 512])
    ln_b_d = din("ln_b", [DEPTH, 512])
    beta_attn_d = din("beta_attn", [DEPTH, 512])
    beta_conv_d = din("beta_conv", [DEPTH, 512])
    w_o_d = din("w_o", [DEPTH, D, D])
    peer_wq_d = din("peer_wq", [DEPTH, D, D])
    peer_keys_d = din("peer_keys", [DEPTH, 8, 2, 128, 64])
    peer_u_d = din("peer_u", [DEPTH, NPEER, D])
    peer_v_d = din("peer_v", [DEPTH, NPEER, D])
    final_g_d = din("final_g", [1, D])
    cos2_d = din("cos2", [S_LAT, 64])
    sin2_d = din("sin2", [S_LAT, 64])
    peer_u_flat = peer_u_d.rearrange("l n d -> (l n) d")
    peer_v_flat = peer_v_d.rearrange("l n d -> (l n) d")
    out_d = nc.dram_tensor("out", [S_LAT, D], F32, kind="ExternalOutput").ap()
    xs_d = nc.dram_tensor("xs", [S_LAT + S_CTX, D], F32).ap()
    uv_d = nc.dram_tensor("uv", [DEPTH * NPEER, 2 * D], BF16).ap()
    dbg_d = {}
    if dbg:
        for nm, shp in [("d_qkvu", [NT * P, INC]), ("d_x1", [NT * P, D]), ("d_oT", [P, 4 * S_LAT]),
                        ("d_pre", [NT * P, 128]), ("d_eidx", [NT * P, 128]), ("d_w", [NT * P, 128]),
                        ("d_qT", [P, 4 * S_LAT]), ("d_x2", [NT * P, D]), ("d_mod", [P, 4096])]:
            dbg_d[nm] = nc.dram_tensor(nm, shp, F32, kind="ExternalOutput").ap()

    top = ExitStack()
    with top:
        S = Sync(nc, top)

        uid = [0]

        def sbt(stack, name, shape, dt):
            uid[0] += 1
            name = f"{name}_u{uid[0]}"
            return Tile(stack.enter_context(nc.sbuf_tensor(name, shape, dt)), name)

        def pst(stack, name, shape, dt):
            uid[0] += 1
            name = f"{name}_u{uid[0]}"
            return Tile(stack.enter_context(nc.psum_tensor(name, shape, dt)), name)

        out_slot = DmaSlot(S, "out", group=True)
        dbg_slots = {}
        xs_buf = [Buf(f"xs{i}") for i in range(NT)]

        ident_f = sbt(top, "ident_f", [P, P], F32)
        ident_b = sbt(top, "ident_b", [P, P], BF16)
        ones_f = sbt(top, "ones_f", [P, P], F32)
        io_t = sbt(top, "io_t", [P, P], F32)
        pid_t = sbt(top, "pid_t", [P, 1], F32)
        S.op("pool", lambda e: e.iota(io_t[:], pattern=[[1, P]], base=0, channel_multiplier=0,
                                      allow_small_or_imprecise_dtypes=True), w=[io_t])
        S.op("pool", lambda e: e.iota(pid_t[:], pattern=[[0, 1]], base=0, channel_multiplier=1,
                                      allow_small_or_imprecise_dtypes=True), w=[pid_t])
        S.op("dve", lambda e: e.tensor_scalar(ident_f[:], io_t[:], pid_t[:, 0:1], None, op0=ALU.is_equal),
             r=[io_t, pid_t], w=[ident_f])
        S.op("dve", lambda e: e.tensor_copy(ident_b[:], ident_f[:]), r=[ident_f], w=[ident_b])
        S.op("pool", lambda e: e.memset(ones_f[:], 1.0), w=[ones_f])

        cslot = DmaSlot(S, "const", group=True)
        craw = sbt(top, "craw", [P, 2, 8], F32)
        scT = sbt(top, "scT", [P, 8, 2], F32)
        S.dma("sp", lambda e: e.dma_start(out=craw[:, 0, :], in_=cT_d), cslot, w=[craw])
        S.dma("sp", lambda e: e.dma_start(out=craw[:, 1, :], in_=cctxT_d), cslot, w=[craw])
        cslot.close()
        S.op("act", lambda e: e.activation(scT[:].rearrange("p j r -> p r j"), craw[:], AF.Silu), r=[craw], w=[scT])
        modcol = sbt(top, "modcol", [P, 2, 16], F32)
        eps6 = sbt(top, "eps6", [P, 1], F32)
        eps5 = sbt(top, "eps5", [P, 1], F32)
        S.op("pool", lambda e: e.memset(eps6[:], 1e-6), w=[eps6])
        S.op("pool", lambda e: e.memset(eps5[:], 1e-5), w=[eps5])


        def rstd_from_ss(ss, rs, scale, eps_t):
            S.op("act", lambda e: e.activation(rs[:], ss[:], AF.Sqrt, scale=scale, bias=eps_t[:, 0:1]), r=[ss, eps_t], w=[rs])
            S.op("dve", lambda e: e.reciprocal(rs[:], rs[:]), r=[rs], w=[rs])

        def dbg_store(name, rows, tile_ap, rtiles):
            if dbg and name in dbg_d:
                if name not in dbg_slots:
                    dbg_slots[name] = (DmaSlot(S, name), Buf(name))
                S.dma("sp", lambda e: e.dma_start(out=dbg_d[name][rows], in_=tile_ap), dbg_slots[name][0], r=rtiles, w=[dbg_slots[name][1]])


        def convert_tables():
            with ExitStack() as ph:
                NB = 4
                stg = [sbt(ph, f"cvi{k}", [P, 4, D], F32) for k in range(NB)]
                obf = [sbt(ph, f"cvo{k}", [P, 4, D], BF16) for k in range(NB)]
                islot = [DmaSlot(S, f"cvi{k}") for k in range(NB)]
                oslot = [DmaSlot(S, f"cvo{k}") for k in range(NB)]
                n = 0
                for l in range(DEPTH):
                    for tab, col0 in ((peer_u_d, 0), (peer_v_d, D)):
                        for t in range(NPEER // 512):
                            k = n % NB
                            src = tab[l, t * 512:(t + 1) * 512, :].rearrange("(p r) d -> p r d", r=4)
                            dst = uv_d[l * NPEER + t * 512:l * NPEER + (t + 1) * 512, col0:col0 + D].rearrange("(p r) d -> p r d", r=4)
                            S.dma("sp", lambda e, k=k, src=src: e.dma_start(out=stg[k][:], in_=src), islot[k], w=[stg[k]])
                            if n % 2 == 0:
                                S.op("act", lambda e, k=k: e.activation(obf[k][:], stg[k][:], AF.Copy), r=[stg[k]], w=[obf[k]])
                            else:
                                S.op("dve", lambda e, k=k: e.tensor_copy(obf[k][:], stg[k][:]), r=[stg[k]], w=[obf[k]])
                            S.dma("pool", lambda e, k=k, dst=dst: e.dma_start(out=dst, in_=obf[k][:]), oslot[k], r=[obf[k]])
                            n += 1
                S.barrier()

        def mod_cols(l):
            with ExitStack() as ph:
                wm = [sbt(ph, f"wm{k}", [P, 8, P], F32) for k in range(2)]
                wslot = [DmaSlot(S, f"wm{k}") for k in range(2)]
                bcol = sbt(ph, "bcol", [P, 16], F32)
                pcol = pst(ph, "pcol", [P, 16, 2], F32)
                S.dma("sp", lambda e: e.dma_start(out=bcol[:], in_=b_mod_d[l, 0:2048].rearrange("(c p) -> p c", p=P),
                                                  allow_slow_non_contiguous=True), DmaSlot(S, "bcol"), w=[bcol])
                for cc in range(16):
                    t = wm[cc % 2]
                    S.dma("sp", lambda e, t=t, cc=cc: e.dma_start(
                        out=t[:], in_=w_mod_d[l, :, cc * P:(cc + 1) * P].rearrange("(j p) m -> p j m", p=P)),
                          wslot[cc % 2], w=[t])
                    for j in range(8):
                        S.op("pe", lambda e, t=t, j=j, cc=cc: e.matmul(pcol[:, cc, :], lhsT=t[:, j, :],
                                                                     rhs=scT[:, j, :], start=(j == 0), stop=(j == 7)),
                             r=[t, scT], w=[pcol])
                S.op("dve", lambda e: e.tensor_tensor(modcol[:].rearrange("p r c -> p c r"), pcol[:],
                                                      bcol[:].unsqueeze(2).to_broadcast([P, 16, 2]), op=ALU.add),
                     r=[pcol, bcol], w=[modcol])
                S.op("dve", lambda e: e.tensor_scalar(modcol[:, :, 8:16], modcol[:, :, 8:16], 1.0, None, op0=ALU.add),
                     r=[modcol], w=[modcol])
                S.barrier()

        def mod_bc(l, r, modbc):
            with ExitStack() as ph:
                NW = 4
                wm = [sbt(ph, f"wmb{k}", [P, 512], F32) for k in range(NW)]
                wslot = [DmaSlot(S, f"wmb{k}") for k in range(NW)]
                bbc = [sbt(ph, f"bbc{k}", [P, 512], F32) for k in range(2)]
                bslot = [DmaSlot(S, f"bbc{k}") for k in range(2)]
                screp = sbt(ph, "screp", [P, 8, P], F32)
                pb = pst(ph, "pbm", [P, 512], F32)
                S.op("dve", lambda e: e.tensor_copy(screp[:], scT[:, :, r].unsqueeze(2).to_broadcast([P, 8, P])),
                     r=[scT], w=[screp])
                n = 0
                for cc in range(8):
                    c0 = 2048 + cc * 512
                    S.dma("sp", lambda e, cc=cc, c0=c0: e.dma_start(out=bbc[cc % 2][:], in_=b_mod_d[l, c0:c0 + 512].partition_broadcast(P)),
                          bslot[cc % 2], w=[bbc[cc % 2]])
                    for j in range(8):
                        t = wm[n % NW]
                        S.dma("sp", lambda e, t=t, j=j, c0=c0: e.dma_start(out=t[:], in_=w_mod_d[l, j * P:(j + 1) * P, c0:c0 + 512]),
                              wslot[n % NW], w=[t])
                        n += 1
                        S.op("pe", lambda e, t=t, j=j: e.matmul(pb[:], lhsT=screp[:, j, :], rhs=t[:], start=(j == 0), stop=(j == 7)),
                             r=[t, screp], w=[pb])
                    S.op("dve", lambda e, cc=cc: e.tensor_tensor(modbc[:, cc * 512:(cc + 1) * 512], pb[:], bbc[cc % 2][:], op=ALU.add),
                         r=[pb, bbc[cc % 2]], w=[modbc])
                S.op("dve", lambda e: e.tensor_scalar(modbc[:, 2048:3072], modbc[:, 2048:3072], 1.0, None, op0=ALU.add),
                     r=[modbc], w=[modbc])
                S.barrier()

        def phase_a(l, M, src_aps, last):
            with ExitStack() as ph:
                win = sbt(ph, "win", [P, 8, INC], BF16)
                stg = sbt(ph, "stg", [P, INC], F32)
                stg_slot = DmaSlot(S, "stg")
                bias_bc = sbt(ph, "bias_bc", [P, INC], F32)
                sh1rep = sbt(ph, "sh1rep", [P, 2, 8, P], BF16)
                gain = sbt(ph, "gain", [P, 640], F32)
                graw = sbt(ph, "graw", [P, 128], F32)
                cos2 = sbt(ph, "cos2", [P, NTL, 64], F32)
                sin2 = sbt(ph, "sin2", [P, NTL, 64], F32)
                xt = [sbt(ph, f"xt{k}", [P, D], F32) for k in range(2)]
                xt_slot = [DmaSlot(S, f"xt{k}") for k in range(2)]
                xb = [sbt(ph, f"xb{k}", [P, D], BF16) for k in range(2)]
                xT = [sbt(ph, f"xT{k}", [P, 8, P], BF16) for k in range(2)]
                junk = sbt(ph, "junkA", [P, D], BF16)
                ss = [sbt(ph, f"ssA{k}", [P, 1], F32) for k in range(2)]
                rstd = [sbt(ph, f"rstdA{k}", [P, 1], F32) for k in range(2)]
                qkvu = sbt(ph, "qkvu", [P, INC], F32)
                sq = sbt(ph, "sq", [P, 640], F32)
                ssq = sbt(ph, "ssq", [P, 10], F32)
                rsq = sbt(ph, "rsq", [P, 10], F32)
                qn = sbt(ph, "qn", [P, 640], F32)
                rb = sbt(ph, "rb", [P, 640], F32)
                qr = sbt(ph, "qr", [P, 4, 2, 64], BF16)
                kr = sbt(ph, "kr", [P, 128], BF16)
                sig = sbt(ph, "sig", [P, 512], F32)
                gg = sbt(ph, "gg", [P, 512], BF16)
                ptb = pst(ph, "ptb", [P, 8, P], BF16)
                pq = [pst(ph, f"pq{k}", [P, 512], F32) for k in range(4)]
                ptq = pst(ph, "ptq", [P, 5, P], BF16)
                ptg = pst(ph, "ptg", [P, 4, P], BF16)
                ra = sq
                qT, qTc, kT, Vp, gT, gTc = M["qT"], M["qTc"], M["kT"], M["Vp"], M["gT"], M["gTc"]

                cslot2 = DmaSlot(S, "rope", group=True)
                S.dma("sp", lambda e: e.dma_start(out=cos2[:], in_=cos2_d.rearrange("(n p) f -> p n f", p=P)), cslot2, w=[cos2])
                S.dma("sp", lambda e: e.dma_start(out=sin2[:], in_=sin2_d.rearrange("(n p) f -> p n f", p=P)), cslot2, w=[sin2])
                S.dma("sp", lambda e: e.dma_start(out=graw[:, 0:64], in_=q_gain_d[l].partition_broadcast(P)), cslot2, w=[graw])
                S.dma("sp", lambda e: e.dma_start(out=graw[:, 64:128], in_=k_gain_d[l].partition_broadcast(P)), cslot2, w=[graw])
                cslot2.close()
                for j in range(8):
                    S.dma("sp", lambda e, j=j: e.dma_start(out=stg[:], in_=w_in_d[l, j * P:(j + 1) * P, :]), stg_slot, w=[stg])
                    S.op("act", lambda e, j=j: e.activation(win[:, j, :], stg[:], AF.Copy), r=[stg], w=[win])
                S.op("dve", lambda e: e.tensor_copy(gain[:, 0:512].rearrange("p (h d) -> p h d", h=8),
                                                    graw[:, 0:64].unsqueeze(1).to_broadcast([P, 8, 64])), r=[graw], w=[gain])
                S.op("dve", lambda e: e.tensor_copy(gain[:, 512:640].rearrange("p (h d) -> p h d", h=2),
                                                    graw[:, 64:128].unsqueeze(1).to_broadcast([P, 2, 64])), r=[graw], w=[gain])
                S.op("dve", lambda e: e.tensor_copy(sh1rep[:], modcol[:, :, 0:8].unsqueeze(3).to_broadcast([P, 2, 8, P])),
                     r=[modcol], w=[sh1rep])

                def make_bias(r_):
                    for cc in range(4):
                        w_ = min(512, INC - cc * 512)
                        for j in range(8):
                            S.op("pe", lambda e, cc=cc, j=j, w_=w_: e.matmul(
                                pq[cc][:, 0:w_], lhsT=sh1rep[:, r_, j, :], rhs=win[:, j, cc * 512:cc * 512 + w_],
                                start=(j == 0), stop=(j == 7)), r=[sh1rep, win], w=[pq[cc]])
                        S.op("act", lambda e, cc=cc, w_=w_: e.activation(
                            bias_bc[:, cc * 512:cc * 512 + w_], pq[cc][:, 0:w_], AF.Copy), r=[pq[cc]], w=[bias_bc])

                def load(i):
                    k = i % 2
                    S.dma("sp", lambda e: e.dma_start(out=xt[k][:], in_=src_aps[i]), xt_slot[k], r=[xs_buf[i]], w=[xt[k]])

                make_bias(0)
                load(0)
                for i in range(NT if stop not in ("a0", "a1") else (0 if stop == "a0" else 1)):
                    k = i % 2
                    r_ = 0 if i < NTL else 1
                    is_ctx = i >= NTL
                    if i == NTL:
                        make_bias(1)
                    if i + 1 < NT:
                        load(i + 1)
                    S.op("act", lambda e: e.activation(junk[:], xt[k][:], AF.Square, accum_out=ss[k][:]), r=[xt[k]], w=[junk, ss[k]])
                    S.op("act", lambda e: e.activation(xb[k][:], xt[k][:], AF.Copy), r=[xt[k]], w=[xb[k]])
                    rstd_from_ss(ss[k], rstd[k], 1.0 / D, eps6)
                    for j in range(8):
                        S.op("pe", lambda e, j=j: e.transpose(ptb[:, j, :], xb[k][:, j * P:(j + 1) * P], ident_b[:]),
                             r=[xb[k], ident_b], w=[ptb])
                    S.op("dve", lambda e: e.tensor_tensor(xT[k][:], ptb[:],
                                                          modcol[:, r_, 8:16].unsqueeze(2).to_broadcast([P, 8, P]), op=ALU.mult),
                         r=[ptb, modcol], w=[xT[k]])
                    if CUT <= 1:
                        continue
                    for cc in range(4):
                        w_ = min(512, INC - cc * 512)
                        for j in range(8):
                            S.op("pe", lambda e, cc=cc, j=j, w_=w_: e.matmul(
                                pq[cc][:, 0:w_], lhsT=xT[k][:, j, :], rhs=win[:, j, cc * 512:cc * 512 + w_],
                                start=(j == 0), stop=(j == 7)), r=[xT[k], win], w=[pq[cc]])
                        S.op("dve", lambda e, cc=cc, w_=w_: e.scalar_tensor_tensor(
                            out=qkvu[:, cc * 512:cc * 512 + w_], in0=pq[cc][:, 0:w_], scalar=rstd[k][:, 0:1],
                            in1=bias_bc[:, cc * 512:cc * 512 + w_], op0=ALU.mult, op1=ALU.add),
                             r=[pq[cc], rstd[k], bias_bc], w=[qkvu])
                    if l == 0:
                        dbg_store("d_qkvu", slice(i * P, (i + 1) * P), qkvu[:], [qkvu])
                    if CUT <= 2:
                        continue
                    S.op("pool", lambda e: e.tensor_tensor(sq[:], qkvu[:, 0:640], qkvu[:, 0:640], op=ALU.mult), r=[qkvu], w=[sq])
                    S.op("dve", lambda e: e.tensor_reduce(out=ssq[:], in_=sq[:].rearrange("p (h d) -> p h d", h=10),
                                                          axis=AX.X, op=ALU.add), r=[sq], w=[ssq])
                    rstd_from_ss(ssq, rsq, 1.0 / 64, eps6)
                    S.op("dve", lambda e: e.tensor_tensor(qn[:].rearrange("p (h d) -> p h d", h=10),
                                                          qkvu[:, 0:640].rearrange("p (h d) -> p h d", h=10),
                                                          rsq[:].unsqueeze(2).to_broadcast([P, 10, 64]), op=ALU.mult),
                         r=[qkvu, rsq], w=[qn])
                    S.op("pool", lambda e: e.tensor_tensor(qn[:], qn[:], gain[:], op=ALU.mult), r=[qn, gain], w=[qn])
                    if CUT <= 3:
                        continue
                    if not is_ctx:
                        qn3 = qn[:].rearrange("p (h d) -> p h d", h=10)
                        rb3 = rb[:].rearrange("p (h d) -> p h d", h=10)
                        S.op("dve", lambda e: e.tensor_tensor(ra[:].rearrange("p (h d) -> p h d", h=10), qn3,
                                                              cos2[:, i, :].unsqueeze(1).to_broadcast([P, 10, 64]), op=ALU.mult),
                             r=[qn, cos2], w=[ra])
                        S.op("pool", lambda e: e.tensor_tensor(rb3[:, :, 0:32], qn3[:, :, 32:64],
                                                               sin2[:, i, 0:32].unsqueeze(1).to_broadcast([P, 10, 32]), op=ALU.mult),
                             r=[qn, sin2], w=[rb])
                        S.op("pool", lambda e: e.tensor_tensor(rb3[:, :, 32:64], qn3[:, :, 0:32],
                                                               sin2[:, i, 32:64].unsqueeze(1).to_broadcast([P, 10, 32]), op=ALU.mult),
                             r=[qn, sin2], w=[rb])
                        S.op("dve", lambda e: e.tensor_tensor(qr[:].rearrange("p pr hh d -> p hh pr d"),
                                                              ra[:, 0:512].rearrange("p (hh pr d) -> p hh pr d", hh=2, pr=4),
                                                              rb[:, 0:512].rearrange("p (hh pr d) -> p hh pr d", hh=2, pr=4), op=ALU.add),
                             r=[ra, rb], w=[qr])
                        S.op("pool", lambda e: e.tensor_tensor(kr[:], ra[:, 512:640], rb[:, 512:640], op=ALU.add), r=[ra, rb], w=[kr])
                    else:
                        S.op("dve", lambda e: e.tensor_copy(qr[:].rearrange("p pr hh d -> p hh pr d"),
                                                            qn[:, 0:512].rearrange("p (hh pr d) -> p hh pr d", hh=2, pr=4)),
                             r=[qn], w=[qr])
                        S.op("pool", lambda e: e.tensor_copy(kr[:], qn[:, 512:640]), r=[qn], w=[kr])
                    if CUT <= 4:
                        continue
                    need_q = (not is_ctx) or (not last)
                    if need_q:
                        for pr in range(4):
                            S.op("pe", lambda e, pr=pr: e.transpose(ptq[:, pr, :], qr[:, pr, :, :].rearrange("p hh d -> p (hh d)"), ident_b[:]),
                                 r=[qr, ident_b], w=[ptq])
                    S.op("pe", lambda e: e.transpose(ptq[:, 4, :], kr[:], ident_b[:]), r=[kr, ident_b], w=[ptq])
                    if need_q:
                        if not is_ctx:
                            S.op("act", lambda e: e.activation(qT[:, :, i * P:(i + 1) * P], ptq[:, 0:4, :], AF.Copy),
                                 r=[ptq], w=[M["qT_b"][pr][i // 4] for pr in range(4)])
                        else:
                            S.op("act", lambda e: e.activation(qTc[:, :, (i - NTL) * P:(i - NTL + 1) * P], ptq[:, 0:4, :], AF.Copy),
                                 r=[ptq], w=[qTc])
                    S.op("act", lambda e: e.activation(kT[:, i * P:(i + 1) * P], ptq[:, 4, :], AF.Copy), r=[ptq], w=[M["kT_b"][i]])
                    if CUT <= 5:
                        continue
                    S.op("pool", lambda e: e.tensor_copy(Vp[:, i, 0:64], qkvu[:, 640:704]), r=[qkvu], w=[M["Vp_b"][i]])
                    S.op("pool", lambda e: e.tensor_copy(Vp[:, i, 128:192], qkvu[:, 704:768]), r=[qkvu], w=[M["Vp_b"][i]])
                    if CUT <= 6:
                        continue
                    if need_q:
                        S.op("act", lambda e: e.activation(sig[:], qkvu[:, 1280:1792], AF.Sigmoid), r=[qkvu], w=[sig])
                        S.op("dve", lambda e: e.tensor_tensor(gg[:], qkvu[:, 768:1280], sig[:], op=ALU.mult), r=[qkvu, sig], w=[gg])
                        for c in range(4):
                            S.op("pe", lambda e, c=c: e.transpose(ptg[:, c, :], gg[:, c * P:(c + 1) * P], ident_b[:]),
                                 r=[gg, ident_b], w=[ptg])
                        if not is_ctx:
                            S.op("act", lambda e: e.activation(gT[:, :, GPAD + i * P:GPAD + (i + 1) * P], ptg[:], AF.Copy),
                                 r=[ptg], w=[M["gT_b"][i]])
                        else:
                            S.op("act", lambda e: e.activation(gTc[:, :, GPAD + (i - NTL) * P:GPAD + (i - NTL + 1) * P], ptg[:], AF.Copy),
                                 r=[ptg], w=[M["gTc_b"][i - NTL]])
                S.barrier()

        def phase_b(l, M, last):
            with ExitStack() as ph:
                NPT = 4
                NPS = 4
                LA = 1
                pT = [sbt(ph, f"pT{k}", [P, 512], BF16) for k in range(NPT)]
                rd = sbt(ph, "rd", [P, 512], F32)
                bcs = sbt(ph, "bcs", [P, 512], F32)
                ps_s = [pst(ph, f"ps_s{k}", [P, 512], F32) for k in range(NPS)]
                po = [pst(ph, f"po{k}", [P, 512], F32) for k in range(3)]
                pbc = pst(ph, "pbc", [P, 512], F32)
                qT, qTc, kT, Vp = M["qT"], M["qTc"], M["kT"], M["Vp"]
                cnt = [0]
                grp = [0]
                kTz = sbt(ph, "kTz", [P, 2, NT * P], BF16)
                S.op("pool", lambda e: e.memset(kTz[:], 0.0), w=[kTz])
                S.op("dve", lambda e: e.tensor_copy(kTz[0:64, 0, :], kT[0:64, :]), r=M["kT_b"], w=[kTz])
                S.op("act", lambda e: e.activation(kTz[64:128, 1, :], kT[64:128, :], AF.Copy), r=M["kT_b"], w=[kTz])

                def attend(qsrc_fn, qfull, qbuf, n, ktiles):
                    its = [(kt_i, kt, hh) for kt_i, kt in enumerate(ktiles) for hh in range(2)]
                    base = cnt[0]
                    cnt[0] += len(its)
                    pg = [po[(2 * grp[0]) % 3], po[(2 * grp[0] + 1) % 3]]
                    grp[0] += 1

                    def emit_s(m):
                        kt_i, kt, hh = its[m]
                        k = (base + m) % NPS
                        kp = (base + m) % NPT
                        S.op("pe", lambda e: e.matmul(ps_s[k][:, 0:n], lhsT=kTz[:, hh, kt * P:(kt + 1) * P], rhs=qfull,
                                                      start=True, stop=True), r=[kTz, qbuf], w=[ps_s[k]])
                        S.op("act", lambda e: e.activation(pT[kp][:, 0:n], ps_s[k][:, 0:n], AF.Exp, scale=0.125),
                             r=[ps_s[k]], w=[pT[kp]])

                    def emit_pv(m):
                        kt_i, kt, hh = its[m]
                        kp = (base + m) % NPT
                        S.op("pe", lambda e: e.matmul(pg[hh][:, 0:n], lhsT=Vp[:, kt, hh * 64:hh * 64 + 128], rhs=pT[kp][:, 0:n],
                                                      start=(kt_i == 0), stop=(kt_i == len(ktiles) - 1)), r=[M["Vp_b"][kt], pT[kp]], w=[pg[hh]])

                    npair = len(its) // 2
                    for j in range(npair + LA):
                        if j < npair:
                            emit_s(2 * j)
                            emit_s(2 * j + 1)
                        if j - LA >= 0:
                            emit_pv(2 * (j - LA))
                            emit_pv(2 * (j - LA) + 1)
                    for hh in range(2):
                        dp = 64 if hh == 0 else 0
                        S.op("dve", lambda e, hh=hh, dp=dp: e.reciprocal(rd[dp:dp + 1, 0:n], pg[hh][dp:dp + 1, 0:n]),
                             r=[pg[hh]], w=[rd])
                        S.op("pe", lambda e, dp=dp: e.matmul(pbc[0:64, 0:n], lhsT=ones_f[dp:dp + 1, 0:64], rhs=rd[dp:dp + 1, 0:n],
                                                            start=True, stop=True), r=[ones_f, rd], w=[pbc])
                        S.op("act", lambda e, hh=hh: e.activation(bcs[hh * 64:(hh + 1) * 64, 0:n], pbc[0:64, 0:n], AF.Copy),
                             r=[pbc], w=[bcs])
                        S.op("dve", lambda e, hh=hh: e.tensor_tensor(qsrc_fn(hh), pg[hh][hh * 64:(hh + 1) * 64, 0:n],
                                                                     bcs[hh * 64:(hh + 1) * 64, 0:n], op=ALU.mult),
                             r=[pg[hh], bcs], w=[qbuf])

                for c in range(8):
                    for pr in range(4):
                        attend(lambda hh, c=c, pr=pr: qT[hh * 64:(hh + 1) * 64, pr, c * 512:(c + 1) * 512],
                               qT[:, pr, c * 512:(c + 1) * 512], M["qT_b"][pr][c], 512, list(range(NT)))
                if not last:
                    for pr in range(4):
                        attend(lambda hh, pr=pr: qTc[hh * 64:(hh + 1) * 64, pr, :], qTc[:, pr, :], qTc.b, S_CTX, [NTL, NTL + 1])
                S.barrier()

        def phase_c(l, M, tiles, src_aps, r_, modbc):
            with ExitStack() as ph:
                wcol = sbt(ph, "wcol", [P, 4, 31], F32)
                DG = sbt(ph, "DG", [P, 4, 31, P], BF16)
                wo = sbt(ph, "wo", [P, 8, D], BF16)
                stg = [sbt(ph, f"stgC{k}", [P, D], F32) for k in range(2)]
                stg_slot = [DmaSlot(S, f"stgC{k}") for k in range(2)]
                betaA = sbt(ph, "betaA", [P, 8], F32)
                betac = sbt(ph, "betac", [P, 8], F32)
                cb_bc = sbt(ph, "cb_bc", [P, 512], F32)
                lg_bc = sbt(ph, "lg_bc", [P, 512], F32)
                lb_bc = sbt(ph, "lb_bc", [P, 512], F32)
                xt = [sbt(ph, f"xtC{k}", [P, D], F32) for k in range(2)]
                xt_slot = [DmaSlot(S, f"xtC{k}") for k in range(2)]
                y = sbt(ph, "yC", [P, 512], F32)
                junk = sbt(ph, "junkC", [P, 512], F32)
                st4 = sbt(ph, "st4", [P, 4], F32)
                rln = sbt(ph, "rln", [P, 1], F32)
                msq = sbt(ph, "msq", [P, 1], F32)
                z = sbt(ph, "zC", [P, 512], F32)
                oc = sbt(ph, "oc", [P, 512], F32)
                ocb = sbt(ph, "ocb", [P, 512], BF16)
                ocT = sbt(ph, "ocT", [P, 4, P], BF16)
                sqo = sbt(ph, "sqo", [P, 4, P], F32)
                ssc = sbt(ph, "ssc", [P, 2], F32)
                rsc = sbt(ph, "rsc", [P, 2], F32)
                t1 = sbt(ph, "t1", [P, D], F32)
                x1 = [sbt(ph, f"x1_{k}", [P, D], F32) for k in range(2)]
                x1_slot = [DmaSlot(S, f"x1s{k}") for k in range(2)]
                py = pst(ph, "py", [P, 512], F32)
                ptc = pst(ph, "ptc", [P, 4, P], BF16)
                pss = pst(ph, "pss", [P, 16], F32)
                pa = [pst(ph, f"pa{k}", [P, 512], F32) for k in range(2)]
                pc = [pst(ph, f"pc{k}", [P, 512], F32) for k in range(2)]
                qT, qTc, gT, gTc = M["qT"], M["qTc"], M["gT"], M["gTc"]

                vs = DmaSlot(S, "vecC", group=True)
                for c in range(4):
                    S.dma("sp", lambda e, c=c: e.dma_start(out=wcol[:, c, :], in_=conv_w_d[l, :, c * P:(c + 1) * P].rearrange("j p -> p j"),
                                                           allow_slow_non_contiguous=True), vs, w=[wcol])
                for half in range(2):
                    S.dma("sp", lambda e, half=half: e.dma_start(out=betaA[half * 64:(half + 1) * 64, :],
                                                                 in_=beta_attn_d[l].rearrange("(h d) -> d h", d=64),
                                                                 allow_slow_non_contiguous=True), vs, w=[betaA])
                S.dma("sp", lambda e: e.dma_start(out=betac[:, 4:8], in_=beta_conv_d[l].rearrange("(c p) -> p c", p=P),
                                                  allow_slow_non_contiguous=True), vs, w=[betac])
                S.dma("sp", lambda e: e.dma_start(out=cb_bc[:], in_=conv_b_d[l].partition_broadcast(P)), vs, w=[cb_bc])
                S.dma("sp", lambda e: e.dma_start(out=lg_bc[:], in_=ln_g_d[l].partition_broadcast(P)), vs, w=[lg_bc])
                S.dma("sp", lambda e: e.dma_start(out=lb_bc[:], in_=ln_b_d[l].partition_broadcast(P)), vs, w=[lb_bc])
                vs.close()
                S.op("dve", lambda e: e.tensor_copy(betac[0:64, 0:4], betaA[0:64, 0:4]), r=[betaA], w=[betac])
                S.op("dve", lambda e: e.tensor_copy(betac[64:128, 0:4], betaA[64:128, 4:8]), r=[betaA], w=[betac])
                for kk in range(8):
                    t = stg[kk % 2]
                    sl = stg_slot[kk % 2]
                    if kk < 4:
                        S.dma("sp", lambda e, t=t, kk=kk: e.dma_start(out=t[0:64, :], in_=w_o_d[l, kk * 64:(kk + 1) * 64, :]), sl, w=[t])
                        S.dma("sp", lambda e, t=t, kk=kk: e.dma_start(out=t[64:128, :], in_=w_o_d[l, (kk + 4) * 64:(kk + 5) * 64, :]), sl, w=[t])
                    else:
                        S.dma("sp", lambda e, t=t, kk=kk: e.dma_start(out=t[:], in_=w_o_d[l, 512 + (kk - 4) * P:512 + (kk - 3) * P, :]), sl, w=[t])
                    S.op("act", lambda e, t=t, kk=kk: e.activation(wo[:, kk, :], t[:], AF.Identity, scale=betac[:, kk:kk + 1]),
                         r=[t, betac], w=[wo])
                for c in range(4):
                    for j in range(31):
                        S.op("pool", lambda e, c=c, j=j: e.tensor_scalar(DG[:, c, j, :], ident_f[:], wcol[:, c, j:j + 1], None, op0=ALU.mult),
                             r=[ident_f, wcol], w=[DG])

                def load(n_):
                    i = tiles[n_]
                    k = n_ % 2
                    S.dma("sp", lambda e: e.dma_start(out=xt[k][:], in_=src_aps[i]), xt_slot[k], r=[xs_buf[i]], w=[xt[k]])

                load(0)
                for n_, i in enumerate(tiles):
                    k = n_ % 2
                    is_ctx = i >= NTL
                    if n_ + 1 < len(tiles):
                        load(n_ + 1)
                    if not is_ctx:
                        gsrc, gb, t0 = gT, M["gT_b"], i * P
                        lo, hi = max(0, i - 1), min(NTL - 1, i + 1)
                        osrc = lambda pr: qT[:, pr, i * P:(i + 1) * P]
                        obufs = [M["qT_b"][pr][i // 4] for pr in range(4)]
                    else:
                        gsrc, gb, t0 = gTc, M["gTc_b"], (i - NTL) * P
                        lo, hi = max(0, i - NTL - 1), min(NTC - 1, i - NTL + 1)
                        osrc = lambda pr: qTc[:, pr, (i - NTL) * P:(i - NTL + 1) * P]
                        obufs = [qTc.b]
                    for c in range(4):
                        for j in range(31):
                            S.op("pe", lambda e, c=c, j=j: e.matmul(py[:, c * P:(c + 1) * P], lhsT=gsrc[:, c, t0 + j + GPAD - 15:t0 + j + GPAD - 15 + P],
                                                                  rhs=DG[:, c, j, :], start=(j == 0), stop=(j == 30)),
                                 r=[gb[q] for q in range(lo, hi + 1)] + [DG], w=[py])
                    S.op("dve", lambda e: e.tensor_tensor(y[:], py[:], cb_bc[:], op=ALU.add), r=[py, cb_bc], w=[y])
                    S.op("act", lambda e: e.activation(junk[:], y[:], AF.Identity, accum_out=st4[:, 0:1]), r=[y], w=[junk, st4])
                    S.op("act", lambda e: e.activation(junk[:], y[:], AF.Square, accum_out=st4[:, 1:2]), r=[y], w=[junk, st4])
                    S.op("dve", lambda e: e.tensor_scalar(st4[:, 2:4], st4[:, 0:2], 1.0 / 512, None, op0=ALU.mult), r=[st4], w=[st4])
                    S.op("dve", lambda e: e.tensor_tensor(msq[:], st4[:, 2:3], st4[:, 2:3], op=ALU.mult), r=[st4], w=[msq])
                    S.op("dve", lambda e: e.tensor_tensor(msq[:], st4[:, 3:4], msq[:], op=ALU.subtract), r=[st4, msq], w=[msq])
                    rstd_from_ss(msq, rln, 1.0, eps5)
                    S.op("dve", lambda e: e.tensor_scalar(z[:], y[:], st4[:, 2:3], rln[:, 0:1], op0=ALU.subtract, op1=ALU.mult),
                         r=[y, st4, rln], w=[z])
                    S.op("pool", lambda e: e.tensor_tensor(z[:], z[:], lg_bc[:], op=ALU.mult), r=[z, lg_bc], w=[z])
                    S.op("pool", lambda e: e.tensor_tensor(z[:], z[:], lb_bc[:], op=ALU.add), r=[z, lb_bc], w=[z])
                    S.op("act", lambda e: e.activation(oc[:], z[:], AF.Silu), r=[z], w=[oc])
                    S.op("act", lambda e: e.activation(junk[:], oc[:], AF.Square, accum_out=ssc[:, 0:1]), r=[oc], w=[junk, ssc])
                    S.op("pool", lambda e: e.tensor_copy(ocb[:], oc[:]), r=[oc], w=[ocb])
                    for c in range(4):
                        S.op("pe", lambda e, c=c: e.transpose(ptc[:, c, :], ocb[:, c * P:(c + 1) * P], ident_b[:]),
                             r=[ocb, ident_b], w=[ptc])
                    S.op("act", lambda e: e.activation(ocT[:], ptc[:], AF.Copy), r=[ptc], w=[ocT])
                    for pr in range(4):
                        S.op("pool", lambda e, pr=pr: e.tensor_tensor(sqo[:, pr, :], osrc(pr), osrc(pr), op=ALU.mult), r=obufs, w=[sqo])
                    for pr in range(4):
                        S.op("pe", lambda e, pr=pr: e.matmul(pss[:, 0:2], lhsT=sqo[:, pr, :], rhs=ones_f[:, 0:2],
                                                            start=(pr == 0), stop=(pr == 3)), r=[sqo, ones_f], w=[pss])
                    S.op("dve", lambda e: e.tensor_copy(ssc[:, 1:2], pss[:, 0:1]), r=[pss], w=[ssc])
                    rstd_from_ss(ssc, rsc, 1.0 / 512, eps6)
                    for cc in range(2):
                        for pr in range(4):
                            S.op("pe", lambda e, cc=cc, pr=pr: e.matmul(pa[cc][:], lhsT=osrc(pr), rhs=wo[:, pr, cc * 512:(cc + 1) * 512],
                                                                      start=(pr == 0), stop=(pr == 3)), r=obufs + [wo], w=[pa[cc]])
                        for c in range(4):
                            S.op("pe", lambda e, cc=cc, c=c: e.matmul(pc[cc][:], lhsT=ocT[:, c, :], rhs=wo[:, 4 + c, cc * 512:(cc + 1) * 512],
                                                                    start=(c == 0), stop=(c == 3)), r=[ocT, wo], w=[pc[cc]])
                        sl = slice(cc * 512, (cc + 1) * 512)
                        S.op("dve", lambda e, cc=cc, sl=sl: e.tensor_scalar(t1[:, sl], pa[cc][:], rsc[:, 1:2], None, op0=ALU.mult),
                             r=[pa[cc], rsc], w=[t1])
                        S.op("dve", lambda e, cc=cc, sl=sl: e.scalar_tensor_tensor(out=t1[:, sl], in0=pc[cc][:], scalar=rsc[:, 0:1],
                                                                                   in1=t1[:, sl], op0=ALU.mult, op1=ALU.add),
                             r=[pc[cc], rsc, t1], w=[t1])
                    S.op("pool", lambda e: e.tensor_tensor(t1[:], t1[:], modbc[:, 0:1024], op=ALU.mult), r=[t1, modbc], w=[t1])
                    S.op("pool", lambda e: e.tensor_tensor(x1[k][:], t1[:], xt[k][:], op=ALU.add), r=[t1, xt[k]], w=[x1[k]])
                    S.dma("sp", lambda e: e.dma_start(out=xs_d[i * P:(i + 1) * P, :], in_=x1[k][:]), x1_slot[k], r=[x1[k]], w=[xs_buf[i]])
                    if l == 0:
                        dbg_store("d_x1", slice(i * P, (i + 1) * P), x1[k][:], [x1[k]])
                S.barrier()

        def phase_p(l, tiles, modbc, last):
            with ExitStack() as ph:
                wq = sbt(ph, "wq", [P, 8, D], F32)
                keysTz = sbt(ph, "keysTz", [P, 8, 2, P], F32)
                iota16 = sbt(ph, "iota16", [P, 16], F32)
                xt = [sbt(ph, f"xtP{k}", [P, D], F32) for k in range(2)]
                xt_slot = [DmaSlot(S, f"xtP{k}") for k in range(2)]
                junk = sbt(ph, "junkP", [P, D], BF16)
                junkr = sbt(ph, "junkR", [P, D], BF16)
                prod = [sbt(ph, f"prod{k}", [P, D], BF16) for k in range(3)]
                hb = [sbt(ph, f"hb{k}", [P, D], BF16) for k in range(2)]
                pi = [0]
                ss = sbt(ph, "ssP", [P, 1], F32)
                rstd = sbt(ph, "rstdP", [P, 1], F32)
                ssf = sbt(ph, "ssF", [P, 1], F32)
                rstdf = sbt(ph, "rstdF", [P, 1], F32)
                h = [sbt(ph, "hP", [P, D], F32)] * 2
                hT = sbt(ph, "hT", [P, 8, P], F32)
                qpT = hT
                qp = sbt(ph, "qp", [P, D], F32)
                sc = sbt(ph, "scP", [P, 16, 128], F32)
                wk = sbt(ph, "wkP", [P, 16, 128], F32)
                eq_ap = wk[:].rearrange("p a b -> p (a b)").rearrange("p (x c) -> p x c", c=16)
                topv = sbt(ph, "topv", [P, 16, 16], F32)
                idx = sbt(ph, "idxP", [P, 16, 16], U32)
                idxf = sbt(ph, "idxf", [P, 16, 16], F32)
                cand = sbt(ph, "cand", [P, 8, 256], F32)
                wk2_ap = sc[:].rearrange("p a b -> p (a b)").rearrange("p (h x) -> p h x", h=8)
                best = sbt(ph, "best", [P, 8, 16], F32)
                pos = sbt(ph, "pos", [P, 8, 16], U32)
                pab = sbt(ph, "pab", [P, 2, 128], U32)
                abf = sbt(ph, "abf", [P, 2, 128], F32)
                isel = sbt(ph, "isel", [P, 2, 128], F32)
                ef = [sbt(ph, f"ef{k}", [P, 128], F32) for k in range(2)]
                eidx = [sbt(ph, f"eidx{k}", [P, 128], I32) for k in range(2)]
                ew = sbt(ph, "ew", [P, 128], F32)
                se = sbt(ph, "se", [P, 8], F32)
                wgt = [sbt(ph, f"wgt{k}", [P, 128], F32) for k in range(2)]
                pre = sbt(ph, "pre", [P, 128], F32)
                aw = sbt(ph, "aw", [P, 128], F32)
                gbuf = [sbt(ph, f"gbuf{k}", [P, 2 * D], BF16) for k in range(NSLOT)]
                diag = [sbt(ph, f"diag{k}", [P, P], BF16) for k in range(6)]
                pre_b = [Buf(f"pre_b{k}") for k in range(4)]
                pre_d = [Buf(f"pre_d{k}") for k in range(4)]
                di = [0]
                gslot = [DmaSlot(S, f"g{k}") for k in range(NSLOT)]
                x2 = [sbt(ph, "x2_0", [P, D], F32)] * 2
                x2_slot = [DmaSlot(S, f"x2s{k}") for k in range(2)]
                fg_bc = sbt(ph, "fg_bc", [P, D], F32)
                pT2 = pst(ph, "pT2", [P, 8, P], F32)
                pqp = [pst(ph, f"pqp{k}", [P, 512], F32) for k in range(2)]
                pcs = [pst(ph, f"pcs{k}", [P, 512], F32) for k in range(2)]
                pva = [pst(ph, f"pva{k}", [P, 512], F32) for k in range(2)]
                psc = pqp + pcs
                print(f"[build] phase_p sbuf remaining {nc.sbuf_bytes_remaining}")

                S.op("pool", lambda e: e.iota(iota16[:], pattern=[[1, 16]], base=0, channel_multiplier=0,
                                              allow_small_or_imprecise_dtypes=True), w=[iota16])
                ws = DmaSlot(S, "wqload", group=True)
                for j in range(8):
                    S.dma("sp", lambda e, j=j: e.dma_start(out=wq[:, j, :], in_=peer_wq_d[l, j * P:(j + 1) * P, :]), ws, w=[wq])
                kraw_ap = sc[:].rearrange("p a b -> p (a b)")[:, 0:1024].rearrange("p (a d) -> p a d", d=64)
                keysT_ap = cand[:].rearrange("p a b -> p (a b)")[:, 0:1024].rearrange("p (a k) -> p a k", k=P)
                S.dma("sp", lambda e: e.dma_start(out=kraw_ap, in_=peer_keys_d[l].rearrange("h c k d -> k (h c) d")), ws, w=[sc])
                if last:
                    S.dma("sp", lambda e: e.dma_start(out=fg_bc[:], in_=final_g_d[0].partition_broadcast(P)), ws, w=[fg_bc])
                ws.close()
                for hh in range(8):
                    S.op("pe", lambda e, hh=hh: e.transpose(pT2[:, hh, :], kraw_ap[:, 2 * hh:2 * hh + 2, :].rearrange("p c d -> p (c d)"), ident_f[:]),
                         r=[sc, ident_f], w=[pT2])
                S.op("act", lambda e: e.activation(keysT_ap, pT2[:], AF.Copy), r=[pT2], w=[cand])
                S.op("pool", lambda e: e.memset(keysTz[:], 0.0), w=[keysTz])
                S.op("dve", lambda e: e.tensor_copy(keysTz[0:64, :, 0, :], keysT_ap[0:64, :, :]), r=[cand], w=[keysTz])
                S.op("dve", lambda e: e.tensor_copy(keysTz[64:128, :, 1, :], keysT_ap[64:128, :, :]), r=[cand], w=[keysTz])

                gi = [0]

                def routing(n_):
                    rq = []
                    i = tiles[n_]
                    k = n_ % 2
                    xk, hk, efk, eik, wgk = xt[k], h[k], ef[k], eidx[k], wgt[k]
                    hbk = hb[k]

                    def op(eng, fn, r=(), w=()):
                        rq.append(("op", eng, fn, r, w))

                    rq.append(("dma", "sp", lambda e: e.dma_start(out=xk[:], in_=xs_d[i * P:(i + 1) * P, :]), xt_slot[k], [xs_buf[i]], [xk]))
                    op("act", lambda e: e.activation(junkr[:], xk[:], AF.Square, accum_out=ss[:]), r=[xk], w=[junkr, ss])
                    op("act", lambda e: e.activation(rstd[:], ss[:], AF.Sqrt, scale=1.0 / D, bias=eps6[:, 0:1]), r=[ss, eps6], w=[rstd])
                    op("dve", lambda e: e.reciprocal(rstd[:], rstd[:]), r=[rstd], w=[rstd])
                    op("dve", lambda e: e.scalar_tensor_tensor(out=hk[:], in0=xk[:], scalar=rstd[:, 0:1], in1=modbc[:, 2048:3072],
                                                               op0=ALU.mult, op1=ALU.mult), r=[xk, rstd, modbc], w=[hk])
                    op("dve", lambda e: e.tensor_tensor(hk[:], hk[:], modbc[:, 1024:2048], op=ALU.add), r=[hk, modbc], w=[hk])
                    op("act", lambda e: e.activation(hbk[:], hk[:], AF.Copy), r=[hk], w=[hbk])
                    for j in range(8):
                        op("pe", lambda e, j=j: e.transpose(pT2[:, j, :], hk[:, j * P:(j + 1) * P], ident_f[:]), r=[hk, ident_f], w=[pT2])
                    op("act", lambda e: e.activation(hT[:], pT2[:], AF.Copy), r=[pT2], w=[hT])
                    for cc in range(2):
                        for j in range(8):
                            op("pe", lambda e, cc=cc, j=j: e.matmul(pqp[cc][:], lhsT=hT[:, j, :], rhs=wq[:, j, cc * 512:(cc + 1) * 512],
                                                                  start=(j == 0), stop=(j == 7)), r=[hT, wq], w=[pqp[cc]])
                        op("act", lambda e, cc=cc: e.activation(qp[:, cc * 512:(cc + 1) * 512], pqp[cc][:], AF.Copy), r=[pqp[cc]], w=[qp])
                    for hh in range(8):
                        op("pe", lambda e, hh=hh: e.transpose(pT2[:, hh, :], qp[:, hh * P:(hh + 1) * P], ident_f[:]), r=[qp, ident_f], w=[pT2])
                    op("act", lambda e: e.activation(qpT[:], pT2[:], AF.Copy), r=[pT2], w=[qpT])
                    for hh in range(8):
                        pv = psc[hh // 2][:].rearrange("p (a k) -> p a k", k=P)
                        op("pe", lambda e, hh=hh, pv=pv: e.matmul(pv[:, (hh % 2) * 2:(hh % 2) * 2 + 2, :], lhsT=qpT[:, hh, :],
                                                                 rhs=keysTz[:, hh, :, :], start=True, stop=True),
                           r=[qpT, keysTz], w=[psc[hh // 2]])
                    for q in range(4):
                        op("act", lambda e, q=q: e.activation(sc[:, 4 * q:4 * q + 4, :], psc[q][:].rearrange("p (a k) -> p a k", k=P), AF.Copy),
                           r=[psc[q]], w=[sc])
                    for g in range(16):
                        op("dve", lambda e, g=g: e.max(out=topv[:, g, 0:8], in_=sc[:, g, :]), r=[sc], w=[topv])
                        op("dve", lambda e, g=g: e.max_index(out=idx[:, g, 0:8], in_max=topv[:, g, 0:8], in_values=sc[:, g, :]),
                           r=[sc, topv], w=[idx])
                        op("dve", lambda e, g=g: e.match_replace(out=wk[:, g, :], in_to_replace=topv[:, g, 0:8], in_values=sc[:, g, :],
                                                                 imm_value=-1e30), r=[sc, topv], w=[wk])
                        op("dve", lambda e, g=g: e.max(out=topv[:, g, 8:16], in_=wk[:, g, :]), r=[wk], w=[topv])
                        op("dve", lambda e, g=g: e.max_index(out=idx[:, g, 8:16], in_max=topv[:, g, 8:16], in_values=wk[:, g, :]),
                           r=[wk, topv], w=[idx])
                    op("dve", lambda e: e.tensor_copy(idxf[:], idx[:]), r=[idx], w=[idxf])
                    tv4 = topv[:].rearrange("p (h c) a -> p h c a", c=2)
                    if4 = idxf[:].rearrange("p (h c) a -> p h c a", c=2)
                    op("dve", lambda e: e.tensor_tensor(cand[:].rearrange("p h (a b) -> p h a b", a=16),
                                                        tv4[:, :, 0, :].unsqueeze(3).to_broadcast([P, 8, 16, 16]),
                                                        tv4[:, :, 1, :].unsqueeze(2).to_broadcast([P, 8, 16, 16]), op=ALU.add),
                       r=[topv], w=[cand])
                    for hh in range(8):
                        op("dve", lambda e, hh=hh: e.max(out=best[:, hh, 0:8], in_=cand[:, hh, :]), r=[cand], w=[best])
                        op("dve", lambda e, hh=hh: e.max_index(out=pos[:, hh, 0:8], in_max=best[:, hh, 0:8], in_values=cand[:, hh, :]),
                           r=[cand, best], w=[pos])
                        op("dve", lambda e, hh=hh: e.match_replace(out=wk2_ap[:, hh, :], in_to_replace=best[:, hh, 0:8],
                                                                   in_values=cand[:, hh, :], imm_value=-1e30), r=[cand, best], w=[sc])
                        op("dve", lambda e, hh=hh: e.max(out=best[:, hh, 8:16], in_=wk2_ap[:, hh, :]), r=[sc], w=[best])
                        op("dve", lambda e, hh=hh: e.max_index(out=pos[:, hh, 8:16], in_max=best[:, hh, 8:16], in_values=wk2_ap[:, hh, :]),
                           r=[sc, best], w=[pos])
                    posf = pos[:].rearrange("p h s -> p (h s)")
                    op("dve", lambda e: e.tensor_single_scalar(pab[:, 0, :], posf, 4, op=ALU.logical_shift_right), r=[pos], w=[pab])
                    op("dve", lambda e: e.tensor_single_scalar(pab[:, 1, :], posf, 15, op=ALU.bitwise_and), r=[pos], w=[pab])
                    op("dve", lambda e: e.tensor_copy(abf[:], pab[:]), r=[pab], w=[abf])
                    for c in range(2):
                        op("dve", lambda e, c=c: e.tensor_tensor(eq_ap, iota16[:].unsqueeze(1).to_broadcast([P, 128, 16]), abf[:, c, :].unsqueeze(2).to_broadcast([P, 128, 16]),
                                                                 op=ALU.is_equal), r=[iota16, abf], w=[wk])
                        op("dve", lambda e, c=c: e.tensor_tensor(eq_ap.rearrange("p (h s) a -> p h s a", h=8),
                                                                 eq_ap.rearrange("p (h s) a -> p h s a", h=8),
                                                                 if4[:, :, c, :].unsqueeze(2).to_broadcast([P, 8, 16, 16]), op=ALU.mult),
                           r=[wk, idxf], w=[wk])
                        op("dve", lambda e, c=c: e.tensor_reduce(out=isel[:, c, :], in_=eq_ap, axis=AX.X, op=ALU.add), r=[wk], w=[isel])
                    op("dve", lambda e: e.scalar_tensor_tensor(out=efk[:], in0=isel[:, 0, :], scalar=128.0, in1=isel[:, 1, :],
                                                               op0=ALU.mult, op1=ALU.add), r=[isel], w=[efk])
                    op("dve", lambda e: e.tensor_scalar(efk[:], efk[:], float(l * NPEER), None, op0=ALU.add), r=[efk], w=[efk])
                    op("dve", lambda e: e.tensor_copy(eik[:], efk[:]), r=[efk], w=[eik])
                    op("dve", lambda e: e.tensor_tensor(ew[:].rearrange("p (h s) -> p h s", h=8), best[:],
                                                        best[:, :, 0:1].to_broadcast([P, 8, 16]), op=ALU.subtract), r=[best], w=[ew])
                    op("act", lambda e: e.activation(ew[:], ew[:], AF.Exp), r=[ew], w=[ew])
                    op("dve", lambda e: e.tensor_reduce(out=se[:], in_=ew[:].rearrange("p (h s) -> p h s", h=8), axis=AX.X, op=ALU.add),
                       r=[ew], w=[se])
                    op("dve", lambda e: e.reciprocal(se[:], se[:]), r=[se], w=[se])
                    op("dve", lambda e: e.tensor_tensor(wgk[:].rearrange("p (h s) -> p h s", h=8), ew[:].rearrange("p (h s) -> p h s", h=8),
                                                        se[:].unsqueeze(2).to_broadcast([P, 8, 16]), op=ALU.mult), r=[ew, se], w=[wgk])
                    return rq

                def emit(rq, n):
                    for _ in range(n):
                        if not rq:
                            return
                        t = rq.pop(0)
                        if t[0] == "op":
                            S.op(t[1], t[2], t[3], t[4])
                        else:
                            S.dma(t[1], t[2], t[3], t[4], t[5])

                rq = routing(0)
                emit(rq, len(rq))
                for n_, i in enumerate(tiles):
                    k = n_ % 2
                    hbk, efk, eik, wgk = hb[k], ef[k], eidx[k], wgt[k]
                    rq = routing(n_ + 1) if n_ + 1 < len(tiles) else []
                    per_slot = -(-len(rq) // 112)
                    for g_ in range(128 // GRP):
                        pb_ = pre_b[g_ % 4]
                        pd_ = pre_d[g_ % 4]
                        for s_ in range(g_ * GRP, (g_ + 1) * GRP):
                            b_ = gi[0] % NSLOT
                            gi[0] += 1
                            S.dma("pool", lambda e, s_=s_, b_=b_: e.indirect_dma_start(
                                out=gbuf[b_][:], out_offset=None, in_=uv_d,
                                in_offset=bass.IndirectOffsetOnAxis(ap=eik[:, s_:s_ + 1], axis=0)), gslot[b_], r=[eik], w=[gbuf[b_]])
                            if False:
                                S.op("dve", lambda e, s_=s_, b_=b_: e.scalar_tensor_tensor(
                                    out=junk[:], in0=hbk[:], scalar=1.0, in1=gbuf[b_][:, 0:D], op0=ALU.mult, op1=ALU.mult,
                                    accum_out=pre[:, s_:s_ + 1]), r=[hbk, gbuf[b_]], w=[junk, pd_])
                            else:
                                pr_ = prod[pi[0] % len(prod)]
                                pi[0] += 1
                                S.op("dve", lambda e, b_=b_, pr_=pr_: e.tensor_tensor(pr_[:], hbk[:], gbuf[b_][:, 0:D], op=ALU.mult),
                                     r=[hbk, gbuf[b_]], w=[pr_])
                                S.op("act", lambda e, s_=s_, pr_=pr_: e.activation(junkr[:], pr_[:], AF.Identity, accum_out=pre[:, s_:s_ + 1]),
                                     r=[pr_], w=[junkr, pb_])
                            emit(rq, per_slot)
                        gs = slice(g_ * GRP, (g_ + 1) * GRP)
                        S.op("act", lambda e, gs=gs: e.activation(aw[:, gs], pre[:, gs], AF.Gelu), r=[pb_, pd_], w=[pb_])
                        for s_ in range(g_ * GRP, (g_ + 1) * GRP):
                            b_ = (gi[0] - (g_ + 1) * GRP + s_) % NSLOT
                            dg = diag[di[0] % len(diag)]
                            di[0] += 1
                            S.op("dve", lambda e, s_=s_, dg=dg: e.tensor_scalar(dg[:], ident_b[:], aw[:, s_:s_ + 1], wgk[:, s_:s_ + 1],
                                                                               op0=ALU.mult, op1=ALU.mult),
                                 r=[ident_b, pb_, wgk], w=[dg])
                            for cc in range(2):
                                S.op("pe", lambda e, s_=s_, b_=b_, dg=dg, cc=cc: e.matmul(
                                    pva[cc][:], lhsT=dg[:], rhs=gbuf[b_][:, D + cc * 512:D + (cc + 1) * 512],
                                    start=(s_ == 0), stop=(s_ == 127)), r=[dg, gbuf[b_]], w=[pva[cc]])
                    if l == 0:
                        dbg_store("d_pre", slice(i * P, (i + 1) * P), pre[:], pre_b + pre_d)
                        dbg_store("d_eidx", slice(i * P, (i + 1) * P), efk[:], [efk])
                        dbg_store("d_w", slice(i * P, (i + 1) * P), wgk[:], [wgk])
                    for cc in range(2):
                        sl = slice(cc * 512, (cc + 1) * 512)
                        S.op("dve", lambda e, cc=cc, sl=sl: e.tensor_tensor(x2[k][:, sl], pva[cc][:], modbc[:, 3072 + cc * 512:3072 + (cc + 1) * 512],
                                                                           op=ALU.mult), r=[pva[cc], modbc], w=[x2[k]])
                    S.op("dve", lambda e: e.tensor_tensor(x2[k][:], x2[k][:], xt[k][:], op=ALU.add), r=[x2[k], xt[k]], w=[x2[k]])
                    if l == 0:
                        dbg_store("d_x2", slice(i * P, (i + 1) * P), x2[k][:], [x2[k]])
                    if last:
                        S.op("act", lambda e: e.activation(junk[:], x2[k][:], AF.Square, accum_out=ssf[:]), r=[x2[k]], w=[junk, ssf])
                        rstd_from_ss(ssf, rstdf, 1.0 / D, eps6)
                        S.op("dve", lambda e: e.scalar_tensor_tensor(out=x2[k][:], in0=x2[k][:], scalar=rstdf[:, 0:1], in1=fg_bc[:],
                                                                     op0=ALU.mult, op1=ALU.mult), r=[x2[k], rstdf, fg_bc], w=[x2[k]])
                        S.dma("sp", lambda e: e.dma_start(out=out_d[i * P:(i + 1) * P, :], in_=x2[k][:]), x2_slot[k], r=[x2[k]])
                    else:
                        S.dma("sp", lambda e: e.dma_start(out=xs_d[i * P:(i + 1) * P, :], in_=x2[k][:]), x2_slot[k], r=[x2[k]], w=[xs_buf[i]])
                    emit(rq, len(rq))
                S.barrier()

        lat_tiles = list(range(NTL))
        ctx_tiles = list(range(NTL, NT))
        if stop in (None, "p1"):
            convert_tables()
        for l in range(depth):
            last = (l == DEPTH - 1)
            if l == 0:
                src_aps = [x_d[i * P:(i + 1) * P, :] for i in range(NTL)] + [ctx_d[i * P:(i + 1) * P, :] for i in range(NTC)]
            else:
                src_aps = [xs_d[i * P:(i + 1) * P, :] for i in range(NT)]
            mod_cols(l)
            if l == 0:
                dbg_store("d_mod", (slice(0, P), slice(0, 32)), modcol[:].rearrange("p r c -> p (r c)"), [modcol])
                dbg_store("d_mod", (slice(0, P), slice(32, 48)), scT[:].rearrange("p j r -> p (j r)"), [scT])
            if stop == "m":
                break
            with ExitStack() as mix:
                M = {}
                M["qT"] = sbt(mix, "qT", [P, 4, S_LAT], BF16)
                M["qT_b"] = [[Buf(f"qT{pr}_{c}") for c in range(8)] for pr in range(4)]
                M["qTc"] = sbt(mix, "qTc", [P, 4, S_CTX], BF16)
                M["gT"] = sbt(mix, "gT", [P, 4, S_LAT + 2 * GPAD], BF16)
                M["gT_b"] = [Buf(f"gT{i}") for i in range(NTL)]
                M["gTc"] = sbt(mix, "gTc", [P, 4, S_CTX + 2 * GPAD], BF16)
                M["gTc_b"] = [Buf(f"gTc{i}") for i in range(NTC)]
                S.op("pool", lambda e: e.memset(M["gT"][:], 0.0), w=M["gT_b"])
                S.op("pool", lambda e: e.memset(M["gTc"][:], 0.0), w=M["gTc_b"])
                with ExitStack() as ab:
                    M["kT"] = sbt(ab, "kT", [P, NT * P], BF16)
                    M["kT_b"] = [Buf(f"kT{i}") for i in range(NT)]
                    M["Vp"] = sbt(ab, "Vp", [P, NT, 192], BF16)
                    M["Vp_b"] = [Buf(f"Vp{i}") for i in range(NT)]
                    S.op("pool", lambda e: e.memset(M["Vp"][:], 1.0), w=M["Vp_b"])
                    phase_a(l, M, src_aps, last)
                    if stop in ("a", "a0", "a1"):
                        break
                    phase_b(l, M, last)
                if stop == "b":
                    break
                with ExitStack() as cs:
                    modbc = sbt(cs, "modbcC", [P, 4096], F32)
                    mod_bc(l, 0, modbc)
                    phase_c(l, M, lat_tiles, src_aps, 0, modbc)
                    if not last:
                        mod_bc(l, 1, modbc)
                        phase_c(l, M, ctx_tiles, src_aps, 1, modbc)
            if stop == "c":
                break
            with ExitStack() as pp:
                modbc = sbt(pp, "modbcP", [P, 4096], F32)
                mod_bc(l, 0, modbc)
                phase_p(l, lat_tiles if stop not in ("p1", "p0") else lat_tiles[:2], modbc, last)
                if not last and stop not in ("p1", "p0"):
                    mod_bc(l, 1, modbc)
                    phase_p(l, ctx_tiles, modbc, last)
            if stop in ("p1", "p0"):
                break
        out_slot.close()
        if out_slot.sem is not None:
            for en in ("sp", "act", "pool"):
                S.E[en].eng.wait_ge(out_slot.sem, out_slot.val)
        S.barrier()
        print(f"[build] instr={S.ninstr} waits={S.nwait} sems={S.nsem}")
    return nc


_NC_CACHE = {}


def _rope_tables():
    n = 16
    inv = (10000.0 ** (-np.arange(n, dtype=np.float32) / n)).astype(np.float32)
    row = np.repeat(np.arange(64), 64).astype(np.float32)
    col = np.tile(np.arange(64), 64).astype(np.float32)
    ang = np.concatenate([row[:, None] * inv, col[:, None] * inv], axis=-1).astype(np.float32)
    cos = np.cos(ang).astype(np.float32)
    sin = np.sin(ang).astype(np.float32)
    cos2 = np.concatenate([cos, cos], axis=-1)
    sin2 = np.concatenate([-sin, sin], axis=-1)
    return np.ascontiguousarray(cos2), np.ascontiguousarray(sin2)


def make_in_maps(inputs, cores):
    f = lambda a: np.ascontiguousarray(np.asarray(a, dtype=np.float32))
    cos2, sin2 = _rope_tables()
    shared = {k: f(inputs[k]) for k in ["w_mod", "b_mod", "w_in", "q_gain", "k_gain", "conv_w", "conv_b", "ln_g", "ln_b",
                                        "beta_attn", "beta_conv", "w_o", "peer_wq", "peer_keys", "peer_u", "peer_v"]}
    shared["final_g"] = f(inputs["final_g"]).reshape(1, D)
    shared["cos2"] = cos2
    shared["sin2"] = sin2
    shared["cctxT"] = np.ascontiguousarray(f(inputs["c_ctx"]).reshape(8, P).T)
    x = f(inputs["x"])
    ctx = f(inputs["ctx"])
    c = f(inputs["c"])
    maps = []
    for b in cores:
        m = dict(shared)
        m["x"] = x[b]
        m["ctx"] = ctx[b]
        m["cT"] = np.ascontiguousarray(c[b].reshape(8, P).T)
        maps.append(m)
    return maps


def kernel(**inputs):
    if "nc" not in _NC_CACHE:
        _NC_CACHE["nc"] = build()
    nc = _NC_CACHE["nc"]
    B = np.asarray(inputs["x"]).shape[0]
    maps = make_in_maps(inputs, list(range(B)))
    res = run_bass_kernel_spmd(nc, maps, core_ids=list(range(B)))
    out = np.stack([np.asarray(r["out"], dtype=np.float32) for r in res.results], axis=0)
    return out
```

```python
import numpy as np
from contextlib import ExitStack
import concourse.bass as bass
import concourse.mybir as mybir
from concourse.bass_utils import run_bass_kernel_spmd

F32 = mybir.dt.float32
BF16 = mybir.dt.bfloat16
I32 = mybir.dt.int32
U32 = mybir.dt.uint32
ALU = mybir.AluOpType
AF = mybir.ActivationFunctionType
AX = mybir.AxisListType

import os
CUT = int(os.environ.get("KCUT", "99"))
SEM_LIMIT = 30000


class Buf:
    __slots__ = ("name", "w", "r")

    def __init__(self, name):
        self.name = name
        self.w = None
        self.r = {}


class Eng:
    def __init__(self, sync, name, eng):
        self.sync = sync
        self.name = name
        self.eng = eng
        self.sem = None
        self.count = 0
        self.known = {}
        self.last = None

    def new_event(self):
        if self.sem is None or self.count >= SEM_LIMIT:
            self.sem = self.sync.new_sem(self.name)
            self.count = 0
        self.count += 1
        self.last = [self.sem, self.count, self.name]
        return self.last


def DmaSlot(sync, name, group=False):
    if name not in sync.slot_by_name:
        sync.slot_by_name[name] = _DmaSlot(sync, name, group)
    return sync.slot_by_name[name]


class _DmaSlot:
    def __init__(self, sync, name, group=False):
        self.name = name
        self.sem = None
        self.val = 0
        self.group = group
        self.pending = []
        sync.slots.append(self)

    def bump(self, sync):
        if self.sem is None or (self.val >= SEM_LIMIT and not self.pending):
            self.sem = sync.new_sem("d" + self.name)
            self.val = 0
        self.val += 16
        ev = [self.sem, self.val, "dma"]
        if self.group:
            self.pending.append(ev)
        return ev

    def close(self):
        for ev in self.pending:
            ev[1] = self.val
        self.pending = []


class Sync:
    def __init__(self, nc, stack):
        self.nc = nc
        self.stack = stack
        self.nsem = 0
        self.slots = []
        self.slot_by_name = {}
        self.E = {
            "pe": Eng(self, "pe", nc.tensor),
            "dve": Eng(self, "dve", nc.vector),
            "act": Eng(self, "act", nc.scalar),
            "pool": Eng(self, "pool", nc.gpsimd),
            "sp": Eng(self, "sp", nc.sync),
        }
        self.ninstr = 0
        self.nwait = 0

    def new_sem(self, name):
        self.nsem += 1
        return self.stack.enter_context(self.nc.semaphore(f"s{self.nsem}_{name}"))

    def _wait(self, E, evs):
        best = {}
        for ev in evs:
            if ev is None:
                continue
            if E.name == "pe" and ev[2] == "pe":
                continue
            k = id(ev[0])
            if k not in best or best[k][1] < ev[1]:
                best[k] = ev
        for ev in best.values():
            sem, val, src = ev
            if E.known.get(id(sem), 0) >= val:
                continue
            E.eng.wait_ge(sem, val)
            self.nwait += 1
            E.known[id(sem)] = val

    def _deps(self, E, reads, writes):
        evs = []
        for b in reads:
            evs.append(b.w)
        for b in writes:
            if b.w is not None and b.w[2] != E.name:
                evs.append(b.w)
            for ev in b.r.values():
                if ev[2] == E.name:
                    continue
                evs.append(ev)
        return evs

    @staticmethod
    def _bufs(lst):
        return [t.b if hasattr(t, "b") else t for t in lst]

    def op(self, engname, fn, r=(), w=()):
        E = self.E[engname]
        r = self._bufs(r)
        w = self._bufs(w)
        self._wait(E, self._deps(E, r, w))
        ins = fn(E.eng)
        ev = E.new_event()
        ins.then_inc(ev[0], 1)
        self.ninstr += 1
        key = ev[2] if ev[2] != "dma" else id(ev[0])
        for b in r:
            b.r[key] = ev
        for b in w:
            b.w = ev
            b.r = {}
        return ev

    def dma(self, qname, fn, slot, r=(), w=()):
        E = self.E[qname]
        r = self._bufs(r)
        w = self._bufs(w)
        self._wait(E, self._deps(E, r, w))
        ins = fn(E.eng)
        ev = slot.bump(self)
        ins.then_inc(ev[0], 16)
        self.ninstr += 1
        key = ev[2] if ev[2] != "dma" else id(ev[0])
        for b in r:
            b.r[key] = ev
        for b in w:
            b.w = ev
            b.r = {}
        return ev

    def barrier(self):
        for s in self.slots:
            if s.group:
                s.close()
        evs = []
        for E in self.E.values():
            if E.last is not None:
                evs.append(E.last)
        for s in self.slots:
            if s.sem is not None:
                evs.append([s.sem, s.val, "dma"])
        for E in self.E.values():
            self._wait(E, [ev for ev in evs if not (E.name == "pe" and ev[2] == "pe")])


class Tile:
    def __init__(self, h, name):
        self.h = h
        self.b = Buf(name)

    def __getitem__(self, k):
        return self.h[k]


P = 128
D = 1024
S_LAT = 4096
S_CTX = 256
NTL = 32
NTC = 2
NT = NTL + NTC
DEPTH = 2
INC = 1792
EPS = 1e-6
NPEER = 16384
GPAD = 16
NSLOT = 17
GRP = 4


def build(depth=DEPTH, dbg=False, stop=None):
    nc = bass.Bass("TRN2", target_bir_lowering=False)

    def din(name, shape, dt=F32):
        return nc.dram_tensor(name, shape, dt, kind="ExternalInput").ap()

    x_d = din("x", [S_LAT, D])
    ctx_d = din("ctx", [S_CTX, D])
    cT_d = din("cT", [P, 8])
    cctxT_d = din("cctxT", [P, 8])
    w_mod_d = din("w_mod", [DEPTH, D, 6 * D])
    b_mod_d = din("b_mod", [DEPTH, 6 * D])
    w_in_d = din("w_in", [DEPTH, D, INC])
    q_gain_d = din("q_gain", [DEPTH, 64])
    k_gain_d = din("k_gain", [DEPTH, 64])
    conv_w_d = din("conv_w", [DEPTH, 31, 512])
    conv_b_d = din("conv_b", [DEPTH, 512])
    ln_g_d = din("ln_g", [DEPTH, 512])
    ln_b_d = din("ln_b", [DEPTH, 512])
    beta_attn_d = din("beta_attn", [DEPTH, 512])
    beta_conv_d = din("beta_conv", [DEPTH, 512])
    w_o_d = din("w_o", [DEPTH, D, D])
    peer_wq_d = din("peer_wq", [DEPTH, D, D])
    peer_keys_d = din("peer_keys", [DEPTH, 8, 2, 128, 64])
    peer_u_d = din("peer_u", [DEPTH, NPEER, D])
    peer_v_d = din("peer_v", [DEPTH, NPEER, D])
    final_g_d = din("final_g", [1, D])
    cos2_d = din("cos2", [S_LAT, 64])
    sin2_d = din("sin2", [S_LAT, 64])
    peer_u_flat = peer_u_d.rearrange("l n d -> (l n) d")
    peer_v_flat = peer_v_d.rearrange("l n d -> (l n) d")
    out_d = nc.dram_tensor("out", [S_LAT, D], F32, kind="ExternalOutput").ap()
    xs_d = nc.dram_tensor("xs", [S_LAT + S_CTX, D], F32).ap()
    uv_d = nc.dram_tensor("uv", [DEPTH * NPEER, 2 * D], BF16).ap()
    dbg_d = {}
    if dbg:
        for nm, shp in [("d_qkvu", [NT * P, INC]), ("d_x1", [NT * P, D]), ("d_oT", [P, 4 * S_LAT]),
                        ("d_pre", [NT * P, 128]), ("d_eidx", [NT * P, 128]), ("d_w", [NT * P, 128]),
                        ("d_qT", [P, 4 * S_LAT]), ("d_x2", [NT * P, D]), ("d_mod", [P, 4096])]:
            dbg_d[nm] = nc.dram_tensor(nm, shp, F32, kind="ExternalOutput").ap()

    top = ExitStack()
    with top:
        S = Sync(nc, top)

        uid = [0]

        def sbt(stack, name, shape, dt):
            uid[0] += 1
            name = f"{name}_u{uid[0]}"
            return Tile(stack.enter_context(nc.sbuf_tensor(name, shape, dt)), name)

        def pst(stack, name, shape, dt):
            uid[0] += 1
            name = f"{name}_u{uid[0]}"
            return Tile(stack.enter_context(nc.psum_tensor(name, shape, dt)), name)

        out_slot = DmaSlot(S, "out", group=True)
        dbg_slots = {}
        xs_buf = [Buf(f"xs{i}") for i in range(NT)]

        ident_f = sbt(top, "ident_f", [P, P], F32)
        ident_b = sbt(top, "ident_b", [P, P], BF16)
        ones_f = sbt(top, "ones_f", [P, P], F32)
        io_t = sbt(top, "io_t", [P, P], F32)
        pid_t = sbt(top, "pid_t", [P, 1], F32)
        S.op("pool", lambda e: e.iota(io_t[:], pattern=[[1, P]], base=0, channel_multiplier=0,
                                      allow_small_or_imprecise_dtypes=True), w=[io_t])
        S.op("pool", lambda e: e.iota(pid_t[:], pattern=[[0, 1]], base=0, channel_multiplier=1,
                                      allow_small_or_imprecise_dtypes=True), w=[pid_t])
        S.op("dve", lambda e: e.tensor_scalar(ident_f[:], io_t[:], pid_t[:, 0:1], None, op0=ALU.is_equal),
             r=[io_t, pid_t], w=[ident_f])
        S.op("dve", lambda e: e.tensor_copy(ident_b[:], ident_f[:]), r=[ident_f], w=[ident_b])
        S.op("pool", lambda e: e.memset(ones_f[:], 1.0), w=[ones_f])

        cslot = DmaSlot(S, "const", group=True)
        craw = sbt(top, "craw", [P, 2, 8], F32)
        scT = sbt(top, "scT", [P, 8, 2], F32)
        S.dma("sp", lambda e: e.dma_start(out=craw[:, 0, :], in_=cT_d), cslot, w=[craw])
        S.dma("sp", lambda e: e.dma_start(out=craw[:, 1, :], in_=cctxT_d), cslot, w=[craw])
        cslot.close()
        S.op("act", lambda e: e.activation(scT[:].rearrange("p j r -> p r j"), craw[:], AF.Silu), r=[craw], w=[scT])
        modcol = sbt(top, "modcol", [P, 2, 16], F32)
        eps6 = sbt(top, "eps6", [P, 1], F32)
        eps5 = sbt(top, "eps5", [P, 1], F32)
        S.op("pool", lambda e: e.memset(eps6[:], 1e-6), w=[eps6])
        S.op("pool", lambda e: e.memset(eps5[:], 1e-5), w=[eps5])


        def rstd_from_ss(ss, rs, scale, eps_t):
            S.op("act", lambda e: e.activation(rs[:], ss[:], AF.Sqrt, scale=scale, bias=eps_t[:, 0:1]), r=[ss, eps_t], w=[rs])
            S.op("dve", lambda e: e.reciprocal(rs[:], rs[:]), r=[rs], w=[rs])

        def dbg_store(name, rows, tile_ap, rtiles):
            if dbg and name in dbg_d:
                if name not in dbg_slots:
                    dbg_slots[name] = (DmaSlot(S, name), Buf(name))
                S.dma("sp", lambda e: e.dma_start(out=dbg_d[name][rows], in_=tile_ap), dbg_slots[name][0], r=rtiles, w=[dbg_slots[name][1]])


        def convert_tables():
            with ExitStack() as ph:
                NB = 4
                stg = [sbt(ph, f"cvi{k}", [P, 4, D], F32) for k in range(NB)]
                obf = [sbt(ph, f"cvo{k}", [P, 4, D], BF16) for k in range(NB)]
                islot = [DmaSlot(S, f"cvi{k}") for k in range(NB)]
                oslot = [DmaSlot(S, f"cvo{k}") for k in range(NB)]
                n = 0
                for l in range(DEPTH):
                    for tab, col0 in ((peer_u_d, 0), (peer_v_d, D)):
                        for t in range(NPEER // 512):
                            k = n % NB
                            src = tab[l, t * 512:(t + 1) * 512, :].rearrange("(p r) d -> p r d", r=4)
                            dst = uv_d[l * NPEER + t * 512:l * NPEER + (t + 1) * 512, col0:col0 + D].rearrange("(p r) d -> p r d", r=4)
                            S.dma("sp", lambda e, k=k, src=src: e.dma_start(out=stg[k][:], in_=src), islot[k], w=[stg[k]])
                            if n % 2 == 0:
                                S.op("act", lambda e, k=k: e.activation(obf[k][:], stg[k][:], AF.Copy), r=[stg[k]], w=[obf[k]])
                            else:
                                S.op("dve", lambda e, k=k: e.tensor_copy(obf[k][:], stg[k][:]), r=[stg[k]], w=[obf[k]])
                            S.dma("pool", lambda e, k=k, dst=dst: e.dma_start(out=dst, in_=obf[k][:]), oslot[k], r=[obf[k]])
                            n += 1
                S.barrier()

        def mod_cols(l):
            with ExitStack() as ph:
                wm = [sbt(ph, f"wm{k}", [P, 8, P], F32) for k in range(2)]
                wslot = [DmaSlot(S, f"wm{k}") for k in range(2)]
                bcol = sbt(ph, "bcol", [P, 16], F32)
                pcol = pst(ph, "pcol", [P, 16, 2], F32)
                S.dma("sp", lambda e: e.dma_start(out=bcol[:], in_=b_mod_d[l, 0:2048].rearrange("(c p) -> p c", p=P),
                                                  allow_slow_non_contiguous=True), DmaSlot(S, "bcol"), w=[bcol])
                for cc in range(16):
                    t = wm[cc % 2]
                    S.dma("sp", lambda e, t=t, cc=cc: e.dma_start(
                        out=t[:], in_=w_mod_d[l, :, cc * P:(cc + 1) * P].rearrange("(j p) m -> p j m", p=P)),
                          wslot[cc % 2], w=[t])
                    for j in range(8):
                        S.op("pe", lambda e, t=t, j=j, cc=cc: e.matmul(pcol[:, cc, :], lhsT=t[:, j, :],
                                                                     rhs=scT[:, j, :], start=(j == 0), stop=(j == 7)),
                             r=[t, scT], w=[pcol])
                S.op("dve", lambda e: e.tensor_tensor(modcol[:].rearrange("p r c -> p c r"), pcol[:],
                                                      bcol[:].unsqueeze(2).to_broadcast([P, 16, 2]), op=ALU.add),
                     r=[pcol, bcol], w=[modcol])
                S.op("dve", lambda e: e.tensor_scalar(modcol[:, :, 8:16], modcol[:, :, 8:16], 1.0, None, op0=ALU.add),
                     r=[modcol], w=[modcol])
                S.barrier()

        def mod_bc(l, r, modbc):
            with ExitStack() as ph:
                NW = 4
                wm = [sbt(ph, f"wmb{k}", [P, 512], F32) for k in range(NW)]
                wslot = [DmaSlot(S, f"wmb{k}") for k in range(NW)]
                bbc = [sbt(ph, f"bbc{k}", [P, 512], F32) for k in range(2)]
                bslot = [DmaSlot(S, f"bbc{k}") for k in range(2)]
                screp = sbt(ph, "screp", [P, 8, P], F32)
                pb = pst(ph, "pbm", [P, 512], F32)
                S.op("dve", lambda e: e.tensor_copy(screp[:], scT[:, :, r].unsqueeze(2).to_broadcast([P, 8, P])),
                     r=[scT], w=[screp])
                n = 0
                for cc in range(8):
                    c0 = 2048 + cc * 512
                    S.dma("sp", lambda e, cc=cc, c0=c0: e.dma_start(out=bbc[cc % 2][:], in_=b_mod_d[l, c0:c0 + 512].partition_broadcast(P)),
                          bslot[cc % 2], w=[bbc[cc % 2]])
                    for j in range(8):
                        t = wm[n % NW]
                        S.dma("sp", lambda e, t=t, j=j, c0=c0: e.dma_start(out=t[:], in_=w_mod_d[l, j * P:(j + 1) * P, c0:c0 + 512]),
                              wslot[n % NW], w=[t])
                        n += 1
                        S.op("pe", lambda e, t=t, j=j: e.matmul(pb[:], lhsT=screp[:, j, :], rhs=t[:], start=(j == 0), stop=(j == 7)),
                             r=[t, screp], w=[pb])
                    S.op("dve", lambda e, cc=cc: e.tensor_tensor(modbc[:, cc * 512:(cc + 1) * 512], pb[:], bbc[cc % 2][:], op=ALU.add),
                         r=[pb, bbc[cc % 2]], w=[modbc])
                S.op("dve", lambda e: e.tensor_scalar(modbc[:, 2048:3072], modbc[:, 2048:3072], 1.0, None, op0=ALU.add),
                     r=[modbc], w=[modbc])
                S.barrier()

        def phase_a(l, M, src_aps, last):
            with ExitStack() as ph:
                win = sbt(ph, "win", [P, 8, INC], BF16)
                stg = sbt(ph, "stg", [P, INC], F32)
                stg_slot = DmaSlot(S, "stg")
                bias_bc = sbt(ph, "bias_bc", [P, INC], F32)
                sh1rep = sbt(ph, "sh1rep", [P, 2, 8, P], BF16)
                gain = sbt(ph, "gain", [P, 640], F32)
                graw = sbt(ph, "graw", [P, 128], F32)
                cos2 = sbt(ph, "cos2", [P, NTL, 64], F32)
                sin2 = sbt(ph, "sin2", [P, NTL, 64], F32)
                xt = [sbt(ph, f"xt{k}", [P, D], F32) for k in range(2)]
                xt_slot = [DmaSlot(S, f"xt{k}") for k in range(2)]
                xb = [sbt(ph, f"xb{k}", [P, D], BF16) for k in range(2)]
                xT = [sbt(ph, f"xT{k}", [P, 8, P], BF16) for k in range(2)]
                junk = sbt(ph, "junkA", [P, D], BF16)
                ss = [sbt(ph, f"ssA{k}", [P, 1], F32) for k in range(2)]
                rstd = [sbt(ph, f"rstdA{k}", [P, 1], F32) for k in range(2)]
                qkvu = sbt(ph, "qkvu", [P, INC], F32)
                sq = sbt(ph, "sq", [P, 640], F32)
                ssq = sbt(ph, "ssq", [P, 10], F32)
                rsq = sbt(ph, "rsq", [P, 10], F32)
                qn = sbt(ph, "qn", [P, 640], F32)
                rb = sbt(ph, "rb", [P, 640], F32)
                qr = sbt(ph, "qr", [P, 4, 2, 64], BF16)
                kr = sbt(ph, "kr", [P, 128], BF16)
                sig = sbt(ph, "sig", [P, 512], F32)
                gg = sbt(ph, "gg", [P, 512], BF16)
                ptb = pst(ph, "ptb", [P, 8, P], BF16)
                pq = [pst(ph, f"pq{k}", [P, 512], F32) for k in range(4)]
                ptq = pst(ph, "ptq", [P, 5, P], BF16)
                ptg = pst(ph, "ptg", [P, 4, P], BF16)
                ra = sq
                qT, qTc, kT, Vp, gT, gTc = M["qT"], M["qTc"], M["kT"], M["Vp"], M["gT"], M["gTc"]

                cslot2 = DmaSlot(S, "rope", group=True)
                S.dma("sp", lambda e: e.dma_start(out=cos2[:], in_=cos2_d.rearrange("(n p) f -> p n f", p=P)), cslot2, w=[cos2])
                S.dma("sp", lambda e: e.dma_start(out=sin2[:], in_=sin2_d.rearrange("(n p) f -> p n f", p=P)), cslot2, w=[sin2])
                S.dma("sp", lambda e: e.dma_start(out=graw[:, 0:64], in_=q_gain_d[l].partition_broadcast(P)), cslot2, w=[graw])
                S.dma("sp", lambda e: e.dma_start(out=graw[:, 64:128], in_=k_gain_d[l].partition_broadcast(P)), cslot2, w=[graw])
                cslot2.close()
                for j in range(8):
                    S.dma("sp", lambda e, j=j: e.dma_start(out=stg[:], in_=w_in_d[l, j * P:(j + 1) * P, :]), stg_slot, w=[stg])
                    S.op("act", lambda e, j=j: e.activation(win[:, j, :], stg[:], AF.Copy), r=[stg], w=[win])
                S.op("dve", lambda e: e.tensor_copy(gain[:, 0:512].rearrange("p (h d) -> p h d", h=8),
                                                    graw[:, 0:64].unsqueeze(1).to_broadcast([P, 8, 64])), r=[graw], w=[gain])
                S.op("dve", lambda e: e.tensor_copy(gain[:, 512:640].rearrange("p (h d) -> p h d", h=2),
                                                    graw[:, 64:128].unsqueeze(1).to_broadcast([P, 2, 64])), r=[graw], w=[gain])
                S.op("dve", lambda e: e.tensor_copy(sh1rep[:], modcol[:, :, 0:8].unsqueeze(3).to_broadcast([P, 2, 8, P])),
                     r=[modcol], w=[sh1rep])

                def make_bias(r_):
                    for cc in range(4):
                        w_ = min(512, INC - cc * 512)
                        for j in range(8):
                            S.op("pe", lambda e, cc=cc, j=j, w_=w_: e.matmul(
                                pq[cc][:, 0:w_], lhsT=sh1rep[:, r_, j, :], rhs=win[:, j, cc * 512:cc * 512 + w_],
                                start=(j == 0), stop=(j == 7)), r=[sh1rep, win], w=[pq[cc]])
                        S.op("act", lambda e, cc=cc, w_=w_: e.activation(
                            bias_bc[:, cc * 512:cc * 512 + w_], pq[cc][:, 0:w_], AF.Copy), r=[pq[cc]], w=[bias_bc])

                def load(i):
                    k = i % 2
                    S.dma("sp", lambda e: e.dma_start(out=xt[k][:], in_=src_aps[i]), xt_slot[k], r=[xs_buf[i]], w=[xt[k]])

                make_bias(0)
                load(0)
                for i in range(NT if stop not in ("a0", "a1") else (0 if stop == "a0" else 1)):
                    k = i % 2
                    r_ = 0 if i < NTL else 1
                    is_ctx = i >= NTL
                    if i == NTL:
                        make_bias(1)
                    if i + 1 < NT:
                        load(i + 1)
                    S.op("act", lambda e: e.activation(junk[:], xt[k][:], AF.Square, accum_out=ss[k][:]), r=[xt[k]], w=[junk, ss[k]])
                    S.op("act", lambda e: e.activation(xb[k][:], xt[k][:], AF.Copy), r=[xt[k]], w=[xb[k]])
                    rstd_from_ss(ss[k], rstd[k], 1.0 / D, eps6)
                    for j in range(8):
                        S.op("pe", lambda e, j=j: e.transpose(ptb[:, j, :], xb[k][:, j * P:(j + 1) * P], ident_b[:]),
                             r=[xb[k], ident_b], w=[ptb])
                    S.op("dve", lambda e: e.tensor_tensor(xT[k][:], ptb[:],
                                                          modcol[:, r_, 8:16].unsqueeze(2).to_broadcast([P, 8, P]), op=ALU.mult),
                         r=[ptb, modcol], w=[xT[k]])
                    if CUT <= 1:
                        continue
                    for cc in range(4):
                        w_ = min(512, INC - cc * 512)
                        for j in range(8):
                            S.op("pe", lambda e, cc=cc, j=j, w_=w_: e.matmul(
                                pq[cc][:, 0:w_], lhsT=xT[k][:, j, :], rhs=win[:, j, cc * 512:cc * 512 + w_],
                                start=(j == 0), stop=(j == 7)), r=[xT[k], win], w=[pq[cc]])
                        S.op("dve", lambda e, cc=cc, w_=w_: e.scalar_tensor_tensor(
                            out=qkvu[:, cc * 512:cc * 512 + w_], in0=pq[cc][:, 0:w_], scalar=rstd[k][:, 0:1],
                            in1=bias_bc[:, cc * 512:cc * 512 + w_], op0=ALU.mult, op1=ALU.add),
                             r=[pq[cc], rstd[k], bias_bc], w=[qkvu])
                    if l == 0:
                        dbg_store("d_qkvu", slice(i * P, (i + 1) * P), qkvu[:], [qkvu])
                    if CUT <= 2:
                        continue
                    S.op("pool", lambda e: e.tensor_tensor(sq[:], qkvu[:, 0:640], qkvu[:, 0:640], op=ALU.mult), r=[qkvu], w=[sq])
                    S.op("dve", lambda e: e.tensor_reduce(out=ssq[:], in_=sq[:].rearrange("p (h d) -> p h d", h=10),
                                                          axis=AX.X, op=ALU.add), r=[sq], w=[ssq])
                    rstd_from_ss(ssq, rsq, 1.0 / 64, eps6)
                    S.op("dve", lambda e: e.tensor_tensor(qn[:].rearrange("p (h d) -> p h d", h=10),
                                                          qkvu[:, 0:640].rearrange("p (h d) -> p h d", h=10),
                                                          rsq[:].unsqueeze(2).to_broadcast([P, 10, 64]), op=ALU.mult),
                         r=[qkvu, rsq], w=[qn])
                    S.op("pool", lambda e: e.tensor_tensor(qn[:], qn[:], gain[:], op=ALU.mult), r=[qn, gain], w=[qn])
                    if CUT <= 3:
                        continue
                    if not is_ctx:
                        qn3 = qn[:].rearrange("p (h d) -> p h d", h=10)
                        rb3 = rb[:].rearrange("p (h d) -> p h d", h=10)
                        S.op("dve", lambda e: e.tensor_tensor(ra[:].rearrange("p (h d) -> p h d", h=10), qn3,
                                                              cos2[:, i, :].unsqueeze(1).to_broadcast([P, 10, 64]), op=ALU.mult),
                             r=[qn, cos2], w=[ra])
                        S.op("pool", lambda e: e.tensor_tensor(rb3[:, :, 0:32], qn3[:, :, 32:64],
                                                               sin2[:, i, 0:32].unsqueeze(1).to_broadcast([P, 10, 32]), op=ALU.mult),
                             r=[qn, sin2], w=[rb])
                        S.op("pool", lambda e: e.tensor_tensor(rb3[:, :, 32:64], qn3[:, :, 0:32],
                                                               sin2[:, i, 32:64].unsqueeze(1).to_broadcast([P, 10, 32]), op=ALU.mult),
                             r=[qn, sin2], w=[rb])
                        S.op("dve", lambda e: e.tensor_tensor(qr[:].rearrange("p pr hh d -> p hh pr d"),
                                                              ra[:, 0:512].rearrange("p (hh pr d) -> p hh pr d", hh=2, pr=4),
                                                              rb[:, 0:512].rearrange("p (hh pr d) -> p hh pr d", hh=2, pr=4), op=ALU.add),
                             r=[ra, rb], w=[qr])
                        S.op("pool", lambda e: e.tensor_tensor(kr[:], ra[:, 512:640], rb[:, 512:640], op=ALU.add), r=[ra, rb], w=[kr])
                    else:
                        S.op("dve", lambda e: e.tensor_copy(qr[:].rearrange("p pr hh d -> p hh pr d"),
                                                            qn[:, 0:512].rearrange("p (hh pr d) -> p hh pr d", hh=2, pr=4)),
                             r=[qn], w=[qr])
                        S.op("pool", lambda e: e.tensor_copy(kr[:], qn[:, 512:640]), r=[qn], w=[kr])
                    if CUT <= 4:
                        continue
                    need_q = (not is_ctx) or (not last)
                    if need_q:
                        for pr in range(4):
                            S.op("pe", lambda e, pr=pr: e.transpose(ptq[:, pr, :], qr[:, pr, :, :].rearrange("p hh d -> p (hh d)"), ident_b[:]),
                                 r=[qr, ident_b], w=[ptq])
                    S.op("pe", lambda e: e.transpose(ptq[:, 4, :], kr[:], ident_b[:]), r=[kr, ident_b], w=[ptq])
                    if need_q:
                        if not is_ctx:
                            S.op("act", lambda e: e.activation(qT[:, :, i * P:(i + 1) * P], ptq[:, 0:4, :], AF.Copy),
                                 r=[ptq], w=[M["qT_b"][pr][i // 4] for pr in range(4)])
                        else:
                            S.op("act", lambda e: e.activation(qTc[:, :, (i - NTL) * P:(i - NTL + 1) * P], ptq[:, 0:4, :], AF.Copy),
                                 r=[ptq], w=[qTc])
                    S.op("act", lambda e: e.activation(kT[:, i * P:(i + 1) * P], ptq[:, 4, :], AF.Copy), r=[ptq], w=[M["kT_b"][i]])
                    if CUT <= 5:
                        continue
                    S.op("pool", lambda e: e.tensor_copy(Vp[:, i, 0:64], qkvu[:, 640:704]), r=[qkvu], w=[M["Vp_b"][i]])
                    S.op("pool", lambda e: e.tensor_copy(Vp[:, i, 128:192], qkvu[:, 704:768]), r=[qkvu], w=[M["Vp_b"][i]])
                    if CUT <= 6:
                        continue
                    if need_q:
                        S.op("act", lambda e: e.activation(sig[:], qkvu[:, 1280:1792], AF.Sigmoid), r=[qkvu], w=[sig])
                        S.op("dve", lambda e: e.tensor_tensor(gg[:], qkvu[:, 768:1280], sig[:], op=ALU.mult), r=[qkvu, sig], w=[gg])
                        for c in range(4):
                            S.op("pe", lambda e, c=c: e.transpose(ptg[:, c, :], gg[:, c * P:(c + 1) * P], ident_b[:]),
                                 r=[gg, ident_b], w=[ptg])
                        if not is_ctx:
                            S.op("act", lambda e: e.activation(gT[:, :, GPAD + i * P:GPAD + (i + 1) * P], ptg[:], AF.Copy),
                                 r=[ptg], w=[M["gT_b"][i]])
                        else:
                            S.op("act", lambda e: e.activation(gTc[:, :, GPAD + (i - NTL) * P:GPAD + (i - NTL + 1) * P], ptg[:], AF.Copy),
                                 r=[ptg], w=[M["gTc_b"][i - NTL]])
                S.barrier()

        def phase_b(l, M, last):
            with ExitStack() as ph:
                NPT = 4
                NPS = 4
                LA = 1
                pT = [sbt(ph, f"pT{k}", [P, 512], BF16) for k in range(NPT)]
                rd = sbt(ph, "rd", [P, 512], F32)
                bcs = sbt(ph, "bcs", [P, 512], F32)
                ps_s = [pst(ph, f"ps_s{k}", [P, 512], F32) for k in range(NPS)]
                po = [pst(ph, f"po{k}", [P, 512], F32) for k in range(3)]
                pbc = pst(ph, "pbc", [P, 512], F32)
                qT, qTc, kT, Vp = M["qT"], M["qTc"], M["kT"], M["Vp"]
                cnt = [0]
                grp = [0]
                kTz = sbt(ph, "kTz", [P, 2, NT * P], BF16)
                S.op("pool", lambda e: e.memset(kTz[:], 0.0), w=[kTz])
                S.op("dve", lambda e: e.tensor_copy(kTz[0:64, 0, :], kT[0:64, :]), r=M["kT_b"], w=[kTz])
                S.op("act", lambda e: e.activation(kTz[64:128, 1, :], kT[64:128, :], AF.Copy), r=M["kT_b"], w=[kTz])

                def attend(qsrc_fn, qfull, qbuf, n, ktiles):
                    its = [(kt_i, kt, hh) for kt_i, kt in enumerate(ktiles) for hh in range(2)]
                    base = cnt[0]
                    cnt[0] += len(its)
                    pg = [po[(2 * grp[0]) % 3], po[(2 * grp[0] + 1) % 3]]
                    grp[0] += 1

                    def emit_s(m):
                        kt_i, kt, hh = its[m]
                        k = (base + m) % NPS
                        kp = (base + m) % NPT
                        S.op("pe", lambda e: e.matmul(ps_s[k][:, 0:n], lhsT=kTz[:, hh, kt * P:(kt + 1) * P], rhs=qfull,
                                                      start=True, stop=True), r=[kTz, qbuf], w=[ps_s[k]])
                        S.op("act", lambda e: e.activation(pT[kp][:, 0:n], ps_s[k][:, 0:n], AF.Exp, scale=0.125),
                             r=[ps_s[k]], w=[pT[kp]])

                    def emit_pv(m):
                        kt_i, kt, hh = its[m]
                        kp = (base + m) % NPT
                        S.op("pe", lambda e: e.matmul(pg[hh][:, 0:n], lhsT=Vp[:, kt, hh * 64:hh * 64 + 128], rhs=pT[kp][:, 0:n],
                                                      start=(kt_i == 0), stop=(kt_i == len(ktiles) - 1)), r=[M["Vp_b"][kt], pT[kp]], w=[pg[hh]])

                    npair = len(its) // 2
                    for j in range(npair + LA):
                        if j < npair:
                            emit_s(2 * j)
                            emit_s(2 * j + 1)
                        if j - LA >= 0:
                            emit_pv(2 * (j - LA))
                            emit_pv(2 * (j - LA) + 1)
                    for hh in range(2):
                        dp = 64 if hh == 0 else 0
                        S.op("dve", lambda e, hh=hh, dp=dp: e.reciprocal(rd[dp:dp + 1, 0:n], pg[hh][dp:dp + 1, 0:n]),
                             r=[pg[hh]], w=[rd])
                        S.op("pe", lambda e, dp=dp: e.matmul(pbc[0:64, 0:n], lhsT=ones_f[dp:dp + 1, 0:64], rhs=rd[dp:dp + 1, 0:n],
                                                            start=True, stop=True), r=[ones_f, rd], w=[pbc])
                        S.op("act", lambda e, hh=hh: e.activation(bcs[hh * 64:(hh + 1) * 64, 0:n], pbc[0:64, 0:n], AF.Copy),
                             r=[pbc], w=[bcs])
                        S.op("dve", lambda e, hh=hh: e.tensor_tensor(qsrc_fn(hh), pg[hh][hh * 64:(hh + 1) * 64, 0:n],
                                                                     bcs[hh * 64:(hh + 1) * 64, 0:n], op=ALU.mult),
                             r=[pg[hh], bcs], w=[qbuf])

                for c in range(8):
                    for pr in range(4):
                        attend(lambda hh, c=c, pr=pr: qT[hh * 64:(hh + 1) * 64, pr, c * 512:(c + 1) * 512],
                               qT[:, pr, c * 512:(c + 1) * 512], M["qT_b"][pr][c], 512, list(range(NT)))
                if not last:
                    for pr in range(4):
                        attend(lambda hh, pr=pr: qTc[hh * 64:(hh + 1) * 64, pr, :], qTc[:, pr, :], qTc.b, S_CTX, [NTL, NTL + 1])
                S.barrier()

        def phase_c(l, M, tiles, src_aps, r_, modbc):
            with ExitStack() as ph:
                wcol = sbt(ph, "wcol", [P, 4, 31], F32)
                DG = sbt(ph, "DG", [P, 4, 31, P], BF16)
                wo = sbt(ph, "wo", [P, 8, D], BF16)
                stg = [sbt(ph, f"stgC{k}", [P, D], F32) for k in range(2)]
                stg_slot = [DmaSlot(S, f"stgC{k}") for k in range(2)]
                betaA = sbt(ph, "betaA", [P, 8], F32)
                betac = sbt(ph, "betac", [P, 8], F32)
                cb_bc = sbt(ph, "cb_bc", [P, 512], F32)
                lg_bc = sbt(ph, "lg_bc", [P, 512], F32)
                lb_bc = sbt(ph, "lb_bc", [P, 512], F32)
                xt = [sbt(ph, f"xtC{k}", [P, D], F32) for k in range(2)]
                xt_slot = [DmaSlot(S, f"xtC{k}") for k in range(2)]
                y = sbt(ph, "yC", [P, 512], F32)
                junk = sbt(ph, "junkC", [P, 512], F32)
                st4 = sbt(ph, "st4", [P, 4], F32)
                rln = sbt(ph, "rln", [P, 1], F32)
                msq = sbt(ph, "msq", [P, 1], F32)
                z = sbt(ph, "zC", [P, 512], F32)
                oc = sbt(ph, "oc", [P, 512], F32)
                ocb = sbt(ph, "ocb", [P, 512], BF16)
                ocT = sbt(ph, "ocT", [P, 4, P], BF16)
                sqo = sbt(ph, "sqo", [P, 4, P], F32)
                ssc = sbt(ph, "ssc", [P, 2], F32)
                rsc = sbt(ph, "rsc", [P, 2], F32)
                t1 = sbt(ph, "t1", [P, D], F32)
                x1 = [sbt(ph, f"x1_{k}", [P, D], F32) for k in range(2)]
                x1_slot = [DmaSlot(S, f"x1s{k}") for k in range(2)]
                py = pst(ph, "py", [P, 512], F32)
                ptc = pst(ph, "ptc", [P, 4, P], BF16)
                pss = pst(ph, "pss", [P, 16], F32)
                pa = [pst(ph, f"pa{k}", [P, 512], F32) for k in range(2)]
                pc = [pst(ph, f"pc{k}", [P, 512], F32) for k in range(2)]
                qT, qTc, gT, gTc = M["qT"], M["qTc"], M["gT"], M["gTc"]

                vs = DmaSlot(S, "vecC", group=True)
                for c in range(4):
                    S.dma("sp", lambda e, c=c: e.dma_start(out=wcol[:, c, :], in_=conv_w_d[l, :, c * P:(c + 1) * P].rearrange("j p -> p j"),
                                                           allow_slow_non_contiguous=True), vs, w=[wcol])
                for half in range(2):
                    S.dma("sp", lambda e, half=half: e.dma_start(out=betaA[half * 64:(half + 1) * 64, :],
                                                                 in_=beta_attn_d[l].rearrange("(h d) -> d h", d=64),
                                                                 allow_slow_non_contiguous=True), vs, w=[betaA])
                S.dma("sp", lambda e: e.dma_start(out=betac[:, 4:8], in_=beta_conv_d[l].rearrange("(c p) -> p c", p=P),
                                                  allow_slow_non_contiguous=True), vs, w=[betac])
                S.dma("sp", lambda e: e.dma_start(out=cb_bc[:], in_=conv_b_d[l].partition_broadcast(P)), vs, w=[cb_bc])
                S.dma("sp", lambda e: e.dma_start(out=lg_bc[:], in_=ln_g_d[l].partition_broadcast(P)), vs, w=[lg_bc])
                S.dma("sp", lambda e: e.dma_start(out=lb_bc[:], in_=ln_b_d[l].partition_broadcast(P)), vs, w=[lb_bc])
                vs.close()
                S.op("dve", lambda e: e.tensor_copy(betac[0:64, 0:4], betaA[0:64, 0:4]), r=[betaA], w=[betac])
                S.op("dve", lambda e: e.tensor_copy(betac[64:128, 0:4], betaA[64:128, 4:8]), r=[betaA], w=[betac])
                for kk in range(8):
                    t = stg[kk % 2]
                    sl = stg_slot[kk % 2]
                    if kk < 4:
                        S.dma("sp", lambda e, t=t, kk=kk: e.dma_start(out=t[0:64, :], in_=w_o_d[l, kk * 64:(kk + 1) * 64, :]), sl, w=[t])
                        S.dma("sp", lambda e, t=t, kk=kk: e.dma_start(out=t[64:128, :], in_=w_o_d[l, (kk + 4) * 64:(kk + 5) * 64, :]), sl, w=[t])
                    else:
                        S.dma("sp", lambda e, t=t, kk=kk: e.dma_start(out=t[:], in_=w_o_d[l, 512 + (kk - 4) * P:512 + (kk - 3) * P, :]), sl, w=[t])
                    S.op("act", lambda e, t=t, kk=kk: e.activation(wo[:, kk, :], t[:], AF.Identity, scale=betac[:, kk:kk + 1]),
                         r=[t, betac], w=[wo])
                for c in range(4):
                    for j in range(31):
                        S.op("pool", lambda e, c=c, j=j: e.tensor_scalar(DG[:, c, j, :], ident_f[:], wcol[:, c, j:j + 1], None, op0=ALU.mult),
                             r=[ident_f, wcol], w=[DG])

                def load(n_):
                    i = tiles[n_]
                    k = n_ % 2
                    S.dma("sp", lambda e: e.dma_start(out=xt[k][:], in_=src_aps[i]), xt_slot[k], r=[xs_buf[i]], w=[xt[k]])

                load(0)
                for n_, i in enumerate(tiles):
                    k = n_ % 2
                    is_ctx = i >= NTL
                    if n_ + 1 < len(tiles):
                        load(n_ + 1)
                    if not is_ctx:
                        gsrc, gb, t0 = gT, M["gT_b"], i * P
                        lo, hi = max(0, i - 1), min(NTL - 1, i + 1)
                        osrc = lambda pr: qT[:, pr, i * P:(i + 1) * P]
                        obufs = [M["qT_b"][pr][i // 4] for pr in range(4)]
                    else:
                        gsrc, gb, t0 = gTc, M["gTc_b"], (i - NTL) * P
                        lo, hi = max(0, i - NTL - 1), min(NTC - 1, i - NTL + 1)
                        osrc = lambda pr: qTc[:, pr, (i - NTL) * P:(i - NTL + 1) * P]
                        obufs = [qTc.b]
                    for c in range(4):
                        for j in range(31):
                            S.op("pe", lambda e, c=c, j=j: e.matmul(py[:, c * P:(c + 1) * P], lhsT=gsrc[:, c, t0 + j + GPAD - 15:t0 + j + GPAD - 15 + P],
                                                                  rhs=DG[:, c, j, :], start=(j == 0), stop=(j == 30)),
                                 r=[gb[q] for q in range(lo, hi + 1)] + [DG], w=[py])
                    S.op("dve", lambda e: e.tensor_tensor(y[:], py[:], cb_bc[:], op=ALU.add), r=[py, cb_bc], w=[y])
                    S.op("act", lambda e: e.activation(junk[:], y[:], AF.Identity, accum_out=st4[:, 0:1]), r=[y], w=[junk, st4])
                    S.op("act", lambda e: e.activation(junk[:], y[:], AF.Square, accum_out=st4[:, 1:2]), r=[y], w=[junk, st4])
                    S.op("dve", lambda e: e.tensor_scalar(st4[:, 2:4], st4[:, 0:2], 1.0 / 512, None, op0=ALU.mult), r=[st4], w=[st4])
                    S.op("dve", lambda e: e.tensor_tensor(msq[:], st4[:, 2:3], st4[:, 2:3], op=ALU.mult), r=[st4], w=[msq])
                    S.op("dve", lambda e: e.tensor_tensor(msq[:], st4[:, 3:4], msq[:], op=ALU.subtract), r=[st4, msq], w=[msq])
                    rstd_from_ss(msq, rln, 1.0, eps5)
                    S.op("dve", lambda e: e.tensor_scalar(z[:], y[:], st4[:, 2:3], rln[:, 0:1], op0=ALU.subtract, op1=ALU.mult),
                         r=[y, st4, rln], w=[z])
                    S.op("pool", lambda e: e.tensor_tensor(z[:], z[:], lg_bc[:], op=ALU.mult), r=[z, lg_bc], w=[z])
                    S.op("pool", lambda e: e.tensor_tensor(z[:], z[:], lb_bc[:], op=ALU.add), r=[z, lb_bc], w=[z])
                    S.op("act", lambda e: e.activation(oc[:], z[:], AF.Silu), r=[z], w=[oc])
                    S.op("act", lambda e: e.activation(junk[:], oc[:], AF.Square, accum_out=ssc[:, 0:1]), r=[oc], w=[junk, ssc])
                    S.op("pool", lambda e: e.tensor_copy(ocb[:], oc[:]), r=[oc], w=[ocb])
                    for c in range(4):
                        S.op("pe", lambda e, c=c: e.transpose(ptc[:, c, :], ocb[:, c * P:(c + 1) * P], ident_b[:]),
                             r=[ocb, ident_b], w=[ptc])
                    S.op("act", lambda e: e.activation(ocT[:], ptc[:], AF.Copy), r=[ptc], w=[ocT])
                    for pr in range(4):
                        S.op("pool", lambda e, pr=pr: e.tensor_tensor(sqo[:, pr, :], osrc(pr), osrc(pr), op=ALU.mult), r=obufs, w=[sqo])
                    for pr in range(4):
                        S.op("pe", lambda e, pr=pr: e.matmul(pss[:, 0:2], lhsT=sqo[:, pr, :], rhs=ones_f[:, 0:2],
                                                            start=(pr == 0), stop=(pr == 3)), r=[sqo, ones_f], w=[pss])
                    S.op("dve", lambda e: e.tensor_copy(ssc[:, 1:2], pss[:, 0:1]), r=[pss], w=[ssc])
                    rstd_from_ss(ssc, rsc, 1.0 / 512, eps6)
                    for cc in range(2):
                        for pr in range(4):
                            S.op("pe", lambda e, cc=cc, pr=pr: e.matmul(pa[cc][:], lhsT=osrc(pr), rhs=wo[:, pr, cc * 512:(cc + 1) * 512],
                                                                      start=(pr == 0), stop=(pr == 3)), r=obufs + [wo], w=[pa[cc]])
                        for c in range(4):
                            S.op("pe", lambda e, cc=cc, c=c: e.matmul(pc[cc][:], lhsT=ocT[:, c, :], rhs=wo[:, 4 + c, cc * 512:(cc + 1) * 512],
                                                                    start=(c == 0), stop=(c == 3)), r=[ocT, wo], w=[pc[cc]])
                        sl = slice(cc * 512, (cc + 1) * 512)
                        S.op("dve", lambda e, cc=cc, sl=sl: e.tensor_scalar(t1[:, sl], pa[cc][:], rsc[:, 1:2], None, op0=ALU.mult),
                             r=[pa[cc], rsc], w=[t1])
                        S.op("dve", lambda e, cc=cc, sl=sl: e.scalar_tensor_tensor(out=t1[:, sl], in0=pc[cc][:], scalar=rsc[:, 0:1],
                                                                                   in1=t1[:, sl], op0=ALU.mult, op1=ALU.add),
                             r=[pc[cc], rsc, t1], w=[t1])
                    S.op("pool", lambda e: e.tensor_tensor(t1[:], t1[:], modbc[:, 0:1024], op=ALU.mult), r=[t1, modbc], w=[t1])
                    S.op("pool", lambda e: e.tensor_tensor(x1[k][:], t1[:], xt[k][:], op=ALU.add), r=[t1, xt[k]], w=[x1[k]])
                    S.dma("sp", lambda e: e.dma_start(out=xs_d[i * P:(i + 1) * P, :], in_=x1[k][:]), x1_slot[k], r=[x1[k]], w=[xs_buf[i]])
                    if l == 0:
                        dbg_store("d_x1", slice(i * P, (i + 1) * P), x1[k][:], [x1[k]])
                S.barrier()

        def phase_p(l, tiles, modbc, last):
            with ExitStack() as ph:
                wq = sbt(ph, "wq", [P, 8, D], F32)
                keysTz = sbt(ph, "keysTz", [P, 8, 2, P], F32)
                iota16 = sbt(ph, "iota16", [P, 16], F32)
                xt = [sbt(ph, f"xtP{k}", [P, D], F32) for k in range(2)]
                xt_slot = [DmaSlot(S, f"xtP{k}") for k in range(2)]
                junk = sbt(ph, "junkP", [P, D], BF16)
                junkr = sbt(ph, "junkR", [P, D], BF16)
                prod = [sbt(ph, f"prod{k}", [P, D], BF16) for k in range(3)]
                hb = [sbt(ph, f"hb{k}", [P, D], BF16) for k in range(2)]
                pi = [0]
                ss = sbt(ph, "ssP", [P, 1], F32)
                rstd = sbt(ph, "rstdP", [P, 1], F32)
                ssf = sbt(ph, "ssF", [P, 1], F32)
                rstdf = sbt(ph, "rstdF", [P, 1], F32)
                h = [sbt(ph, "hP", [P, D], F32)] * 2
                hT = sbt(ph, "hT", [P, 8, P], F32)
                qpT = hT
                qp = sbt(ph, "qp", [P, D], F32)
                sc = sbt(ph, "scP", [P, 16, 128], F32)
                wk = sbt(ph, "wkP", [P, 16, 128], F32)
                eq_ap = wk[:].rearrange("p a b -> p (a b)").rearrange("p (x c) -> p x c", c=16)
                topv = sbt(ph, "topv", [P, 16, 16], F32)
                idx = sbt(ph, "idxP", [P, 16, 16], U32)
                idxf = sbt(ph, "idxf", [P, 16, 16], F32)
                cand = sbt(ph, "cand", [P, 8, 256], F32)
                wk2_ap = sc[:].rearrange("p a b -> p (a b)").rearrange("p (h x) -> p h x", h=8)
                best = sbt(ph, "best", [P, 8, 16], F32)
                pos = sbt(ph, "pos", [P, 8, 16], U32)
                pab = sbt(ph, "pab", [P, 2, 128], U32)
                abf = sbt(ph, "abf", [P, 2, 128], F32)
                isel = sbt(ph, "isel", [P, 2, 128], F32)
                ef = [sbt(ph, f"ef{k}", [P, 128], F32) for k in range(2)]
                eidx = [sbt(ph, f"eidx{k}", [P, 128], I32) for k in range(2)]
                ew = sbt(ph, "ew", [P, 128], F32)
                se = sbt(ph, "se", [P, 8], F32)
                wgt = [sbt(ph, f"wgt{k}", [P, 128], F32) for k in range(2)]
                pre = sbt(ph, "pre", [P, 128], F32)
                aw = sbt(ph, "aw", [P, 128], F32)
                gbuf = [sbt(ph, f"gbuf{k}", [P, 2 * D], BF16) for k in range(NSLOT)]
                diag = [sbt(ph, f"diag{k}", [P, P], BF16) for k in range(6)]
                pre_b = [Buf(f"pre_b{k}") for k in range(4)]
                pre_d = [Buf(f"pre_d{k}") for k in range(4)]
                di = [0]
                gslot = [DmaSlot(S, f"g{k}") for k in range(NSLOT)]
                x2 = [sbt(ph, "x2_0", [P, D], F32)] * 2
                x2_slot = [DmaSlot(S, f"x2s{k}") for k in range(2)]
                fg_bc = sbt(ph, "fg_bc", [P, D], F32)
                pT2 = pst(ph, "pT2", [P, 8, P], F32)
                pqp = [pst(ph, f"pqp{k}", [P, 512], F32) for k in range(2)]
                pcs = [pst(ph, f"pcs{k}", [P, 512], F32) for k in range(2)]
                pva = [pst(ph, f"pva{k}", [P, 512], F32) for k in range(2)]
                psc = pqp + pcs
                print(f"[build] phase_p sbuf remaining {nc.sbuf_bytes_remaining}")

                S.op("pool", lambda e: e.iota(iota16[:], pattern=[[1, 16]], base=0, channel_multiplier=0,
                                              allow_small_or_imprecise_dtypes=True), w=[iota16])
                ws = DmaSlot(S, "wqload", group=True)
                for j in range(8):
                    S.dma("sp", lambda e, j=j: e.dma_start(out=wq[:, j, :], in_=peer_wq_d[l, j * P:(j + 1) * P, :]), ws, w=[wq])
                kraw_ap = sc[:].rearrange("p a b -> p (a b)")[:, 0:1024].rearrange("p (a d) -> p a d", d=64)
                keysT_ap = cand[:].rearrange("p a b -> p (a b)")[:, 0:1024].rearrange("p (a k) -> p a k", k=P)
                S.dma("sp", lambda e: e.dma_start(out=kraw_ap, in_=peer_keys_d[l].rearrange("h c k d -> k (h c) d")), ws, w=[sc])
                if last:
                    S.dma("sp", lambda e: e.dma_start(out=fg_bc[:], in_=final_g_d[0].partition_broadcast(P)), ws, w=[fg_bc])
                ws.close()
                for hh in range(8):
                    S.op("pe", lambda e, hh=hh: e.transpose(pT2[:, hh, :], kraw_ap[:, 2 * hh:2 * hh + 2, :].rearrange("p c d -> p (c d)"), ident_f[:]),
                         r=[sc, ident_f], w=[pT2])
                S.op("act", lambda e: e.activation(keysT_ap, pT2[:], AF.Copy), r=[pT2], w=[cand])
                S.op("pool", lambda e: e.memset(keysTz[:], 0.0), w=[keysTz])
                S.op("dve", lambda e: e.tensor_copy(keysTz[0:64, :, 0, :], keysT_ap[0:64, :, :]), r=[cand], w=[keysTz])
                S.op("dve", lambda e: e.tensor_copy(keysTz[64:128, :, 1, :], keysT_ap[64:128, :, :]), r=[cand], w=[keysTz])

                gi = [0]

                def routing(n_):
                    rq = []
                    i = tiles[n_]
                    k = n_ % 2
                    xk, hk, efk, eik, wgk = xt[k], h[k], ef[k], eidx[k], wgt[k]
                    hbk = hb[k]

                    def op(eng, fn, r=(), w=()):
                        rq.append(("op", eng, fn, r, w))

                    rq.append(("dma", "sp", lambda e: e.dma_start(out=xk[:], in_=xs_d[i * P:(i + 1) * P, :]), xt_slot[k], [xs_buf[i]], [xk]))
                    op("act", lambda e: e.activation(junkr[:], xk[:], AF.Square, accum_out=ss[:]), r=[xk], w=[junkr, ss])
                    op("act", lambda e: e.activation(rstd[:], ss[:], AF.Sqrt, scale=1.0 / D, bias=eps6[:, 0:1]), r=[ss, eps6], w=[rstd])
                    op("dve", lambda e: e.reciprocal(rstd[:], rstd[:]), r=[rstd], w=[rstd])
                    op("dve", lambda e: e.scalar_tensor_tensor(out=hk[:], in0=xk[:], scalar=rstd[:, 0:1], in1=modbc[:, 2048:3072],
                                                               op0=ALU.mult, op1=ALU.mult), r=[xk, rstd, modbc], w=[hk])
                    op("dve", lambda e: e.tensor_tensor(hk[:], hk[:], modbc[:, 1024:2048], op=ALU.add), r=[hk, modbc], w=[hk])
                    op("act", lambda e: e.activation(hbk[:], hk[:], AF.Copy), r=[hk], w=[hbk])
                    for j in range(8):
                        op("pe", lambda e, j=j: e.transpose(pT2[:, j, :], hk[:, j * P:(j + 1) * P], ident_f[:]), r=[hk, ident_f], w=[pT2])
                    op("act", lambda e: e.activation(hT[:], pT2[:], AF.Copy), r=[pT2], w=[hT])
                    for cc in range(2):
                        for j in range(8):
                            op("pe", lambda e, cc=cc, j=j: e.matmul(pqp[cc][:], lhsT=hT[:, j, :], rhs=wq[:, j, cc * 512:(cc + 1) * 512],
                                                                  start=(j == 0), stop=(j == 7)), r=[hT, wq], w=[pqp[cc]])
                        op("act", lambda e, cc=cc: e.activation(qp[:, cc * 512:(cc + 1) * 512], pqp[cc][:], AF.Copy), r=[pqp[cc]], w=[qp])
                    for hh in range(8):
                        op("pe", lambda e, hh=hh: e.transpose(pT2[:, hh, :], qp[:, hh * P:(hh + 1) * P], ident_f[:]), r=[qp, ident_f], w=[pT2])
                    op("act", lambda e: e.activation(qpT[:], pT2[:], AF.Copy), r=[pT2], w=[qpT])
                    for hh in range(8):
                        pv = psc[hh // 2][:].rearrange("p (a k) -> p a k", k=P)
                        op("pe", lambda e, hh=hh, pv=pv: e.matmul(pv[:, (hh % 2) * 2:(hh % 2) * 2 + 2, :], lhsT=qpT[:, hh, :],
                                                                 rhs=keysTz[:, hh, :, :], start=True, stop=True),
                           r=[qpT, keysTz], w=[psc[hh // 2]])
                    for q in range(4):
                        op("act", lambda e, q=q: e.activation(sc[:, 4 * q:4 * q + 4, :], psc[q][:].rearrange("p (a k) -> p a k", k=P), AF.Copy),
                           r=[psc[q]], w=[sc])
                    for g in range(16):
                        op("dve", lambda e, g=g: e.max(out=topv[:, g, 0:8], in_=sc[:, g, :]), r=[sc], w=[topv])
                        op("dve", lambda e, g=g: e.max_index(out=idx[:, g, 0:8], in_max=topv[:, g, 0:8], in_values=sc[:, g, :]),
                           r=[sc, topv], w=[idx])
                        op("dve", lambda e, g=g: e.match_replace(out=wk[:, g, :], in_to_replace=topv[:, g, 0:8], in_values=sc[:, g, :],
                                                                 imm_value=-1e30), r=[sc, topv], w=[wk])
                        op("dve", lambda e, g=g: e.max(out=topv[:, g, 8:16], in_=wk[:, g, :]), r=[wk], w=[topv])
                        op("dve", lambda e, g=g: e.max_index(out=idx[:, g, 8:16], in_max=topv[:, g, 8:16], in_values=wk[:, g, :]),
                           r=[wk, topv], w=[idx])
                    op("dve", lambda e: e.tensor_copy(idxf[:], idx[:]), r=[idx], w=[idxf])
                    tv4 = topv[:].rearrange("p (h c) a -> p h c a", c=2)
                    if4 = idxf[:].rearrange("p (h c) a -> p h c a", c=2)
                    op("dve", lambda e: e.tensor_tensor(cand[:].rearrange("p h (a b) -> p h a b", a=16),
                                                        tv4[:, :, 0, :].unsqueeze(3).to_broadcast([P, 8, 16, 16]),
                                                        tv4[:, :, 1, :].unsqueeze(2).to_broadcast([P, 8, 16, 16]), op=ALU.add),
                       r=[topv], w=[cand])
                    for hh in range(8):
                        op("dve", lambda e, hh=hh: e.max(out=best[:, hh, 0:8], in_=cand[:, hh, :]), r=[cand], w=[best])
                        op("dve", lambda e, hh=hh: e.max_index(out=pos[:, hh, 0:8], in_max=best[:, hh, 0:8], in_values=cand[:, hh, :]),
                           r=[cand, best], w=[pos])
                        op("dve", lambda e, hh=hh: e.match_replace(out=wk2_ap[:, hh, :], in_to_replace=best[:, hh, 0:8],
                                                                   in_values=cand[:, hh, :], imm_value=-1e30), r=[cand, best], w=[sc])
                        op("dve", lambda e, hh=hh: e.max(out=best[:, hh, 8:16], in_=wk2_ap[:, hh, :]), r=[sc], w=[best])
                        op("dve", lambda e, hh=hh: e.max_index(out=pos[:, hh, 8:16], in_max=best[:, hh, 8:16], in_values=wk2_ap[:, hh, :]),
                           r=[sc, best], w=[pos])
                    posf = pos[:].rearrange("p h s -> p (h s)")
                    op("dve", lambda e: e.tensor_single_scalar(pab[:, 0, :], posf, 4, op=ALU.logical_shift_right), r=[pos], w=[pab])
                    op("dve", lambda e: e.tensor_single_scalar(pab[:, 1, :], posf, 15, op=ALU.bitwise_and), r=[pos], w=[pab])
                    op("dve", lambda e: e.tensor_copy(abf[:], pab[:]), r=[pab], w=[abf])
                    for c in range(2):
                        op("dve", lambda e, c=c: e.tensor_tensor(eq_ap, iota16[:].unsqueeze(1).to_broadcast([P, 128, 16]), abf[:, c, :].unsqueeze(2).to_broadcast([P, 128, 16]),
                                                                 op=ALU.is_equal), r=[iota16, abf], w=[wk])
                        op("dve", lambda e, c=c: e.tensor_tensor(eq_ap.rearrange("p (h s) a -> p h s a", h=8),
                                                                 eq_ap.rearrange("p (h s) a -> p h s a", h=8),
                                                                 if4[:, :, c, :].unsqueeze(2).to_broadcast([P, 8, 16, 16]), op=ALU.mult),
                           r=[wk, idxf], w=[wk])
                        op("dve", lambda e, c=c: e.tensor_reduce(out=isel[:, c, :], in_=eq_ap, axis=AX.X, op=ALU.add), r=[wk], w=[isel])
                    op("dve", lambda e: e.scalar_tensor_tensor(out=efk[:], in0=isel[:, 0, :], scalar=128.0, in1=isel[:, 1, :],
                                                               op0=ALU.mult, op1=ALU.add), r=[isel], w=[efk])
                    op("dve", lambda e: e.tensor_scalar(efk[:], efk[:], float(l * NPEER), None, op0=ALU.add), r=[efk], w=[efk])
                    op("dve", lambda e: e.tensor_copy(eik[:], efk[:]), r=[efk], w=[eik])
                    op("dve", lambda e: e.tensor_tensor(ew[:].rearrange("p (h s) -> p h s", h=8), best[:],
                                                        best[:, :, 0:1].to_broadcast([P, 8, 16]), op=ALU.subtract), r=[best], w=[ew])
                    op("act", lambda e: e.activation(ew[:], ew[:], AF.Exp), r=[ew], w=[ew])
                    op("dve", lambda e: e.tensor_reduce(out=se[:], in_=ew[:].rearrange("p (h s) -> p h s", h=8), axis=AX.X, op=ALU.add),
                       r=[ew], w=[se])
                    op("dve", lambda e: e.reciprocal(se[:], se[:]), r=[se], w=[se])
                    op("dve", lambda e: e.tensor_tensor(wgk[:].rearrange("p (h s) -> p h s", h=8), ew[:].rearrange("p (h s) -> p h s", h=8),
                                                        se[:].unsqueeze(2).to_broadcast([P, 8, 16]), op=ALU.mult), r=[ew, se], w=[wgk])
                    return rq

                def emit(rq, n):
                    for _ in range(n):
                        if not rq:
                            return
                        t = rq.pop(0)
                        if t[0] == "op":
                            S.op(t[1], t[2], t[3], t[4])
                        else:
                            S.dma(t[1], t[2], t[3], t[4], t[5])

                rq = routing(0)
                emit(rq, len(rq))
                for n_, i in enumerate(tiles):
                    k = n_ % 2
                    hbk, efk, eik, wgk = hb[k], ef[k], eidx[k], wgt[k]
                    rq = routing(n_ + 1) if n_ + 1 < len(tiles) else []
                    per_slot = -(-len(rq) // 112)
                    NG = 128 // GRP
                    gb = {}
                    for g_ in range(NG + 1):
                        if g_ < NG:
                            pb_ = pre_b[g_ % 4]
                            pd_ = pre_d[g_ % 4]
                            for s_ in range(g_ * GRP, (g_ + 1) * GRP):
                                b_ = gi[0] % NSLOT
                                gi[0] += 1
                                gb[s_] = b_
                                S.dma("pool", lambda e, s_=s_, b_=b_: e.indirect_dma_start(
                                    out=gbuf[b_][:], out_offset=None, in_=uv_d,
                                    in_offset=bass.IndirectOffsetOnAxis(ap=eik[:, s_:s_ + 1], axis=0)), gslot[b_], r=[eik], w=[gbuf[b_]])
                                pr_ = prod[pi[0] % len(prod)]
                                pi[0] += 1
                                S.op("dve", lambda e, b_=b_, pr_=pr_: e.tensor_tensor(pr_[:], hbk[:], gbuf[b_][:, 0:D], op=ALU.mult),
                                     r=[hbk, gbuf[b_]], w=[pr_])
                                S.op("act", lambda e, s_=s_, pr_=pr_: e.activation(junkr[:], pr_[:], AF.Identity, accum_out=pre[:, s_:s_ + 1]),
                                     r=[pr_], w=[junkr, pb_])
                                emit(rq, per_slot)
                            gs = slice(g_ * GRP, (g_ + 1) * GRP)
                            S.op("act", lambda e, gs=gs: e.activation(aw[:, gs], pre[:, gs], AF.Gelu), r=[pb_, pd_], w=[pb_])
                        if g_ >= 1:
                            gq = g_ - 1
                            pbq = pre_b[gq % 4]
                            for s_ in range(gq * GRP, (gq + 1) * GRP):
                                b_ = gb[s_]
                                dg = diag[di[0] % len(diag)]
                                di[0] += 1
                                S.op("dve", lambda e, s_=s_, dg=dg: e.tensor_scalar(dg[:], ident_b[:], aw[:, s_:s_ + 1], wgk[:, s_:s_ + 1],
                                                                                   op0=ALU.mult, op1=ALU.mult),
                                     r=[ident_b, pbq, wgk], w=[dg])
                                for cc in range(2):
                                    S.op("pe", lambda e, s_=s_, b_=b_, dg=dg, cc=cc: e.matmul(
                                        pva[cc][:], lhsT=dg[:], rhs=gbuf[b_][:, D + cc * 512:D + (cc + 1) * 512],
                                        start=(s_ == 0), stop=(s_ == 127)), r=[dg, gbuf[b_]], w=[pva[cc]])
                    if l == 0:
                        dbg_store("d_pre", slice(i * P, (i + 1) * P), pre[:], pre_b + pre_d)
                        dbg_store("d_eidx", slice(i * P, (i + 1) * P), efk[:], [efk])
                        dbg_store("d_w", slice(i * P, (i + 1) * P), wgk[:], [wgk])
                    for cc in range(2):
                        sl = slice(cc * 512, (cc + 1) * 512)
                        S.op("dve", lambda e, cc=cc, sl=sl: e.tensor_tensor(x2[k][:, sl], pva[cc][:], modbc[:, 3072 + cc * 512:3072 + (cc + 1) * 512],
                                                                           op=ALU.mult), r=[pva[cc], modbc], w=[x2[k]])
                    S.op("dve", lambda e: e.tensor_tensor(x2[k][:], x2[k][:], xt[k][:], op=ALU.add), r=[x2[k], xt[k]], w=[x2[k]])
                    if l == 0:
                        dbg_store("d_x2", slice(i * P, (i + 1) * P), x2[k][:], [x2[k]])
                    if last:
                        S.op("act", lambda e: e.activation(junk[:], x2[k][:], AF.Square, accum_out=ssf[:]), r=[x2[k]], w=[junk, ssf])
                        rstd_from_ss(ssf, rstdf, 1.0 / D, eps6)
                        S.op("dve", lambda e: e.scalar_tensor_tensor(out=x2[k][:], in0=x2[k][:], scalar=rstdf[:, 0:1], in1=fg_bc[:],
                                                                     op0=ALU.mult, op1=ALU.mult), r=[x2[k], rstdf, fg_bc], w=[x2[k]])
                        S.dma("sp", lambda e: e.dma_start(out=out_d[i * P:(i + 1) * P, :], in_=x2[k][:]), x2_slot[k], r=[x2[k]])
                    else:
                        S.dma("sp", lambda e: e.dma_start(out=xs_d[i * P:(i + 1) * P, :], in_=x2[k][:]), x2_slot[k], r=[x2[k]], w=[xs_buf[i]])
                    emit(rq, len(rq))
                S.barrier()

        lat_tiles = list(range(NTL))
        ctx_tiles = list(range(NTL, NT))
        if stop in (None, "p1"):
            convert_tables()
        for l in range(depth):
            last = (l == DEPTH - 1)
            if l == 0:
                src_aps = [x_d[i * P:(i + 1) * P, :] for i in range(NTL)] + [ctx_d[i * P:(i + 1) * P, :] for i in range(NTC)]
            else:
                src_aps = [xs_d[i * P:(i + 1) * P, :] for i in range(NT)]
            mod_cols(l)
            if l == 0:
                dbg_store("d_mod", (slice(0, P), slice(0, 32)), modcol[:].rearrange("p r c -> p (r c)"), [modcol])
                dbg_store("d_mod", (slice(0, P), slice(32, 48)), scT[:].rearrange("p j r -> p (j r)"), [scT])
            if stop == "m":
                break
            with ExitStack() as mix:
                M = {}
                M["qT"] = sbt(mix, "qT", [P, 4, S_LAT], BF16)
                M["qT_b"] = [[Buf(f"qT{pr}_{c}") for c in range(8)] for pr in range(4)]
                M["qTc"] = sbt(mix, "qTc", [P, 4, S_CTX], BF16)
                M["gT"] = sbt(mix, "gT", [P, 4, S_LAT + 2 * GPAD], BF16)
                M["gT_b"] = [Buf(f"gT{i}") for i in range(NTL)]
                M["gTc"] = sbt(mix, "gTc", [P, 4, S_CTX + 2 * GPAD], BF16)
                M["gTc_b"] = [Buf(f"gTc{i}") for i in range(NTC)]
                S.op("pool", lambda e: e.memset(M["gT"][:], 0.0), w=M["gT_b"])
                S.op("pool", lambda e: e.memset(M["gTc"][:], 0.0), w=M["gTc_b"])
                with ExitStack() as ab:
                    M["kT"] = sbt(ab, "kT", [P, NT * P], BF16)
                    M["kT_b"] = [Buf(f"kT{i}") for i in range(NT)]
                    M["Vp"] = sbt(ab, "Vp", [P, NT, 192], BF16)
                    M["Vp_b"] = [Buf(f"Vp{i}") for i in range(NT)]
                    S.op("pool", lambda e: e.memset(M["Vp"][:], 1.0), w=M["Vp_b"])
                    phase_a(l, M, src_aps, last)
                    if stop in ("a", "a0", "a1"):
                        break
                    phase_b(l, M, last)
                if stop == "b":
                    break
                with ExitStack() as cs:
                    modbc = sbt(cs, "modbcC", [P, 4096], F32)
                    mod_bc(l, 0, modbc)
                    phase_c(l, M, lat_tiles, src_aps, 0, modbc)
                    if not last:
                        mod_bc(l, 1, modbc)
                        phase_c(l, M, ctx_tiles, src_aps, 1, modbc)
            if stop == "c":
                break
            with ExitStack() as pp:
                modbc = sbt(pp, "modbcP", [P, 4096], F32)
                mod_bc(l, 0, modbc)
                phase_p(l, lat_tiles if stop not in ("p1", "p0") else lat_tiles[:2], modbc, last)
                if not last and stop not in ("p1", "p0"):
                    mod_bc(l, 1, modbc)
                    phase_p(l, ctx_tiles, modbc, last)
            if stop in ("p1", "p0"):
                break
        out_slot.close()
        if out_slot.sem is not None:
            for en in ("sp", "act", "pool"):
                S.E[en].eng.wait_ge(out_slot.sem, out_slot.val)
        S.barrier()
        print(f"[build] instr={S.ninstr} waits={S.nwait} sems={S.nsem}")
    return nc


_NC_CACHE = {}


def _rope_tables():
    n = 16
    inv = (10000.0 ** (-np.arange(n, dtype=np.float32) / n)).astype(np.float32)
    row = np.repeat(np.arange(64), 64).astype(np.float32)
    col = np.tile(np.arange(64), 64).astype(np.float32)
    ang = np.concatenate([row[:, None] * inv, col[:, None] * inv], axis=-1).astype(np.float32)
    cos = np.cos(ang).astype(np.float32)
    sin = np.sin(ang).astype(np.float32)
    cos2 = np.concatenate([cos, cos], axis=-1)
    sin2 = np.concatenate([-sin, sin], axis=-1)
    return np.ascontiguousarray(cos2), np.ascontiguousarray(sin2)


def make_in_maps(inputs, cores):
    f = lambda a: np.ascontiguousarray(np.asarray(a, dtype=np.float32))
    cos2, sin2 = _rope_tables()
    shared = {k: f(inputs[k]) for k in ["w_mod", "b_mod", "w_in", "q_gain", "k_gain", "conv_w", "conv_b", "ln_g", "ln_b",
                                        "beta_attn", "beta_conv", "w_o", "peer_wq", "peer_keys", "peer_u", "peer_v"]}
    shared["final_g"] = f(inputs["final_g"]).reshape(1, D)
    shared["cos2"] = cos2
    shared["sin2"] = sin2
    shared["cctxT"] = np.ascontiguousarray(f(inputs["c_ctx"]).reshape(8, P).T)
    x = f(inputs["x"])
    ctx = f(inputs["ctx"])
    c = f(inputs["c"])
    maps = []
    for b in cores:
        m = dict(shared)
        m["x"] = x[b]
        m["ctx"] = ctx[b]
        m["cT"] = np.ascontiguousarray(c[b].reshape(8, P).T)
        maps.append(m)
    return maps


def kernel(**inputs):
    if "nc" not in _NC_CACHE:
        _NC_CACHE["nc"] = build()
    nc = _NC_CACHE["nc"]
    B = np.asarray(inputs["x"]).shape[0]
    maps = make_in_maps(inputs, list(range(B)))
    res = run_bass_kernel_spmd(nc, maps, core_ids=list(range(B)))
    out = np.stack([np.asarray(r["out"], dtype=np.float32) for r in res.results], axis=0)
    return out
```

```python
import numpy as np
from contextlib import ExitStack
import concourse.bass as bass
import concourse.mybir as mybir
from concourse.bass_utils import run_bass_kernel_spmd

F32 = mybir.dt.float32
BF16 = mybir.dt.bfloat16
I32 = mybir.dt.int32
U32 = mybir.dt.uint32
ALU = mybir.AluOpType
AF = mybir.ActivationFunctionType
AX = mybir.AxisListType

import os
CUT = int(os.environ.get("KCUT", "99"))
SEM_LIMIT = 30000


class Buf:
    __slots__ = ("name", "w", "r")

    def __init__(self, name):
        self.name = name
        self.w = None
        self.r = {}


class Eng:
    def __init__(self, sync, name, eng):
        self.sync = sync
        self.name = name
        self.eng = eng
        self.sem = None
        self.count = 0
        self.known = {}
        self.last = None

    def new_event(self):
        if self.sem is None or self.count >= SEM_LIMIT:
            self.sem = self.sync.new_sem(self.name)
            self.count = 0
        self.count += 1
        self.last = [self.sem, self.count, self.name]
        return self.last


def DmaSlot(sync, name, group=False):
    if name not in sync.slot_by_name:
        sync.slot_by_name[name] = _DmaSlot(sync, name, group)
    return sync.slot_by_name[name]


class _DmaSlot:
    def __init__(self, sync, name, group=False):
        self.name = name
        self.sem = None
        self.val = 0
        self.group = group
        self.pending = []
        sync.slots.append(self)

    def bump(self, sync):
        if self.sem is None or (self.val >= SEM_LIMIT and not self.pending):
            self.sem = sync.new_sem("d" + self.name)
            self.val = 0
        self.val += 16
        ev = [self.sem, self.val, "dma"]
        if self.group:
            self.pending.append(ev)
        return ev

    def close(self):
        for ev in self.pending:
            ev[1] = self.val
        self.pending = []


class Sync:
    def __init__(self, nc, stack):
        self.nc = nc
        self.stack = stack
        self.nsem = 0
        self.slots = []
        self.slot_by_name = {}
        self.E = {
            "pe": Eng(self, "pe", nc.tensor),
            "dve": Eng(self, "dve", nc.vector),
            "act": Eng(self, "act", nc.scalar),
            "pool": Eng(self, "pool", nc.gpsimd),
            "sp": Eng(self, "sp", nc.sync),
        }
        self.ninstr = 0
        self.nwait = 0

    def new_sem(self, name):
        self.nsem += 1
        return self.stack.enter_context(self.nc.semaphore(f"s{self.nsem}_{name}"))

    def _wait(self, E, evs):
        best = {}
        for ev in evs:
            if ev is None:
                continue
            if E.name == "pe" and ev[2] == "pe":
                continue
            k = id(ev[0])
            if k not in best or best[k][1] < ev[1]:
                best[k] = ev
        for ev in best.values():
            sem, val, src = ev
            if E.known.get(id(sem), 0) >= val:
                continue
            E.eng.wait_ge(sem, val)
            self.nwait += 1
            E.known[id(sem)] = val

    def _deps(self, E, reads, writes):
        evs = []
        for b in reads:
            evs.append(b.w)
        for b in writes:
            if b.w is not None and b.w[2] != E.name:
                evs.append(b.w)
            for ev in b.r.values():
                if ev[2] == E.name:
                    continue
                evs.append(ev)
        return evs

    @staticmethod
    def _bufs(lst):
        return [t.b if hasattr(t, "b") else t for t in lst]

    def op(self, engname, fn, r=(), w=()):
        E = self.E[engname]
        r = self._bufs(r)
        w = self._bufs(w)
        self._wait(E, self._deps(E, r, w))
        ins = fn(E.eng)
        ev = E.new_event()
        ins.then_inc(ev[0], 1)
        self.ninstr += 1
        key = ev[2] if ev[2] != "dma" else id(ev[0])
        for b in r:
            b.r[key] = ev
        for b in w:
            b.w = ev
            b.r = {}
        return ev

    def dma(self, qname, fn, slot, r=(), w=()):
        E = self.E[qname]
        r = self._bufs(r)
        w = self._bufs(w)
        self._wait(E, self._deps(E, r, w))
        ins = fn(E.eng)
        ev = slot.bump(self)
        ins.then_inc(ev[0], 16)
        self.ninstr += 1
        key = ev[2] if ev[2] != "dma" else id(ev[0])
        for b in r:
            b.r[key] = ev
        for b in w:
            b.w = ev
            b.r = {}
        return ev

    def barrier(self):
        for s in self.slots:
            if s.group:
                s.close()
        evs = []
        for E in self.E.values():
            if E.last is not None:
                evs.append(E.last)
        for s in self.slots:
            if s.sem is not None:
                evs.append([s.sem, s.val, "dma"])
        for E in self.E.values():
            self._wait(E, [ev for ev in evs if not (E.name == "pe" and ev[2] == "pe")])


class Tile:
    def __init__(self, h, name):
        self.h = h
        self.b = Buf(name)

    def __getitem__(self, k):
        return self.h[k]


P = 128
D = 1024
S_LAT = 4096
S_CTX = 256
NTL = 32
NTC = 2
NT = NTL + NTC
DEPTH = 2
INC = 1792
EPS = 1e-6
NPEER = 16384
GPAD = 16
NSLOT = 17
GRP = 4


def build(depth=DEPTH, dbg=False, stop=None):
    nc = bass.Bass("TRN2", target_bir_lowering=False)

    def din(name, shape, dt=F32):
        return nc.dram_tensor(name, shape, dt, kind="ExternalInput").ap()

    x_d = din("x", [S_LAT, D])
    ctx_d = din("ctx", [S_CTX, D])
    cT_d = din("cT", [P, 8])
    cctxT_d = din("cctxT", [P, 8])
    w_mod_d = din("w_mod", [DEPTH, D, 6 * D])
    b_mod_d = din("b_mod", [DEPTH, 6 * D])
    w_in_d = din("w_in", [DEPTH, D, INC])
    q_gain_d = din("q_gain", [DEPTH, 64])
    k_gain_d = din("k_gain", [DEPTH, 64])
    conv_w_d = din("conv_w", [DEPTH, 31, 512])
    conv_b_d = din("conv_b", [DEPTH, 512])
    ln_g_d = din("ln_g", [DEPTH, 512])
    ln_b_d = din("ln_b", [DEPTH, 512])
    beta_attn_d = din("beta_attn", [DEPTH, 512])
    beta_conv_d = din("beta_conv", [DEPTH, 512])
    w_o_d = din("w_o", [DEPTH, D, D])
    peer_wq_d = din("peer_wq", [DEPTH, D, D])
    peer_keys_d = din("peer_keys", [DEPTH, 8, 2, 128, 64])
    peer_u_d = din("peer_u", [DEPTH, NPEER, D])
    peer_v_d = din("peer_v", [DEPTH, NPEER, D])
    final_g_d = din("final_g", [1, D])
    cos2_d = din("cos2", [S_LAT, 64])
    sin2_d = din("sin2", [S_LAT, 64])
    peer_u_flat = peer_u_d.rearrange("l n d -> (l n) d")
    peer_v_flat = peer_v_d.rearrange("l n d -> (l n) d")
    out_d = nc.dram_tensor("out", [S_LAT, D], F32, kind="ExternalOutput").ap()
    xs_d = nc.dram_tensor("xs", [S_LAT + S_CTX, D], F32).ap()
    uv_d = nc.dram_tensor("uv", [DEPTH * NPEER, 2 * D], BF16).ap()
    modscr_d = nc.dram_tensor("modscr", [2, P, 4096], F32).ap()
    dbg_d = {}
    if dbg:
        for nm, shp in [("d_qkvu", [NT * P, INC]), ("d_x1", [NT * P, D]), ("d_oT", [P, 4 * S_LAT]),
                        ("d_pre", [NT * P, 128]), ("d_eidx", [NT * P, 128]), ("d_w", [NT * P, 128]),
                        ("d_qT", [P, 4 * S_LAT]), ("d_x2", [NT * P, D]), ("d_mod", [P, 4096])]:
            dbg_d[nm] = nc.dram_tensor(nm, shp, F32, kind="ExternalOutput").ap()

    top = ExitStack()
    with top:
        S = Sync(nc, top)

        uid = [0]

        def sbt(stack, name, shape, dt):
            uid[0] += 1
            name = f"{name}_u{uid[0]}"
            return Tile(stack.enter_context(nc.sbuf_tensor(name, shape, dt)), name)

        def pst(stack, name, shape, dt):
            uid[0] += 1
            name = f"{name}_u{uid[0]}"
            return Tile(stack.enter_context(nc.psum_tensor(name, shape, dt)), name)

        out_slot = DmaSlot(S, "out", group=True)
        dbg_slots = {}
        xs_buf = [Buf(f"xs{i}") for i in range(NT)]

        ident_f = sbt(top, "ident_f", [P, P], F32)
        ident_b = sbt(top, "ident_b", [P, P], BF16)
        ones_f = sbt(top, "ones_f", [P, P], F32)
        io_t = sbt(top, "io_t", [P, P], F32)
        pid_t = sbt(top, "pid_t", [P, 1], F32)
        S.op("pool", lambda e: e.iota(io_t[:], pattern=[[1, P]], base=0, channel_multiplier=0,
                                      allow_small_or_imprecise_dtypes=True), w=[io_t])
        S.op("pool", lambda e: e.iota(pid_t[:], pattern=[[0, 1]], base=0, channel_multiplier=1,
                                      allow_small_or_imprecise_dtypes=True), w=[pid_t])
        S.op("dve", lambda e: e.tensor_scalar(ident_f[:], io_t[:], pid_t[:, 0:1], None, op0=ALU.is_equal),
             r=[io_t, pid_t], w=[ident_f])
        S.op("dve", lambda e: e.tensor_copy(ident_b[:], ident_f[:]), r=[ident_f], w=[ident_b])
        S.op("pool", lambda e: e.memset(ones_f[:], 1.0), w=[ones_f])

        cslot = DmaSlot(S, "const", group=True)
        craw = sbt(top, "craw", [P, 2, 8], F32)
        scT = sbt(top, "scT", [P, 8, 2], F32)
        S.dma("sp", lambda e: e.dma_start(out=craw[:, 0, :], in_=cT_d), cslot, w=[craw])
        S.dma("sp", lambda e: e.dma_start(out=craw[:, 1, :], in_=cctxT_d), cslot, w=[craw])
        cslot.close()
        S.op("act", lambda e: e.activation(scT[:].rearrange("p j r -> p r j"), craw[:], AF.Silu), r=[craw], w=[scT])
        modcol = sbt(top, "modcol", [P, 2, 16], F32)
        eps6 = sbt(top, "eps6", [P, 1], F32)
        eps5 = sbt(top, "eps5", [P, 1], F32)
        S.op("pool", lambda e: e.memset(eps6[:], 1e-6), w=[eps6])
        S.op("pool", lambda e: e.memset(eps5[:], 1e-5), w=[eps5])


        def rstd_from_ss(ss, rs, scale, eps_t):
            S.op("act", lambda e: e.activation(rs[:], ss[:], AF.Sqrt, scale=scale, bias=eps_t[:, 0:1]), r=[ss, eps_t], w=[rs])
            S.op("dve", lambda e: e.reciprocal(rs[:], rs[:]), r=[rs], w=[rs])

        def dbg_store(name, rows, tile_ap, rtiles):
            if dbg and name in dbg_d:
                if name not in dbg_slots:
                    dbg_slots[name] = (DmaSlot(S, name), Buf(name))
                S.dma("sp", lambda e: e.dma_start(out=dbg_d[name][rows], in_=tile_ap), dbg_slots[name][0], r=rtiles, w=[dbg_slots[name][1]])


        def convert_thunks(ph, NB=3):
            stg = [sbt(ph, f"cvi{k}", [P, 4, D], F32) for k in range(NB)]
            obf = [sbt(ph, f"cvo{k}", [P, 4, D], BF16) for k in range(NB)]
            islot = [DmaSlot(S, f"cvi{k}") for k in range(NB)]
            oslot = [DmaSlot(S, f"cvo{k}") for k in range(NB)]
            cq = []
            n = 0
            for l in range(DEPTH):
                for tab, col0 in ((peer_u_d, 0), (peer_v_d, D)):
                    for t in range(NPEER // 512):
                        k = n % NB
                        src = tab[l, t * 512:(t + 1) * 512, :].rearrange("(p r) d -> p r d", r=4)
                        dst = uv_d[l * NPEER + t * 512:l * NPEER + (t + 1) * 512, col0:col0 + D].rearrange("(p r) d -> p r d", r=4)
                        cq.append(("dma", "sp", lambda e, k=k, src=src: e.dma_start(out=stg[k][:], in_=src), islot[k], [], [stg[k]]))
                        if n % 2 == 0:
                            cq.append(("op", "dve", lambda e, k=k: e.tensor_copy(obf[k][:], stg[k][:]), [stg[k]], [obf[k]]))
                        else:
                            cq.append(("op", "pool", lambda e, k=k: e.tensor_copy(obf[k][:], stg[k][:]), [stg[k]], [obf[k]]))
                        cq.append(("dma", "pool", lambda e, k=k, dst=dst: e.dma_start(out=dst, in_=obf[k][:]), oslot[k], [obf[k]], []))
                        n += 1
            return cq

        def emit(rq, n):
            for _ in range(n):
                if not rq:
                    return
                t = rq.pop(0)
                if t[0] == "op":
                    S.op(t[1], t[2], t[3], t[4])
                else:
                    S.dma(t[1], t[2], t[3], t[4], t[5])

        def mod_cols(l):
            with ExitStack() as ph:
                wm = [sbt(ph, f"wm{k}", [P, 8, P], F32) for k in range(2)]
                wslot = [DmaSlot(S, f"wm{k}") for k in range(2)]
                bcol = sbt(ph, "bcol", [P, 16], F32)
                pcol = pst(ph, "pcol", [P, 16, 2], F32)
                S.dma("sp", lambda e: e.dma_start(out=bcol[:], in_=b_mod_d[l, 0:2048].rearrange("(c p) -> p c", p=P),
                                                  allow_slow_non_contiguous=True), DmaSlot(S, "bcol"), w=[bcol])
                for cc in range(16):
                    t = wm[cc % 2]
                    S.dma("sp", lambda e, t=t, cc=cc: e.dma_start(
                        out=t[:], in_=w_mod_d[l, :, cc * P:(cc + 1) * P].rearrange("(j p) m -> p j m", p=P)),
                          wslot[cc % 2], w=[t])
                    for j in range(8):
                        S.op("pe", lambda e, t=t, j=j, cc=cc: e.matmul(pcol[:, cc, :], lhsT=t[:, j, :],
                                                                     rhs=scT[:, j, :], start=(j == 0), stop=(j == 7)),
                             r=[t, scT], w=[pcol])
                S.op("dve", lambda e: e.tensor_tensor(modcol[:].rearrange("p r c -> p c r"), pcol[:],
                                                      bcol[:].unsqueeze(2).to_broadcast([P, 16, 2]), op=ALU.add),
                     r=[pcol, bcol], w=[modcol])
                S.op("dve", lambda e: e.tensor_scalar(modcol[:, :, 8:16], modcol[:, :, 8:16], 1.0, None, op0=ALU.add),
                     r=[modcol], w=[modcol])
                S.barrier()

        def mod_bc(l, r, modbc):
            with ExitStack() as ph:
                NW = 4
                wm = [sbt(ph, f"wmb{k}", [P, 512], F32) for k in range(NW)]
                wslot = [DmaSlot(S, f"wmb{k}") for k in range(NW)]
                bbc = [sbt(ph, f"bbc{k}", [P, 512], F32) for k in range(2)]
                bslot = [DmaSlot(S, f"bbc{k}") for k in range(2)]
                screp = sbt(ph, "screp", [P, 8, P], F32)
                pb = pst(ph, "pbm", [P, 512], F32)
                S.op("dve", lambda e: e.tensor_copy(screp[:], scT[:, :, r].unsqueeze(2).to_broadcast([P, 8, P])),
                     r=[scT], w=[screp])
                n = 0
                for cc in range(8):
                    c0 = 2048 + cc * 512
                    S.dma("sp", lambda e, cc=cc, c0=c0: e.dma_start(out=bbc[cc % 2][:], in_=b_mod_d[l, c0:c0 + 512].partition_broadcast(P)),
                          bslot[cc % 2], w=[bbc[cc % 2]])
                    for j in range(8):
                        t = wm[n % NW]
                        S.dma("sp", lambda e, t=t, j=j, c0=c0: e.dma_start(out=t[:], in_=w_mod_d[l, j * P:(j + 1) * P, c0:c0 + 512]),
                              wslot[n % NW], w=[t])
                        n += 1
                        S.op("pe", lambda e, t=t, j=j: e.matmul(pb[:], lhsT=screp[:, j, :], rhs=t[:], start=(j == 0), stop=(j == 7)),
                             r=[t, screp], w=[pb])
                    S.op("dve", lambda e, cc=cc: e.tensor_tensor(modbc[:, cc * 512:(cc + 1) * 512], pb[:], bbc[cc % 2][:], op=ALU.add),
                         r=[pb, bbc[cc % 2]], w=[modbc])
                S.op("dve", lambda e: e.tensor_scalar(modbc[:, 2048:3072], modbc[:, 2048:3072], 1.0, None, op0=ALU.add),
                     r=[modbc], w=[modbc])
                S.barrier()

        def phase_a(l, M, src_aps, last):
            with ExitStack() as ph:
                win = sbt(ph, "win", [P, 8, INC], BF16)
                stg = sbt(ph, "stg", [P, INC], F32)
                stg_slot = DmaSlot(S, "stg")
                bias_bc = sbt(ph, "bias_bc", [P, INC], F32)
                sh1rep = sbt(ph, "sh1rep", [P, 2, 8, P], BF16)
                gain = sbt(ph, "gain", [P, 640], F32)
                graw = sbt(ph, "graw", [P, 128], F32)
                cos2 = sbt(ph, "cos2", [P, NTL, 64], F32)
                sin2 = sbt(ph, "sin2", [P, NTL, 64], F32)
                xt = [sbt(ph, f"xt{k}", [P, D], F32) for k in range(2)]
                xt_slot = [DmaSlot(S, f"xt{k}") for k in range(2)]
                xb = [sbt(ph, f"xb{k}", [P, D], BF16) for k in range(2)]
                xT = [sbt(ph, f"xT{k}", [P, 8, P], BF16) for k in range(2)]
                junk = sbt(ph, "junkA", [P, D], BF16)
                ss = [sbt(ph, f"ssA{k}", [P, 1], F32) for k in range(2)]
                rstd = [sbt(ph, f"rstdA{k}", [P, 1], F32) for k in range(2)]
                qkvu = sbt(ph, "qkvu", [P, INC], F32)
                sq = sbt(ph, "sq", [P, 640], F32)
                ssq = sbt(ph, "ssq", [P, 10], F32)
                rsq = sbt(ph, "rsq", [P, 10], F32)
                qn = sbt(ph, "qn", [P, 640], F32)
                rb = sbt(ph, "rb", [P, 640], F32)
                qr = sbt(ph, "qr", [P, 4, 2, 64], BF16)
                kr = sbt(ph, "kr", [P, 128], BF16)
                sig = sbt(ph, "sig", [P, 512], F32)
                gg = sbt(ph, "gg", [P, 512], BF16)
                ptb = pst(ph, "ptb", [P, 8, P], BF16)
                pq = [pst(ph, f"pq{k}", [P, 512], F32) for k in range(4)]
                ptq = pst(ph, "ptq", [P, 5, P], BF16)
                ptg = pst(ph, "ptg", [P, 4, P], BF16)
                ra = sq
                qT, qTc, kT, Vp, gT, gTc = M["qT"], M["qTc"], M["kT"], M["Vp"], M["gT"], M["gTc"]

                cslot2 = DmaSlot(S, "rope", group=True)
                S.dma("sp", lambda e: e.dma_start(out=cos2[:], in_=cos2_d.rearrange("(n p) f -> p n f", p=P)), cslot2, w=[cos2])
                S.dma("sp", lambda e: e.dma_start(out=sin2[:], in_=sin2_d.rearrange("(n p) f -> p n f", p=P)), cslot2, w=[sin2])
                S.dma("sp", lambda e: e.dma_start(out=graw[:, 0:64], in_=q_gain_d[l].partition_broadcast(P)), cslot2, w=[graw])
                S.dma("sp", lambda e: e.dma_start(out=graw[:, 64:128], in_=k_gain_d[l].partition_broadcast(P)), cslot2, w=[graw])
                cslot2.close()
                for j in range(8):
                    S.dma("sp", lambda e, j=j: e.dma_start(out=stg[:], in_=w_in_d[l, j * P:(j + 1) * P, :]), stg_slot, w=[stg])
                    S.op("act", lambda e, j=j: e.activation(win[:, j, :], stg[:], AF.Copy), r=[stg], w=[win])
                S.op("dve", lambda e: e.tensor_copy(gain[:, 0:512].rearrange("p (h d) -> p h d", h=8),
                                                    graw[:, 0:64].unsqueeze(1).to_broadcast([P, 8, 64])), r=[graw], w=[gain])
                S.op("dve", lambda e: e.tensor_copy(gain[:, 512:640].rearrange("p (h d) -> p h d", h=2),
                                                    graw[:, 64:128].unsqueeze(1).to_broadcast([P, 2, 64])), r=[graw], w=[gain])
                S.op("dve", lambda e: e.tensor_copy(sh1rep[:], modcol[:, :, 0:8].unsqueeze(3).to_broadcast([P, 2, 8, P])),
                     r=[modcol], w=[sh1rep])

                def make_bias(r_):
                    for cc in range(4):
                        w_ = min(512, INC - cc * 512)
                        for j in range(8):
                            S.op("pe", lambda e, cc=cc, j=j, w_=w_: e.matmul(
                                pq[cc][:, 0:w_], lhsT=sh1rep[:, r_, j, :], rhs=win[:, j, cc * 512:cc * 512 + w_],
                                start=(j == 0), stop=(j == 7)), r=[sh1rep, win], w=[pq[cc]])
                        S.op("act", lambda e, cc=cc, w_=w_: e.activation(
                            bias_bc[:, cc * 512:cc * 512 + w_], pq[cc][:, 0:w_], AF.Copy), r=[pq[cc]], w=[bias_bc])

                def load(i):
                    k = i % 2
                    S.dma("sp", lambda e: e.dma_start(out=xt[k][:], in_=src_aps[i]), xt_slot[k], r=[xs_buf[i]], w=[xt[k]])

                make_bias(0)
                load(0)
                for i in range(NT if stop not in ("a0", "a1") else (0 if stop == "a0" else 1)):
                    k = i % 2
                    r_ = 0 if i < NTL else 1
                    is_ctx = i >= NTL
                    if i == NTL:
                        make_bias(1)
                    if i + 1 < NT:
                        load(i + 1)
                    S.op("act", lambda e: e.activation(junk[:], xt[k][:], AF.Square, accum_out=ss[k][:]), r=[xt[k]], w=[junk, ss[k]])
                    S.op("act", lambda e: e.activation(xb[k][:], xt[k][:], AF.Copy), r=[xt[k]], w=[xb[k]])
                    rstd_from_ss(ss[k], rstd[k], 1.0 / D, eps6)
                    for j in range(8):
                        S.op("pe", lambda e, j=j: e.transpose(ptb[:, j, :], xb[k][:, j * P:(j + 1) * P], ident_b[:]),
                             r=[xb[k], ident_b], w=[ptb])
                    S.op("dve", lambda e: e.tensor_tensor(xT[k][:], ptb[:],
                                                          modcol[:, r_, 8:16].unsqueeze(2).to_broadcast([P, 8, P]), op=ALU.mult),
                         r=[ptb, modcol], w=[xT[k]])
                    if CUT <= 1:
                        continue
                    for cc in range(4):
                        w_ = min(512, INC - cc * 512)
                        for j in range(8):
                            S.op("pe", lambda e, cc=cc, j=j, w_=w_: e.matmul(
                                pq[cc][:, 0:w_], lhsT=xT[k][:, j, :], rhs=win[:, j, cc * 512:cc * 512 + w_],
                                start=(j == 0), stop=(j == 7)), r=[xT[k], win], w=[pq[cc]])
                        S.op("dve", lambda e, cc=cc, w_=w_: e.scalar_tensor_tensor(
                            out=qkvu[:, cc * 512:cc * 512 + w_], in0=pq[cc][:, 0:w_], scalar=rstd[k][:, 0:1],
                            in1=bias_bc[:, cc * 512:cc * 512 + w_], op0=ALU.mult, op1=ALU.add),
                             r=[pq[cc], rstd[k], bias_bc], w=[qkvu])
                    if l == 0:
                        dbg_store("d_qkvu", slice(i * P, (i + 1) * P), qkvu[:], [qkvu])
                    if CUT <= 2:
                        continue
                    S.op("pool", lambda e: e.tensor_tensor(sq[:], qkvu[:, 0:640], qkvu[:, 0:640], op=ALU.mult), r=[qkvu], w=[sq])
                    S.op("dve", lambda e: e.tensor_reduce(out=ssq[:], in_=sq[:].rearrange("p (h d) -> p h d", h=10),
                                                          axis=AX.X, op=ALU.add), r=[sq], w=[ssq])
                    rstd_from_ss(ssq, rsq, 1.0 / 64, eps6)
                    S.op("dve", lambda e: e.tensor_tensor(qn[:].rearrange("p (h d) -> p h d", h=10),
                                                          qkvu[:, 0:640].rearrange("p (h d) -> p h d", h=10),
                                                          rsq[:].unsqueeze(2).to_broadcast([P, 10, 64]), op=ALU.mult),
                         r=[qkvu, rsq], w=[qn])
                    S.op("pool", lambda e: e.tensor_tensor(qn[:], qn[:], gain[:], op=ALU.mult), r=[qn, gain], w=[qn])
                    if CUT <= 3:
                        continue
                    if not is_ctx:
                        qn3 = qn[:].rearrange("p (h d) -> p h d", h=10)
                        rb3 = rb[:].rearrange("p (h d) -> p h d", h=10)
                        S.op("dve", lambda e: e.tensor_tensor(ra[:].rearrange("p (h d) -> p h d", h=10), qn3,
                                                              cos2[:, i, :].unsqueeze(1).to_broadcast([P, 10, 64]), op=ALU.mult),
                             r=[qn, cos2], w=[ra])
                        S.op("pool", lambda e: e.tensor_tensor(rb3[:, :, 0:32], qn3[:, :, 32:64],
                                                               sin2[:, i, 0:32].unsqueeze(1).to_broadcast([P, 10, 32]), op=ALU.mult),
                             r=[qn, sin2], w=[rb])
                        S.op("pool", lambda e: e.tensor_tensor(rb3[:, :, 32:64], qn3[:, :, 0:32],
                                                               sin2[:, i, 32:64].unsqueeze(1).to_broadcast([P, 10, 32]), op=ALU.mult),
                             r=[qn, sin2], w=[rb])
                        S.op("dve", lambda e: e.tensor_tensor(qr[:].rearrange("p pr hh d -> p hh pr d"),
                                                              ra[:, 0:512].rearrange("p (hh pr d) -> p hh pr d", hh=2, pr=4),
                                                              rb[:, 0:512].rearrange("p (hh pr d) -> p hh pr d", hh=2, pr=4), op=ALU.add),
                             r=[ra, rb], w=[qr])
                        S.op("pool", lambda e: e.tensor_tensor(kr[:], ra[:, 512:640], rb[:, 512:640], op=ALU.add), r=[ra, rb], w=[kr])
                    else:
                        S.op("dve", lambda e: e.tensor_copy(qr[:].rearrange("p pr hh d -> p hh pr d"),
                                                            qn[:, 0:512].rearrange("p (hh pr d) -> p hh pr d", hh=2, pr=4)),
                             r=[qn], w=[qr])
                        S.op("pool", lambda e: e.tensor_copy(kr[:], qn[:, 512:640]), r=[qn], w=[kr])
                    if CUT <= 4:
                        continue
                    need_q = (not is_ctx) or (not last)
                    if need_q:
                        for pr in range(4):
                            S.op("pe", lambda e, pr=pr: e.transpose(ptq[:, pr, :], qr[:, pr, :, :].rearrange("p hh d -> p (hh d)"), ident_b[:]),
                                 r=[qr, ident_b], w=[ptq])
                    S.op("pe", lambda e: e.transpose(ptq[:, 4, :], kr[:], ident_b[:]), r=[kr, ident_b], w=[ptq])
                    if need_q:
                        if not is_ctx:
                            S.op("act", lambda e: e.activation(qT[:, :, i * P:(i + 1) * P], ptq[:, 0:4, :], AF.Copy),
                                 r=[ptq], w=[M["qT_b"][pr][i // 4] for pr in range(4)])
                        else:
                            S.op("act", lambda e: e.activation(qTc[:, :, (i - NTL) * P:(i - NTL + 1) * P], ptq[:, 0:4, :], AF.Copy),
                                 r=[ptq], w=[qTc])
                    S.op("act", lambda e: e.activation(kT[:, i * P:(i + 1) * P], ptq[:, 4, :], AF.Copy), r=[ptq], w=[M["kT_b"][i]])
                    if CUT <= 5:
                        continue
                    S.op("pool", lambda e: e.tensor_copy(Vp[:, i, 0:64], qkvu[:, 640:704]), r=[qkvu], w=[M["Vp_b"][i]])
                    S.op("pool", lambda e: e.tensor_copy(Vp[:, i, 128:192], qkvu[:, 704:768]), r=[qkvu], w=[M["Vp_b"][i]])
                    if CUT <= 6:
                        continue
                    if need_q:
                        S.op("act", lambda e: e.activation(sig[:], qkvu[:, 1280:1792], AF.Sigmoid), r=[qkvu], w=[sig])
                        S.op("dve", lambda e: e.tensor_tensor(gg[:], qkvu[:, 768:1280], sig[:], op=ALU.mult), r=[qkvu, sig], w=[gg])
                        for c in range(4):
                            S.op("pe", lambda e, c=c: e.transpose(ptg[:, c, :], gg[:, c * P:(c + 1) * P], ident_b[:]),
                                 r=[gg, ident_b], w=[ptg])
                        if not is_ctx:
                            S.op("act", lambda e: e.activation(gT[:, :, GPAD + i * P:GPAD + (i + 1) * P], ptg[:], AF.Copy),
                                 r=[ptg], w=[M["gT_b"][i]])
                        else:
                            S.op("act", lambda e: e.activation(gTc[:, :, GPAD + (i - NTL) * P:GPAD + (i - NTL + 1) * P], ptg[:], AF.Copy),
                                 r=[ptg], w=[M["gTc_b"][i - NTL]])
                S.barrier()

        def phase_b(l, M, last):
            with ExitStack() as ph:
                NPT = 4
                NPS = 4
                LA = 1
                pT = [sbt(ph, f"pT{k}", [P, 512], BF16) for k in range(NPT)]
                rd = sbt(ph, "rd", [P, 512], F32)
                bcs = sbt(ph, "bcs", [P, 512], F32)
                ps_s = [pst(ph, f"ps_s{k}", [P, 512], F32) for k in range(NPS)]
                po = [pst(ph, f"po{k}", [P, 512], F32) for k in range(3)]
                pbc = pst(ph, "pbc", [P, 512], F32)
                qT, qTc, kT, Vp = M["qT"], M["qTc"], M["kT"], M["Vp"]
                cnt = [0]
                grp = [0]
                kTz = sbt(ph, "kTz", [P, 2, NT * P], BF16)
                S.op("pool", lambda e: e.memset(kTz[:], 0.0), w=[kTz])
                S.op("dve", lambda e: e.tensor_copy(kTz[0:64, 0, :], kT[0:64, :]), r=M["kT_b"], w=[kTz])
                S.op("act", lambda e: e.activation(kTz[64:128, 1, :], kT[64:128, :], AF.Copy), r=M["kT_b"], w=[kTz])
                cq = convert_thunks(ph) if (l == 0 and stop in (None, "p1")) else []

                def attend(qsrc_fn, qfull, qbuf, n, ktiles):
                    its = [(kt_i, kt, hh) for kt_i, kt in enumerate(ktiles) for hh in range(2)]
                    base = cnt[0]
                    cnt[0] += len(its)
                    pg = [po[(2 * grp[0]) % 3], po[(2 * grp[0] + 1) % 3]]
                    grp[0] += 1

                    def emit_s(m):
                        kt_i, kt, hh = its[m]
                        k = (base + m) % NPS
                        kp = (base + m) % NPT
                        S.op("pe", lambda e: e.matmul(ps_s[k][:, 0:n], lhsT=kTz[:, hh, kt * P:(kt + 1) * P], rhs=qfull,
                                                      start=True, stop=True), r=[kTz, qbuf], w=[ps_s[k]])
                        S.op("act", lambda e: e.activation(pT[kp][:, 0:n], ps_s[k][:, 0:n], AF.Exp, scale=0.125),
                             r=[ps_s[k]], w=[pT[kp]])

                    def emit_pv(m):
                        kt_i, kt, hh = its[m]
                        kp = (base + m) % NPT
                        S.op("pe", lambda e: e.matmul(pg[hh][:, 0:n], lhsT=Vp[:, kt, hh * 64:hh * 64 + 128], rhs=pT[kp][:, 0:n],
                                                      start=(kt_i == 0), stop=(kt_i == len(ktiles) - 1)), r=[M["Vp_b"][kt], pT[kp]], w=[pg[hh]])

                    npair = len(its) // 2
                    for j in range(npair + LA):
                        if j < npair:
                            emit_s(2 * j)
                            emit_s(2 * j + 1)
                        if j - LA >= 0:
                            emit_pv(2 * (j - LA))
                            emit_pv(2 * (j - LA) + 1)
                        emit(cq, 1)
                    for hh in range(2):
                        dp = 64 if hh == 0 else 0
                        S.op("dve", lambda e, hh=hh, dp=dp: e.reciprocal(rd[dp:dp + 1, 0:n], pg[hh][dp:dp + 1, 0:n]),
                             r=[pg[hh]], w=[rd])
                        S.op("pe", lambda e, dp=dp: e.matmul(pbc[0:64, 0:n], lhsT=ones_f[dp:dp + 1, 0:64], rhs=rd[dp:dp + 1, 0:n],
                                                            start=True, stop=True), r=[ones_f, rd], w=[pbc])
                        S.op("act", lambda e, hh=hh: e.activation(bcs[hh * 64:(hh + 1) * 64, 0:n], pbc[0:64, 0:n], AF.Copy),
                             r=[pbc], w=[bcs])
                        S.op("dve", lambda e, hh=hh: e.tensor_tensor(qsrc_fn(hh), pg[hh][hh * 64:(hh + 1) * 64, 0:n],
                                                                     bcs[hh * 64:(hh + 1) * 64, 0:n], op=ALU.mult),
                             r=[pg[hh], bcs], w=[qbuf])

                for c in range(8):
                    for pr in range(4):
                        attend(lambda hh, c=c, pr=pr: qT[hh * 64:(hh + 1) * 64, pr, c * 512:(c + 1) * 512],
                               qT[:, pr, c * 512:(c + 1) * 512], M["qT_b"][pr][c], 512, list(range(NT)))
                if not last:
                    for pr in range(4):
                        attend(lambda hh, pr=pr: qTc[hh * 64:(hh + 1) * 64, pr, :], qTc[:, pr, :], qTc.b, S_CTX, [NTL, NTL + 1])
                emit(cq, len(cq))
                S.barrier()

        def phase_c(l, M, tiles, src_aps, r_, modbc):
            with ExitStack() as ph:
                wcol = sbt(ph, "wcol", [P, 4, 31], F32)
                DG = sbt(ph, "DG", [P, 4, 31, P], BF16)
                wo = sbt(ph, "wo", [P, 8, D], BF16)
                stg = [sbt(ph, f"stgC{k}", [P, D], F32) for k in range(2)]
                stg_slot = [DmaSlot(S, f"stgC{k}") for k in range(2)]
                betaA = sbt(ph, "betaA", [P, 8], F32)
                betac = sbt(ph, "betac", [P, 8], F32)
                cb_bc = sbt(ph, "cb_bc", [P, 512], F32)
                lg_bc = sbt(ph, "lg_bc", [P, 512], F32)
                lb_bc = sbt(ph, "lb_bc", [P, 512], F32)
                xt = [sbt(ph, f"xtC{k}", [P, D], F32) for k in range(2)]
                xt_slot = [DmaSlot(S, f"xtC{k}") for k in range(2)]
                y = sbt(ph, "yC", [P, 512], F32)
                junk = sbt(ph, "junkC", [P, 512], F32)
                st4 = sbt(ph, "st4", [P, 4], F32)
                rln = sbt(ph, "rln", [P, 1], F32)
                msq = sbt(ph, "msq", [P, 1], F32)
                z = sbt(ph, "zC", [P, 512], F32)
                oc = sbt(ph, "oc", [P, 512], F32)
                ocb = sbt(ph, "ocb", [P, 512], BF16)
                ocT = sbt(ph, "ocT", [P, 4, P], BF16)
                sqo = sbt(ph, "sqo", [P, 4, P], F32)
                ssc = sbt(ph, "ssc", [P, 2], F32)
                rsc = sbt(ph, "rsc", [P, 2], F32)
                t1 = sbt(ph, "t1", [P, D], F32)
                x1 = [sbt(ph, f"x1_{k}", [P, D], F32) for k in range(2)]
                x1_slot = [DmaSlot(S, f"x1s{k}") for k in range(2)]
                py = pst(ph, "py", [P, 512], F32)
                ptc = pst(ph, "ptc", [P, 4, P], BF16)
                pss = pst(ph, "pss", [P, 16], F32)
                pa = [pst(ph, f"pa{k}", [P, 512], F32) for k in range(2)]
                pc = [pst(ph, f"pc{k}", [P, 512], F32) for k in range(2)]
                qT, qTc, gT, gTc = M["qT"], M["qTc"], M["gT"], M["gTc"]

                vs = DmaSlot(S, "vecC", group=True)
                for c in range(4):
                    S.dma("sp", lambda e, c=c: e.dma_start(out=wcol[:, c, :], in_=conv_w_d[l, :, c * P:(c + 1) * P].rearrange("j p -> p j"),
                                                           allow_slow_non_contiguous=True), vs, w=[wcol])
                for half in range(2):
                    S.dma("sp", lambda e, half=half: e.dma_start(out=betaA[half * 64:(half + 1) * 64, :],
                                                                 in_=beta_attn_d[l].rearrange("(h d) -> d h", d=64),
                                                                 allow_slow_non_contiguous=True), vs, w=[betaA])
                S.dma("sp", lambda e: e.dma_start(out=betac[:, 4:8], in_=beta_conv_d[l].rearrange("(c p) -> p c", p=P),
                                                  allow_slow_non_contiguous=True), vs, w=[betac])
                S.dma("sp", lambda e: e.dma_start(out=cb_bc[:], in_=conv_b_d[l].partition_broadcast(P)), vs, w=[cb_bc])
                S.dma("sp", lambda e: e.dma_start(out=lg_bc[:], in_=ln_g_d[l].partition_broadcast(P)), vs, w=[lg_bc])
                S.dma("sp", lambda e: e.dma_start(out=lb_bc[:], in_=ln_b_d[l].partition_broadcast(P)), vs, w=[lb_bc])
                vs.close()
                S.op("dve", lambda e: e.tensor_copy(betac[0:64, 0:4], betaA[0:64, 0:4]), r=[betaA], w=[betac])
                S.op("dve", lambda e: e.tensor_copy(betac[64:128, 0:4], betaA[64:128, 4:8]), r=[betaA], w=[betac])
                for kk in range(8):
                    t = stg[kk % 2]
                    sl = stg_slot[kk % 2]
                    if kk < 4:
                        S.dma("sp", lambda e, t=t, kk=kk: e.dma_start(out=t[0:64, :], in_=w_o_d[l, kk * 64:(kk + 1) * 64, :]), sl, w=[t])
                        S.dma("sp", lambda e, t=t, kk=kk: e.dma_start(out=t[64:128, :], in_=w_o_d[l, (kk + 4) * 64:(kk + 5) * 64, :]), sl, w=[t])
                    else:
                        S.dma("sp", lambda e, t=t, kk=kk: e.dma_start(out=t[:], in_=w_o_d[l, 512 + (kk - 4) * P:512 + (kk - 3) * P, :]), sl, w=[t])
                    S.op("act", lambda e, t=t, kk=kk: e.activation(wo[:, kk, :], t[:], AF.Identity, scale=betac[:, kk:kk + 1]),
                         r=[t, betac], w=[wo])
                for c in range(4):
                    for j in range(31):
                        S.op("pool", lambda e, c=c, j=j: e.tensor_scalar(DG[:, c, j, :], ident_f[:], wcol[:, c, j:j + 1], None, op0=ALU.mult),
                             r=[ident_f, wcol], w=[DG])

                def load(n_):
                    i = tiles[n_]
                    k = n_ % 2
                    S.dma("sp", lambda e: e.dma_start(out=xt[k][:], in_=src_aps[i]), xt_slot[k], r=[xs_buf[i]], w=[xt[k]])

                load(0)
                for n_, i in enumerate(tiles):
                    k = n_ % 2
                    is_ctx = i >= NTL
                    if n_ + 1 < len(tiles):
                        load(n_ + 1)
                    if not is_ctx:
                        gsrc, gb, t0 = gT, M["gT_b"], i * P
                        lo, hi = max(0, i - 1), min(NTL - 1, i + 1)
                        osrc = lambda pr: qT[:, pr, i * P:(i + 1) * P]
                        obufs = [M["qT_b"][pr][i // 4] for pr in range(4)]
                    else:
                        gsrc, gb, t0 = gTc, M["gTc_b"], (i - NTL) * P
                        lo, hi = max(0, i - NTL - 1), min(NTC - 1, i - NTL + 1)
                        osrc = lambda pr: qTc[:, pr, (i - NTL) * P:(i - NTL + 1) * P]
                        obufs = [qTc.b]
                    for c in range(4):
                        for j in range(31):
                            S.op("pe", lambda e, c=c, j=j: e.matmul(py[:, c * P:(c + 1) * P], lhsT=gsrc[:, c, t0 + j + GPAD - 15:t0 + j + GPAD - 15 + P],
                                                                  rhs=DG[:, c, j, :], start=(j == 0), stop=(j == 30)),
                                 r=[gb[q] for q in range(lo, hi + 1)] + [DG], w=[py])
                    S.op("dve", lambda e: e.tensor_tensor(y[:], py[:], cb_bc[:], op=ALU.add), r=[py, cb_bc], w=[y])
                    S.op("act", lambda e: e.activation(junk[:], y[:], AF.Identity, accum_out=st4[:, 0:1]), r=[y], w=[junk, st4])
                    S.op("act", lambda e: e.activation(junk[:], y[:], AF.Square, accum_out=st4[:, 1:2]), r=[y], w=[junk, st4])
                    S.op("dve", lambda e: e.tensor_scalar(st4[:, 2:4], st4[:, 0:2], 1.0 / 512, None, op0=ALU.mult), r=[st4], w=[st4])
                    S.op("dve", lambda e: e.tensor_tensor(msq[:], st4[:, 2:3], st4[:, 2:3], op=ALU.mult), r=[st4], w=[msq])
                    S.op("dve", lambda e: e.tensor_tensor(msq[:], st4[:, 3:4], msq[:], op=ALU.subtract), r=[st4, msq], w=[msq])
                    rstd_from_ss(msq, rln, 1.0, eps5)
                    S.op("dve", lambda e: e.tensor_scalar(z[:], y[:], st4[:, 2:3], rln[:, 0:1], op0=ALU.subtract, op1=ALU.mult),
                         r=[y, st4, rln], w=[z])
                    S.op("pool", lambda e: e.tensor_tensor(z[:], z[:], lg_bc[:], op=ALU.mult), r=[z, lg_bc], w=[z])
                    S.op("pool", lambda e: e.tensor_tensor(z[:], z[:], lb_bc[:], op=ALU.add), r=[z, lb_bc], w=[z])
                    S.op("act", lambda e: e.activation(oc[:], z[:], AF.Silu), r=[z], w=[oc])
                    S.op("act", lambda e: e.activation(junk[:], oc[:], AF.Square, accum_out=ssc[:, 0:1]), r=[oc], w=[junk, ssc])
                    S.op("pool", lambda e: e.tensor_copy(ocb[:], oc[:]), r=[oc], w=[ocb])
                    for c in range(4):
                        S.op("pe", lambda e, c=c: e.transpose(ptc[:, c, :], ocb[:, c * P:(c + 1) * P], ident_b[:]),
                             r=[ocb, ident_b], w=[ptc])
                    S.op("act", lambda e: e.activation(ocT[:], ptc[:], AF.Copy), r=[ptc], w=[ocT])
                    for pr in range(4):
                        S.op("pool", lambda e, pr=pr: e.tensor_tensor(sqo[:, pr, :], osrc(pr), osrc(pr), op=ALU.mult), r=obufs, w=[sqo])
                    for pr in range(4):
                        S.op("pe", lambda e, pr=pr: e.matmul(pss[:, 0:2], lhsT=sqo[:, pr, :], rhs=ones_f[:, 0:2],
                                                            start=(pr == 0), stop=(pr == 3)), r=[sqo, ones_f], w=[pss])
                    S.op("dve", lambda e: e.tensor_copy(ssc[:, 1:2], pss[:, 0:1]), r=[pss], w=[ssc])
                    rstd_from_ss(ssc, rsc, 1.0 / 512, eps6)
                    for cc in range(2):
                        for pr in range(4):
                            S.op("pe", lambda e, cc=cc, pr=pr: e.matmul(pa[cc][:], lhsT=osrc(pr), rhs=wo[:, pr, cc * 512:(cc + 1) * 512],
                                                                      start=(pr == 0), stop=(pr == 3)), r=obufs + [wo], w=[pa[cc]])
                        for c in range(4):
                            S.op("pe", lambda e, cc=cc, c=c: e.matmul(pc[cc][:], lhsT=ocT[:, c, :], rhs=wo[:, 4 + c, cc * 512:(cc + 1) * 512],
                                                                    start=(c == 0), stop=(c == 3)), r=[ocT, wo], w=[pc[cc]])
                        sl = slice(cc * 512, (cc + 1) * 512)
                        S.op("dve", lambda e, cc=cc, sl=sl: e.tensor_scalar(t1[:, sl], pa[cc][:], rsc[:, 1:2], None, op0=ALU.mult),
                             r=[pa[cc], rsc], w=[t1])
                        S.op("dve", lambda e, cc=cc, sl=sl: e.scalar_tensor_tensor(out=t1[:, sl], in0=pc[cc][:], scalar=rsc[:, 0:1],
                                                                                   in1=t1[:, sl], op0=ALU.mult, op1=ALU.add),
                             r=[pc[cc], rsc, t1], w=[t1])
                    S.op("pool", lambda e: e.tensor_tensor(t1[:], t1[:], modbc[:, 0:1024], op=ALU.mult), r=[t1, modbc], w=[t1])
                    S.op("pool", lambda e: e.tensor_tensor(x1[k][:], t1[:], xt[k][:], op=ALU.add), r=[t1, xt[k]], w=[x1[k]])
                    S.dma("sp", lambda e: e.dma_start(out=xs_d[i * P:(i + 1) * P, :], in_=x1[k][:]), x1_slot[k], r=[x1[k]], w=[xs_buf[i]])
                    if l == 0:
                        dbg_store("d_x1", slice(i * P, (i + 1) * P), x1[k][:], [x1[k]])
                S.barrier()

        def phase_p(l, tiles, modbc, last):
            with ExitStack() as ph:
                wq = sbt(ph, "wq", [P, 8, D], F32)
                keysTz = sbt(ph, "keysTz", [P, 8, 2, P], F32)
                iota16 = sbt(ph, "iota16", [P, 16], F32)
                xt = [sbt(ph, f"xtP{k}", [P, D], F32) for k in range(2)]
                xt_slot = [DmaSlot(S, f"xtP{k}") for k in range(2)]
                junk = sbt(ph, "junkP", [P, D], BF16)
                junkr = sbt(ph, "junkR", [P, D], BF16)
                prod = [sbt(ph, f"prod{k}", [P, D], BF16) for k in range(3)]
                hb = [sbt(ph, f"hb{k}", [P, D], BF16) for k in range(2)]
                pi = [0]
                ss = sbt(ph, "ssP", [P, 1], F32)
                rstd = sbt(ph, "rstdP", [P, 1], F32)
                ssf = sbt(ph, "ssF", [P, 1], F32)
                rstdf = sbt(ph, "rstdF", [P, 1], F32)
                h = [sbt(ph, "hP", [P, D], F32)] * 2
                hT = sbt(ph, "hT", [P, 8, P], F32)
                qpT = hT
                qp = sbt(ph, "qp", [P, D], F32)
                sc = sbt(ph, "scP", [P, 16, 128], F32)
                wk = sbt(ph, "wkP", [P, 16, 128], F32)
                eq_ap = wk[:].rearrange("p a b -> p (a b)").rearrange("p (x c) -> p x c", c=16)
                topv = sbt(ph, "topv", [P, 16, 16], F32)
                idx = sbt(ph, "idxP", [P, 16, 16], U32)
                idxf = sbt(ph, "idxf", [P, 16, 16], F32)
                cand = sbt(ph, "cand", [P, 8, 256], F32)
                wk2_ap = sc[:].rearrange("p a b -> p (a b)").rearrange("p (h x) -> p h x", h=8)
                best = sbt(ph, "best", [P, 8, 16], F32)
                pos = sbt(ph, "pos", [P, 8, 16], U32)
                pab = sbt(ph, "pab", [P, 2, 128], U32)
                abf = sbt(ph, "abf", [P, 2, 128], F32)
                isel = sbt(ph, "isel", [P, 2, 128], F32)
                ef = [sbt(ph, f"ef{k}", [P, 128], F32) for k in range(2)]
                eidx = [sbt(ph, f"eidx{k}", [P, 128], I32) for k in range(2)]
                ew = sbt(ph, "ew", [P, 128], F32)
                se = sbt(ph, "se", [P, 8], F32)
                wgt = [sbt(ph, f"wgt{k}", [P, 128], F32) for k in range(2)]
                pre = sbt(ph, "pre", [P, 128], F32)
                aw = sbt(ph, "aw", [P, 128], F32)
                gbuf = [sbt(ph, f"gbuf{k}", [P, 2 * D], BF16) for k in range(NSLOT)]
                diag = [sbt(ph, f"diag{k}", [P, P], BF16) for k in range(6)]
                pre_b = [Buf(f"pre_b{k}") for k in range(4)]
                pre_d = [Buf(f"pre_d{k}") for k in range(4)]
                di = [0]
                gslot = [DmaSlot(S, f"g{k}") for k in range(NSLOT)]
                x2 = [sbt(ph, "x2_0", [P, D], F32)] * 2
                x2_slot = [DmaSlot(S, f"x2s{k}") for k in range(2)]
                fg_bc = sbt(ph, "fg_bc", [P, D], F32)
                pT2 = pst(ph, "pT2", [P, 8, P], F32)
                pqp = [pst(ph, f"pqp{k}", [P, 512], F32) for k in range(2)]
                pcs = [pst(ph, f"pcs{k}", [P, 512], F32) for k in range(2)]
                pva = [pst(ph, f"pva{k}", [P, 512], F32) for k in range(2)]
                psc = pqp + pcs
                print(f"[build] phase_p sbuf remaining {nc.sbuf_bytes_remaining}")

                S.op("pool", lambda e: e.iota(iota16[:], pattern=[[1, 16]], base=0, channel_multiplier=0,
                                              allow_small_or_imprecise_dtypes=True), w=[iota16])
                ws = DmaSlot(S, "wqload", group=True)
                for j in range(8):
                    S.dma("sp", lambda e, j=j: e.dma_start(out=wq[:, j, :], in_=peer_wq_d[l, j * P:(j + 1) * P, :]), ws, w=[wq])
                kraw_ap = sc[:].rearrange("p a b -> p (a b)")[:, 0:1024].rearrange("p (a d) -> p a d", d=64)
                keysT_ap = cand[:].rearrange("p a b -> p (a b)")[:, 0:1024].rearrange("p (a k) -> p a k", k=P)
                S.dma("sp", lambda e: e.dma_start(out=kraw_ap, in_=peer_keys_d[l].rearrange("h c k d -> k (h c) d")), ws, w=[sc])
                if last:
                    S.dma("sp", lambda e: e.dma_start(out=fg_bc[:], in_=final_g_d[0].partition_broadcast(P)), ws, w=[fg_bc])
                ws.close()
                for hh in range(8):
                    S.op("pe", lambda e, hh=hh: e.transpose(pT2[:, hh, :], kraw_ap[:, 2 * hh:2 * hh + 2, :].rearrange("p c d -> p (c d)"), ident_f[:]),
                         r=[sc, ident_f], w=[pT2])
                S.op("act", lambda e: e.activation(keysT_ap, pT2[:], AF.Copy), r=[pT2], w=[cand])
                S.op("pool", lambda e: e.memset(keysTz[:], 0.0), w=[keysTz])
                S.op("dve", lambda e: e.tensor_copy(keysTz[0:64, :, 0, :], keysT_ap[0:64, :, :]), r=[cand], w=[keysTz])
                S.op("dve", lambda e: e.tensor_copy(keysTz[64:128, :, 1, :], keysT_ap[64:128, :, :]), r=[cand], w=[keysTz])

                gi = [0]

                def routing(n_):
                    rq = []
                    i = tiles[n_]
                    k = n_ % 2
                    xk, hk, efk, eik, wgk = xt[k], h[k], ef[k], eidx[k], wgt[k]
                    hbk = hb[k]

                    def op(eng, fn, r=(), w=()):
                        rq.append(("op", eng, fn, r, w))

                    rq.append(("dma", "sp", lambda e: e.dma_start(out=xk[:], in_=xs_d[i * P:(i + 1) * P, :]), xt_slot[k], [xs_buf[i]], [xk]))
                    op("act", lambda e: e.activation(junkr[:], xk[:], AF.Square, accum_out=ss[:]), r=[xk], w=[junkr, ss])
                    op("act", lambda e: e.activation(rstd[:], ss[:], AF.Sqrt, scale=1.0 / D, bias=eps6[:, 0:1]), r=[ss, eps6], w=[rstd])
                    op("dve", lambda e: e.reciprocal(rstd[:], rstd[:]), r=[rstd], w=[rstd])
                    op("dve", lambda e: e.scalar_tensor_tensor(out=hk[:], in0=xk[:], scalar=rstd[:, 0:1], in1=modbc[:, 2048:3072],
                                                               op0=ALU.mult, op1=ALU.mult), r=[xk, rstd, modbc], w=[hk])
                    op("dve", lambda e: e.tensor_tensor(hk[:], hk[:], modbc[:, 1024:2048], op=ALU.add), r=[hk, modbc], w=[hk])
                    op("act", lambda e: e.activation(hbk[:], hk[:], AF.Copy), r=[hk], w=[hbk])
                    for j in range(8):
                        op("pe", lambda e, j=j: e.transpose(pT2[:, j, :], hk[:, j * P:(j + 1) * P], ident_f[:]), r=[hk, ident_f], w=[pT2])
                    op("act", lambda e: e.activation(hT[:], pT2[:], AF.Copy), r=[pT2], w=[hT])
                    for cc in range(2):
                        for j in range(8):
                            op("pe", lambda e, cc=cc, j=j: e.matmul(pqp[cc][:], lhsT=hT[:, j, :], rhs=wq[:, j, cc * 512:(cc + 1) * 512],
                                                                  start=(j == 0), stop=(j == 7)), r=[hT, wq], w=[pqp[cc]])
                        op("act", lambda e, cc=cc: e.activation(qp[:, cc * 512:(cc + 1) * 512], pqp[cc][:], AF.Copy), r=[pqp[cc]], w=[qp])
                    for hh in range(8):
                        op("pe", lambda e, hh=hh: e.transpose(pT2[:, hh, :], qp[:, hh * P:(hh + 1) * P], ident_f[:]), r=[qp, ident_f], w=[pT2])
                    op("act", lambda e: e.activation(qpT[:], pT2[:], AF.Copy), r=[pT2], w=[qpT])
                    for hh in range(8):
                        pv = psc[hh // 2][:].rearrange("p (a k) -> p a k", k=P)
                        op("pe", lambda e, hh=hh, pv=pv: e.matmul(pv[:, (hh % 2) * 2:(hh % 2) * 2 + 2, :], lhsT=qpT[:, hh, :],
                                                                 rhs=keysTz[:, hh, :, :], start=True, stop=True),
                           r=[qpT, keysTz], w=[psc[hh // 2]])
                    for q in range(4):
                        op("act", lambda e, q=q: e.activation(sc[:, 4 * q:4 * q + 4, :], psc[q][:].rearrange("p (a k) -> p a k", k=P), AF.Copy),
                           r=[psc[q]], w=[sc])
                    for g in range(16):
                        op("dve", lambda e, g=g: e.max(out=topv[:, g, 0:8], in_=sc[:, g, :]), r=[sc], w=[topv])
                        op("dve", lambda e, g=g: e.max_index(out=idx[:, g, 0:8], in_max=topv[:, g, 0:8], in_values=sc[:, g, :]),
                           r=[sc, topv], w=[idx])
                        op("dve", lambda e, g=g: e.match_replace(out=wk[:, g, :], in_to_replace=topv[:, g, 0:8], in_values=sc[:, g, :],
                                                                 imm_value=-1e30), r=[sc, topv], w=[wk])
                        op("dve", lambda e, g=g: e.max(out=topv[:, g, 8:16], in_=wk[:, g, :]), r=[wk], w=[topv])
                        op("dve", lambda e, g=g: e.max_index(out=idx[:, g, 8:16], in_max=topv[:, g, 8:16], in_values=wk[:, g, :]),
                           r=[wk, topv], w=[idx])
                    op("dve", lambda e: e.tensor_copy(idxf[:], idx[:]), r=[idx], w=[idxf])
                    tv4 = topv[:].rearrange("p (h c) a -> p h c a", c=2)
                    if4 = idxf[:].rearrange("p (h c) a -> p h c a", c=2)
                    op("dve", lambda e: e.tensor_tensor(cand[:].rearrange("p h (a b) -> p h a b", a=16),
                                                        tv4[:, :, 0, :].unsqueeze(3).to_broadcast([P, 8, 16, 16]),
                                                        tv4[:, :, 1, :].unsqueeze(2).to_broadcast([P, 8, 16, 16]), op=ALU.add),
                       r=[topv], w=[cand])
                    for hh in range(8):
                        op("dve", lambda e, hh=hh: e.max(out=best[:, hh, 0:8], in_=cand[:, hh, :]), r=[cand], w=[best])
                        op("dve", lambda e, hh=hh: e.max_index(out=pos[:, hh, 0:8], in_max=best[:, hh, 0:8], in_values=cand[:, hh, :]),
                           r=[cand, best], w=[pos])
                        op("dve", lambda e, hh=hh: e.match_replace(out=wk2_ap[:, hh, :], in_to_replace=best[:, hh, 0:8],
                                                                   in_values=cand[:, hh, :], imm_value=-1e30), r=[cand, best], w=[sc])
                        op("dve", lambda e, hh=hh: e.max(out=best[:, hh, 8:16], in_=wk2_ap[:, hh, :]), r=[sc], w=[best])
                        op("dve", lambda e, hh=hh: e.max_index(out=pos[:, hh, 8:16], in_max=best[:, hh, 8:16], in_values=wk2_ap[:, hh, :]),
                           r=[sc, best], w=[pos])
                    posf = pos[:].rearrange("p h s -> p (h s)")
                    op("dve", lambda e: e.tensor_single_scalar(pab[:, 0, :], posf, 4, op=ALU.logical_shift_right), r=[pos], w=[pab])
                    op("dve", lambda e: e.tensor_single_scalar(pab[:, 1, :], posf, 15, op=ALU.bitwise_and), r=[pos], w=[pab])
                    op("dve", lambda e: e.tensor_copy(abf[:], pab[:]), r=[pab], w=[abf])
                    for c in range(2):
                        op("dve", lambda e, c=c: e.tensor_tensor(eq_ap, iota16[:].unsqueeze(1).to_broadcast([P, 128, 16]), abf[:, c, :].unsqueeze(2).to_broadcast([P, 128, 16]),
                                                                 op=ALU.is_equal), r=[iota16, abf], w=[wk])
                        op("dve", lambda e, c=c: e.tensor_tensor(eq_ap.rearrange("p (h s) a -> p h s a", h=8),
                                                                 eq_ap.rearrange("p (h s) a -> p h s a", h=8),
                                                                 if4[:, :, c, :].unsqueeze(2).to_broadcast([P, 8, 16, 16]), op=ALU.mult),
                           r=[wk, idxf], w=[wk])
                        op("dve", lambda e, c=c: e.tensor_reduce(out=isel[:, c, :], in_=eq_ap, axis=AX.X, op=ALU.add), r=[wk], w=[isel])
                    op("dve", lambda e: e.scalar_tensor_tensor(out=efk[:], in0=isel[:, 0, :], scalar=128.0, in1=isel[:, 1, :],
                                                               op0=ALU.mult, op1=ALU.add), r=[isel], w=[efk])
                    op("dve", lambda e: e.tensor_scalar(efk[:], efk[:], float(l * NPEER), None, op0=ALU.add), r=[efk], w=[efk])
                    op("dve", lambda e: e.tensor_copy(eik[:], efk[:]), r=[efk], w=[eik])
                    op("dve", lambda e: e.tensor_tensor(ew[:].rearrange("p (h s) -> p h s", h=8), best[:],
                                                        best[:, :, 0:1].to_broadcast([P, 8, 16]), op=ALU.subtract), r=[best], w=[ew])
                    op("act", lambda e: e.activation(ew[:], ew[:], AF.Exp), r=[ew], w=[ew])
                    op("dve", lambda e: e.tensor_reduce(out=se[:], in_=ew[:].rearrange("p (h s) -> p h s", h=8), axis=AX.X, op=ALU.add),
                       r=[ew], w=[se])
                    op("dve", lambda e: e.reciprocal(se[:], se[:]), r=[se], w=[se])
                    op("dve", lambda e: e.tensor_tensor(wgk[:].rearrange("p (h s) -> p h s", h=8), ew[:].rearrange("p (h s) -> p h s", h=8),
                                                        se[:].unsqueeze(2).to_broadcast([P, 8, 16]), op=ALU.mult), r=[ew, se], w=[wgk])
                    return rq

                rq = routing(0)
                emit(rq, len(rq))
                for n_, i in enumerate(tiles):
                    k = n_ % 2
                    hbk, efk, eik, wgk = hb[k], ef[k], eidx[k], wgt[k]
                    rq = routing(n_ + 1) if n_ + 1 < len(tiles) else []
                    per_slot = -(-len(rq) // 112)
                    NG = 128 // GRP
                    gb = {}
                    for g_ in range(NG + 1):
                        if g_ < NG:
                            pb_ = pre_b[g_ % 4]
                            pd_ = pre_d[g_ % 4]
                            for s_ in range(g_ * GRP, (g_ + 1) * GRP):
                                b_ = gi[0] % NSLOT
                                gi[0] += 1
                                gb[s_] = b_
                                S.dma("pool", lambda e, s_=s_, b_=b_: e.indirect_dma_start(
                                    out=gbuf[b_][:], out_offset=None, in_=uv_d,
                                    in_offset=bass.IndirectOffsetOnAxis(ap=eik[:, s_:s_ + 1], axis=0)), gslot[b_], r=[eik], w=[gbuf[b_]])
                                pr_ = prod[pi[0] % len(prod)]
                                pi[0] += 1
                                S.op("dve", lambda e, b_=b_, pr_=pr_: e.tensor_tensor(pr_[:], hbk[:], gbuf[b_][:, 0:D], op=ALU.mult),
                                     r=[hbk, gbuf[b_]], w=[pr_])
                                S.op("act", lambda e, s_=s_, pr_=pr_: e.activation(junkr[:], pr_[:], AF.Identity, accum_out=pre[:, s_:s_ + 1]),
                                     r=[pr_], w=[junkr, pb_])
                                emit(rq, per_slot)
                            gs = slice(g_ * GRP, (g_ + 1) * GRP)
                            S.op("act", lambda e, gs=gs: e.activation(aw[:, gs], pre[:, gs], AF.Gelu), r=[pb_, pd_], w=[pb_])
                        if g_ >= 1:
                            gq = g_ - 1
                            pbq = pre_b[gq % 4]
                            for s_ in range(gq * GRP, (gq + 1) * GRP):
                                b_ = gb[s_]
                                dg = diag[di[0] % len(diag)]
                                di[0] += 1
                                S.op("dve", lambda e, s_=s_, dg=dg: e.tensor_scalar(dg[:], ident_b[:], aw[:, s_:s_ + 1], wgk[:, s_:s_ + 1],
                                                                                   op0=ALU.mult, op1=ALU.mult),
                                     r=[ident_b, pbq, wgk], w=[dg])
                                for cc in range(2):
                                    S.op("pe", lambda e, s_=s_, b_=b_, dg=dg, cc=cc: e.matmul(
                                        pva[cc][:], lhsT=dg[:], rhs=gbuf[b_][:, D + cc * 512:D + (cc + 1) * 512],
                                        start=(s_ == 0), stop=(s_ == 127)), r=[dg, gbuf[b_]], w=[pva[cc]])
                    if l == 0:
                        dbg_store("d_pre", slice(i * P, (i + 1) * P), pre[:], pre_b + pre_d)
                        dbg_store("d_eidx", slice(i * P, (i + 1) * P), efk[:], [efk])
                        dbg_store("d_w", slice(i * P, (i + 1) * P), wgk[:], [wgk])
                    for cc in range(2):
                        sl = slice(cc * 512, (cc + 1) * 512)
                        S.op("dve", lambda e, cc=cc, sl=sl: e.tensor_tensor(x2[k][:, sl], pva[cc][:], modbc[:, 3072 + cc * 512:3072 + (cc + 1) * 512],
                                                                           op=ALU.mult), r=[pva[cc], modbc], w=[x2[k]])
                    S.op("dve", lambda e: e.tensor_tensor(x2[k][:], x2[k][:], xt[k][:], op=ALU.add), r=[x2[k], xt[k]], w=[x2[k]])
                    if l == 0:
                        dbg_store("d_x2", slice(i * P, (i + 1) * P), x2[k][:], [x2[k]])
                    if last:
                        S.op("act", lambda e: e.activation(junk[:], x2[k][:], AF.Square, accum_out=ssf[:]), r=[x2[k]], w=[junk, ssf])
                        rstd_from_ss(ssf, rstdf, 1.0 / D, eps6)
                        S.op("dve", lambda e: e.scalar_tensor_tensor(out=x2[k][:], in0=x2[k][:], scalar=rstdf[:, 0:1], in1=fg_bc[:],
                                                                     op0=ALU.mult, op1=ALU.mult), r=[x2[k], rstdf, fg_bc], w=[x2[k]])
                        S.dma("sp", lambda e: e.dma_start(out=out_d[i * P:(i + 1) * P, :], in_=x2[k][:]), x2_slot[k], r=[x2[k]])
                    else:
                        S.dma("sp", lambda e: e.dma_start(out=xs_d[i * P:(i + 1) * P, :], in_=x2[k][:]), x2_slot[k], r=[x2[k]], w=[xs_buf[i]])
                    emit(rq, len(rq))
                S.barrier()

        def load_modbc(r_, modbc):
            S.dma("sp", lambda e: e.dma_start(out=modbc[:], in_=modscr_d[r_]), DmaSlot(S, "modld"), w=[modbc])

        lat_tiles = list(range(NTL))
        ctx_tiles = list(range(NTL, NT))
        for l in range(depth):
            last = (l == DEPTH - 1)
            if l == 0:
                src_aps = [x_d[i * P:(i + 1) * P, :] for i in range(NTL)] + [ctx_d[i * P:(i + 1) * P, :] for i in range(NTC)]
            else:
                src_aps = [xs_d[i * P:(i + 1) * P, :] for i in range(NT)]
            mod_cols(l)
            if l == 0:
                dbg_store("d_mod", (slice(0, P), slice(0, 32)), modcol[:].rearrange("p r c -> p (r c)"), [modcol])
                dbg_store("d_mod", (slice(0, P), slice(32, 48)), scT[:].rearrange("p j r -> p (j r)"), [scT])
            if stop == "m":
                break
            with ExitStack() as mix:
                M = {}
                M["qT"] = sbt(mix, "qT", [P, 4, S_LAT], BF16)
                M["qT_b"] = [[Buf(f"qT{pr}_{c}") for c in range(8)] for pr in range(4)]
                M["qTc"] = sbt(mix, "qTc", [P, 4, S_CTX], BF16)
                M["gT"] = sbt(mix, "gT", [P, 4, S_LAT + 2 * GPAD], BF16)
                M["gT_b"] = [Buf(f"gT{i}") for i in range(NTL)]
                M["gTc"] = sbt(mix, "gTc", [P, 4, S_CTX + 2 * GPAD], BF16)
                M["gTc_b"] = [Buf(f"gTc{i}") for i in range(NTC)]
                S.op("pool", lambda e: e.memset(M["gT"][:], 0.0), w=M["gT_b"])
                S.op("pool", lambda e: e.memset(M["gTc"][:], 0.0), w=M["gTc_b"])
                with ExitStack() as ab:
                    M["kT"] = sbt(ab, "kT", [P, NT * P], BF16)
                    M["kT_b"] = [Buf(f"kT{i}") for i in range(NT)]
                    M["Vp"] = sbt(ab, "Vp", [P, NT, 192], BF16)
                    M["Vp_b"] = [Buf(f"Vp{i}") for i in range(NT)]
                    S.op("pool", lambda e: e.memset(M["Vp"][:], 1.0), w=M["Vp_b"])
                    phase_a(l, M, src_aps, last)
                    if stop in ("a", "a0", "a1"):
                        break
                    phase_b(l, M, last)
                if stop == "b":
                    break
                with ExitStack() as cs:
                    modbc = sbt(cs, "modbcC", [P, 4096], F32)
                    for r_ in ((0,) if last else (0, 1)):
                        mod_bc(l, r_, modbc)
                        S.dma("sp", lambda e, r_=r_: e.dma_start(out=modscr_d[r_], in_=modbc[:]), DmaSlot(S, "modst"), r=[modbc])
                        S.barrier()
                    load_modbc(0, modbc)
                    phase_c(l, M, lat_tiles, src_aps, 0, modbc)
                    if not last:
                        load_modbc(1, modbc)
                        phase_c(l, M, ctx_tiles, src_aps, 1, modbc)
            if stop == "c":
                break
            with ExitStack() as pp:
                modbc = sbt(pp, "modbcP", [P, 4096], F32)
                load_modbc(0, modbc)
                phase_p(l, lat_tiles if stop not in ("p1", "p0") else lat_tiles[:2], modbc, last)
                if not last and stop not in ("p1", "p0"):
                    load_modbc(1, modbc)
                    phase_p(l, ctx_tiles, modbc, last)
            if stop in ("p1", "p0"):
                break
        out_slot.close()
        if out_slot.sem is not None:
            for en in ("sp", "act", "pool"):
                S.E[en].eng.wait_ge(out_slot.sem, out_slot.val)
        S.barrier()
        print(f"[build] instr={S.ninstr} waits={S.nwait} sems={S.nsem}")
    return nc


_NC_CACHE = {}


def _rope_tables():
    n = 16
    inv = (10000.0 ** (-np.arange(n, dtype=np.float32) / n)).astype(np.float32)
    row = np.repeat(np.arange(64), 64).astype(np.float32)
    col = np.tile(np.arange(64), 64).astype(np.float32)
    ang = np.concatenate([row[:, None] * inv, col[:, None] * inv], axis=-1).astype(np.float32)
    cos = np.cos(ang).astype(np.float32)
    sin = np.sin(ang).astype(np.float32)
    cos2 = np.concatenate([cos, cos], axis=-1)
    sin2 = np.concatenate([-sin, sin], axis=-1)
    return np.ascontiguousarray(cos2), np.ascontiguousarray(sin2)


def make_in_maps(inputs, cores):
    f = lambda a: np.ascontiguousarray(np.asarray(a, dtype=np.float32))
    cos2, sin2 = _rope_tables()
    shared = {k: f(inputs[k]) for k in ["w_mod", "b_mod", "w_in", "q_gain", "k_gain", "conv_w", "conv_b", "ln_g", "ln_b",
                                        "beta_attn", "beta_conv", "w_o", "peer_wq", "peer_keys", "peer_u", "peer_v"]}
    shared["final_g"] = f(inputs["final_g"]).reshape(1, D)
    shared["cos2"] = cos2
    shared["sin2"] = sin2
    shared["cctxT"] = np.ascontiguousarray(f(inputs["c_ctx"]).reshape(8, P).T)
    x = f(inputs["x"])
    ctx = f(inputs["ctx"])
    c = f(inputs["c"])
    maps = []
    for b in cores:
        m = dict(shared)
        m["x"] = x[b]
        m["ctx"] = ctx[b]
        m["cT"] = np.ascontiguousarray(c[b].reshape(8, P).T)
        maps.append(m)
    return maps


def kernel(**inputs):
    if "nc" not in _NC_CACHE:
        _NC_CACHE["nc"] = build()
    nc = _NC_CACHE["nc"]
    B = np.asarray(inputs["x"]).shape[0]
    maps = make_in_maps(inputs, list(range(B)))
    res = run_bass_kernel_spmd(nc, maps, core_ids=list(range(B)))
    out = np.stack([np.asarray(r["out"], dtype=np.float32) for r in res.results], axis=0)
    return out
```

```python
import numpy as np
from contextlib import ExitStack
import concourse.bass as bass
import concourse.mybir as mybir
from concourse.bass_utils import run_bass_kernel_spmd

F32 = mybir.dt.float32
BF16 = mybir.dt.bfloat16
I32 = mybir.dt.int32
U32 = mybir.dt.uint32
ALU = mybir.AluOpType
AF = mybir.ActivationFunctionType
AX = mybir.AxisListType

import os
CUT = int(os.environ.get("KCUT", "99"))
SEM_LIMIT = 30000


class Buf:
    __slots__ = ("name", "w", "r")

    def __init__(self, name):
        self.name = name
        self.w = None
        self.r = {}


class Eng:
    def __init__(self, sync, name, eng):
        self.sync = sync
        self.name = name
        self.eng = eng
        self.sem = None
        self.count = 0
        self.known = {}
        self.last = None

    def new_event(self):
        if self.sem is None or self.count >= SEM_LIMIT:
            self.sem = self.sync.new_sem(self.name)
            self.count = 0
        self.count += 1
        self.last = [self.sem, self.count, self.name]
        return self.last


def DmaSlot(sync, name, group=False):
    if name not in sync.slot_by_name:
        sync.slot_by_name[name] = _DmaSlot(sync, name, group)
    return sync.slot_by_name[name]


class _DmaSlot:
    def __init__(self, sync, name, group=False):
        self.name = name
        self.sem = None
        self.val = 0
        self.group = group
        self.pending = []
        sync.slots.append(self)

    def bump(self, sync):
        if self.sem is None or (self.val >= SEM_LIMIT and not self.pending):
            self.sem = sync.new_sem("d" + self.name)
            self.val = 0
        self.val += 16
        ev = [self.sem, self.val, "dma"]
        if self.group:
            self.pending.append(ev)
        return ev

    def close(self):
        for ev in self.pending:
            ev[1] = self.val
        self.pending = []


class Sync:
    def __init__(self, nc, stack):
        self.nc = nc
        self.stack = stack
        self.nsem = 0
        self.slots = []
        self.slot_by_name = {}
        self.E = {
            "pe": Eng(self, "pe", nc.tensor),
            "dve": Eng(self, "dve", nc.vector),
            "act": Eng(self, "act", nc.scalar),
            "pool": Eng(self, "pool", nc.gpsimd),
            "sp": Eng(self, "sp", nc.sync),
        }
        self.ninstr = 0
        self.nwait = 0

    def new_sem(self, name):
        self.nsem += 1
        return self.stack.enter_context(self.nc.semaphore(f"s{self.nsem}_{name}"))

    def _wait(self, E, evs):
        best = {}
        for ev in evs:
            if ev is None:
                continue
            if E.name == "pe" and ev[2] == "pe":
                continue
            k = id(ev[0])
            if k not in best or best[k][1] < ev[1]:
                best[k] = ev
        for ev in best.values():
            sem, val, src = ev
            if E.known.get(id(sem), 0) >= val:
                continue
            E.eng.wait_ge(sem, val)
            self.nwait += 1
            E.known[id(sem)] = val

    def _deps(self, E, reads, writes):
        evs = []
        for b in reads:
            evs.append(b.w)
        for b in writes:
            if b.w is not None and b.w[2] != E.name:
                evs.append(b.w)
            for ev in b.r.values():
                if ev[2] == E.name:
                    continue
                evs.append(ev)
        return evs

    @staticmethod
    def _bufs(lst):
        return [t.b if hasattr(t, "b") else t for t in lst]

    def op(self, engname, fn, r=(), w=()):
        E = self.E[engname]
        r = self._bufs(r)
        w = self._bufs(w)
        self._wait(E, self._deps(E, r, w))
        ins = fn(E.eng)
        ev = E.new_event()
        ins.then_inc(ev[0], 1)
        self.ninstr += 1
        key = ev[2] if ev[2] != "dma" else id(ev[0])
        for b in r:
            b.r[key] = ev
        for b in w:
            b.w = ev
            b.r = {}
        return ev

    def dma(self, qname, fn, slot, r=(), w=()):
        E = self.E[qname]
        r = self._bufs(r)
        w = self._bufs(w)
        self._wait(E, self._deps(E, r, w))
        ins = fn(E.eng)
        ev = slot.bump(self)
        ins.then_inc(ev[0], 16)
        self.ninstr += 1
        key = ev[2] if ev[2] != "dma" else id(ev[0])
        for b in r:
            b.r[key] = ev
        for b in w:
            b.w = ev
            b.r = {}
        return ev

    def barrier(self):
        for s in self.slots:
            if s.group:
                s.close()
        evs = []
        for E in self.E.values():
            if E.last is not None:
                evs.append(E.last)
        for s in self.slots:
            if s.sem is not None:
                evs.append([s.sem, s.val, "dma"])
        for E in self.E.values():
            self._wait(E, [ev for ev in evs if not (E.name == "pe" and ev[2] == "pe")])


class Tile:
    def __init__(self, h, name):
        self.h = h
        self.b = Buf(name)

    def __getitem__(self, k):
        return self.h[k]


P = 128
D = 1024
S_LAT = 4096
S_CTX = 256
NTL = 32
NTC = 2
NT = NTL + NTC
DEPTH = 2
INC = 1792
EPS = 1e-6
NPEER = 16384
GPAD = 16
NSLOT = 17
GRP = 4


def build(depth=DEPTH, dbg=False, stop=None):
    nc = bass.Bass("TRN2", target_bir_lowering=False)

    def din(name, shape, dt=F32):
        return nc.dram_tensor(name, shape, dt, kind="ExternalInput").ap()

    x_d = din("x", [S_LAT, D])
    ctx_d = din("ctx", [S_CTX, D])
    cT_d = din("cT", [P, 8])
    cctxT_d = din("cctxT", [P, 8])
    w_mod_d = din("w_mod", [DEPTH, D, 6 * D])
    b_mod_d = din("b_mod", [DEPTH, 6 * D])
    w_in_d = din("w_in", [DEPTH, D, INC])
    q_gain_d = din("q_gain", [DEPTH, 64])
    k_gain_d = din("k_gain", [DEPTH, 64])
    conv_w_d = din("conv_w", [DEPTH, 31, 512])
    conv_b_d = din("conv_b", [DEPTH, 512])
    ln_g_d = din("ln_g", [DEPTH, 512])
    ln_b_d = din("ln_b", [DEPTH, 512])
    beta_attn_d = din("beta_attn", [DEPTH, 512])
    beta_conv_d = din("beta_conv", [DEPTH, 512])
    w_o_d = din("w_o", [DEPTH, D, D])
    peer_wq_d = din("peer_wq", [DEPTH, D, D])
    peer_keys_d = din("peer_keys", [DEPTH, 8, 2, 128, 64])
    peer_u_d = din("peer_u", [DEPTH, NPEER, D])
    peer_v_d = din("peer_v", [DEPTH, NPEER, D])
    final_g_d = din("final_g", [1, D])
    cos2_d = din("cos2", [S_LAT, 64])
    sin2_d = din("sin2", [S_LAT, 64])
    peer_u_flat = peer_u_d.rearrange("l n d -> (l n) d")
    peer_v_flat = peer_v_d.rearrange("l n d -> (l n) d")
    out_d = nc.dram_tensor("out", [S_LAT, D], F32, kind="ExternalOutput").ap()
    xs_d = nc.dram_tensor("xs", [S_LAT + S_CTX, D], F32).ap()
    uv_d = nc.dram_tensor("uv", [DEPTH * NPEER, 2 * D], BF16).ap()
    modscr_d = nc.dram_tensor("modscr", [2, P, 4096], F32).ap()
    dbg_d = {}
    if dbg:
        for nm, shp in [("d_qkvu", [NT * P, INC]), ("d_x1", [NT * P, D]), ("d_oT", [P, 4 * S_LAT]),
                        ("d_pre", [NT * P, 128]), ("d_eidx", [NT * P, 128]), ("d_w", [NT * P, 128]),
                        ("d_qT", [P, 4 * S_LAT]), ("d_x2", [NT * P, D]), ("d_mod", [P, 4096])]:
            dbg_d[nm] = nc.dram_tensor(nm, shp, F32, kind="ExternalOutput").ap()

    top = ExitStack()
    with top:
        S = Sync(nc, top)

        uid = [0]

        def sbt(stack, name, shape, dt):
            uid[0] += 1
            name = f"{name}_u{uid[0]}"
            return Tile(stack.enter_context(nc.sbuf_tensor(name, shape, dt)), name)

        def pst(stack, name, shape, dt):
            uid[0] += 1
            name = f"{name}_u{uid[0]}"
            return Tile(stack.enter_context(nc.psum_tensor(name, shape, dt)), name)

        out_slot = DmaSlot(S, "out", group=True)
        dbg_slots = {}
        xs_buf = [Buf(f"xs{i}") for i in range(NT)]

        ident_f = sbt(top, "ident_f", [P, P], F32)
        ident_b = sbt(top, "ident_b", [P, P], BF16)
        ones_f = sbt(top, "ones_f", [P, P], F32)
        io_t = sbt(top, "io_t", [P, P], F32)
        pid_t = sbt(top, "pid_t", [P, 1], F32)
        S.op("pool", lambda e: e.iota(io_t[:], pattern=[[1, P]], base=0, channel_multiplier=0,
                                      allow_small_or_imprecise_dtypes=True), w=[io_t])
        S.op("pool", lambda e: e.iota(pid_t[:], pattern=[[0, 1]], base=0, channel_multiplier=1,
                                      allow_small_or_imprecise_dtypes=True), w=[pid_t])
        S.op("dve", lambda e: e.tensor_scalar(ident_f[:], io_t[:], pid_t[:, 0:1], None, op0=ALU.is_equal),
             r=[io_t, pid_t], w=[ident_f])
        S.op("dve", lambda e: e.tensor_copy(ident_b[:], ident_f[:]), r=[ident_f], w=[ident_b])
        S.op("pool", lambda e: e.memset(ones_f[:], 1.0), w=[ones_f])

        cslot = DmaSlot(S, "const", group=True)
        craw = sbt(top, "craw", [P, 2, 8], F32)
        scT = sbt(top, "scT", [P, 8, 2], F32)
        S.dma("sp", lambda e: e.dma_start(out=craw[:, 0, :], in_=cT_d), cslot, w=[craw])
        S.dma("sp", lambda e: e.dma_start(out=craw[:, 1, :], in_=cctxT_d), cslot, w=[craw])
        cslot.close()
        S.op("act", lambda e: e.activation(scT[:].rearrange("p j r -> p r j"), craw[:], AF.Silu), r=[craw], w=[scT])
        modcol = sbt(top, "modcol", [P, 2, 16], F32)
        eps6 = sbt(top, "eps6", [P, 1], F32)
        eps5 = sbt(top, "eps5", [P, 1], F32)
        S.op("pool", lambda e: e.memset(eps6[:], 1e-6), w=[eps6])
        S.op("pool", lambda e: e.memset(eps5[:], 1e-5), w=[eps5])


        def rstd_from_ss(ss, rs, scale, eps_t):
            S.op("act", lambda e: e.activation(rs[:], ss[:], AF.Sqrt, scale=scale, bias=eps_t[:, 0:1]), r=[ss, eps_t], w=[rs])
            S.op("dve", lambda e: e.reciprocal(rs[:], rs[:]), r=[rs], w=[rs])

        def dbg_store(name, rows, tile_ap, rtiles):
            if dbg and name in dbg_d:
                if name not in dbg_slots:
                    dbg_slots[name] = (DmaSlot(S, name), Buf(name))
                S.dma("sp", lambda e: e.dma_start(out=dbg_d[name][rows], in_=tile_ap), dbg_slots[name][0], r=rtiles, w=[dbg_slots[name][1]])


        def convert_thunks(ph, NB=3):
            stg = [sbt(ph, f"cvi{k}", [P, 4, D], F32) for k in range(NB)]
            obf = [sbt(ph, f"cvo{k}", [P, 4, D], BF16) for k in range(NB)]
            islot = [DmaSlot(S, f"cvi{k}") for k in range(NB)]
            oslot = [DmaSlot(S, f"cvo{k}") for k in range(NB)]
            steps = []
            n = 0
            for l in range(DEPTH):
                for tab, col0 in ((peer_u_d, 0), (peer_v_d, D)):
                    for t in range(NPEER // 512):
                        k = n % NB
                        src = tab[l, t * 512:(t + 1) * 512, :].rearrange("(p r) d -> p r d", r=4)
                        dst = uv_d[l * NPEER + t * 512:l * NPEER + (t + 1) * 512, col0:col0 + D].rearrange("(p r) d -> p r d", r=4)
                        steps.append((
                            ("dma", "sp", lambda e, k=k, src=src: e.dma_start(out=stg[k][:], in_=src), islot[k], [], [stg[k]]),
                            ("op", "dve", lambda e, k=k: e.tensor_copy(obf[k][:], stg[k][:]), [stg[k]], [obf[k]]),
                            ("dma", "pool", lambda e, k=k, dst=dst: e.dma_start(out=dst, in_=obf[k][:]), oslot[k], [obf[k]], []),
                        ))
                        n += 1
            cq = []
            for n in range(len(steps) + 2):
                if n < len(steps):
                    cq.append(steps[n][0])
                if n >= 2:
                    cq.append(steps[n - 2][1])
                    cq.append(steps[n - 2][2])
            return cq

        def emit(rq, n):
            for _ in range(n):
                if not rq:
                    return
                t = rq.pop(0)
                if t[0] == "op":
                    S.op(t[1], t[2], t[3], t[4])
                else:
                    S.dma(t[1], t[2], t[3], t[4], t[5])

        def mod_cols(l):
            with ExitStack() as ph:
                wm = [sbt(ph, f"wm{k}", [P, 8, P], F32) for k in range(2)]
                wslot = [DmaSlot(S, f"wm{k}") for k in range(2)]
                bcol = sbt(ph, "bcol", [P, 16], F32)
                pcol = pst(ph, "pcol", [P, 16, 2], F32)
                S.dma("sp", lambda e: e.dma_start(out=bcol[:], in_=b_mod_d[l, 0:2048].rearrange("(c p) -> p c", p=P),
                                                  allow_slow_non_contiguous=True), DmaSlot(S, "bcol"), w=[bcol])
                for cc in range(16):
                    t = wm[cc % 2]
                    S.dma("sp", lambda e, t=t, cc=cc: e.dma_start(
                        out=t[:], in_=w_mod_d[l, :, cc * P:(cc + 1) * P].rearrange("(j p) m -> p j m", p=P)),
                          wslot[cc % 2], w=[t])
                    for j in range(8):
                        S.op("pe", lambda e, t=t, j=j, cc=cc: e.matmul(pcol[:, cc, :], lhsT=t[:, j, :],
                                                                     rhs=scT[:, j, :], start=(j == 0), stop=(j == 7)),
                             r=[t, scT], w=[pcol])
                S.op("dve", lambda e: e.tensor_tensor(modcol[:].rearrange("p r c -> p c r"), pcol[:],
                                                      bcol[:].unsqueeze(2).to_broadcast([P, 16, 2]), op=ALU.add),
                     r=[pcol, bcol], w=[modcol])
                S.op("dve", lambda e: e.tensor_scalar(modcol[:, :, 8:16], modcol[:, :, 8:16], 1.0, None, op0=ALU.add),
                     r=[modcol], w=[modcol])
                S.barrier()

        def mod_bc(l, r, modbc):
            with ExitStack() as ph:
                NW = 4
                wm = [sbt(ph, f"wmb{k}", [P, 512], F32) for k in range(NW)]
                wslot = [DmaSlot(S, f"wmb{k}") for k in range(NW)]
                bbc = [sbt(ph, f"bbc{k}", [P, 512], F32) for k in range(2)]
                bslot = [DmaSlot(S, f"bbc{k}") for k in range(2)]
                screp = sbt(ph, "screp", [P, 8, P], F32)
                pb = pst(ph, "pbm", [P, 512], F32)
                S.op("dve", lambda e: e.tensor_copy(screp[:], scT[:, :, r].unsqueeze(2).to_broadcast([P, 8, P])),
                     r=[scT], w=[screp])
                n = 0
                for cc in range(8):
                    c0 = 2048 + cc * 512
                    S.dma("sp", lambda e, cc=cc, c0=c0: e.dma_start(out=bbc[cc % 2][:], in_=b_mod_d[l, c0:c0 + 512].partition_broadcast(P)),
                          bslot[cc % 2], w=[bbc[cc % 2]])
                    for j in range(8):
                        t = wm[n % NW]
                        S.dma("sp", lambda e, t=t, j=j, c0=c0: e.dma_start(out=t[:], in_=w_mod_d[l, j * P:(j + 1) * P, c0:c0 + 512]),
                              wslot[n % NW], w=[t])
                        n += 1
                        S.op("pe", lambda e, t=t, j=j: e.matmul(pb[:], lhsT=screp[:, j, :], rhs=t[:], start=(j == 0), stop=(j == 7)),
                             r=[t, screp], w=[pb])
                    S.op("dve", lambda e, cc=cc: e.tensor_tensor(modbc[:, cc * 512:(cc + 1) * 512], pb[:], bbc[cc % 2][:], op=ALU.add),
                         r=[pb, bbc[cc % 2]], w=[modbc])
                S.op("dve", lambda e: e.tensor_scalar(modbc[:, 2048:3072], modbc[:, 2048:3072], 1.0, None, op0=ALU.add),
                     r=[modbc], w=[modbc])
                S.barrier()

        def phase_a(l, M, src_aps, last):
            with ExitStack() as ph:
                win = sbt(ph, "win", [P, 8, INC], BF16)
                stg = sbt(ph, "stg", [P, INC], F32)
                stg_slot = DmaSlot(S, "stg")
                bias_bc = sbt(ph, "bias_bc", [P, INC], F32)
                sh1rep = sbt(ph, "sh1rep", [P, 2, 8, P], BF16)
                gain = sbt(ph, "gain", [P, 640], F32)
                graw = sbt(ph, "graw", [P, 128], F32)
                cos2 = sbt(ph, "cos2", [P, NTL, 64], F32)
                sin2 = sbt(ph, "sin2", [P, NTL, 64], F32)
                xt = [sbt(ph, f"xt{k}", [P, D], F32) for k in range(2)]
                xt_slot = [DmaSlot(S, f"xt{k}") for k in range(2)]
                xb = [sbt(ph, f"xb{k}", [P, D], BF16) for k in range(2)]
                xT = [sbt(ph, f"xT{k}", [P, 8, P], BF16) for k in range(2)]
                junk = sbt(ph, "junkA", [P, D], BF16)
                ss = [sbt(ph, f"ssA{k}", [P, 1], F32) for k in range(2)]
                rstd = [sbt(ph, f"rstdA{k}", [P, 1], F32) for k in range(2)]
                qkvu = sbt(ph, "qkvu", [P, INC], F32)
                sq = sbt(ph, "sq", [P, 640], F32)
                ssq = sbt(ph, "ssq", [P, 10], F32)
                rsq = sbt(ph, "rsq", [P, 10], F32)
                qn = sbt(ph, "qn", [P, 640], F32)
                rb = sbt(ph, "rb", [P, 640], F32)
                qr = sbt(ph, "qr", [P, 4, 2, 64], BF16)
                kr = sbt(ph, "kr", [P, 128], BF16)
                sig = sbt(ph, "sig", [P, 512], F32)
                gg = sbt(ph, "gg", [P, 512], BF16)
                ptb = pst(ph, "ptb", [P, 8, P], BF16)
                pq = [pst(ph, f"pq{k}", [P, 512], F32) for k in range(4)]
                ptq = pst(ph, "ptq", [P, 5, P], BF16)
                ptg = pst(ph, "ptg", [P, 4, P], BF16)
                ra = sq
                qT, qTc, kT, Vp, gT, gTc = M["qT"], M["qTc"], M["kT"], M["Vp"], M["gT"], M["gTc"]

                cslot2 = DmaSlot(S, "rope", group=True)
                S.dma("sp", lambda e: e.dma_start(out=cos2[:], in_=cos2_d.rearrange("(n p) f -> p n f", p=P)), cslot2, w=[cos2])
                S.dma("sp", lambda e: e.dma_start(out=sin2[:], in_=sin2_d.rearrange("(n p) f -> p n f", p=P)), cslot2, w=[sin2])
                S.dma("sp", lambda e: e.dma_start(out=graw[:, 0:64], in_=q_gain_d[l].partition_broadcast(P)), cslot2, w=[graw])
                S.dma("sp", lambda e: e.dma_start(out=graw[:, 64:128], in_=k_gain_d[l].partition_broadcast(P)), cslot2, w=[graw])
                cslot2.close()
                for j in range(8):
                    S.dma("sp", lambda e, j=j: e.dma_start(out=stg[:], in_=w_in_d[l, j * P:(j + 1) * P, :]), stg_slot, w=[stg])
                    S.op("act", lambda e, j=j: e.activation(win[:, j, :], stg[:], AF.Copy), r=[stg], w=[win])
                S.op("dve", lambda e: e.tensor_copy(gain[:, 0:512].rearrange("p (h d) -> p h d", h=8),
                                                    graw[:, 0:64].unsqueeze(1).to_broadcast([P, 8, 64])), r=[graw], w=[gain])
                S.op("dve", lambda e: e.tensor_copy(gain[:, 512:640].rearrange("p (h d) -> p h d", h=2),
                                                    graw[:, 64:128].unsqueeze(1).to_broadcast([P, 2, 64])), r=[graw], w=[gain])
                S.op("dve", lambda e: e.tensor_copy(sh1rep[:], modcol[:, :, 0:8].unsqueeze(3).to_broadcast([P, 2, 8, P])),
                     r=[modcol], w=[sh1rep])

                def make_bias(r_):
                    for cc in range(4):
                        w_ = min(512, INC - cc * 512)
                        for j in range(8):
                            S.op("pe", lambda e, cc=cc, j=j, w_=w_: e.matmul(
                                pq[cc][:, 0:w_], lhsT=sh1rep[:, r_, j, :], rhs=win[:, j, cc * 512:cc * 512 + w_],
                                start=(j == 0), stop=(j == 7)), r=[sh1rep, win], w=[pq[cc]])
                        S.op("act", lambda e, cc=cc, w_=w_: e.activation(
                            bias_bc[:, cc * 512:cc * 512 + w_], pq[cc][:, 0:w_], AF.Copy), r=[pq[cc]], w=[bias_bc])

                def load(i):
                    k = i % 2
                    S.dma("sp", lambda e: e.dma_start(out=xt[k][:], in_=src_aps[i]), xt_slot[k], r=[xs_buf[i]], w=[xt[k]])

                make_bias(0)
                load(0)
                for i in range(NT if stop not in ("a0", "a1") else (0 if stop == "a0" else 1)):
                    k = i % 2
                    r_ = 0 if i < NTL else 1
                    is_ctx = i >= NTL
                    if i == NTL:
                        make_bias(1)
                    if i + 1 < NT:
                        load(i + 1)
                    S.op("act", lambda e: e.activation(junk[:], xt[k][:], AF.Square, accum_out=ss[k][:]), r=[xt[k]], w=[junk, ss[k]])
                    S.op("act", lambda e: e.activation(xb[k][:], xt[k][:], AF.Copy), r=[xt[k]], w=[xb[k]])
                    rstd_from_ss(ss[k], rstd[k], 1.0 / D, eps6)
                    for j in range(8):
                        S.op("pe", lambda e, j=j: e.transpose(ptb[:, j, :], xb[k][:, j * P:(j + 1) * P], ident_b[:]),
                             r=[xb[k], ident_b], w=[ptb])
                    S.op("dve", lambda e: e.tensor_tensor(xT[k][:], ptb[:],
                                                          modcol[:, r_, 8:16].unsqueeze(2).to_broadcast([P, 8, P]), op=ALU.mult),
                         r=[ptb, modcol], w=[xT[k]])
                    if CUT <= 1:
                        continue
                    for cc in range(4):
                        w_ = min(512, INC - cc * 512)
                        for j in range(8):
                            S.op("pe", lambda e, cc=cc, j=j, w_=w_: e.matmul(
                                pq[cc][:, 0:w_], lhsT=xT[k][:, j, :], rhs=win[:, j, cc * 512:cc * 512 + w_],
                                start=(j == 0), stop=(j == 7)), r=[xT[k], win], w=[pq[cc]])
                        S.op("dve", lambda e, cc=cc, w_=w_: e.scalar_tensor_tensor(
                            out=qkvu[:, cc * 512:cc * 512 + w_], in0=pq[cc][:, 0:w_], scalar=rstd[k][:, 0:1],
                            in1=bias_bc[:, cc * 512:cc * 512 + w_], op0=ALU.mult, op1=ALU.add),
                             r=[pq[cc], rstd[k], bias_bc], w=[qkvu])
                    if l == 0:
                        dbg_store("d_qkvu", slice(i * P, (i + 1) * P), qkvu[:], [qkvu])
                    if CUT <= 2:
                        continue
                    S.op("pool", lambda e: e.tensor_tensor(sq[:], qkvu[:, 0:640], qkvu[:, 0:640], op=ALU.mult), r=[qkvu], w=[sq])
                    S.op("dve", lambda e: e.tensor_reduce(out=ssq[:], in_=sq[:].rearrange("p (h d) -> p h d", h=10),
                                                          axis=AX.X, op=ALU.add), r=[sq], w=[ssq])
                    rstd_from_ss(ssq, rsq, 1.0 / 64, eps6)
                    S.op("dve", lambda e: e.tensor_tensor(qn[:].rearrange("p (h d) -> p h d", h=10),
                                                          qkvu[:, 0:640].rearrange("p (h d) -> p h d", h=10),
                                                          rsq[:].unsqueeze(2).to_broadcast([P, 10, 64]), op=ALU.mult),
                         r=[qkvu, rsq], w=[qn])
                    S.op("pool", lambda e: e.tensor_tensor(qn[:], qn[:], gain[:], op=ALU.mult), r=[qn, gain], w=[qn])
                    if CUT <= 3:
                        continue
                    if not is_ctx:
                        qn3 = qn[:].rearrange("p (h d) -> p h d", h=10)
                        rb3 = rb[:].rearrange("p (h d) -> p h d", h=10)
                        S.op("dve", lambda e: e.tensor_tensor(ra[:].rearrange("p (h d) -> p h d", h=10), qn3,
                                                              cos2[:, i, :].unsqueeze(1).to_broadcast([P, 10, 64]), op=ALU.mult),
                             r=[qn, cos2], w=[ra])
                        S.op("pool", lambda e: e.tensor_tensor(rb3[:, :, 0:32], qn3[:, :, 32:64],
                                                               sin2[:, i, 0:32].unsqueeze(1).to_broadcast([P, 10, 32]), op=ALU.mult),
                             r=[qn, sin2], w=[rb])
                        S.op("pool", lambda e: e.tensor_tensor(rb3[:, :, 32:64], qn3[:, :, 0:32],
                                                               sin2[:, i, 32:64].unsqueeze(1).to_broadcast([P, 10, 32]), op=ALU.mult),
                             r=[qn, sin2], w=[rb])
                        S.op("dve", lambda e: e.tensor_tensor(qr[:].rearrange("p pr hh d -> p hh pr d"),
                                                              ra[:, 0:512].rearrange("p (hh pr d) -> p hh pr d", hh=2, pr=4),
                                                              rb[:, 0:512].rearrange("p (hh pr d) -> p hh pr d", hh=2, pr=4), op=ALU.add),
                             r=[ra, rb], w=[qr])
                        S.op("pool", lambda e: e.tensor_tensor(kr[:], ra[:, 512:640], rb[:, 512:640], op=ALU.add), r=[ra, rb], w=[kr])
                    else:
                        S.op("dve", lambda e: e.tensor_copy(qr[:].rearrange("p pr hh d -> p hh pr d"),
                                                            qn[:, 0:512].rearrange("p (hh pr d) -> p hh pr d", hh=2, pr=4)),
                             r=[qn], w=[qr])
                        S.op("pool", lambda e: e.tensor_copy(kr[:], qn[:, 512:640]), r=[qn], w=[kr])
                    if CUT <= 4:
                        continue
                    need_q = (not is_ctx) or (not last)
                    if need_q:
                        for pr in range(4):
                            S.op("pe", lambda e, pr=pr: e.transpose(ptq[:, pr, :], qr[:, pr, :, :].rearrange("p hh d -> p (hh d)"), ident_b[:]),
                                 r=[qr, ident_b], w=[ptq])
                    S.op("pe", lambda e: e.transpose(ptq[:, 4, :], kr[:], ident_b[:]), r=[kr, ident_b], w=[ptq])
                    if need_q:
                        if not is_ctx:
                            S.op("act", lambda e: e.activation(qT[:, :, i * P:(i + 1) * P], ptq[:, 0:4, :], AF.Copy),
                                 r=[ptq], w=[M["qT_b"][pr][i // 4] for pr in range(4)])
                        else:
                            S.op("act", lambda e: e.activation(qTc[:, :, (i - NTL) * P:(i - NTL + 1) * P], ptq[:, 0:4, :], AF.Copy),
                                 r=[ptq], w=[qTc])
                    S.op("act", lambda e: e.activation(kT[:, i * P:(i + 1) * P], ptq[:, 4, :], AF.Copy), r=[ptq], w=[M["kT_b"][i]])
                    if CUT <= 5:
                        continue
                    S.op("pool", lambda e: e.tensor_copy(Vp[:, i, 0:64], qkvu[:, 640:704]), r=[qkvu], w=[M["Vp_b"][i]])
                    S.op("pool", lambda e: e.tensor_copy(Vp[:, i, 128:192], qkvu[:, 704:768]), r=[qkvu], w=[M["Vp_b"][i]])
                    if CUT <= 6:
                        continue
                    if need_q:
                        S.op("act", lambda e: e.activation(sig[:], qkvu[:, 1280:1792], AF.Sigmoid), r=[qkvu], w=[sig])
                        S.op("dve", lambda e: e.tensor_tensor(gg[:], qkvu[:, 768:1280], sig[:], op=ALU.mult), r=[qkvu, sig], w=[gg])
                        for c in range(4):
                            S.op("pe", lambda e, c=c: e.transpose(ptg[:, c, :], gg[:, c * P:(c + 1) * P], ident_b[:]),
                                 r=[gg, ident_b], w=[ptg])
                        if not is_ctx:
                            S.op("act", lambda e: e.activation(gT[:, :, GPAD + i * P:GPAD + (i + 1) * P], ptg[:], AF.Copy),
                                 r=[ptg], w=[M["gT_b"][i]])
                        else:
                            S.op("act", lambda e: e.activation(gTc[:, :, GPAD + (i - NTL) * P:GPAD + (i - NTL + 1) * P], ptg[:], AF.Copy),
                                 r=[ptg], w=[M["gTc_b"][i - NTL]])
                S.barrier()

        def phase_b(l, M, last):
            with ExitStack() as ph:
                NPT = 4
                NPS = 4
                LA = 1
                pT = [sbt(ph, f"pT{k}", [P, 512], BF16) for k in range(NPT)]
                rd = sbt(ph, "rd", [P, 512], F32)
                bcs = sbt(ph, "bcs", [P, 512], F32)
                ps_s = [pst(ph, f"ps_s{k}", [P, 512], F32) for k in range(NPS)]
                po = [pst(ph, f"po{k}", [P, 512], F32) for k in range(3)]
                pbc = pst(ph, "pbc", [P, 512], F32)
                qT, qTc, kT, Vp = M["qT"], M["qTc"], M["kT"], M["Vp"]
                cnt = [0]
                grp = [0]
                kTz = sbt(ph, "kTz", [P, 2, NT * P], BF16)
                S.op("pool", lambda e: e.memset(kTz[:], 0.0), w=[kTz])
                S.op("dve", lambda e: e.tensor_copy(kTz[0:64, 0, :], kT[0:64, :]), r=M["kT_b"], w=[kTz])
                S.op("act", lambda e: e.activation(kTz[64:128, 1, :], kT[64:128, :], AF.Copy), r=M["kT_b"], w=[kTz])
                cq = convert_thunks(ph) if (l == 0 and stop in (None, "p1")) else []
                cqn = [0]

                def attend(qsrc_fn, qfull, qbuf, n, ktiles):
                    its = [(kt_i, kt, hh) for kt_i, kt in enumerate(ktiles) for hh in range(2)]
                    base = cnt[0]
                    cnt[0] += len(its)
                    pg = [po[(2 * grp[0]) % 3], po[(2 * grp[0] + 1) % 3]]
                    grp[0] += 1

                    def emit_s(m):
                        kt_i, kt, hh = its[m]
                        k = (base + m) % NPS
                        kp = (base + m) % NPT
                        S.op("pe", lambda e: e.matmul(ps_s[k][:, 0:n], lhsT=kTz[:, hh, kt * P:(kt + 1) * P], rhs=qfull,
                                                      start=True, stop=True), r=[kTz, qbuf], w=[ps_s[k]])
                        S.op("act", lambda e: e.activation(pT[kp][:, 0:n], ps_s[k][:, 0:n], AF.Exp, scale=0.125),
                             r=[ps_s[k]], w=[pT[kp]])

                    def emit_pv(m):
                        kt_i, kt, hh = its[m]
                        kp = (base + m) % NPT
                        S.op("pe", lambda e: e.matmul(pg[hh][:, 0:n], lhsT=Vp[:, kt, hh * 64:hh * 64 + 128], rhs=pT[kp][:, 0:n],
                                                      start=(kt_i == 0), stop=(kt_i == len(ktiles) - 1)), r=[M["Vp_b"][kt], pT[kp]], w=[pg[hh]])

                    npair = len(its) // 2
                    for j in range(npair + LA):
                        if j < npair:
                            emit_s(2 * j)
                            emit_s(2 * j + 1)
                        if j - LA >= 0:
                            emit_pv(2 * (j - LA))
                            emit_pv(2 * (j - LA) + 1)
                        cqn[0] += 1
                        if cqn[0] % 4 != 3:
                            emit(cq, 1)
                    for hh in range(2):
                        dp = 64 if hh == 0 else 0
                        S.op("dve", lambda e, hh=hh, dp=dp: e.reciprocal(rd[dp:dp + 1, 0:n], pg[hh][dp:dp + 1, 0:n]),
                             r=[pg[hh]], w=[rd])
                        S.op("pe", lambda e, dp=dp: e.matmul(pbc[0:64, 0:n], lhsT=ones_f[dp:dp + 1, 0:64], rhs=rd[dp:dp + 1, 0:n],
                                                            start=True, stop=True), r=[ones_f, rd], w=[pbc])
                        S.op("act", lambda e, hh=hh: e.activation(bcs[hh * 64:(hh + 1) * 64, 0:n], pbc[0:64, 0:n], AF.Copy),
                             r=[pbc], w=[bcs])
                        S.op("dve", lambda e, hh=hh: e.tensor_tensor(qsrc_fn(hh), pg[hh][hh * 64:(hh + 1) * 64, 0:n],
                                                                     bcs[hh * 64:(hh + 1) * 64, 0:n], op=ALU.mult),
                             r=[pg[hh], bcs], w=[qbuf])

                for c in range(8):
                    for pr in range(4):
                        attend(lambda hh, c=c, pr=pr: qT[hh * 64:(hh + 1) * 64, pr, c * 512:(c + 1) * 512],
                               qT[:, pr, c * 512:(c + 1) * 512], M["qT_b"][pr][c], 512, list(range(NT)))
                if not last:
                    for pr in range(4):
                        attend(lambda hh, pr=pr: qTc[hh * 64:(hh + 1) * 64, pr, :], qTc[:, pr, :], qTc.b, S_CTX, [NTL, NTL + 1])
                emit(cq, len(cq))
                S.barrier()

        def phase_c(l, M, tiles, src_aps, r_, modbc):
            with ExitStack() as ph:
                wcol = sbt(ph, "wcol", [P, 4, 31], F32)
                DG = sbt(ph, "DG", [P, 4, 31, P], BF16)
                wo = sbt(ph, "wo", [P, 8, D], BF16)
                stg = [sbt(ph, f"stgC{k}", [P, D], F32) for k in range(2)]
                stg_slot = [DmaSlot(S, f"stgC{k}") for k in range(2)]
                betaA = sbt(ph, "betaA", [P, 8], F32)
                betac = sbt(ph, "betac", [P, 8], F32)
                cb_bc = sbt(ph, "cb_bc", [P, 512], F32)
                lg_bc = sbt(ph, "lg_bc", [P, 512], F32)
                lb_bc = sbt(ph, "lb_bc", [P, 512], F32)
                xt = [sbt(ph, f"xtC{k}", [P, D], F32) for k in range(2)]
                xt_slot = [DmaSlot(S, f"xtC{k}") for k in range(2)]
                y = sbt(ph, "yC", [P, 512], F32)
                junk = sbt(ph, "junkC", [P, 512], F32)
                st4 = sbt(ph, "st4", [P, 4], F32)
                rln = sbt(ph, "rln", [P, 1], F32)
                msq = sbt(ph, "msq", [P, 1], F32)
                z = sbt(ph, "zC", [P, 512], F32)
                oc = sbt(ph, "oc", [P, 512], F32)
                ocb = sbt(ph, "ocb", [P, 512], BF16)
                ocT = sbt(ph, "ocT", [P, 4, P], BF16)
                sqo = sbt(ph, "sqo", [P, 4, P], F32)
                ssc = sbt(ph, "ssc", [P, 2], F32)
                rsc = sbt(ph, "rsc", [P, 2], F32)
                t1 = sbt(ph, "t1", [P, D], F32)
                x1 = [sbt(ph, f"x1_{k}", [P, D], F32) for k in range(2)]
                x1_slot = [DmaSlot(S, f"x1s{k}") for k in range(2)]
                py = pst(ph, "py", [P, 512], F32)
                ptc = pst(ph, "ptc", [P, 4, P], BF16)
                pss = pst(ph, "pss", [P, 16], F32)
                pa = [pst(ph, f"pa{k}", [P, 512], F32) for k in range(2)]
                pc = [pst(ph, f"pc{k}", [P, 512], F32) for k in range(2)]
                qT, qTc, gT, gTc = M["qT"], M["qTc"], M["gT"], M["gTc"]

                vs = DmaSlot(S, "vecC", group=True)
                for c in range(4):
                    S.dma("sp", lambda e, c=c: e.dma_start(out=wcol[:, c, :], in_=conv_w_d[l, :, c * P:(c + 1) * P].rearrange("j p -> p j"),
                                                           allow_slow_non_contiguous=True), vs, w=[wcol])
                for half in range(2):
                    S.dma("sp", lambda e, half=half: e.dma_start(out=betaA[half * 64:(half + 1) * 64, :],
                                                                 in_=beta_attn_d[l].rearrange("(h d) -> d h", d=64),
                                                                 allow_slow_non_contiguous=True), vs, w=[betaA])
                S.dma("sp", lambda e: e.dma_start(out=betac[:, 4:8], in_=beta_conv_d[l].rearrange("(c p) -> p c", p=P),
                                                  allow_slow_non_contiguous=True), vs, w=[betac])
                S.dma("sp", lambda e: e.dma_start(out=cb_bc[:], in_=conv_b_d[l].partition_broadcast(P)), vs, w=[cb_bc])
                S.dma("sp", lambda e: e.dma_start(out=lg_bc[:], in_=ln_g_d[l].partition_broadcast(P)), vs, w=[lg_bc])
                S.dma("sp", lambda e: e.dma_start(out=lb_bc[:], in_=ln_b_d[l].partition_broadcast(P)), vs, w=[lb_bc])
                vs.close()
                S.op("dve", lambda e: e.tensor_copy(betac[0:64, 0:4], betaA[0:64, 0:4]), r=[betaA], w=[betac])
                S.op("dve", lambda e: e.tensor_copy(betac[64:128, 0:4], betaA[64:128, 4:8]), r=[betaA], w=[betac])
                for kk in range(8):
                    t = stg[kk % 2]
                    sl = stg_slot[kk % 2]
                    if kk < 4:
                        S.dma("sp", lambda e, t=t, kk=kk: e.dma_start(out=t[0:64, :], in_=w_o_d[l, kk * 64:(kk + 1) * 64, :]), sl, w=[t])
                        S.dma("sp", lambda e, t=t, kk=kk: e.dma_start(out=t[64:128, :], in_=w_o_d[l, (kk + 4) * 64:(kk + 5) * 64, :]), sl, w=[t])
                    else:
                        S.dma("sp", lambda e, t=t, kk=kk: e.dma_start(out=t[:], in_=w_o_d[l, 512 + (kk - 4) * P:512 + (kk - 3) * P, :]), sl, w=[t])
                    S.op("act", lambda e, t=t, kk=kk: e.activation(wo[:, kk, :], t[:], AF.Identity, scale=betac[:, kk:kk + 1]),
                         r=[t, betac], w=[wo])
                for c in range(4):
                    for j in range(31):
                        S.op("pool", lambda e, c=c, j=j: e.tensor_scalar(DG[:, c, j, :], ident_f[:], wcol[:, c, j:j + 1], None, op0=ALU.mult),
                             r=[ident_f, wcol], w=[DG])

                def load(n_):
                    i = tiles[n_]
                    k = n_ % 2
                    S.dma("sp", lambda e: e.dma_start(out=xt[k][:], in_=src_aps[i]), xt_slot[k], r=[xs_buf[i]], w=[xt[k]])

                load(0)
                for n_, i in enumerate(tiles):
                    k = n_ % 2
                    is_ctx = i >= NTL
                    if n_ + 1 < len(tiles):
                        load(n_ + 1)
                    if not is_ctx:
                        gsrc, gb, t0 = gT, M["gT_b"], i * P
                        lo, hi = max(0, i - 1), min(NTL - 1, i + 1)
                        osrc = lambda pr: qT[:, pr, i * P:(i + 1) * P]
                        obufs = [M["qT_b"][pr][i // 4] for pr in range(4)]
                    else:
                        gsrc, gb, t0 = gTc, M["gTc_b"], (i - NTL) * P
                        lo, hi = max(0, i - NTL - 1), min(NTC - 1, i - NTL + 1)
                        osrc = lambda pr: qTc[:, pr, (i - NTL) * P:(i - NTL + 1) * P]
                        obufs = [qTc.b]
                    for c in range(4):
                        for j in range(31):
                            S.op("pe", lambda e, c=c, j=j: e.matmul(py[:, c * P:(c + 1) * P], lhsT=gsrc[:, c, t0 + j + GPAD - 15:t0 + j + GPAD - 15 + P],
                                                                  rhs=DG[:, c, j, :], start=(j == 0), stop=(j == 30)),
                                 r=[gb[q] for q in range(lo, hi + 1)] + [DG], w=[py])
                    S.op("dve", lambda e: e.tensor_tensor(y[:], py[:], cb_bc[:], op=ALU.add), r=[py, cb_bc], w=[y])
                    S.op("act", lambda e: e.activation(junk[:], y[:], AF.Identity, accum_out=st4[:, 0:1]), r=[y], w=[junk, st4])
                    S.op("act", lambda e: e.activation(junk[:], y[:], AF.Square, accum_out=st4[:, 1:2]), r=[y], w=[junk, st4])
                    S.op("dve", lambda e: e.tensor_scalar(st4[:, 2:4], st4[:, 0:2], 1.0 / 512, None, op0=ALU.mult), r=[st4], w=[st4])
                    S.op("dve", lambda e: e.tensor_tensor(msq[:], st4[:, 2:3], st4[:, 2:3], op=ALU.mult), r=[st4], w=[msq])
                    S.op("dve", lambda e: e.tensor_tensor(msq[:], st4[:, 3:4], msq[:], op=ALU.subtract), r=[st4, msq], w=[msq])
                    rstd_from_ss(msq, rln, 1.0, eps5)
                    S.op("dve", lambda e: e.tensor_scalar(z[:], y[:], st4[:, 2:3], rln[:, 0:1], op0=ALU.subtract, op1=ALU.mult),
                         r=[y, st4, rln], w=[z])
                    S.op("pool", lambda e: e.tensor_tensor(z[:], z[:], lg_bc[:], op=ALU.mult), r=[z, lg_bc], w=[z])
                    S.op("pool", lambda e: e.tensor_tensor(z[:], z[:], lb_bc[:], op=ALU.add), r=[z, lb_bc], w=[z])
                    S.op("act", lambda e: e.activation(oc[:], z[:], AF.Silu), r=[z], w=[oc])
                    S.op("act", lambda e: e.activation(junk[:], oc[:], AF.Square, accum_out=ssc[:, 0:1]), r=[oc], w=[junk, ssc])
                    S.op("pool", lambda e: e.tensor_copy(ocb[:], oc[:]), r=[oc], w=[ocb])
                    for c in range(4):
                        S.op("pe", lambda e, c=c: e.transpose(ptc[:, c, :], ocb[:, c * P:(c + 1) * P], ident_b[:]),
                             r=[ocb, ident_b], w=[ptc])
                    S.op("act", lambda e: e.activation(ocT[:], ptc[:], AF.Copy), r=[ptc], w=[ocT])
                    for pr in range(4):
                        S.op("pool", lambda e, pr=pr: e.tensor_tensor(sqo[:, pr, :], osrc(pr), osrc(pr), op=ALU.mult), r=obufs, w=[sqo])
                    for pr in range(4):
                        S.op("pe", lambda e, pr=pr: e.matmul(pss[:, 0:2], lhsT=sqo[:, pr, :], rhs=ones_f[:, 0:2],
                                                            start=(pr == 0), stop=(pr == 3)), r=[sqo, ones_f], w=[pss])
                    S.op("dve", lambda e: e.tensor_copy(ssc[:, 1:2], pss[:, 0:1]), r=[pss], w=[ssc])
                    rstd_from_ss(ssc, rsc, 1.0 / 512, eps6)
                    for cc in range(2):
                        for pr in range(4):
                            S.op("pe", lambda e, cc=cc, pr=pr: e.matmul(pa[cc][:], lhsT=osrc(pr), rhs=wo[:, pr, cc * 512:(cc + 1) * 512],
                                                                      start=(pr == 0), stop=(pr == 3)), r=obufs + [wo], w=[pa[cc]])
                        for c in range(4):
                            S.op("pe", lambda e, cc=cc, c=c: e.matmul(pc[cc][:], lhsT=ocT[:, c, :], rhs=wo[:, 4 + c, cc * 512:(cc + 1) * 512],
                                                                    start=(c == 0), stop=(c == 3)), r=[ocT, wo], w=[pc[cc]])
                        sl = slice(cc * 512, (cc + 1) * 512)
                        S.op("dve", lambda e, cc=cc, sl=sl: e.tensor_scalar(t1[:, sl], pa[cc][:], rsc[:, 1:2], None, op0=ALU.mult),
                             r=[pa[cc], rsc], w=[t1])
                        S.op("dve", lambda e, cc=cc, sl=sl: e.scalar_tensor_tensor(out=t1[:, sl], in0=pc[cc][:], scalar=rsc[:, 0:1],
                                                                                   in1=t1[:, sl], op0=ALU.mult, op1=ALU.add),
                             r=[pc[cc], rsc, t1], w=[t1])
                    S.op("pool", lambda e: e.tensor_tensor(t1[:], t1[:], modbc[:, 0:1024], op=ALU.mult), r=[t1, modbc], w=[t1])
                    S.op("pool", lambda e: e.tensor_tensor(x1[k][:], t1[:], xt[k][:], op=ALU.add), r=[t1, xt[k]], w=[x1[k]])
                    S.dma("sp", lambda e: e.dma_start(out=xs_d[i * P:(i + 1) * P, :], in_=x1[k][:]), x1_slot[k], r=[x1[k]], w=[xs_buf[i]])
                    if l == 0:
                        dbg_store("d_x1", slice(i * P, (i + 1) * P), x1[k][:], [x1[k]])
                S.barrier()

        def phase_p(l, tiles, modbc, last):
            with ExitStack() as ph:
                wq = sbt(ph, "wq", [P, 8, D], F32)
                keysTz = sbt(ph, "keysTz", [P, 8, 2, P], F32)
                iota16 = sbt(ph, "iota16", [P, 16], F32)
                xt = [sbt(ph, f"xtP{k}", [P, D], F32) for k in range(2)]
                xt_slot = [DmaSlot(S, f"xtP{k}") for k in range(2)]
                junk = sbt(ph, "junkP", [P, D], BF16)
                junkr = sbt(ph, "junkR", [P, D], BF16)
                prod = [sbt(ph, f"prod{k}", [P, D], BF16) for k in range(3)]
                hb = [sbt(ph, f"hb{k}", [P, D], BF16) for k in range(2)]
                pi = [0]
                ss = sbt(ph, "ssP", [P, 1], F32)
                rstd = sbt(ph, "rstdP", [P, 1], F32)
                ssf = sbt(ph, "ssF", [P, 1], F32)
                rstdf = sbt(ph, "rstdF", [P, 1], F32)
                h = [sbt(ph, "hP", [P, D], F32)] * 2
                hT = sbt(ph, "hT", [P, 8, P], F32)
                qpT = hT
                qp = sbt(ph, "qp", [P, D], F32)
                sc = sbt(ph, "scP", [P, 16, 128], F32)
                wk = sbt(ph, "wkP", [P, 16, 128], F32)
                eq_ap = wk[:].rearrange("p a b -> p (a b)").rearrange("p (x c) -> p x c", c=16)
                topv = sbt(ph, "topv", [P, 16, 16], F32)
                idx = sbt(ph, "idxP", [P, 16, 16], U32)
                idxf = sbt(ph, "idxf", [P, 16, 16], F32)
                cand = sbt(ph, "cand", [P, 8, 256], F32)
                wk2_ap = sc[:].rearrange("p a b -> p (a b)").rearrange("p (h x) -> p h x", h=8)
                best = sbt(ph, "best", [P, 8, 16], F32)
                pos = sbt(ph, "pos", [P, 8, 16], U32)
                pab = sbt(ph, "pab", [P, 2, 128], U32)
                abf = sbt(ph, "abf", [P, 2, 128], F32)
                isel = sbt(ph, "isel", [P, 2, 128], F32)
                ef = [sbt(ph, f"ef{k}", [P, 128], F32) for k in range(2)]
                eidx = [sbt(ph, f"eidx{k}", [P, 128], I32) for k in range(2)]
                ew = sbt(ph, "ew", [P, 128], F32)
                se = sbt(ph, "se", [P, 8], F32)
                wgt = [sbt(ph, f"wgt{k}", [P, 128], F32) for k in range(2)]
                pre = sbt(ph, "pre", [P, 128], F32)
                aw = sbt(ph, "aw", [P, 128], F32)
                gbuf = [sbt(ph, f"gbuf{k}", [P, 2 * D], BF16) for k in range(NSLOT)]
                diag = [sbt(ph, f"diag{k}", [P, P], BF16) for k in range(6)]
                pre_b = [Buf(f"pre_b{k}") for k in range(4)]
                pre_d = [Buf(f"pre_d{k}") for k in range(4)]
                di = [0]
                gslot = [DmaSlot(S, f"g{k}") for k in range(NSLOT)]
                x2 = [sbt(ph, "x2_0", [P, D], F32)] * 2
                x2_slot = [DmaSlot(S, f"x2s{k}") for k in range(2)]
                fg_bc = sbt(ph, "fg_bc", [P, D], F32)
                pT2 = pst(ph, "pT2", [P, 8, P], F32)
                pqp = [pst(ph, f"pqp{k}", [P, 512], F32) for k in range(2)]
                pcs = [pst(ph, f"pcs{k}", [P, 512], F32) for k in range(2)]
                pva = [pst(ph, f"pva{k}", [P, 512], F32) for k in range(2)]
                psc = pqp + pcs
                print(f"[build] phase_p sbuf remaining {nc.sbuf_bytes_remaining}")

                S.op("pool", lambda e: e.iota(iota16[:], pattern=[[1, 16]], base=0, channel_multiplier=0,
                                              allow_small_or_imprecise_dtypes=True), w=[iota16])
                ws = DmaSlot(S, "wqload", group=True)
                for j in range(8):
                    S.dma("sp", lambda e, j=j: e.dma_start(out=wq[:, j, :], in_=peer_wq_d[l, j * P:(j + 1) * P, :]), ws, w=[wq])
                kraw_ap = sc[:].rearrange("p a b -> p (a b)")[:, 0:1024].rearrange("p (a d) -> p a d", d=64)
                keysT_ap = cand[:].rearrange("p a b -> p (a b)")[:, 0:1024].rearrange("p (a k) -> p a k", k=P)
                S.dma("sp", lambda e: e.dma_start(out=kraw_ap, in_=peer_keys_d[l].rearrange("h c k d -> k (h c) d")), ws, w=[sc])
                if last:
                    S.dma("sp", lambda e: e.dma_start(out=fg_bc[:], in_=final_g_d[0].partition_broadcast(P)), ws, w=[fg_bc])
                ws.close()
                for hh in range(8):
                    S.op("pe", lambda e, hh=hh: e.transpose(pT2[:, hh, :], kraw_ap[:, 2 * hh:2 * hh + 2, :].rearrange("p c d -> p (c d)"), ident_f[:]),
                         r=[sc, ident_f], w=[pT2])
                S.op("act", lambda e: e.activation(keysT_ap, pT2[:], AF.Copy), r=[pT2], w=[cand])
                S.op("pool", lambda e: e.memset(keysTz[:], 0.0), w=[keysTz])
                S.op("dve", lambda e: e.tensor_copy(keysTz[0:64, :, 0, :], keysT_ap[0:64, :, :]), r=[cand], w=[keysTz])
                S.op("dve", lambda e: e.tensor_copy(keysTz[64:128, :, 1, :], keysT_ap[64:128, :, :]), r=[cand], w=[keysTz])

                gi = [0]

                def routing(n_):
                    rq = []
                    i = tiles[n_]
                    k = n_ % 2
                    xk, hk, efk, eik, wgk = xt[k], h[k], ef[k], eidx[k], wgt[k]
                    hbk = hb[k]

                    def op(eng, fn, r=(), w=()):
                        rq.append(("op", eng, fn, r, w))

                    rq.append(("dma", "sp", lambda e: e.dma_start(out=xk[:], in_=xs_d[i * P:(i + 1) * P, :]), xt_slot[k], [xs_buf[i]], [xk]))
                    op("act", lambda e: e.activation(junkr[:], xk[:], AF.Square, accum_out=ss[:]), r=[xk], w=[junkr, ss])
                    op("act", lambda e: e.activation(rstd[:], ss[:], AF.Sqrt, scale=1.0 / D, bias=eps6[:, 0:1]), r=[ss, eps6], w=[rstd])
                    op("dve", lambda e: e.reciprocal(rstd[:], rstd[:]), r=[rstd], w=[rstd])
                    op("dve", lambda e: e.scalar_tensor_tensor(out=hk[:], in0=xk[:], scalar=rstd[:, 0:1], in1=modbc[:, 2048:3072],
                                                               op0=ALU.mult, op1=ALU.mult), r=[xk, rstd, modbc], w=[hk])
                    op("dve", lambda e: e.tensor_tensor(hk[:], hk[:], modbc[:, 1024:2048], op=ALU.add), r=[hk, modbc], w=[hk])
                    op("act", lambda e: e.activation(hbk[:], hk[:], AF.Copy), r=[hk], w=[hbk])
                    for j in range(8):
                        op("pe", lambda e, j=j: e.transpose(pT2[:, j, :], hk[:, j * P:(j + 1) * P], ident_f[:]), r=[hk, ident_f], w=[pT2])
                    op("act", lambda e: e.activation(hT[:], pT2[:], AF.Copy), r=[pT2], w=[hT])
                    for cc in range(2):
                        for j in range(8):
                            op("pe", lambda e, cc=cc, j=j: e.matmul(pqp[cc][:], lhsT=hT[:, j, :], rhs=wq[:, j, cc * 512:(cc + 1) * 512],
                                                                  start=(j == 0), stop=(j == 7)), r=[hT, wq], w=[pqp[cc]])
                        op("act", lambda e, cc=cc: e.activation(qp[:, cc * 512:(cc + 1) * 512], pqp[cc][:], AF.Copy), r=[pqp[cc]], w=[qp])
                    for hh in range(8):
                        op("pe", lambda e, hh=hh: e.transpose(pT2[:, hh, :], qp[:, hh * P:(hh + 1) * P], ident_f[:]), r=[qp, ident_f], w=[pT2])
                    op("act", lambda e: e.activation(qpT[:], pT2[:], AF.Copy), r=[pT2], w=[qpT])
                    for hh in range(8):
                        pv = psc[hh // 2][:].rearrange("p (a k) -> p a k", k=P)
                        op("pe", lambda e, hh=hh, pv=pv: e.matmul(pv[:, (hh % 2) * 2:(hh % 2) * 2 + 2, :], lhsT=qpT[:, hh, :],
                                                                 rhs=keysTz[:, hh, :, :], start=True, stop=True),
                           r=[qpT, keysTz], w=[psc[hh // 2]])
                    for q in range(4):
                        op("act", lambda e, q=q: e.activation(sc[:, 4 * q:4 * q + 4, :], psc[q][:].rearrange("p (a k) -> p a k", k=P), AF.Copy),
                           r=[psc[q]], w=[sc])
                    for g in range(16):
                        op("dve", lambda e, g=g: e.max(out=topv[:, g, 0:8], in_=sc[:, g, :]), r=[sc], w=[topv])
                        op("dve", lambda e, g=g: e.max_index(out=idx[:, g, 0:8], in_max=topv[:, g, 0:8], in_values=sc[:, g, :]),
                           r=[sc, topv], w=[idx])
                        op("dve", lambda e, g=g: e.match_replace(out=wk[:, g, :], in_to_replace=topv[:, g, 0:8], in_values=sc[:, g, :],
                                                                 imm_value=-1e30), r=[sc, topv], w=[wk])
                        op("dve", lambda e, g=g: e.max(out=topv[:, g, 8:16], in_=wk[:, g, :]), r=[wk], w=[topv])
                        op("dve", lambda e, g=g: e.max_index(out=idx[:, g, 8:16], in_max=topv[:, g, 8:16], in_values=wk[:, g, :]),
                           r=[wk, topv], w=[idx])
                    op("dve", lambda e: e.tensor_copy(idxf[:], idx[:]), r=[idx], w=[idxf])
                    tv4 = topv[:].rearrange("p (h c) a -> p h c a", c=2)
                    if4 = idxf[:].rearrange("p (h c) a -> p h c a", c=2)
                    op("dve", lambda e: e.tensor_tensor(cand[:].rearrange("p h (a b) -> p h a b", a=16),
                                                        tv4[:, :, 0, :].unsqueeze(3).to_broadcast([P, 8, 16, 16]),
                                                        tv4[:, :, 1, :].unsqueeze(2).to_broadcast([P, 8, 16, 16]), op=ALU.add),
                       r=[topv], w=[cand])
                    for hh in range(8):
                        op("dve", lambda e, hh=hh: e.max(out=best[:, hh, 0:8], in_=cand[:, hh, :]), r=[cand], w=[best])
                        op("dve", lambda e, hh=hh: e.max_index(out=pos[:, hh, 0:8], in_max=best[:, hh, 0:8], in_values=cand[:, hh, :]),
                           r=[cand, best], w=[pos])
                        op("dve", lambda e, hh=hh: e.match_replace(out=wk2_ap[:, hh, :], in_to_replace=best[:, hh, 0:8],
                                                                   in_values=cand[:, hh, :], imm_value=-1e30), r=[cand, best], w=[sc])
                        op("dve", lambda e, hh=hh: e.max(out=best[:, hh, 8:16], in_=wk2_ap[:, hh, :]), r=[sc], w=[best])
                        op("dve", lambda e, hh=hh: e.max_index(out=pos[:, hh, 8:16], in_max=best[:, hh, 8:16], in_values=wk2_ap[:, hh, :]),
                           r=[sc, best], w=[pos])
                    posf = pos[:].rearrange("p h s -> p (h s)")
                    op("dve", lambda e: e.tensor_single_scalar(pab[:, 0, :], posf, 4, op=ALU.logical_shift_right), r=[pos], w=[pab])
                    op("dve", lambda e: e.tensor_single_scalar(pab[:, 1, :], posf, 15, op=ALU.bitwise_and), r=[pos], w=[pab])
                    op("dve", lambda e: e.tensor_copy(abf[:], pab[:]), r=[pab], w=[abf])
                    for c in range(2):
                        op("dve", lambda e, c=c: e.tensor_tensor(eq_ap, iota16[:].unsqueeze(1).to_broadcast([P, 128, 16]), abf[:, c, :].unsqueeze(2).to_broadcast([P, 128, 16]),
                                                                 op=ALU.is_equal), r=[iota16, abf], w=[wk])
                        op("dve", lambda e, c=c: e.tensor_tensor(eq_ap.rearrange("p (h s) a -> p h s a", h=8),
                                                                 eq_ap.rearrange("p (h s) a -> p h s a", h=8),
                                                                 if4[:, :, c, :].unsqueeze(2).to_broadcast([P, 8, 16, 16]), op=ALU.mult),
                           r=[wk, idxf], w=[wk])
                        op("dve", lambda e, c=c: e.tensor_reduce(out=isel[:, c, :], in_=eq_ap, axis=AX.X, op=ALU.add), r=[wk], w=[isel])
                    op("dve", lambda e: e.scalar_tensor_tensor(out=efk[:], in0=isel[:, 0, :], scalar=128.0, in1=isel[:, 1, :],
                                                               op0=ALU.mult, op1=ALU.add), r=[isel], w=[efk])
                    op("dve", lambda e: e.tensor_scalar(efk[:], efk[:], float(l * NPEER), None, op0=ALU.add), r=[efk], w=[efk])
                    op("dve", lambda e: e.tensor_copy(eik[:], efk[:]), r=[efk], w=[eik])
                    op("dve", lambda e: e.tensor_tensor(ew[:].rearrange("p (h s) -> p h s", h=8), best[:],
                                                        best[:, :, 0:1].to_broadcast([P, 8, 16]), op=ALU.subtract), r=[best], w=[ew])
                    op("act", lambda e: e.activation(ew[:], ew[:], AF.Exp), r=[ew], w=[ew])
                    op("dve", lambda e: e.tensor_reduce(out=se[:], in_=ew[:].rearrange("p (h s) -> p h s", h=8), axis=AX.X, op=ALU.add),
                       r=[ew], w=[se])
                    op("dve", lambda e: e.reciprocal(se[:], se[:]), r=[se], w=[se])
                    op("dve", lambda e: e.tensor_tensor(wgk[:].rearrange("p (h s) -> p h s", h=8), ew[:].rearrange("p (h s) -> p h s", h=8),
                                                        se[:].unsqueeze(2).to_broadcast([P, 8, 16]), op=ALU.mult), r=[ew, se], w=[wgk])
                    return rq

                rq = routing(0)
                emit(rq, len(rq))
                for n_, i in enumerate(tiles):
                    k = n_ % 2
                    hbk, efk, eik, wgk = hb[k], ef[k], eidx[k], wgt[k]
                    rq = routing(n_ + 1) if n_ + 1 < len(tiles) else []
                    per_slot = -(-len(rq) // 112)
                    NG = 128 // GRP
                    gb = {}
                    for g_ in range(NG + 1):
                        if g_ < NG:
                            pb_ = pre_b[g_ % 4]
                            pd_ = pre_d[g_ % 4]
                            for s_ in range(g_ * GRP, (g_ + 1) * GRP):
                                b_ = gi[0] % NSLOT
                                gi[0] += 1
                                gb[s_] = b_
                                S.dma("pool", lambda e, s_=s_, b_=b_: e.indirect_dma_start(
                                    out=gbuf[b_][:], out_offset=None, in_=uv_d,
                                    in_offset=bass.IndirectOffsetOnAxis(ap=eik[:, s_:s_ + 1], axis=0)), gslot[b_], r=[eik], w=[gbuf[b_]])
                                pr_ = prod[pi[0] % len(prod)]
                                pi[0] += 1
                                S.op("dve", lambda e, b_=b_, pr_=pr_: e.tensor_tensor(pr_[:], hbk[:], gbuf[b_][:, 0:D], op=ALU.mult),
                                     r=[hbk, gbuf[b_]], w=[pr_])
                                S.op("act", lambda e, s_=s_, pr_=pr_: e.activation(junkr[:], pr_[:], AF.Identity, accum_out=pre[:, s_:s_ + 1]),
                                     r=[pr_], w=[junkr, pb_])
                                emit(rq, per_slot)
                            gs = slice(g_ * GRP, (g_ + 1) * GRP)
                            S.op("act", lambda e, gs=gs: e.activation(aw[:, gs], pre[:, gs], AF.Gelu), r=[pb_, pd_], w=[pb_])
                        if g_ >= 1:
                            gq = g_ - 1
                            pbq = pre_b[gq % 4]
                            for s_ in range(gq * GRP, (gq + 1) * GRP):
                                b_ = gb[s_]
                                dg = diag[di[0] % len(diag)]
                                di[0] += 1
                                S.op("dve", lambda e, s_=s_, dg=dg: e.tensor_scalar(dg[:], ident_b[:], aw[:, s_:s_ + 1], wgk[:, s_:s_ + 1],
                                                                                   op0=ALU.mult, op1=ALU.mult),
                                     r=[ident_b, pbq, wgk], w=[dg])
                                for cc in range(2):
                                    S.op("pe", lambda e, s_=s_, b_=b_, dg=dg, cc=cc: e.matmul(
                                        pva[cc][:], lhsT=dg[:], rhs=gbuf[b_][:, D + cc * 512:D + (cc + 1) * 512],
                                        start=(s_ == 0), stop=(s_ == 127)), r=[dg, gbuf[b_]], w=[pva[cc]])
                    if l == 0:
                        dbg_store("d_pre", slice(i * P, (i + 1) * P), pre[:], pre_b + pre_d)
                        dbg_store("d_eidx", slice(i * P, (i + 1) * P), efk[:], [efk])
                        dbg_store("d_w", slice(i * P, (i + 1) * P), wgk[:], [wgk])
                    for cc in range(2):
                        sl = slice(cc * 512, (cc + 1) * 512)
                        S.op("dve", lambda e, cc=cc, sl=sl: e.tensor_tensor(x2[k][:, sl], pva[cc][:], modbc[:, 3072 + cc * 512:3072 + (cc + 1) * 512],
                                                                           op=ALU.mult), r=[pva[cc], modbc], w=[x2[k]])
                    S.op("dve", lambda e: e.tensor_tensor(x2[k][:], x2[k][:], xt[k][:], op=ALU.add), r=[x2[k], xt[k]], w=[x2[k]])
                    if l == 0:
                        dbg_store("d_x2", slice(i * P, (i + 1) * P), x2[k][:], [x2[k]])
                    if last:
                        S.op("act", lambda e: e.activation(junk[:], x2[k][:], AF.Square, accum_out=ssf[:]), r=[x2[k]], w=[junk, ssf])
                        rstd_from_ss(ssf, rstdf, 1.0 / D, eps6)
                        S.op("dve", lambda e: e.scalar_tensor_tensor(out=x2[k][:], in0=x2[k][:], scalar=rstdf[:, 0:1], in1=fg_bc[:],
                                                                     op0=ALU.mult, op1=ALU.mult), r=[x2[k], rstdf, fg_bc], w=[x2[k]])
                        S.dma("sp", lambda e: e.dma_start(out=out_d[i * P:(i + 1) * P, :], in_=x2[k][:]), x2_slot[k], r=[x2[k]])
                    else:
                        S.dma("sp", lambda e: e.dma_start(out=xs_d[i * P:(i + 1) * P, :], in_=x2[k][:]), x2_slot[k], r=[x2[k]], w=[xs_buf[i]])
                    emit(rq, len(rq))
                S.barrier()

        def load_modbc(r_, modbc):
            S.dma("sp", lambda e: e.dma_start(out=modbc[:], in_=modscr_d[r_]), DmaSlot(S, "modld"), w=[modbc])

        lat_tiles = list(range(NTL))
        ctx_tiles = list(range(NTL, NT))
        for l in range(depth):
            last = (l == DEPTH - 1)
            if l == 0:
                src_aps = [x_d[i * P:(i + 1) * P, :] for i in range(NTL)] + [ctx_d[i * P:(i + 1) * P, :] for i in range(NTC)]
            else:
                src_aps = [xs_d[i * P:(i + 1) * P, :] for i in range(NT)]
            mod_cols(l)
            if l == 0:
                dbg_store("d_mod", (slice(0, P), slice(0, 32)), modcol[:].rearrange("p r c -> p (r c)"), [modcol])
                dbg_store("d_mod", (slice(0, P), slice(32, 48)), scT[:].rearrange("p j r -> p (j r)"), [scT])
            if stop == "m":
                break
            with ExitStack() as mix:
                M = {}
                M["qT"] = sbt(mix, "qT", [P, 4, S_LAT], BF16)
                M["qT_b"] = [[Buf(f"qT{pr}_{c}") for c in range(8)] for pr in range(4)]
                M["qTc"] = sbt(mix, "qTc", [P, 4, S_CTX], BF16)
                M["gT"] = sbt(mix, "gT", [P, 4, S_LAT + 2 * GPAD], BF16)
                M["gT_b"] = [Buf(f"gT{i}") for i in range(NTL)]
                M["gTc"] = sbt(mix, "gTc", [P, 4, S_CTX + 2 * GPAD], BF16)
                M["gTc_b"] = [Buf(f"gTc{i}") for i in range(NTC)]
                S.op("pool", lambda e: e.memset(M["gT"][:], 0.0), w=M["gT_b"])
                S.op("pool", lambda e: e.memset(M["gTc"][:], 0.0), w=M["gTc_b"])
                with ExitStack() as ab:
                    M["kT"] = sbt(ab, "kT", [P, NT * P], BF16)
                    M["kT_b"] = [Buf(f"kT{i}") for i in range(NT)]
                    M["Vp"] = sbt(ab, "Vp", [P, NT, 192], BF16)
                    M["Vp_b"] = [Buf(f"Vp{i}") for i in range(NT)]
                    S.op("pool", lambda e: e.memset(M["Vp"][:], 1.0), w=M["Vp_b"])
                    phase_a(l, M, src_aps, last)
                    if stop in ("a", "a0", "a1"):
                        break
                    phase_b(l, M, last)
                if stop == "b":
                    break
                with ExitStack() as cs:
                    modbc = sbt(cs, "modbcC", [P, 4096], F32)
                    for r_ in ((0,) if last else (0, 1)):
                        mod_bc(l, r_, modbc)
                        S.dma("sp", lambda e, r_=r_: e.dma_start(out=modscr_d[r_], in_=modbc[:]), DmaSlot(S, "modst"), r=[modbc])
                        S.barrier()
                    load_modbc(0, modbc)
                    phase_c(l, M, lat_tiles, src_aps, 0, modbc)
                    if not last:
                        load_modbc(1, modbc)
                        phase_c(l, M, ctx_tiles, src_aps, 1, modbc)
            if stop == "c":
                break
            with ExitStack() as pp:
                modbc = sbt(pp, "modbcP", [P, 4096], F32)
                load_modbc(0, modbc)
                phase_p(l, lat_tiles if stop not in ("p1", "p0") else lat_tiles[:2], modbc, last)
                if not last and stop not in ("p1", "p0"):
                    load_modbc(1, modbc)
                    phase_p(l, ctx_tiles, modbc, last)
            if stop in ("p1", "p0"):
                break
        out_slot.close()
        if out_slot.sem is not None:
            for en in ("sp", "act", "pool"):
                S.E[en].eng.wait_ge(out_slot.sem, out_slot.val)
        S.barrier()
        print(f"[build] instr={S.ninstr} waits={S.nwait} sems={S.nsem}")
    return nc


_NC_CACHE = {}


def _rope_tables():
    n = 16
    inv = (10000.0 ** (-np.arange(n, dtype=np.float32) / n)).astype(np.float32)
    row = np.repeat(np.arange(64), 64).astype(np.float32)
    col = np.tile(np.arange(64), 64).astype(np.float32)
    ang = np.concatenate([row[:, None] * inv, col[:, None] * inv], axis=-1).astype(np.float32)
    cos = np.cos(ang).astype(np.float32)
    sin = np.sin(ang).astype(np.float32)
    cos2 = np.concatenate([cos, cos], axis=-1)
    sin2 = np.concatenate([-sin, sin], axis=-1)
    return np.ascontiguousarray(cos2), np.ascontiguousarray(sin2)


def make_in_maps(inputs, cores):
    f = lambda a: np.ascontiguousarray(np.asarray(a, dtype=np.float32))
    cos2, sin2 = _rope_tables()
    shared = {k: f(inputs[k]) for k in ["w_mod", "b_mod", "w_in", "q_gain", "k_gain", "conv_w", "conv_b", "ln_g", "ln_b",
                                        "beta_attn", "beta_conv", "w_o", "peer_wq", "peer_keys", "peer_u", "peer_v"]}
    shared["final_g"] = f(inputs["final_g"]).reshape(1, D)
    shared["cos2"] = cos2
    shared["sin2"] = sin2
    shared["cctxT"] = np.ascontiguousarray(f(inputs["c_ctx"]).reshape(8, P).T)
    x = f(inputs["x"])
    ctx = f(inputs["ctx"])
    c = f(inputs["c"])
    maps = []
    for b in cores:
        m = dict(shared)
        m["x"] = x[b]
        m["ctx"] = ctx[b]
        m["cT"] = np.ascontiguousarray(c[b].reshape(8, P).T)
        maps.append(m)
    return maps


def kernel(**inputs):
    if "nc" not in _NC_CACHE:
        _NC_CACHE["nc"] = build()
    nc = _NC_CACHE["nc"]
    B = np.asarray(inputs["x"]).shape[0]
    maps = make_in_maps(inputs, list(range(B)))
    res = run_bass_kernel_spmd(nc, maps, core_ids=list(range(B)))
    out = np.stack([np.asarray(r["out"], dtype=np.float32) for r in res.results], axis=0)
    return out
```

```python
import numpy as np
from contextlib import ExitStack
import concourse.bass as bass
import concourse.mybir as mybir
from concourse.bass_utils import run_bass_kernel_spmd

F32 = mybir.dt.float32
BF16 = mybir.dt.bfloat16
I32 = mybir.dt.int32
U32 = mybir.dt.uint32
ALU = mybir.AluOpType
AF = mybir.ActivationFunctionType
AX = mybir.AxisListType

import os
CUT = int(os.environ.get("KCUT", "99"))
SEM_LIMIT = 30000


class Buf:
    __slots__ = ("name", "w", "r")

    def __init__(self, name):
        self.name = name
        self.w = None
        self.r = {}


class Eng:
    def __init__(self, sync, name, eng):
        self.sync = sync
        self.name = name
        self.eng = eng
        self.sem = None
        self.count = 0
        self.known = {}
        self.last = None

    def new_event(self):
        if self.sem is None or self.count >= SEM_LIMIT:
            self.sem = self.sync.new_sem(self.name)
            self.count = 0
        self.count += 1
        self.last = [self.sem, self.count, self.name]
        return self.last


def DmaSlot(sync, name, group=False):
    if name not in sync.slot_by_name:
        sync.slot_by_name[name] = _DmaSlot(sync, name, group)
    return sync.slot_by_name[name]


class _DmaSlot:
    def __init__(self, sync, name, group=False):
        self.name = name
        self.sem = None
        self.val = 0
        self.group = group
        self.pending = []
        sync.slots.append(self)

    def bump(self, sync):
        if self.sem is None or (self.val >= SEM_LIMIT and not self.pending):
            self.sem = sync.new_sem("d" + self.name)
            self.val = 0
        self.val += 16
        ev = [self.sem, self.val, "dma"]
        if self.group:
            self.pending.append(ev)
        return ev

    def close(self):
        for ev in self.pending:
            ev[1] = self.val
        self.pending = []


class Sync:
    def __init__(self, nc, stack):
        self.nc = nc
        self.stack = stack
        self.nsem = 0
        self.slots = []
        self.slot_by_name = {}
        self.E = {
            "pe": Eng(self, "pe", nc.tensor),
            "dve": Eng(self, "dve", nc.vector),
            "act": Eng(self, "act", nc.scalar),
            "pool": Eng(self, "pool", nc.gpsimd),
            "sp": Eng(self, "sp", nc.sync),
        }
        self.ninstr = 0
        self.nwait = 0

    def new_sem(self, name):
        self.nsem += 1
        return self.stack.enter_context(self.nc.semaphore(f"s{self.nsem}_{name}"))

    def _wait(self, E, evs):
        best = {}
        for ev in evs:
            if ev is None:
                continue
            if E.name == "pe" and ev[2] == "pe":
                continue
            k = id(ev[0])
            if k not in best or best[k][1] < ev[1]:
                best[k] = ev
        for ev in best.values():
            sem, val, src = ev
            if E.known.get(id(sem), 0) >= val:
                continue
            E.eng.wait_ge(sem, val)
            self.nwait += 1
            E.known[id(sem)] = val

    def _deps(self, E, reads, writes):
        evs = []
        for b in reads:
            evs.append(b.w)
        for b in writes:
            if b.w is not None and b.w[2] != E.name:
                evs.append(b.w)
            for ev in b.r.values():
                if ev[2] == E.name:
                    continue
                evs.append(ev)
        return evs

    @staticmethod
    def _bufs(lst):
        return [t.b if hasattr(t, "b") else t for t in lst]

    def op(self, engname, fn, r=(), w=()):
        E = self.E[engname]
        r = self._bufs(r)
        w = self._bufs(w)
        self._wait(E, self._deps(E, r, w))
        ins = fn(E.eng)
        ev = E.new_event()
        ins.then_inc(ev[0], 1)
        self.ninstr += 1
        key = ev[2] if ev[2] != "dma" else id(ev[0])
        for b in r:
            b.r[key] = ev
        for b in w:
            b.w = ev
            b.r = {}
        return ev

    def dma(self, qname, fn, slot, r=(), w=()):
        E = self.E[qname]
        r = self._bufs(r)
        w = self._bufs(w)
        self._wait(E, self._deps(E, r, w))
        ins = fn(E.eng)
        ev = slot.bump(self)
        ins.then_inc(ev[0], 16)
        self.ninstr += 1
        key = ev[2] if ev[2] != "dma" else id(ev[0])
        for b in r:
            b.r[key] = ev
        for b in w:
            b.w = ev
            b.r = {}
        return ev

    def barrier(self):
        for s in self.slots:
            if s.group:
                s.close()
        evs = []
        for E in self.E.values():
            if E.last is not None:
                evs.append(E.last)
        for s in self.slots:
            if s.sem is not None:
                evs.append([s.sem, s.val, "dma"])
        for E in self.E.values():
            self._wait(E, [ev for ev in evs if not (E.name == "pe" and ev[2] == "pe")])


class Tile:
    def __init__(self, h, name):
        self.h = h
        self.b = Buf(name)

    def __getitem__(self, k):
        return self.h[k]


P = 128
D = 1024
S_LAT = 4096
S_CTX = 256
NTL = 32
NTC = 2
NT = NTL + NTC
DEPTH = 2
INC = 1792
EPS = 1e-6
NPEER = 16384
GPAD = 16
NSLOT = 17
GRP = 4


def build(depth=DEPTH, dbg=False, stop=None):
    nc = bass.Bass("TRN2", target_bir_lowering=False)

    def din(name, shape, dt=F32):
        return nc.dram_tensor(name, shape, dt, kind="ExternalInput").ap()

    x_d = din("x", [S_LAT, D])
    ctx_d = din("ctx", [S_CTX, D])
    cT_d = din("cT", [P, 8])
    cctxT_d = din("cctxT", [P, 8])
    w_mod_d = din("w_mod", [DEPTH, D, 6 * D])
    b_mod_d = din("b_mod", [DEPTH, 6 * D])
    w_in_d = din("w_in", [DEPTH, D, INC])
    q_gain_d = din("q_gain", [DEPTH, 64])
    k_gain_d = din("k_gain", [DEPTH, 64])
    conv_w_d = din("conv_w", [DEPTH, 31, 512])
    conv_b_d = din("conv_b", [DEPTH, 512])
    ln_g_d = din("ln_g", [DEPTH, 512])
    ln_b_d = din("ln_b", [DEPTH, 512])
    beta_attn_d = din("beta_attn", [DEPTH, 512])
    beta_conv_d = din("beta_conv", [DEPTH, 512])
    w_o_d = din("w_o", [DEPTH, D, D])
    peer_wq_d = din("peer_wq", [DEPTH, D, D])
    peer_keys_d = din("peer_keys", [DEPTH, 8, 2, 128, 64])
    peer_u_d = din("peer_u", [DEPTH, NPEER, D])
    peer_v_d = din("peer_v", [DEPTH, NPEER, D])
    final_g_d = din("final_g", [1, D])
    cos2_d = din("cos2", [S_LAT, 64])
    sin2_d = din("sin2", [S_LAT, 64])
    peer_u_flat = peer_u_d.rearrange("l n d -> (l n) d")
    peer_v_flat = peer_v_d.rearrange("l n d -> (l n) d")
    out_d = nc.dram_tensor("out", [S_LAT, D], F32, kind="ExternalOutput").ap()
    xs_d = nc.dram_tensor("xs", [S_LAT + S_CTX, D], F32).ap()
    uv_d = nc.dram_tensor("uv", [DEPTH * NPEER, 2 * D], BF16).ap()
    modscr_d = nc.dram_tensor("modscr", [2, P, 4096], F32).ap()
    dbg_d = {}
    if dbg:
        for nm, shp in [("d_qkvu", [NT * P, INC]), ("d_x1", [NT * P, D]), ("d_oT", [P, 4 * S_LAT]),
                        ("d_pre", [NT * P, 128]), ("d_eidx", [NT * P, 128]), ("d_w", [NT * P, 128]),
                        ("d_qT", [P, 4 * S_LAT]), ("d_x2", [NT * P, D]), ("d_mod", [P, 4096])]:
            dbg_d[nm] = nc.dram_tensor(nm, shp, F32, kind="ExternalOutput").ap()

    top = ExitStack()
    with top:
        S = Sync(nc, top)

        uid = [0]

        def sbt(stack, name, shape, dt):
            uid[0] += 1
            name = f"{name}_u{uid[0]}"
            return Tile(stack.enter_context(nc.sbuf_tensor(name, shape, dt)), name)

        def pst(stack, name, shape, dt):
            uid[0] += 1
            name = f"{name}_u{uid[0]}"
            return Tile(stack.enter_context(nc.psum_tensor(name, shape, dt)), name)

        out_slot = DmaSlot(S, "out", group=True)
        dbg_slots = {}
        xs_buf = [Buf(f"xs{i}") for i in range(NT)]

        ident_f = sbt(top, "ident_f", [P, P], F32)
        ident_b = sbt(top, "ident_b", [P, P], BF16)
        ones_f = sbt(top, "ones_f", [P, P], F32)
        io_t = sbt(top, "io_t", [P, P], F32)
        pid_t = sbt(top, "pid_t", [P, 1], F32)
        S.op("pool", lambda e: e.iota(io_t[:], pattern=[[1, P]], base=0, channel_multiplier=0,
                                      allow_small_or_imprecise_dtypes=True), w=[io_t])
        S.op("pool", lambda e: e.iota(pid_t[:], pattern=[[0, 1]], base=0, channel_multiplier=1,
                                      allow_small_or_imprecise_dtypes=True), w=[pid_t])
        S.op("dve", lambda e: e.tensor_scalar(ident_f[:], io_t[:], pid_t[:, 0:1], None, op0=ALU.is_equal),
             r=[io_t, pid_t], w=[ident_f])
        S.op("dve", lambda e: e.tensor_copy(ident_b[:], ident_f[:]), r=[ident_f], w=[ident_b])
        S.op("pool", lambda e: e.memset(ones_f[:], 1.0), w=[ones_f])

        cslot = DmaSlot(S, "const", group=True)
        craw = sbt(top, "craw", [P, 2, 8], F32)
        scT = sbt(top, "scT", [P, 8, 2], F32)
        S.dma("sp", lambda e: e.dma_start(out=craw[:, 0, :], in_=cT_d), cslot, w=[craw])
        S.dma("sp", lambda e: e.dma_start(out=craw[:, 1, :], in_=cctxT_d), cslot, w=[craw])
        cslot.close()
        S.op("act", lambda e: e.activation(scT[:].rearrange("p j r -> p r j"), craw[:], AF.Silu), r=[craw], w=[scT])
        modcol = sbt(top, "modcol", [P, 2, 16], F32)
        eps6 = sbt(top, "eps6", [P, 1], F32)
        eps5 = sbt(top, "eps5", [P, 1], F32)
        S.op("pool", lambda e: e.memset(eps6[:], 1e-6), w=[eps6])
        S.op("pool", lambda e: e.memset(eps5[:], 1e-5), w=[eps5])


        def rstd_from_ss(ss, rs, scale, eps_t):
            S.op("act", lambda e: e.activation(rs[:], ss[:], AF.Sqrt, scale=scale, bias=eps_t[:, 0:1]), r=[ss, eps_t], w=[rs])
            S.op("dve", lambda e: e.reciprocal(rs[:], rs[:]), r=[rs], w=[rs])

        def dbg_store(name, rows, tile_ap, rtiles):
            if dbg and name in dbg_d:
                if name not in dbg_slots:
                    dbg_slots[name] = (DmaSlot(S, name), Buf(name))
                S.dma("sp", lambda e: e.dma_start(out=dbg_d[name][rows], in_=tile_ap), dbg_slots[name][0], r=rtiles, w=[dbg_slots[name][1]])


        def convert_thunks(ph, NB=3):
            stg = [sbt(ph, f"cvi{k}", [P, 4, D], F32) for k in range(NB)]
            obf = [sbt(ph, f"cvo{k}", [P, 4, D], BF16) for k in range(NB)]
            islot = [DmaSlot(S, f"cvi{k}") for k in range(NB)]
            oslot = [DmaSlot(S, f"cvo{k}") for k in range(NB)]
            steps = []
            n = 0
            for l in range(DEPTH):
                for tab, col0 in ((peer_u_d, 0), (peer_v_d, D)):
                    for t in range(NPEER // 512):
                        k = n % NB
                        src = tab[l, t * 512:(t + 1) * 512, :].rearrange("(p r) d -> p r d", r=4)
                        dst = uv_d[l * NPEER + t * 512:l * NPEER + (t + 1) * 512, col0:col0 + D].rearrange("(p r) d -> p r d", r=4)
                        steps.append((
                            ("dma", "sp", lambda e, k=k, src=src: e.dma_start(out=stg[k][:], in_=src), islot[k], [], [stg[k]]),
                            ("op", "dve", lambda e, k=k: e.tensor_copy(obf[k][:], stg[k][:]), [stg[k]], [obf[k]]),
                            ("dma", "pool", lambda e, k=k, dst=dst: e.dma_start(out=dst, in_=obf[k][:]), oslot[k], [obf[k]], []),
                        ))
                        n += 1
            cq = []
            for n in range(len(steps) + 2):
                if n < len(steps):
                    cq.append(steps[n][0])
                if n >= 2:
                    cq.append(steps[n - 2][1])
                    cq.append(steps[n - 2][2])
            return cq

        def emit(rq, n):
            for _ in range(n):
                if not rq:
                    return
                t = rq.pop(0)
                if t[0] == "op":
                    S.op(t[1], t[2], t[3], t[4])
                else:
                    S.dma(t[1], t[2], t[3], t[4], t[5])

        def mod_cols(l):
            with ExitStack() as ph:
                wm = [sbt(ph, f"wm{k}", [P, 8, P], F32) for k in range(2)]
                wslot = [DmaSlot(S, f"wm{k}") for k in range(2)]
                bcol = sbt(ph, "bcol", [P, 16], F32)
                pcol = pst(ph, "pcol", [P, 16, 2], F32)
                S.dma("sp", lambda e: e.dma_start(out=bcol[:], in_=b_mod_d[l, 0:2048].rearrange("(c p) -> p c", p=P),
                                                  allow_slow_non_contiguous=True), DmaSlot(S, "bcol"), w=[bcol])
                for cc in range(16):
                    t = wm[cc % 2]
                    S.dma("sp", lambda e, t=t, cc=cc: e.dma_start(
                        out=t[:], in_=w_mod_d[l, :, cc * P:(cc + 1) * P].rearrange("(j p) m -> p j m", p=P)),
                          wslot[cc % 2], w=[t])
                    for j in range(8):
                        S.op("pe", lambda e, t=t, j=j, cc=cc: e.matmul(pcol[:, cc, :], lhsT=t[:, j, :],
                                                                     rhs=scT[:, j, :], start=(j == 0), stop=(j == 7)),
                             r=[t, scT], w=[pcol])
                S.op("dve", lambda e: e.tensor_tensor(modcol[:].rearrange("p r c -> p c r"), pcol[:],
                                                      bcol[:].unsqueeze(2).to_broadcast([P, 16, 2]), op=ALU.add),
                     r=[pcol, bcol], w=[modcol])
                S.op("dve", lambda e: e.tensor_scalar(modcol[:, :, 8:16], modcol[:, :, 8:16], 1.0, None, op0=ALU.add),
                     r=[modcol], w=[modcol])
                S.barrier()

        def mod_bc(l, r, modbc):
            with ExitStack() as ph:
                NW = 4
                wm = [sbt(ph, f"wmb{k}", [P, 512], F32) for k in range(NW)]
                wslot = [DmaSlot(S, f"wmb{k}") for k in range(NW)]
                bbc = [sbt(ph, f"bbc{k}", [P, 512], F32) for k in range(2)]
                bslot = [DmaSlot(S, f"bbc{k}") for k in range(2)]
                screp = sbt(ph, "screp", [P, 8, P], F32)
                pb = pst(ph, "pbm", [P, 512], F32)
                S.op("dve", lambda e: e.tensor_copy(screp[:], scT[:, :, r].unsqueeze(2).to_broadcast([P, 8, P])),
                     r=[scT], w=[screp])
                n = 0
                for cc in range(8):
                    c0 = 2048 + cc * 512
                    S.dma("sp", lambda e, cc=cc, c0=c0: e.dma_start(out=bbc[cc % 2][:], in_=b_mod_d[l, c0:c0 + 512].partition_broadcast(P)),
                          bslot[cc % 2], w=[bbc[cc % 2]])
                    for j in range(8):
                        t = wm[n % NW]
                        S.dma("sp", lambda e, t=t, j=j, c0=c0: e.dma_start(out=t[:], in_=w_mod_d[l, j * P:(j + 1) * P, c0:c0 + 512]),
                              wslot[n % NW], w=[t])
                        n += 1
                        S.op("pe", lambda e, t=t, j=j: e.matmul(pb[:], lhsT=screp[:, j, :], rhs=t[:], start=(j == 0), stop=(j == 7)),
                             r=[t, screp], w=[pb])
                    S.op("dve", lambda e, cc=cc: e.tensor_tensor(modbc[:, cc * 512:(cc + 1) * 512], pb[:], bbc[cc % 2][:], op=ALU.add),
                         r=[pb, bbc[cc % 2]], w=[modbc])
                S.op("dve", lambda e: e.tensor_scalar(modbc[:, 2048:3072], modbc[:, 2048:3072], 1.0, None, op0=ALU.add),
                     r=[modbc], w=[modbc])
                S.barrier()

        def phase_a(l, M, src_aps, last):
            with ExitStack() as ph:
                win = sbt(ph, "win", [P, 8, INC], BF16)
                stg = sbt(ph, "stg", [P, INC], F32)
                stg_slot = DmaSlot(S, "stg")
                bias_bc = sbt(ph, "bias_bc", [P, INC], F32)
                sh1rep = sbt(ph, "sh1rep", [P, 2, 8, P], BF16)
                gain = sbt(ph, "gain", [P, 640], F32)
                graw = sbt(ph, "graw", [P, 128], F32)
                cos2 = sbt(ph, "cos2", [P, NTL, 64], F32)
                sin2 = sbt(ph, "sin2", [P, NTL, 64], F32)
                xt = [sbt(ph, f"xt{k}", [P, D], F32) for k in range(2)]
                xt_slot = [DmaSlot(S, f"xt{k}") for k in range(2)]
                xb = [sbt(ph, f"xb{k}", [P, D], BF16) for k in range(2)]
                xT = [sbt(ph, f"xT{k}", [P, 8, P], BF16) for k in range(2)]
                junk = sbt(ph, "junkA", [P, D], BF16)
                ss = [sbt(ph, f"ssA{k}", [P, 1], F32) for k in range(2)]
                rstd = [sbt(ph, f"rstdA{k}", [P, 1], F32) for k in range(2)]
                qkvu = sbt(ph, "qkvu", [P, INC], F32)
                sq = sbt(ph, "sq", [P, 640], F32)
                ssq = sbt(ph, "ssq", [P, 10], F32)
                rsq = sbt(ph, "rsq", [P, 10], F32)
                qn = sbt(ph, "qn", [P, 640], F32)
                rb = sbt(ph, "rb", [P, 640], F32)
                qr = sbt(ph, "qr", [P, 4, 2, 64], BF16)
                kr = sbt(ph, "kr", [P, 128], BF16)
                sig = sbt(ph, "sig", [P, 512], F32)
                gg = sbt(ph, "gg", [P, 512], BF16)
                ptb = pst(ph, "ptb", [P, 8, P], BF16)
                pq = [pst(ph, f"pq{k}", [P, 512], F32) for k in range(4)]
                ptq = pst(ph, "ptq", [P, 5, P], BF16)
                ptg = pst(ph, "ptg", [P, 4, P], BF16)
                ra = sq
                qT, qTc, kT, Vp, gT, gTc = M["qT"], M["qTc"], M["kT"], M["Vp"], M["gT"], M["gTc"]

                cslot2 = DmaSlot(S, "rope", group=True)
                S.dma("sp", lambda e: e.dma_start(out=cos2[:], in_=cos2_d.rearrange("(n p) f -> p n f", p=P)), cslot2, w=[cos2])
                S.dma("sp", lambda e: e.dma_start(out=sin2[:], in_=sin2_d.rearrange("(n p) f -> p n f", p=P)), cslot2, w=[sin2])
                S.dma("sp", lambda e: e.dma_start(out=graw[:, 0:64], in_=q_gain_d[l].partition_broadcast(P)), cslot2, w=[graw])
                S.dma("sp", lambda e: e.dma_start(out=graw[:, 64:128], in_=k_gain_d[l].partition_broadcast(P)), cslot2, w=[graw])
                cslot2.close()
                for j in range(8):
                    S.dma("sp", lambda e, j=j: e.dma_start(out=stg[:], in_=w_in_d[l, j * P:(j + 1) * P, :]), stg_slot, w=[stg])
                    S.op("act", lambda e, j=j: e.activation(win[:, j, :], stg[:], AF.Copy), r=[stg], w=[win])
                S.op("dve", lambda e: e.tensor_copy(gain[:, 0:512].rearrange("p (h d) -> p h d", h=8),
                                                    graw[:, 0:64].unsqueeze(1).to_broadcast([P, 8, 64])), r=[graw], w=[gain])
                S.op("dve", lambda e: e.tensor_copy(gain[:, 512:640].rearrange("p (h d) -> p h d", h=2),
                                                    graw[:, 64:128].unsqueeze(1).to_broadcast([P, 2, 64])), r=[graw], w=[gain])
                S.op("dve", lambda e: e.tensor_copy(sh1rep[:], modcol[:, :, 0:8].unsqueeze(3).to_broadcast([P, 2, 8, P])),
                     r=[modcol], w=[sh1rep])

                def make_bias(r_):
                    for cc in range(4):
                        w_ = min(512, INC - cc * 512)
                        for j in range(8):
                            S.op("pe", lambda e, cc=cc, j=j, w_=w_: e.matmul(
                                pq[cc][:, 0:w_], lhsT=sh1rep[:, r_, j, :], rhs=win[:, j, cc * 512:cc * 512 + w_],
                                start=(j == 0), stop=(j == 7)), r=[sh1rep, win], w=[pq[cc]])
                        S.op("act", lambda e, cc=cc, w_=w_: e.activation(
                            bias_bc[:, cc * 512:cc * 512 + w_], pq[cc][:, 0:w_], AF.Copy), r=[pq[cc]], w=[bias_bc])

                def load(i):
                    k = i % 2
                    S.dma("sp", lambda e: e.dma_start(out=xt[k][:], in_=src_aps[i]), xt_slot[k], r=[xs_buf[i]], w=[xt[k]])

                make_bias(0)
                load(0)
                qk2 = [qkvu, stg]

                def stage1(i):
                    k = i % 2
                    r_ = 0 if i < NTL else 1
                    qk_ = qk2[i % 2]
                    S.op("act", lambda e: e.activation(junk[:], xt[k][:], AF.Square, accum_out=ss[k][:]), r=[xt[k]], w=[junk, ss[k]])
                    S.op("act", lambda e: e.activation(xb[k][:], xt[k][:], AF.Copy), r=[xt[k]], w=[xb[k]])
                    rstd_from_ss(ss[k], rstd[k], 1.0 / D, eps6)
                    for j in range(8):
                        S.op("pe", lambda e, j=j: e.transpose(ptb[:, j, :], xb[k][:, j * P:(j + 1) * P], ident_b[:]),
                             r=[xb[k], ident_b], w=[ptb])
                    S.op("dve", lambda e: e.tensor_tensor(xT[k][:], ptb[:],
                                                          modcol[:, r_, 8:16].unsqueeze(2).to_broadcast([P, 8, P]), op=ALU.mult),
                         r=[ptb, modcol], w=[xT[k]])
                    if CUT <= 1:
                        return
                    for cc in range(4):
                        w_ = min(512, INC - cc * 512)
                        for j in range(8):
                            S.op("pe", lambda e, cc=cc, j=j, w_=w_: e.matmul(
                                pq[cc][:, 0:w_], lhsT=xT[k][:, j, :], rhs=win[:, j, cc * 512:cc * 512 + w_],
                                start=(j == 0), stop=(j == 7)), r=[xT[k], win], w=[pq[cc]])
                        S.op("dve", lambda e, cc=cc, w_=w_: e.scalar_tensor_tensor(
                            out=qk_[:, cc * 512:cc * 512 + w_], in0=pq[cc][:, 0:w_], scalar=rstd[k][:, 0:1],
                            in1=bias_bc[:, cc * 512:cc * 512 + w_], op0=ALU.mult, op1=ALU.add),
                             r=[pq[cc], rstd[k], bias_bc], w=[qk_])

                def stage2(i):
                    k = i % 2
                    is_ctx = i >= NTL
                    qk_ = qk2[i % 2]
                    if l == 0:
                        dbg_store("d_qkvu", slice(i * P, (i + 1) * P), qk_[:], [qk_])
                    if CUT <= 2:
                        return
                    S.op("pool", lambda e: e.tensor_tensor(sq[:], qk_[:, 0:640], qk_[:, 0:640], op=ALU.mult), r=[qk_], w=[sq])
                    S.op("dve", lambda e: e.tensor_reduce(out=ssq[:], in_=sq[:].rearrange("p (h d) -> p h d", h=10),
                                                          axis=AX.X, op=ALU.add), r=[sq], w=[ssq])
                    rstd_from_ss(ssq, rsq, 1.0 / 64, eps6)
                    S.op("dve", lambda e: e.tensor_tensor(qn[:].rearrange("p (h d) -> p h d", h=10),
                                                          qk_[:, 0:640].rearrange("p (h d) -> p h d", h=10),
                                                          rsq[:].unsqueeze(2).to_broadcast([P, 10, 64]), op=ALU.mult),
                         r=[qk_, rsq], w=[qn])
                    S.op("pool", lambda e: e.tensor_tensor(qn[:], qn[:], gain[:], op=ALU.mult), r=[qn, gain], w=[qn])
                    if CUT <= 3:
                        return
                    if not is_ctx:
                        qn3 = qn[:].rearrange("p (h d) -> p h d", h=10)
                        rb3 = rb[:].rearrange("p (h d) -> p h d", h=10)
                        S.op("dve", lambda e: e.tensor_tensor(ra[:].rearrange("p (h d) -> p h d", h=10), qn3,
                                                              cos2[:, i, :].unsqueeze(1).to_broadcast([P, 10, 64]), op=ALU.mult),
                             r=[qn, cos2], w=[ra])
                        S.op("pool", lambda e: e.tensor_tensor(rb3[:, :, 0:32], qn3[:, :, 32:64],
                                                               sin2[:, i, 0:32].unsqueeze(1).to_broadcast([P, 10, 32]), op=ALU.mult),
                             r=[qn, sin2], w=[rb])
                        S.op("pool", lambda e: e.tensor_tensor(rb3[:, :, 32:64], qn3[:, :, 0:32],
                                                               sin2[:, i, 32:64].unsqueeze(1).to_broadcast([P, 10, 32]), op=ALU.mult),
                             r=[qn, sin2], w=[rb])
                        S.op("dve", lambda e: e.tensor_tensor(qr[:].rearrange("p pr hh d -> p hh pr d"),
                                                              ra[:, 0:512].rearrange("p (hh pr d) -> p hh pr d", hh=2, pr=4),
                                                              rb[:, 0:512].rearrange("p (hh pr d) -> p hh pr d", hh=2, pr=4), op=ALU.add),
                             r=[ra, rb], w=[qr])
                        S.op("pool", lambda e: e.tensor_tensor(kr[:], ra[:, 512:640], rb[:, 512:640], op=ALU.add), r=[ra, rb], w=[kr])
                    else:
                        S.op("dve", lambda e: e.tensor_copy(qr[:].rearrange("p pr hh d -> p hh pr d"),
                                                            qn[:, 0:512].rearrange("p (hh pr d) -> p hh pr d", hh=2, pr=4)),
                             r=[qn], w=[qr])
                        S.op("pool", lambda e: e.tensor_copy(kr[:], qn[:, 512:640]), r=[qn], w=[kr])
                    if CUT <= 4:
                        return
                    need_q = (not is_ctx) or (not last)
                    if need_q:
                        for pr in range(4):
                            S.op("pe", lambda e, pr=pr: e.transpose(ptq[:, pr, :], qr[:, pr, :, :].rearrange("p hh d -> p (hh d)"), ident_b[:]),
                                 r=[qr, ident_b], w=[ptq])
                    S.op("pe", lambda e: e.transpose(ptq[:, 4, :], kr[:], ident_b[:]), r=[kr, ident_b], w=[ptq])
                    if need_q:
                        if not is_ctx:
                            S.op("act", lambda e: e.activation(qT[:, :, i * P:(i + 1) * P], ptq[:, 0:4, :], AF.Copy),
                                 r=[ptq], w=[M["qT_b"][pr][i // 4] for pr in range(4)])
                        else:
                            S.op("act", lambda e: e.activation(qTc[:, :, (i - NTL) * P:(i - NTL + 1) * P], ptq[:, 0:4, :], AF.Copy),
                                 r=[ptq], w=[qTc])
                    S.op("act", lambda e: e.activation(kT[:, i * P:(i + 1) * P], ptq[:, 4, :], AF.Copy), r=[ptq], w=[M["kT_b"][i]])
                    if CUT <= 5:
                        return
                    S.op("pool", lambda e: e.tensor_copy(Vp[:, i, 0:64], qk_[:, 640:704]), r=[qk_], w=[M["Vp_b"][i]])
                    S.op("pool", lambda e: e.tensor_copy(Vp[:, i, 128:192], qk_[:, 704:768]), r=[qk_], w=[M["Vp_b"][i]])
                    if CUT <= 6:
                        return
                    if need_q:
                        S.op("act", lambda e: e.activation(sig[:], qk_[:, 1280:1792], AF.Sigmoid), r=[qk_], w=[sig])
                        S.op("dve", lambda e: e.tensor_tensor(gg[:], qk_[:, 768:1280], sig[:], op=ALU.mult), r=[qk_, sig], w=[gg])
                        for c in range(4):
                            S.op("pe", lambda e, c=c: e.transpose(ptg[:, c, :], gg[:, c * P:(c + 1) * P], ident_b[:]),
                                 r=[gg, ident_b], w=[ptg])
                        if not is_ctx:
                            S.op("act", lambda e: e.activation(gT[:, :, GPAD + i * P:GPAD + (i + 1) * P], ptg[:], AF.Copy),
                                 r=[ptg], w=[M["gT_b"][i]])
                        else:
                            S.op("act", lambda e: e.activation(gTc[:, :, GPAD + (i - NTL) * P:GPAD + (i - NTL + 1) * P], ptg[:], AF.Copy),
                                 r=[ptg], w=[M["gTc_b"][i - NTL]])


                ntile = NT if stop not in ("a0", "a1") else (0 if stop == "a0" else 1)
                for i in range(ntile):
                    k = i % 2
                    r_ = 0 if i < NTL else 1
                    is_ctx = i >= NTL
                    if i == NTL:
                        make_bias(1)
                    if i + 1 < NT:
                        load(i + 1)
                    stage1(i)
                    if i >= 1:
                        stage2(i - 1)
                if ntile:
                    stage2(ntile - 1)

                S.barrier()

        def phase_b(l, M, last):
            with ExitStack() as ph:
                NPT = 4
                NPS = 4
                LA = 1
                pT = [sbt(ph, f"pT{k}", [P, 512], BF16) for k in range(NPT)]
                rd = sbt(ph, "rd", [P, 512], F32)
                bcs = sbt(ph, "bcs", [P, 512], F32)
                ps_s = [pst(ph, f"ps_s{k}", [P, 512], F32) for k in range(NPS)]
                po = [pst(ph, f"po{k}", [P, 512], F32) for k in range(3)]
                pbc = pst(ph, "pbc", [P, 512], F32)
                qT, qTc, kT, Vp = M["qT"], M["qTc"], M["kT"], M["Vp"]
                cnt = [0]
                grp = [0]
                kTz = sbt(ph, "kTz", [P, 2, NT * P], BF16)
                S.op("pool", lambda e: e.memset(kTz[:], 0.0), w=[kTz])
                S.op("dve", lambda e: e.tensor_copy(kTz[0:64, 0, :], kT[0:64, :]), r=M["kT_b"], w=[kTz])
                S.op("act", lambda e: e.activation(kTz[64:128, 1, :], kT[64:128, :], AF.Copy), r=M["kT_b"], w=[kTz])
                cq = convert_thunks(ph) if (l == 0 and stop in (None, "p1")) else []
                cqn = [0]

                def attend(qsrc_fn, qfull, qbuf, n, ktiles):
                    its = [(kt_i, kt, hh) for kt_i, kt in enumerate(ktiles) for hh in range(2)]
                    base = cnt[0]
                    cnt[0] += len(its)
                    pg = [po[(2 * grp[0]) % 3], po[(2 * grp[0] + 1) % 3]]
                    grp[0] += 1

                    def emit_s(m):
                        kt_i, kt, hh = its[m]
                        k = (base + m) % NPS
                        kp = (base + m) % NPT
                        S.op("pe", lambda e: e.matmul(ps_s[k][:, 0:n], lhsT=kTz[:, hh, kt * P:(kt + 1) * P], rhs=qfull,
                                                      start=True, stop=True), r=[kTz, qbuf], w=[ps_s[k]])
                        S.op("act", lambda e: e.activation(pT[kp][:, 0:n], ps_s[k][:, 0:n], AF.Exp, scale=0.125),
                             r=[ps_s[k]], w=[pT[kp]])

                    def emit_pv(m):
                        kt_i, kt, hh = its[m]
                        kp = (base + m) % NPT
                        S.op("pe", lambda e: e.matmul(pg[hh][:, 0:n], lhsT=Vp[:, kt, hh * 64:hh * 64 + 128], rhs=pT[kp][:, 0:n],
                                                      start=(kt_i == 0), stop=(kt_i == len(ktiles) - 1)), r=[M["Vp_b"][kt], pT[kp]], w=[pg[hh]])

                    npair = len(its) // 2
                    for j in range(npair + LA):
                        if j < npair:
                            emit_s(2 * j)
                            emit_s(2 * j + 1)
                        if j - LA >= 0:
                            emit_pv(2 * (j - LA))
                            emit_pv(2 * (j - LA) + 1)
                        cqn[0] += 1
                        if cqn[0] % 4 != 3:
                            emit(cq, 1)
                    for hh in range(2):
                        dp = 64 if hh == 0 else 0
                        S.op("dve", lambda e, hh=hh, dp=dp: e.reciprocal(rd[dp:dp + 1, 0:n], pg[hh][dp:dp + 1, 0:n]),
                             r=[pg[hh]], w=[rd])
                        S.op("pe", lambda e, dp=dp: e.matmul(pbc[0:64, 0:n], lhsT=ones_f[dp:dp + 1, 0:64], rhs=rd[dp:dp + 1, 0:n],
                                                            start=True, stop=True), r=[ones_f, rd], w=[pbc])
                        S.op("act", lambda e, hh=hh: e.activation(bcs[hh * 64:(hh + 1) * 64, 0:n], pbc[0:64, 0:n], AF.Copy),
                             r=[pbc], w=[bcs])
                        S.op("dve", lambda e, hh=hh: e.tensor_tensor(qsrc_fn(hh), pg[hh][hh * 64:(hh + 1) * 64, 0:n],
                                                                     bcs[hh * 64:(hh + 1) * 64, 0:n], op=ALU.mult),
                             r=[pg[hh], bcs], w=[qbuf])

                for c in range(8):
                    for pr in range(4):
                        attend(lambda hh, c=c, pr=pr: qT[hh * 64:(hh + 1) * 64, pr, c * 512:(c + 1) * 512],
                               qT[:, pr, c * 512:(c + 1) * 512], M["qT_b"][pr][c], 512, list(range(NT)))
                if not last:
                    for pr in range(4):
                        attend(lambda hh, pr=pr: qTc[hh * 64:(hh + 1) * 64, pr, :], qTc[:, pr, :], qTc.b, S_CTX, [NTL, NTL + 1])
                emit(cq, len(cq))
                S.barrier()

        def phase_c(l, M, tiles, src_aps, r_, modbc):
            with ExitStack() as ph:
                wcol = sbt(ph, "wcol", [P, 4, 31], F32)
                DG = sbt(ph, "DG", [P, 4, 31, P], BF16)
                wo = sbt(ph, "wo", [P, 8, D], BF16)
                stg = [sbt(ph, f"stgC{k}", [P, D], F32) for k in range(2)]
                stg_slot = [DmaSlot(S, f"stgC{k}") for k in range(2)]
                betaA = sbt(ph, "betaA", [P, 8], F32)
                betac = sbt(ph, "betac", [P, 8], F32)
                cb_bc = sbt(ph, "cb_bc", [P, 512], F32)
                lg_bc = sbt(ph, "lg_bc", [P, 512], F32)
                lb_bc = sbt(ph, "lb_bc", [P, 512], F32)
                xt = [sbt(ph, f"xtC{k}", [P, D], F32) for k in range(2)]
                xt_slot = [DmaSlot(S, f"xtC{k}") for k in range(2)]
                y = sbt(ph, "yC", [P, 512], F32)
                junk = sbt(ph, "junkC", [P, 512], F32)
                st4 = sbt(ph, "st4", [P, 4], F32)
                rln = sbt(ph, "rln", [P, 1], F32)
                msq = sbt(ph, "msq", [P, 1], F32)
                z = sbt(ph, "zC", [P, 512], F32)
                oc = sbt(ph, "oc", [P, 512], F32)
                ocb = sbt(ph, "ocb", [P, 512], BF16)
                ocT = sbt(ph, "ocT", [P, 4, P], BF16)
                sqo = sbt(ph, "sqo", [P, 4, P], F32)
                ssc = sbt(ph, "ssc", [P, 2], F32)
                rsc = sbt(ph, "rsc", [P, 2], F32)
                t1 = sbt(ph, "t1", [P, D], F32)
                x1 = [sbt(ph, f"x1_{k}", [P, D], F32) for k in range(2)]
                x1_slot = [DmaSlot(S, f"x1s{k}") for k in range(2)]
                py2 = [pst(ph, f"py{k}", [P, 512], F32) for k in range(2)]
                ptc = pst(ph, "ptc", [P, 4, P], BF16)
                pss = pst(ph, "pss", [P, 16], F32)
                pa = [pst(ph, f"pa{k}", [P, 512], F32) for k in range(2)]
                pc = [pst(ph, f"pc{k}", [P, 512], F32) for k in range(2)]
                qT, qTc, gT, gTc = M["qT"], M["qTc"], M["gT"], M["gTc"]

                vs = DmaSlot(S, "vecC", group=True)
                for c in range(4):
                    S.dma("sp", lambda e, c=c: e.dma_start(out=wcol[:, c, :], in_=conv_w_d[l, :, c * P:(c + 1) * P].rearrange("j p -> p j"),
                                                           allow_slow_non_contiguous=True), vs, w=[wcol])
                for half in range(2):
                    S.dma("sp", lambda e, half=half: e.dma_start(out=betaA[half * 64:(half + 1) * 64, :],
                                                                 in_=beta_attn_d[l].rearrange("(h d) -> d h", d=64),
                                                                 allow_slow_non_contiguous=True), vs, w=[betaA])
                S.dma("sp", lambda e: e.dma_start(out=betac[:, 4:8], in_=beta_conv_d[l].rearrange("(c p) -> p c", p=P),
                                                  allow_slow_non_contiguous=True), vs, w=[betac])
                S.dma("sp", lambda e: e.dma_start(out=cb_bc[:], in_=conv_b_d[l].partition_broadcast(P)), vs, w=[cb_bc])
                S.dma("sp", lambda e: e.dma_start(out=lg_bc[:], in_=ln_g_d[l].partition_broadcast(P)), vs, w=[lg_bc])
                S.dma("sp", lambda e: e.dma_start(out=lb_bc[:], in_=ln_b_d[l].partition_broadcast(P)), vs, w=[lb_bc])
                vs.close()
                S.op("dve", lambda e: e.tensor_copy(betac[0:64, 0:4], betaA[0:64, 0:4]), r=[betaA], w=[betac])
                S.op("dve", lambda e: e.tensor_copy(betac[64:128, 0:4], betaA[64:128, 4:8]), r=[betaA], w=[betac])
                for kk in range(8):
                    t = stg[kk % 2]
                    sl = stg_slot[kk % 2]
                    if kk < 4:
                        S.dma("sp", lambda e, t=t, kk=kk: e.dma_start(out=t[0:64, :], in_=w_o_d[l, kk * 64:(kk + 1) * 64, :]), sl, w=[t])
                        S.dma("sp", lambda e, t=t, kk=kk: e.dma_start(out=t[64:128, :], in_=w_o_d[l, (kk + 4) * 64:(kk + 5) * 64, :]), sl, w=[t])
                    else:
                        S.dma("sp", lambda e, t=t, kk=kk: e.dma_start(out=t[:], in_=w_o_d[l, 512 + (kk - 4) * P:512 + (kk - 3) * P, :]), sl, w=[t])
                    S.op("act", lambda e, t=t, kk=kk: e.activation(wo[:, kk, :], t[:], AF.Identity, scale=betac[:, kk:kk + 1]),
                         r=[t, betac], w=[wo])
                for c in range(4):
                    for j in range(31):
                        S.op("pool", lambda e, c=c, j=j: e.tensor_scalar(DG[:, c, j, :], ident_f[:], wcol[:, c, j:j + 1], None, op0=ALU.mult),
                             r=[ident_f, wcol], w=[DG])

                def load(n_):
                    i = tiles[n_]
                    k = n_ % 2
                    S.dma("sp", lambda e: e.dma_start(out=xt[k][:], in_=src_aps[i]), xt_slot[k], r=[xs_buf[i]], w=[xt[k]])

                def conv(n_):
                    i = tiles[n_]
                    pyc = py2[n_ % 2]
                    if i < NTL:
                        gsrc, gb, t0 = gT, M["gT_b"], i * P
                        lo, hi = max(0, i - 1), min(NTL - 1, i + 1)
                    else:
                        gsrc, gb, t0 = gTc, M["gTc_b"], (i - NTL) * P
                        lo, hi = max(0, i - NTL - 1), min(NTC - 1, i - NTL + 1)
                    for c in range(4):
                        for j in range(31):
                            S.op("pe", lambda e, c=c, j=j: e.matmul(pyc[:, c * P:(c + 1) * P],
                                                                  lhsT=gsrc[:, c, t0 + j + GPAD - 15:t0 + j + GPAD - 15 + P],
                                                                  rhs=DG[:, c, j, :], start=(j == 0), stop=(j == 30)),
                                 r=[gb[q] for q in range(lo, hi + 1)] + [DG], w=[pyc])

                load(0)
                conv(0)
                for n_, i in enumerate(tiles):
                    k = n_ % 2
                    is_ctx = i >= NTL
                    if n_ + 1 < len(tiles):
                        load(n_ + 1)
                    if not is_ctx:
                        osrc = lambda pr: qT[:, pr, i * P:(i + 1) * P]
                        obufs = [M["qT_b"][pr][i // 4] for pr in range(4)]
                    else:
                        osrc = lambda pr: qTc[:, pr, (i - NTL) * P:(i - NTL + 1) * P]
                        obufs = [qTc.b]
                    if n_ + 1 < len(tiles):
                        conv(n_ + 1)
                    py = py2[n_ % 2]
                    S.op("dve", lambda e: e.tensor_tensor(y[:], py[:], cb_bc[:], op=ALU.add), r=[py, cb_bc], w=[y])
                    S.op("act", lambda e: e.activation(junk[:], y[:], AF.Identity, accum_out=st4[:, 0:1]), r=[y], w=[junk, st4])
                    S.op("act", lambda e: e.activation(junk[:], y[:], AF.Square, accum_out=st4[:, 1:2]), r=[y], w=[junk, st4])
                    S.op("dve", lambda e: e.tensor_scalar(st4[:, 2:4], st4[:, 0:2], 1.0 / 512, None, op0=ALU.mult), r=[st4], w=[st4])
                    S.op("dve", lambda e: e.tensor_tensor(msq[:], st4[:, 2:3], st4[:, 2:3], op=ALU.mult), r=[st4], w=[msq])
                    S.op("dve", lambda e: e.tensor_tensor(msq[:], st4[:, 3:4], msq[:], op=ALU.subtract), r=[st4, msq], w=[msq])
                    rstd_from_ss(msq, rln, 1.0, eps5)
                    S.op("dve", lambda e: e.tensor_scalar(z[:], y[:], st4[:, 2:3], rln[:, 0:1], op0=ALU.subtract, op1=ALU.mult),
                         r=[y, st4, rln], w=[z])
                    S.op("pool", lambda e: e.tensor_tensor(z[:], z[:], lg_bc[:], op=ALU.mult), r=[z, lg_bc], w=[z])
                    S.op("pool", lambda e: e.tensor_tensor(z[:], z[:], lb_bc[:], op=ALU.add), r=[z, lb_bc], w=[z])
                    S.op("act", lambda e: e.activation(oc[:], z[:], AF.Silu), r=[z], w=[oc])
                    S.op("act", lambda e: e.activation(junk[:], oc[:], AF.Square, accum_out=ssc[:, 0:1]), r=[oc], w=[junk, ssc])
                    S.op("pool", lambda e: e.tensor_copy(ocb[:], oc[:]), r=[oc], w=[ocb])
                    for c in range(4):
                        S.op("pe", lambda e, c=c: e.transpose(ptc[:, c, :], ocb[:, c * P:(c + 1) * P], ident_b[:]),
                             r=[ocb, ident_b], w=[ptc])
                    S.op("act", lambda e: e.activation(ocT[:], ptc[:], AF.Copy), r=[ptc], w=[ocT])
                    for pr in range(4):
                        S.op("pool", lambda e, pr=pr: e.tensor_tensor(sqo[:, pr, :], osrc(pr), osrc(pr), op=ALU.mult), r=obufs, w=[sqo])
                    for pr in range(4):
                        S.op("pe", lambda e, pr=pr: e.matmul(pss[:, 0:2], lhsT=sqo[:, pr, :], rhs=ones_f[:, 0:2],
                                                            start=(pr == 0), stop=(pr == 3)), r=[sqo, ones_f], w=[pss])
                    S.op("dve", lambda e: e.tensor_copy(ssc[:, 1:2], pss[:, 0:1]), r=[pss], w=[ssc])
                    rstd_from_ss(ssc, rsc, 1.0 / 512, eps6)
                    for cc in range(2):
                        for pr in range(4):
                            S.op("pe", lambda e, cc=cc, pr=pr: e.matmul(pa[cc][:], lhsT=osrc(pr), rhs=wo[:, pr, cc * 512:(cc + 1) * 512],
                                                                      start=(pr == 0), stop=(pr == 3)), r=obufs + [wo], w=[pa[cc]])
                        for c in range(4):
                            S.op("pe", lambda e, cc=cc, c=c: e.matmul(pc[cc][:], lhsT=ocT[:, c, :], rhs=wo[:, 4 + c, cc * 512:(cc + 1) * 512],
                                                                    start=(c == 0), stop=(c == 3)), r=[ocT, wo], w=[pc[cc]])
                        sl = slice(cc * 512, (cc + 1) * 512)
                        S.op("dve", lambda e, cc=cc, sl=sl: e.tensor_scalar(t1[:, sl], pa[cc][:], rsc[:, 1:2], None, op0=ALU.mult),
                             r=[pa[cc], rsc], w=[t1])
                        S.op("dve", lambda e, cc=cc, sl=sl: e.scalar_tensor_tensor(out=t1[:, sl], in0=pc[cc][:], scalar=rsc[:, 0:1],
                                                                                   in1=t1[:, sl], op0=ALU.mult, op1=ALU.add),
                             r=[pc[cc], rsc, t1], w=[t1])
                    S.op("pool", lambda e: e.tensor_tensor(t1[:], t1[:], modbc[:, 0:1024], op=ALU.mult), r=[t1, modbc], w=[t1])
                    S.op("pool", lambda e: e.tensor_tensor(x1[k][:], t1[:], xt[k][:], op=ALU.add), r=[t1, xt[k]], w=[x1[k]])
                    S.dma("sp", lambda e: e.dma_start(out=xs_d[i * P:(i + 1) * P, :], in_=x1[k][:]), x1_slot[k], r=[x1[k]], w=[xs_buf[i]])
                    if l == 0:
                        dbg_store("d_x1", slice(i * P, (i + 1) * P), x1[k][:], [x1[k]])
                S.barrier()

        def phase_p(l, tiles, modbc, last):
            with ExitStack() as ph:
                wq = sbt(ph, "wq", [P, 8, D], F32)
                keysTz = sbt(ph, "keysTz", [P, 8, 2, P], F32)
                iota16 = sbt(ph, "iota16", [P, 16], F32)
                xt = [sbt(ph, f"xtP{k}", [P, D], F32) for k in range(2)]
                xt_slot = [DmaSlot(S, f"xtP{k}") for k in range(2)]
                junk = sbt(ph, "junkP", [P, D], BF16)
                junkr = sbt(ph, "junkR", [P, D], BF16)
                prod = [sbt(ph, f"prod{k}", [P, D], BF16) for k in range(3)]
                hb = [sbt(ph, f"hb{k}", [P, D], BF16) for k in range(2)]
                pi = [0]
                ss = sbt(ph, "ssP", [P, 1], F32)
                rstd = sbt(ph, "rstdP", [P, 1], F32)
                ssf = sbt(ph, "ssF", [P, 1], F32)
                rstdf = sbt(ph, "rstdF", [P, 1], F32)
                h = [sbt(ph, "hP", [P, D], F32)] * 2
                hT = sbt(ph, "hT", [P, 8, P], F32)
                qpT = hT
                qp = sbt(ph, "qp", [P, D], F32)
                sc = sbt(ph, "scP", [P, 16, 128], F32)
                wk = sbt(ph, "wkP", [P, 16, 128], F32)
                eq_ap = wk[:].rearrange("p a b -> p (a b)").rearrange("p (x c) -> p x c", c=16)
                topv = sbt(ph, "topv", [P, 16, 16], F32)
                idx = sbt(ph, "idxP", [P, 16, 16], U32)
                idxf = sbt(ph, "idxf", [P, 16, 16], F32)
                cand = sbt(ph, "cand", [P, 8, 256], F32)
                wk2_ap = sc[:].rearrange("p a b -> p (a b)").rearrange("p (h x) -> p h x", h=8)
                best = sbt(ph, "best", [P, 8, 16], F32)
                pos = sbt(ph, "pos", [P, 8, 16], U32)
                pab = sbt(ph, "pab", [P, 2, 128], U32)
                abf = sbt(ph, "abf", [P, 2, 128], F32)
                isel = sbt(ph, "isel", [P, 2, 128], F32)
                ef = [sbt(ph, f"ef{k}", [P, 128], F32) for k in range(2)]
                eidx = [sbt(ph, f"eidx{k}", [P, 128], I32) for k in range(2)]
                ew = sbt(ph, "ew", [P, 128], F32)
                se = sbt(ph, "se", [P, 8], F32)
                wgt = [sbt(ph, f"wgt{k}", [P, 128], F32) for k in range(2)]
                pre = sbt(ph, "pre", [P, 128], F32)
                aw = sbt(ph, "aw", [P, 128], F32)
                gbuf = [sbt(ph, f"gbuf{k}", [P, 2 * D], BF16) for k in range(NSLOT)]
                diag = [sbt(ph, f"diag{k}", [P, P], BF16) for k in range(6)]
                pre_b = [Buf(f"pre_b{k}") for k in range(4)]
                pre_d = [Buf(f"pre_d{k}") for k in range(4)]
                di = [0]
                gslot = [DmaSlot(S, f"g{k}") for k in range(NSLOT)]
                x2 = [sbt(ph, "x2_0", [P, D], F32)] * 2
                x2_slot = [DmaSlot(S, f"x2s{k}") for k in range(2)]
                fg_bc = sbt(ph, "fg_bc", [P, D], F32)
                pT2 = pst(ph, "pT2", [P, 8, P], F32)
                pqp = [pst(ph, f"pqp{k}", [P, 512], F32) for k in range(2)]
                pcs = [pst(ph, f"pcs{k}", [P, 512], F32) for k in range(2)]
                pva = [pst(ph, f"pva{k}", [P, 512], F32) for k in range(2)]
                psc = pqp + pcs
                print(f"[build] phase_p sbuf remaining {nc.sbuf_bytes_remaining}")

                S.op("pool", lambda e: e.iota(iota16[:], pattern=[[1, 16]], base=0, channel_multiplier=0,
                                              allow_small_or_imprecise_dtypes=True), w=[iota16])
                ws = DmaSlot(S, "wqload", group=True)
                for j in range(8):
                    S.dma("sp", lambda e, j=j: e.dma_start(out=wq[:, j, :], in_=peer_wq_d[l, j * P:(j + 1) * P, :]), ws, w=[wq])
                kraw_ap = sc[:].rearrange("p a b -> p (a b)")[:, 0:1024].rearrange("p (a d) -> p a d", d=64)
                keysT_ap = cand[:].rearrange("p a b -> p (a b)")[:, 0:1024].rearrange("p (a k) -> p a k", k=P)
                S.dma("sp", lambda e: e.dma_start(out=kraw_ap, in_=peer_keys_d[l].rearrange("h c k d -> k (h c) d")), ws, w=[sc])
                if last:
                    S.dma("sp", lambda e: e.dma_start(out=fg_bc[:], in_=final_g_d[0].partition_broadcast(P)), ws, w=[fg_bc])
                ws.close()
                for hh in range(8):
                    S.op("pe", lambda e, hh=hh: e.transpose(pT2[:, hh, :], kraw_ap[:, 2 * hh:2 * hh + 2, :].rearrange("p c d -> p (c d)"), ident_f[:]),
                         r=[sc, ident_f], w=[pT2])
                S.op("act", lambda e: e.activation(keysT_ap, pT2[:], AF.Copy), r=[pT2], w=[cand])
                S.op("pool", lambda e: e.memset(keysTz[:], 0.0), w=[keysTz])
                S.op("dve", lambda e: e.tensor_copy(keysTz[0:64, :, 0, :], keysT_ap[0:64, :, :]), r=[cand], w=[keysTz])
                S.op("dve", lambda e: e.tensor_copy(keysTz[64:128, :, 1, :], keysT_ap[64:128, :, :]), r=[cand], w=[keysTz])

                gi = [0]

                def routing(n_):
                    rq = []
                    i = tiles[n_]
                    k = n_ % 2
                    xk, hk, efk, eik, wgk = xt[k], h[k], ef[k], eidx[k], wgt[k]
                    hbk = hb[k]

                    def op(eng, fn, r=(), w=()):
                        rq.append(("op", eng, fn, r, w))

                    rq.append(("dma", "sp", lambda e: e.dma_start(out=xk[:], in_=xs_d[i * P:(i + 1) * P, :]), xt_slot[k], [xs_buf[i]], [xk]))
                    op("act", lambda e: e.activation(junkr[:], xk[:], AF.Square, accum_out=ss[:]), r=[xk], w=[junkr, ss])
                    op("act", lambda e: e.activation(rstd[:], ss[:], AF.Sqrt, scale=1.0 / D, bias=eps6[:, 0:1]), r=[ss, eps6], w=[rstd])
                    op("dve", lambda e: e.reciprocal(rstd[:], rstd[:]), r=[rstd], w=[rstd])
                    op("dve", lambda e: e.scalar_tensor_tensor(out=hk[:], in0=xk[:], scalar=rstd[:, 0:1], in1=modbc[:, 2048:3072],
                                                               op0=ALU.mult, op1=ALU.mult), r=[xk, rstd, modbc], w=[hk])
                    op("dve", lambda e: e.tensor_tensor(hk[:], hk[:], modbc[:, 1024:2048], op=ALU.add), r=[hk, modbc], w=[hk])
                    op("act", lambda e: e.activation(hbk[:], hk[:], AF.Copy), r=[hk], w=[hbk])
                    for j in range(8):
                        op("pe", lambda e, j=j: e.transpose(pT2[:, j, :], hk[:, j * P:(j + 1) * P], ident_f[:]), r=[hk, ident_f], w=[pT2])
                    op("act", lambda e: e.activation(hT[:], pT2[:], AF.Copy), r=[pT2], w=[hT])
                    for cc in range(2):
                        for j in range(8):
                            op("pe", lambda e, cc=cc, j=j: e.matmul(pqp[cc][:], lhsT=hT[:, j, :], rhs=wq[:, j, cc * 512:(cc + 1) * 512],
                                                                  start=(j == 0), stop=(j == 7)), r=[hT, wq], w=[pqp[cc]])
                        op("act", lambda e, cc=cc: e.activation(qp[:, cc * 512:(cc + 1) * 512], pqp[cc][:], AF.Copy), r=[pqp[cc]], w=[qp])
                    for hh in range(8):
                        op("pe", lambda e, hh=hh: e.transpose(pT2[:, hh, :], qp[:, hh * P:(hh + 1) * P], ident_f[:]), r=[qp, ident_f], w=[pT2])
                    op("act", lambda e: e.activation(qpT[:], pT2[:], AF.Copy), r=[pT2], w=[qpT])
                    for hh in range(8):
                        pv = psc[hh // 2][:].rearrange("p (a k) -> p a k", k=P)
                        op("pe", lambda e, hh=hh, pv=pv: e.matmul(pv[:, (hh % 2) * 2:(hh % 2) * 2 + 2, :], lhsT=qpT[:, hh, :],
                                                                 rhs=keysTz[:, hh, :, :], start=True, stop=True),
                           r=[qpT, keysTz], w=[psc[hh // 2]])
                    for q in range(4):
                        op("act", lambda e, q=q: e.activation(sc[:, 4 * q:4 * q + 4, :], psc[q][:].rearrange("p (a k) -> p a k", k=P), AF.Copy),
                           r=[psc[q]], w=[sc])
                    for g in range(16):
                        op("dve", lambda e, g=g: e.max(out=topv[:, g, 0:8], in_=sc[:, g, :]), r=[sc], w=[topv])
                        op("dve", lambda e, g=g: e.max_index(out=idx[:, g, 0:8], in_max=topv[:, g, 0:8], in_values=sc[:, g, :]),
                           r=[sc, topv], w=[idx])
                        op("dve", lambda e, g=g: e.match_replace(out=wk[:, g, :], in_to_replace=topv[:, g, 0:8], in_values=sc[:, g, :],
                                                                 imm_value=-1e30), r=[sc, topv], w=[wk])
                        op("dve", lambda e, g=g: e.max(out=topv[:, g, 8:16], in_=wk[:, g, :]), r=[wk], w=[topv])
                        op("dve", lambda e, g=g: e.max_index(out=idx[:, g, 8:16], in_max=topv[:, g, 8:16], in_values=wk[:, g, :]),
                           r=[wk, topv], w=[idx])
                    op("dve", lambda e: e.tensor_copy(idxf[:], idx[:]), r=[idx], w=[idxf])
                    tv4 = topv[:].rearrange("p (h c) a -> p h c a", c=2)
                    if4 = idxf[:].rearrange("p (h c) a -> p h c a", c=2)
                    op("dve", lambda e: e.tensor_tensor(cand[:].rearrange("p h (a b) -> p h a b", a=16),
                                                        tv4[:, :, 0, :].unsqueeze(3).to_broadcast([P, 8, 16, 16]),
                                                        tv4[:, :, 1, :].unsqueeze(2).to_broadcast([P, 8, 16, 16]), op=ALU.add),
                       r=[topv], w=[cand])
                    for hh in range(8):
                        op("dve", lambda e, hh=hh: e.max(out=best[:, hh, 0:8], in_=cand[:, hh, :]), r=[cand], w=[best])
                        op("dve", lambda e, hh=hh: e.max_index(out=pos[:, hh, 0:8], in_max=best[:, hh, 0:8], in_values=cand[:, hh, :]),
                           r=[cand, best], w=[pos])
                        op("dve", lambda e, hh=hh: e.match_replace(out=wk2_ap[:, hh, :], in_to_replace=best[:, hh, 0:8],
                                                                   in_values=cand[:, hh, :], imm_value=-1e30), r=[cand, best], w=[sc])
                        op("dve", lambda e, hh=hh: e.max(out=best[:, hh, 8:16], in_=wk2_ap[:, hh, :]), r=[sc], w=[best])
                        op("dve", lambda e, hh=hh: e.max_index(out=pos[:, hh, 8:16], in_max=best[:, hh, 8:16], in_values=wk2_ap[:, hh, :]),
                           r=[sc, best], w=[pos])
                    posf = pos[:].rearrange("p h s -> p (h s)")
                    op("dve", lambda e: e.tensor_single_scalar(pab[:, 0, :], posf, 4, op=ALU.logical_shift_right), r=[pos], w=[pab])
                    op("dve", lambda e: e.tensor_single_scalar(pab[:, 1, :], posf, 15, op=ALU.bitwise_and), r=[pos], w=[pab])
                    op("dve", lambda e: e.tensor_copy(abf[:], pab[:]), r=[pab], w=[abf])
                    for c in range(2):
                        op("dve", lambda e, c=c: e.tensor_tensor(eq_ap, iota16[:].unsqueeze(1).to_broadcast([P, 128, 16]), abf[:, c, :].unsqueeze(2).to_broadcast([P, 128, 16]),
                                                                 op=ALU.is_equal), r=[iota16, abf], w=[wk])
                        op("dve", lambda e, c=c: e.tensor_tensor(eq_ap.rearrange("p (h s) a -> p h s a", h=8),
                                                                 eq_ap.rearrange("p (h s) a -> p h s a", h=8),
                                                                 if4[:, :, c, :].unsqueeze(2).to_broadcast([P, 8, 16, 16]), op=ALU.mult),
                           r=[wk, idxf], w=[wk])
                        op("dve", lambda e, c=c: e.tensor_reduce(out=isel[:, c, :], in_=eq_ap, axis=AX.X, op=ALU.add), r=[wk], w=[isel])
                    op("dve", lambda e: e.scalar_tensor_tensor(out=efk[:], in0=isel[:, 0, :], scalar=128.0, in1=isel[:, 1, :],
                                                               op0=ALU.mult, op1=ALU.add), r=[isel], w=[efk])
                    op("dve", lambda e: e.tensor_scalar(efk[:], efk[:], float(l * NPEER), None, op0=ALU.add), r=[efk], w=[efk])
                    op("dve", lambda e: e.tensor_copy(eik[:], efk[:]), r=[efk], w=[eik])
                    op("dve", lambda e: e.tensor_tensor(ew[:].rearrange("p (h s) -> p h s", h=8), best[:],
                                                        best[:, :, 0:1].to_broadcast([P, 8, 16]), op=ALU.subtract), r=[best], w=[ew])
                    op("act", lambda e: e.activation(ew[:], ew[:], AF.Exp), r=[ew], w=[ew])
                    op("dve", lambda e: e.tensor_reduce(out=se[:], in_=ew[:].rearrange("p (h s) -> p h s", h=8), axis=AX.X, op=ALU.add),
                       r=[ew], w=[se])
                    op("dve", lambda e: e.reciprocal(se[:], se[:]), r=[se], w=[se])
                    op("dve", lambda e: e.tensor_tensor(wgk[:].rearrange("p (h s) -> p h s", h=8), ew[:].rearrange("p (h s) -> p h s", h=8),
                                                        se[:].unsqueeze(2).to_broadcast([P, 8, 16]), op=ALU.mult), r=[ew, se], w=[wgk])
                    return rq

                rq = routing(0)
                emit(rq, len(rq))
                for n_, i in enumerate(tiles):
                    k = n_ % 2
                    hbk, efk, eik, wgk = hb[k], ef[k], eidx[k], wgt[k]
                    rq = routing(n_ + 1) if n_ + 1 < len(tiles) else []
                    per_slot = -(-len(rq) // 112)
                    NG = 128 // GRP
                    gb = {}
                    for g_ in range(NG + 1):
                        if g_ < NG:
                            pb_ = pre_b[g_ % 4]
                            pd_ = pre_d[g_ % 4]
                            for s_ in range(g_ * GRP, (g_ + 1) * GRP):
                                b_ = gi[0] % NSLOT
                                gi[0] += 1
                                gb[s_] = b_
                                S.dma("pool", lambda e, s_=s_, b_=b_: e.indirect_dma_start(
                                    out=gbuf[b_][:], out_offset=None, in_=uv_d,
                                    in_offset=bass.IndirectOffsetOnAxis(ap=eik[:, s_:s_ + 1], axis=0)), gslot[b_], r=[eik], w=[gbuf[b_]])
                                pr_ = prod[pi[0] % len(prod)]
                                pi[0] += 1
                                S.op("dve", lambda e, b_=b_, pr_=pr_: e.tensor_tensor(pr_[:], hbk[:], gbuf[b_][:, 0:D], op=ALU.mult),
                                     r=[hbk, gbuf[b_]], w=[pr_])
                                S.op("act", lambda e, s_=s_, pr_=pr_: e.activation(junkr[:], pr_[:], AF.Identity, accum_out=pre[:, s_:s_ + 1]),
                                     r=[pr_], w=[junkr, pb_])
                                emit(rq, per_slot)
                            gs = slice(g_ * GRP, (g_ + 1) * GRP)
                            S.op("act", lambda e, gs=gs: e.activation(aw[:, gs], pre[:, gs], AF.Gelu), r=[pb_, pd_], w=[pb_])
                        if g_ >= 1:
                            gq = g_ - 1
                            pbq = pre_b[gq % 4]
                            for s_ in range(gq * GRP, (gq + 1) * GRP):
                                b_ = gb[s_]
                                dg = diag[di[0] % len(diag)]
                                di[0] += 1
                                S.op("dve", lambda e, s_=s_, dg=dg: e.tensor_scalar(dg[:], ident_b[:], aw[:, s_:s_ + 1], wgk[:, s_:s_ + 1],
                                                                                   op0=ALU.mult, op1=ALU.mult),
                                     r=[ident_b, pbq, wgk], w=[dg])
                                for cc in range(2):
                                    S.op("pe", lambda e, s_=s_, b_=b_, dg=dg, cc=cc: e.matmul(
                                        pva[cc][:], lhsT=dg[:], rhs=gbuf[b_][:, D + cc * 512:D + (cc + 1) * 512],
                                        start=(s_ == 0), stop=(s_ == 127)), r=[dg, gbuf[b_]], w=[pva[cc]])
                    if l == 0:
                        dbg_store("d_pre", slice(i * P, (i + 1) * P), pre[:], pre_b + pre_d)
                        dbg_store("d_eidx", slice(i * P, (i + 1) * P), efk[:], [efk])
                        dbg_store("d_w", slice(i * P, (i + 1) * P), wgk[:], [wgk])
                    for cc in range(2):
                        sl = slice(cc * 512, (cc + 1) * 512)
                        S.op("dve", lambda e, cc=cc, sl=sl: e.tensor_tensor(x2[k][:, sl], pva[cc][:], modbc[:, 3072 + cc * 512:3072 + (cc + 1) * 512],
                                                                           op=ALU.mult), r=[pva[cc], modbc], w=[x2[k]])
                    S.op("dve", lambda e: e.tensor_tensor(x2[k][:], x2[k][:], xt[k][:], op=ALU.add), r=[x2[k], xt[k]], w=[x2[k]])
                    if l == 0:
                        dbg_store("d_x2", slice(i * P, (i + 1) * P), x2[k][:], [x2[k]])
                    if last:
                        S.op("act", lambda e: e.activation(junk[:], x2[k][:], AF.Square, accum_out=ssf[:]), r=[x2[k]], w=[junk, ssf])
                        rstd_from_ss(ssf, rstdf, 1.0 / D, eps6)
                        S.op("dve", lambda e: e.scalar_tensor_tensor(out=x2[k][:], in0=x2[k][:], scalar=rstdf[:, 0:1], in1=fg_bc[:],
                                                                     op0=ALU.mult, op1=ALU.mult), r=[x2[k], rstdf, fg_bc], w=[x2[k]])
                        S.dma("sp", lambda e: e.dma_start(out=out_d[i * P:(i + 1) * P, :], in_=x2[k][:]), x2_slot[k], r=[x2[k]])
                    else:
                        S.dma("sp", lambda e: e.dma_start(out=xs_d[i * P:(i + 1) * P, :], in_=x2[k][:]), x2_slot[k], r=[x2[k]], w=[xs_buf[i]])
                    emit(rq, len(rq))
                S.barrier()

        def load_modbc(r_, modbc):
            S.dma("sp", lambda e: e.dma_start(out=modbc[:], in_=modscr_d[r_]), DmaSlot(S, "modld"), w=[modbc])

        lat_tiles = list(range(NTL))
        ctx_tiles = list(range(NTL, NT))
        for l in range(depth):
            last = (l == DEPTH - 1)
            if l == 0:
                src_aps = [x_d[i * P:(i + 1) * P, :] for i in range(NTL)] + [ctx_d[i * P:(i + 1) * P, :] for i in range(NTC)]
            else:
                src_aps = [xs_d[i * P:(i + 1) * P, :] for i in range(NT)]
            mod_cols(l)
            if l == 0:
                dbg_store("d_mod", (slice(0, P), slice(0, 32)), modcol[:].rearrange("p r c -> p (r c)"), [modcol])
                dbg_store("d_mod", (slice(0, P), slice(32, 48)), scT[:].rearrange("p j r -> p (j r)"), [scT])
            if stop == "m":
                break
            with ExitStack() as mix:
                M = {}
                M["qT"] = sbt(mix, "qT", [P, 4, S_LAT], BF16)
                M["qT_b"] = [[Buf(f"qT{pr}_{c}") for c in range(8)] for pr in range(4)]
                M["qTc"] = sbt(mix, "qTc", [P, 4, S_CTX], BF16)
                M["gT"] = sbt(mix, "gT", [P, 4, S_LAT + 2 * GPAD], BF16)
                M["gT_b"] = [Buf(f"gT{i}") for i in range(NTL)]
                M["gTc"] = sbt(mix, "gTc", [P, 4, S_CTX + 2 * GPAD], BF16)
                M["gTc_b"] = [Buf(f"gTc{i}") for i in range(NTC)]
                S.op("pool", lambda e: e.memset(M["gT"][:], 0.0), w=M["gT_b"])
                S.op("pool", lambda e: e.memset(M["gTc"][:], 0.0), w=M["gTc_b"])
                with ExitStack() as ab:
                    M["kT"] = sbt(ab, "kT", [P, NT * P], BF16)
                    M["kT_b"] = [Buf(f"kT{i}") for i in range(NT)]
                    M["Vp"] = sbt(ab, "Vp", [P, NT, 192], BF16)
                    M["Vp_b"] = [Buf(f"Vp{i}") for i in range(NT)]
                    S.op("pool", lambda e: e.memset(M["Vp"][:], 1.0), w=M["Vp_b"])
                    phase_a(l, M, src_aps, last)
                    if stop in ("a", "a0", "a1"):
                        break
                    phase_b(l, M, last)
                if stop == "b":
                    break
                with ExitStack() as cs:
                    modbc = sbt(cs, "modbcC", [P, 4096], F32)
                    for r_ in ((0,) if last else (0, 1)):
                        mod_bc(l, r_, modbc)
                        S.dma("sp", lambda e, r_=r_: e.dma_start(out=modscr_d[r_], in_=modbc[:]), DmaSlot(S, "modst"), r=[modbc])
                        S.barrier()
                    load_modbc(0, modbc)
                    phase_c(l, M, lat_tiles, src_aps, 0, modbc)
                    if not last:
                        load_modbc(1, modbc)
                        phase_c(l, M, ctx_tiles, src_aps, 1, modbc)
            if stop == "c":
                break
            with ExitStack() as pp:
                modbc = sbt(pp, "modbcP", [P, 4096], F32)
                load_modbc(0, modbc)
                phase_p(l, lat_tiles if stop not in ("p1", "p0") else lat_tiles[:2], modbc, last)
                if not last and stop not in ("p1", "p0"):
                    load_modbc(1, modbc)
                    phase_p(l, ctx_tiles, modbc, last)
            if stop in ("p1", "p0"):
                break
        out_slot.close()
        if out_slot.sem is not None:
            for en in ("sp", "act", "pool"):
                S.E[en].eng.wait_ge(out_slot.sem, out_slot.val)
        S.barrier()
        print(f"[build] instr={S.ninstr} waits={S.nwait} sems={S.nsem}")
    return nc


_NC_CACHE = {}


def _rope_tables():
    n = 16
    inv = (10000.0 ** (-np.arange(n, dtype=np.float32) / n)).astype(np.float32)
    row = np.repeat(np.arange(64), 64).astype(np.float32)
    col = np.tile(np.arange(64), 64).astype(np.float32)
    ang = np.concatenate([row[:, None] * inv, col[:, None] * inv], axis=-1).astype(np.float32)
    cos = np.cos(ang).astype(np.float32)
    sin = np.sin(ang).astype(np.float32)
    cos2 = np.concatenate([cos, cos], axis=-1)
    sin2 = np.concatenate([-sin, sin], axis=-1)
    return np.ascontiguousarray(cos2), np.ascontiguousarray(sin2)


def make_in_maps(inputs, cores):
    f = lambda a: np.ascontiguousarray(np.asarray(a, dtype=np.float32))
    cos2, sin2 = _rope_tables()
    shared = {k: f(inputs[k]) for k in ["w_mod", "b_mod", "w_in", "q_gain", "k_gain", "conv_w", "conv_b", "ln_g", "ln_b",
                                        "beta_attn", "beta_conv", "w_o", "peer_wq", "peer_keys", "peer_u", "peer_v"]}
    shared["final_g"] = f(inputs["final_g"]).reshape(1, D)
    shared["cos2"] = cos2
    shared["sin2"] = sin2
    shared["cctxT"] = np.ascontiguousarray(f(inputs["c_ctx"]).reshape(8, P).T)
    x = f(inputs["x"])
    ctx = f(inputs["ctx"])
    c = f(inputs["c"])
    maps = []
    for b in cores:
        m = dict(shared)
        m["x"] = x[b]
        m["ctx"] = ctx[b]
        m["cT"] = np.ascontiguousarray(c[b].reshape(8, P).T)
        maps.append(m)
    return maps


def kernel(**inputs):
    if "nc" not in _NC_CACHE:
        _NC_CACHE["nc"] = build()
    nc = _NC_CACHE["nc"]
    B = np.asarray(inputs["x"]).shape[0]
    maps = make_in_maps(inputs, list(range(B)))
    res = run_bass_kernel_spmd(nc, maps, core_ids=list(range(B)))
    out = np.stack([np.asarray(r["out"], dtype=np.float32) for r in res.results], axis=0)
    return out
```

```python
import numpy as np
from contextlib import ExitStack
import concourse.bass as bass
import concourse.mybir as mybir
from concourse.bass_utils import run_bass_kernel_spmd

F32 = mybir.dt.float32
BF16 = mybir.dt.bfloat16
I32 = mybir.dt.int32
U32 = mybir.dt.uint32
ALU = mybir.AluOpType
AF = mybir.ActivationFunctionType
AX = mybir.AxisListType

import os
CUT = int(os.environ.get("KCUT", "99"))
SEM_LIMIT = 30000


class Buf:
    __slots__ = ("name", "w", "r")

    def __init__(self, name):
        self.name = name
        self.w = None
        self.r = {}


class Eng:
    def __init__(self, sync, name, eng):
        self.sync = sync
        self.name = name
        self.eng = eng
        self.sem = None
        self.count = 0
        self.known = {}
        self.last = None

    def new_event(self):
        if self.sem is None or self.count >= SEM_LIMIT:
            self.sem = self.sync.new_sem(self.name)
            self.count = 0
        self.count += 1
        self.last = [self.sem, self.count, self.name]
        return self.last


def DmaSlot(sync, name, group=False):
    if name not in sync.slot_by_name:
        sync.slot_by_name[name] = _DmaSlot(sync, name, group)
    return sync.slot_by_name[name]


class _DmaSlot:
    def __init__(self, sync, name, group=False):
        self.name = name
        self.sem = None
        self.val = 0
        self.group = group
        self.pending = []
        sync.slots.append(self)

    def bump(self, sync):
        if self.sem is None or (self.val >= SEM_LIMIT and not self.pending):
            self.sem = sync.new_sem("d" + self.name)
            self.val = 0
        self.val += 16
        ev = [self.sem, self.val, "dma"]
        if self.group:
            self.pending.append(ev)
        return ev

    def close(self):
        for ev in self.pending:
            ev[1] = self.val
        self.pending = []


class Sync:
    def __init__(self, nc, stack):
        self.nc = nc
        self.stack = stack
        self.nsem = 0
        self.slots = []
        self.slot_by_name = {}
        self.E = {
            "pe": Eng(self, "pe", nc.tensor),
            "dve": Eng(self, "dve", nc.vector),
            "act": Eng(self, "act", nc.scalar),
            "pool": Eng(self, "pool", nc.gpsimd),
            "sp": Eng(self, "sp", nc.sync),
        }
        self.ninstr = 0
        self.nwait = 0

    def new_sem(self, name):
        self.nsem += 1
        return self.stack.enter_context(self.nc.semaphore(f"s{self.nsem}_{name}"))

    def _wait(self, E, evs):
        best = {}
        for ev in evs:
            if ev is None:
                continue
            if E.name == "pe" and ev[2] == "pe":
                continue
            k = id(ev[0])
            if k not in best or best[k][1] < ev[1]:
                best[k] = ev
        for ev in best.values():
            sem, val, src = ev
            if E.known.get(id(sem), 0) >= val:
                continue
            E.eng.wait_ge(sem, val)
            self.nwait += 1
            E.known[id(sem)] = val

    def _deps(self, E, reads, writes):
        evs = []
        for b in reads:
            evs.append(b.w)
        for b in writes:
            if b.w is not None and b.w[2] != E.name:
                evs.append(b.w)
            for ev in b.r.values():
                if ev[2] == E.name:
                    continue
                evs.append(ev)
        return evs

    @staticmethod
    def _bufs(lst):
        return [t.b if hasattr(t, "b") else t for t in lst]

    def op(self, engname, fn, r=(), w=()):
        E = self.E[engname]
        r = self._bufs(r)
        w = self._bufs(w)
        self._wait(E, self._deps(E, r, w))
        ins = fn(E.eng)
        ev = E.new_event()
        ins.then_inc(ev[0], 1)
        self.ninstr += 1
        key = ev[2] if ev[2] != "dma" else id(ev[0])
        for b in r:
            b.r[key] = ev
        for b in w:
            b.w = ev
            b.r = {}
        return ev

    def dma(self, qname, fn, slot, r=(), w=()):
        E = self.E[qname]
        r = self._bufs(r)
        w = self._bufs(w)
        self._wait(E, self._deps(E, r, w))
        ins = fn(E.eng)
        ev = slot.bump(self)
        ins.then_inc(ev[0], 16)
        self.ninstr += 1
        key = ev[2] if ev[2] != "dma" else id(ev[0])
        for b in r:
            b.r[key] = ev
        for b in w:
            b.w = ev
            b.r = {}
        return ev

    def barrier(self):
        for s in self.slots:
            if s.group:
                s.close()
        evs = []
        for E in self.E.values():
            if E.last is not None:
                evs.append(E.last)
        for s in self.slots:
            if s.sem is not None:
                evs.append([s.sem, s.val, "dma"])
        for E in self.E.values():
            self._wait(E, [ev for ev in evs if not (E.name == "pe" and ev[2] == "pe")])


class Tile:
    def __init__(self, h, name):
        self.h = h
        self.b = Buf(name)

    def __getitem__(self, k):
        return self.h[k]


P = 128
D = 1024
S_LAT = 4096
S_CTX = 256
NTL = 32
NTC = 2
NT = NTL + NTC
DEPTH = 2
INC = 1792
EPS = 1e-6
NPEER = 16384
GPAD = 16
NSLOT = 17
GRP = 4


def build(depth=DEPTH, dbg=False, stop=None):
    nc = bass.Bass("TRN2", target_bir_lowering=False)

    def din(name, shape, dt=F32):
        return nc.dram_tensor(name, shape, dt, kind="ExternalInput").ap()

    x_d = din("x", [S_LAT, D])
    ctx_d = din("ctx", [S_CTX, D])
    cT_d = din("cT", [P, 8])
    cctxT_d = din("cctxT", [P, 8])
    w_mod_d = din("w_mod", [DEPTH, D, 6 * D])
    b_mod_d = din("b_mod", [DEPTH, 6 * D])
    w_in_d = din("w_in", [DEPTH, D, INC])
    q_gain_d = din("q_gain", [DEPTH, 64])
    k_gain_d = din("k_gain", [DEPTH, 64])
    conv_w_d = din("conv_w", [DEPTH, 31, 512])
    conv_b_d = din("conv_b", [DEPTH, 512])
    ln_g_d = din("ln_g", [DEPTH, 512])
    ln_b_d = din("ln_b", [DEPTH, 512])
    beta_attn_d = din("beta_attn", [DEPTH, 512])
    beta_conv_d = din("beta_conv", [DEPTH, 512])
    w_o_d = din("w_o", [DEPTH, D, D])
    peer_wq_d = din("peer_wq", [DEPTH, D, D])
    peer_keys_d = din("peer_keys", [DEPTH, 8, 2, 128, 64])
    peer_u_d = din("peer_u", [DEPTH, NPEER, D])
    peer_v_d = din("peer_v", [DEPTH, NPEER, D])
    final_g_d = din("final_g", [1, D])
    cos2_d = din("cos2", [S_LAT, 64])
    sin2_d = din("sin2", [S_LAT, 64])
    peer_u_flat = peer_u_d.rearrange("l n d -> (l n) d")
    peer_v_flat = peer_v_d.rearrange("l n d -> (l n) d")
    out_d = nc.dram_tensor("out", [S_LAT, D], F32, kind="ExternalOutput").ap()
    xs_d = nc.dram_tensor("xs", [S_LAT + S_CTX, D], F32).ap()
    uv_d = nc.dram_tensor("uv", [DEPTH * NPEER, 2 * D], BF16).ap()
    modscr_d = nc.dram_tensor("modscr", [2, P, 4096], F32).ap()
    dbg_d = {}
    if dbg:
        for nm, shp in [("d_qkvu", [NT * P, INC]), ("d_x1", [NT * P, D]), ("d_oT", [P, 4 * S_LAT]),
                        ("d_pre", [NT * P, 128]), ("d_eidx", [NT * P, 128]), ("d_w", [NT * P, 128]),
                        ("d_qT", [P, 4 * S_LAT]), ("d_x2", [NT * P, D]), ("d_mod", [P, 4096])]:
            dbg_d[nm] = nc.dram_tensor(nm, shp, F32, kind="ExternalOutput").ap()

    top = ExitStack()
    with top:
        S = Sync(nc, top)

        uid = [0]

        def sbt(stack, name, shape, dt):
            uid[0] += 1
            name = f"{name}_u{uid[0]}"
            return Tile(stack.enter_context(nc.sbuf_tensor(name, shape, dt)), name)

        def pst(stack, name, shape, dt):
            uid[0] += 1
            name = f"{name}_u{uid[0]}"
            return Tile(stack.enter_context(nc.psum_tensor(name, shape, dt)), name)

        out_slot = DmaSlot(S, "out", group=True)
        dbg_slots = {}
        xs_buf = [Buf(f"xs{i}") for i in range(NT)]

        ident_f = sbt(top, "ident_f", [P, P], F32)
        ident_b = sbt(top, "ident_b", [P, P], BF16)
        ones_f = sbt(top, "ones_f", [P, P], F32)
        io_t = sbt(top, "io_t", [P, P], F32)
        pid_t = sbt(top, "pid_t", [P, 1], F32)
        S.op("pool", lambda e: e.iota(io_t[:], pattern=[[1, P]], base=0, channel_multiplier=0,
                                      allow_small_or_imprecise_dtypes=True), w=[io_t])
        S.op("pool", lambda e: e.iota(pid_t[:], pattern=[[0, 1]], base=0, channel_multiplier=1,
                                      allow_small_or_imprecise_dtypes=True), w=[pid_t])
        S.op("dve", lambda e: e.tensor_scalar(ident_f[:], io_t[:], pid_t[:, 0:1], None, op0=ALU.is_equal),
             r=[io_t, pid_t], w=[ident_f])
        S.op("dve", lambda e: e.tensor_copy(ident_b[:], ident_f[:]), r=[ident_f], w=[ident_b])
        S.op("pool", lambda e: e.memset(ones_f[:], 1.0), w=[ones_f])

        cslot = DmaSlot(S, "const", group=True)
        craw = sbt(top, "craw", [P, 2, 8], F32)
        scT = sbt(top, "scT", [P, 8, 2], F32)
        S.dma("sp", lambda e: e.dma_start(out=craw[:, 0, :], in_=cT_d), cslot, w=[craw])
        S.dma("sp", lambda e: e.dma_start(out=craw[:, 1, :], in_=cctxT_d), cslot, w=[craw])
        cslot.close()
        S.op("act", lambda e: e.activation(scT[:].rearrange("p j r -> p r j"), craw[:], AF.Silu), r=[craw], w=[scT])
        modcol = sbt(top, "modcol", [P, 2, 16], F32)
        eps6 = sbt(top, "eps6", [P, 1], F32)
        eps5 = sbt(top, "eps5", [P, 1], F32)
        S.op("pool", lambda e: e.memset(eps6[:], 1e-6), w=[eps6])
        S.op("pool", lambda e: e.memset(eps5[:], 1e-5), w=[eps5])


        def rstd_from_ss(ss, rs, scale, eps_t):
            S.op("act", lambda e: e.activation(rs[:], ss[:], AF.Sqrt, scale=scale, bias=eps_t[:, 0:1]), r=[ss, eps_t], w=[rs])
            S.op("dve", lambda e: e.reciprocal(rs[:], rs[:]), r=[rs], w=[rs])

        def dbg_store(name, rows, tile_ap, rtiles):
            if dbg and name in dbg_d:
                if name not in dbg_slots:
                    dbg_slots[name] = (DmaSlot(S, name), Buf(name))
                S.dma("sp", lambda e: e.dma_start(out=dbg_d[name][rows], in_=tile_ap), dbg_slots[name][0], r=rtiles, w=[dbg_slots[name][1]])


        def convert_thunks(ph, NB=3):
            stg = [sbt(ph, f"cvi{k}", [P, 4, D], F32) for k in range(NB)]
            obf = [sbt(ph, f"cvo{k}", [P, 4, D], BF16) for k in range(NB)]
            islot = [DmaSlot(S, f"cvi{k}") for k in range(NB)]
            oslot = [DmaSlot(S, f"cvo{k}") for k in range(NB)]
            steps = []
            n = 0
            for l in range(DEPTH):
                for tab, col0 in ((peer_u_d, 0), (peer_v_d, D)):
                    for t in range(NPEER // 512):
                        k = n % NB
                        src = tab[l, t * 512:(t + 1) * 512, :].rearrange("(p r) d -> p r d", r=4)
                        dst = uv_d[l * NPEER + t * 512:l * NPEER + (t + 1) * 512, col0:col0 + D].rearrange("(p r) d -> p r d", r=4)
                        steps.append((
                            ("dma", "sp", lambda e, k=k, src=src: e.dma_start(out=stg[k][:], in_=src), islot[k], [], [stg[k]]),
                            ("op", "dve", lambda e, k=k: e.tensor_copy(obf[k][:], stg[k][:]), [stg[k]], [obf[k]]),
                            ("dma", "pool", lambda e, k=k, dst=dst: e.dma_start(out=dst, in_=obf[k][:]), oslot[k], [obf[k]], []),
                        ))
                        n += 1
            cq = []
            for n in range(len(steps) + 2):
                if n < len(steps):
                    cq.append(steps[n][0])
                if n >= 2:
                    cq.append(steps[n - 2][1])
                    cq.append(steps[n - 2][2])
            return cq

        def emit(rq, n):
            for _ in range(n):
                if not rq:
                    return
                t = rq.pop(0)
                if t[0] == "op":
                    S.op(t[1], t[2], t[3], t[4])
                else:
                    S.dma(t[1], t[2], t[3], t[4], t[5])

        def mod_cols(l):
            with ExitStack() as ph:
                wm = [sbt(ph, f"wm{k}", [P, 8, P], F32) for k in range(2)]
                wslot = [DmaSlot(S, f"wm{k}") for k in range(2)]
                bcol = sbt(ph, "bcol", [P, 16], F32)
                pcol = pst(ph, "pcol", [P, 16, 2], F32)
                S.dma("sp", lambda e: e.dma_start(out=bcol[:], in_=b_mod_d[l, 0:2048].rearrange("(c p) -> p c", p=P),
                                                  allow_slow_non_contiguous=True), DmaSlot(S, "bcol"), w=[bcol])
                for cc in range(16):
                    t = wm[cc % 2]
                    S.dma("sp", lambda e, t=t, cc=cc: e.dma_start(
                        out=t[:], in_=w_mod_d[l, :, cc * P:(cc + 1) * P].rearrange("(j p) m -> p j m", p=P)),
                          wslot[cc % 2], w=[t])
                    for j in range(8):
                        S.op("pe", lambda e, t=t, j=j, cc=cc: e.matmul(pcol[:, cc, :], lhsT=t[:, j, :],
                                                                     rhs=scT[:, j, :], start=(j == 0), stop=(j == 7)),
                             r=[t, scT], w=[pcol])
                S.op("dve", lambda e: e.tensor_tensor(modcol[:].rearrange("p r c -> p c r"), pcol[:],
                                                      bcol[:].unsqueeze(2).to_broadcast([P, 16, 2]), op=ALU.add),
                     r=[pcol, bcol], w=[modcol])
                S.op("dve", lambda e: e.tensor_scalar(modcol[:, :, 8:16], modcol[:, :, 8:16], 1.0, None, op0=ALU.add),
                     r=[modcol], w=[modcol])
                S.barrier()

        def mod_bc(l, r, modbc):
            with ExitStack() as ph:
                NW = 4
                wm = [sbt(ph, f"wmb{k}", [P, 512], F32) for k in range(NW)]
                wslot = [DmaSlot(S, f"wmb{k}") for k in range(NW)]
                bbc = [sbt(ph, f"bbc{k}", [P, 512], F32) for k in range(2)]
                bslot = [DmaSlot(S, f"bbc{k}") for k in range(2)]
                screp = sbt(ph, "screp", [P, 8, P], F32)
                pb = pst(ph, "pbm", [P, 512], F32)
                S.op("dve", lambda e: e.tensor_copy(screp[:], scT[:, :, r].unsqueeze(2).to_broadcast([P, 8, P])),
                     r=[scT], w=[screp])
                n = 0
                for cc in range(8):
                    c0 = 2048 + cc * 512
                    S.dma("sp", lambda e, cc=cc, c0=c0: e.dma_start(out=bbc[cc % 2][:], in_=b_mod_d[l, c0:c0 + 512].partition_broadcast(P)),
                          bslot[cc % 2], w=[bbc[cc % 2]])
                    for j in range(8):
                        t = wm[n % NW]
                        S.dma("sp", lambda e, t=t, j=j, c0=c0: e.dma_start(out=t[:], in_=w_mod_d[l, j * P:(j + 1) * P, c0:c0 + 512]),
                              wslot[n % NW], w=[t])
                        n += 1
                        S.op("pe", lambda e, t=t, j=j: e.matmul(pb[:], lhsT=screp[:, j, :], rhs=t[:], start=(j == 0), stop=(j == 7)),
                             r=[t, screp], w=[pb])
                    S.op("dve", lambda e, cc=cc: e.tensor_tensor(modbc[:, cc * 512:(cc + 1) * 512], pb[:], bbc[cc % 2][:], op=ALU.add),
                         r=[pb, bbc[cc % 2]], w=[modbc])
                S.op("dve", lambda e: e.tensor_scalar(modbc[:, 2048:3072], modbc[:, 2048:3072], 1.0, None, op0=ALU.add),
                     r=[modbc], w=[modbc])
                S.barrier()

        def phase_a(l, M, src_aps, last):
            with ExitStack() as ph:
                win = sbt(ph, "win", [P, 8, INC], BF16)
                stg = sbt(ph, "stg", [P, INC], F32)
                stg_slot = DmaSlot(S, "stg")
                bias_bc = sbt(ph, "bias_bc", [P, INC], F32)
                sh1rep = sbt(ph, "sh1rep", [P, 2, 8, P], BF16)
                gain = sbt(ph, "gain", [P, 640], F32)
                graw = sbt(ph, "graw", [P, 128], F32)
                cos2 = sbt(ph, "cos2", [P, NTL, 64], F32)
                sin2 = sbt(ph, "sin2", [P, NTL, 64], F32)
                xt = [sbt(ph, f"xt{k}", [P, D], F32) for k in range(2)]
                xt_slot = [DmaSlot(S, f"xt{k}") for k in range(2)]
                xb = [sbt(ph, f"xb{k}", [P, D], BF16) for k in range(2)]
                xT = [sbt(ph, f"xT{k}", [P, 8, P], BF16) for k in range(2)]
                junk = sbt(ph, "junkA", [P, D], BF16)
                ss = [sbt(ph, f"ssA{k}", [P, 1], F32) for k in range(2)]
                rstd = [sbt(ph, f"rstdA{k}", [P, 1], F32) for k in range(2)]
                qkvu = sbt(ph, "qkvu", [P, INC], F32)
                sq = sbt(ph, "sq", [P, 640], F32)
                ssq = sbt(ph, "ssq", [P, 10], F32)
                rsq = sbt(ph, "rsq", [P, 10], F32)
                qn = sbt(ph, "qn", [P, 640], F32)
                rb = sbt(ph, "rb", [P, 640], F32)
                qr = sbt(ph, "qr", [P, 4, 2, 64], BF16)
                kr = sbt(ph, "kr", [P, 128], BF16)
                sig = sbt(ph, "sig", [P, 512], F32)
                gg = sbt(ph, "gg", [P, 512], BF16)
                ptb = pst(ph, "ptb", [P, 8, P], BF16)
                pq = [pst(ph, f"pq{k}", [P, 512], F32) for k in range(4)]
                ptq = pst(ph, "ptq", [P, 5, P], BF16)
                ptg = pst(ph, "ptg", [P, 4, P], BF16)
                ra = sq
                qT, qTc, kT, Vp, gT, gTc = M["qT"], M["qTc"], M["kT"], M["Vp"], M["gT"], M["gTc"]

                cslot2 = DmaSlot(S, "rope", group=True)
                S.dma("sp", lambda e: e.dma_start(out=cos2[:], in_=cos2_d.rearrange("(n p) f -> p n f", p=P)), cslot2, w=[cos2])
                S.dma("sp", lambda e: e.dma_start(out=sin2[:], in_=sin2_d.rearrange("(n p) f -> p n f", p=P)), cslot2, w=[sin2])
                S.dma("sp", lambda e: e.dma_start(out=graw[:, 0:64], in_=q_gain_d[l].partition_broadcast(P)), cslot2, w=[graw])
                S.dma("sp", lambda e: e.dma_start(out=graw[:, 64:128], in_=k_gain_d[l].partition_broadcast(P)), cslot2, w=[graw])
                cslot2.close()
                for j in range(8):
                    S.dma("sp", lambda e, j=j: e.dma_start(out=stg[:], in_=w_in_d[l, j * P:(j + 1) * P, :]), stg_slot, w=[stg])
                    S.op("act", lambda e, j=j: e.activation(win[:, j, :], stg[:], AF.Copy), r=[stg], w=[win])
                S.op("dve", lambda e: e.tensor_copy(gain[:, 0:512].rearrange("p (h d) -> p h d", h=8),
                                                    graw[:, 0:64].unsqueeze(1).to_broadcast([P, 8, 64])), r=[graw], w=[gain])
                S.op("dve", lambda e: e.tensor_copy(gain[:, 512:640].rearrange("p (h d) -> p h d", h=2),
                                                    graw[:, 64:128].unsqueeze(1).to_broadcast([P, 2, 64])), r=[graw], w=[gain])
                S.op("dve", lambda e: e.tensor_copy(sh1rep[:], modcol[:, :, 0:8].unsqueeze(3).to_broadcast([P, 2, 8, P])),
                     r=[modcol], w=[sh1rep])

                def make_bias(r_):
                    for cc in range(4):
                        w_ = min(512, INC - cc * 512)
                        for j in range(8):
                            S.op("pe", lambda e, cc=cc, j=j, w_=w_: e.matmul(
                                pq[cc][:, 0:w_], lhsT=sh1rep[:, r_, j, :], rhs=win[:, j, cc * 512:cc * 512 + w_],
                                start=(j == 0), stop=(j == 7)), r=[sh1rep, win], w=[pq[cc]])
                        S.op("act", lambda e, cc=cc, w_=w_: e.activation(
                            bias_bc[:, cc * 512:cc * 512 + w_], pq[cc][:, 0:w_], AF.Copy), r=[pq[cc]], w=[bias_bc])

                def load(i):
                    k = i % 2
                    S.dma("sp", lambda e: e.dma_start(out=xt[k][:], in_=src_aps[i]), xt_slot[k], r=[xs_buf[i]], w=[xt[k]])

                make_bias(0)
                load(0)
                qk2 = [qkvu, stg]

                def stage1(i):
                    k = i % 2
                    r_ = 0 if i < NTL else 1
                    qk_ = qk2[i % 2]
                    S.op("act", lambda e: e.activation(junk[:], xt[k][:], AF.Square, accum_out=ss[k][:]), r=[xt[k]], w=[junk, ss[k]])
                    S.op("act", lambda e: e.activation(xb[k][:], xt[k][:], AF.Copy), r=[xt[k]], w=[xb[k]])
                    rstd_from_ss(ss[k], rstd[k], 1.0 / D, eps6)
                    for j in range(8):
                        S.op("pe", lambda e, j=j: e.transpose(ptb[:, j, :], xb[k][:, j * P:(j + 1) * P], ident_b[:]),
                             r=[xb[k], ident_b], w=[ptb])
                    S.op("dve", lambda e: e.tensor_tensor(xT[k][:], ptb[:],
                                                          modcol[:, r_, 8:16].unsqueeze(2).to_broadcast([P, 8, P]), op=ALU.mult),
                         r=[ptb, modcol], w=[xT[k]])
                    if CUT <= 1:
                        return
                    for cc in range(4):
                        w_ = min(512, INC - cc * 512)
                        for j in range(8):
                            S.op("pe", lambda e, cc=cc, j=j, w_=w_: e.matmul(
                                pq[cc][:, 0:w_], lhsT=xT[k][:, j, :], rhs=win[:, j, cc * 512:cc * 512 + w_],
                                start=(j == 0), stop=(j == 7)), r=[xT[k], win], w=[pq[cc]])
                        S.op("dve", lambda e, cc=cc, w_=w_: e.scalar_tensor_tensor(
                            out=qk_[:, cc * 512:cc * 512 + w_], in0=pq[cc][:, 0:w_], scalar=rstd[k][:, 0:1],
                            in1=bias_bc[:, cc * 512:cc * 512 + w_], op0=ALU.mult, op1=ALU.add),
                             r=[pq[cc], rstd[k], bias_bc], w=[qk_])

                def stage2(i):
                    k = i % 2
                    is_ctx = i >= NTL
                    qk_ = qk2[i % 2]
                    if l == 0:
                        dbg_store("d_qkvu", slice(i * P, (i + 1) * P), qk_[:], [qk_])
                    if CUT <= 2:
                        return
                    S.op("pool", lambda e: e.tensor_tensor(sq[:], qk_[:, 0:640], qk_[:, 0:640], op=ALU.mult), r=[qk_], w=[sq])
                    S.op("dve", lambda e: e.tensor_reduce(out=ssq[:], in_=sq[:].rearrange("p (h d) -> p h d", h=10),
                                                          axis=AX.X, op=ALU.add), r=[sq], w=[ssq])
                    rstd_from_ss(ssq, rsq, 1.0 / 64, eps6)
                    S.op("dve", lambda e: e.tensor_tensor(qn[:].rearrange("p (h d) -> p h d", h=10),
                                                          qk_[:, 0:640].rearrange("p (h d) -> p h d", h=10),
                                                          rsq[:].unsqueeze(2).to_broadcast([P, 10, 64]), op=ALU.mult),
                         r=[qk_, rsq], w=[qn])
                    S.op("pool", lambda e: e.tensor_tensor(qn[:], qn[:], gain[:], op=ALU.mult), r=[qn, gain], w=[qn])
                    if CUT <= 3:
                        return
                    if not is_ctx:
                        qn3 = qn[:].rearrange("p (h d) -> p h d", h=10)
                        rb3 = rb[:].rearrange("p (h d) -> p h d", h=10)
                        S.op("dve", lambda e: e.tensor_tensor(ra[:].rearrange("p (h d) -> p h d", h=10), qn3,
                                                              cos2[:, i, :].unsqueeze(1).to_broadcast([P, 10, 64]), op=ALU.mult),
                             r=[qn, cos2], w=[ra])
                        S.op("pool", lambda e: e.tensor_tensor(rb3[:, :, 0:32], qn3[:, :, 32:64],
                                                               sin2[:, i, 0:32].unsqueeze(1).to_broadcast([P, 10, 32]), op=ALU.mult),
                             r=[qn, sin2], w=[rb])
                        S.op("pool", lambda e: e.tensor_tensor(rb3[:, :, 32:64], qn3[:, :, 0:32],
                                                               sin2[:, i, 32:64].unsqueeze(1).to_broadcast([P, 10, 32]), op=ALU.mult),
                             r=[qn, sin2], w=[rb])
                        S.op("dve", lambda e: e.tensor_tensor(qr[:].rearrange("p pr hh d -> p hh pr d"),
                                                              ra[:, 0:512].rearrange("p (hh pr d) -> p hh pr d", hh=2, pr=4),
                                                              rb[:, 0:512].rearrange("p (hh pr d) -> p hh pr d", hh=2, pr=4), op=ALU.add),
                             r=[ra, rb], w=[qr])
                        S.op("pool", lambda e: e.tensor_tensor(kr[:], ra[:, 512:640], rb[:, 512:640], op=ALU.add), r=[ra, rb], w=[kr])
                    else:
                        S.op("dve", lambda e: e.tensor_copy(qr[:].rearrange("p pr hh d -> p hh pr d"),
                                                            qn[:, 0:512].rearrange("p (hh pr d) -> p hh pr d", hh=2, pr=4)),
                             r=[qn], w=[qr])
                        S.op("pool", lambda e: e.tensor_copy(kr[:], qn[:, 512:640]), r=[qn], w=[kr])
                    if CUT <= 4:
                        return
                    need_q = (not is_ctx) or (not last)
                    if need_q:
                        for pr in range(4):
                            S.op("pe", lambda e, pr=pr: e.transpose(ptq[:, pr, :], qr[:, pr, :, :].rearrange("p hh d -> p (hh d)"), ident_b[:]),
                                 r=[qr, ident_b], w=[ptq])
                    S.op("pe", lambda e: e.transpose(ptq[:, 4, :], kr[:], ident_b[:]), r=[kr, ident_b], w=[ptq])
                    if need_q:
                        if not is_ctx:
                            S.op("act", lambda e: e.activation(qT[:, :, i * P:(i + 1) * P], ptq[:, 0:4, :], AF.Copy),
                                 r=[ptq], w=[M["qT_b"][pr][i // 4] for pr in range(4)])
                        else:
                            S.op("act", lambda e: e.activation(qTc[:, :, (i - NTL) * P:(i - NTL + 1) * P], ptq[:, 0:4, :], AF.Copy),
                                 r=[ptq], w=[qTc])
                    S.op("act", lambda e: e.activation(kT[:, i * P:(i + 1) * P], ptq[:, 4, :], AF.Copy), r=[ptq], w=[M["kT_b"][i]])
                    if CUT <= 5:
                        return
                    S.op("pool", lambda e: e.tensor_copy(Vp[:, i, 0:64], qk_[:, 640:704]), r=[qk_], w=[M["Vp_b"][i]])
                    S.op("pool", lambda e: e.tensor_copy(Vp[:, i, 128:192], qk_[:, 704:768]), r=[qk_], w=[M["Vp_b"][i]])
                    if CUT <= 6:
                        return
                    if need_q:
                        S.op("act", lambda e: e.activation(sig[:], qk_[:, 1280:1792], AF.Sigmoid), r=[qk_], w=[sig])
                        S.op("dve", lambda e: e.tensor_tensor(gg[:], qk_[:, 768:1280], sig[:], op=ALU.mult), r=[qk_, sig], w=[gg])
                        for c in range(4):
                            S.op("pe", lambda e, c=c: e.transpose(ptg[:, c, :], gg[:, c * P:(c + 1) * P], ident_b[:]),
                                 r=[gg, ident_b], w=[ptg])
                        if not is_ctx:
                            S.op("act", lambda e: e.activation(gT[:, :, GPAD + i * P:GPAD + (i + 1) * P], ptg[:], AF.Copy),
                                 r=[ptg], w=[M["gT_b"][i]])
                        else:
                            S.op("act", lambda e: e.activation(gTc[:, :, GPAD + (i - NTL) * P:GPAD + (i - NTL + 1) * P], ptg[:], AF.Copy),
                                 r=[ptg], w=[M["gTc_b"][i - NTL]])


                ntile = NT if stop not in ("a0", "a1") else (0 if stop == "a0" else 1)
                for i in range(ntile):
                    k = i % 2
                    r_ = 0 if i < NTL else 1
                    is_ctx = i >= NTL
                    if i == NTL:
                        make_bias(1)
                    if i + 1 < NT:
                        load(i + 1)
                    stage1(i)
                    if i >= 1:
                        stage2(i - 1)
                if ntile:
                    stage2(ntile - 1)

                S.barrier()

        def phase_b(l, M, last):
            with ExitStack() as ph:
                NPT = 4
                NPS = 4
                LA = 1
                pT = [sbt(ph, f"pT{k}", [P, 512], BF16) for k in range(NPT)]
                rd = sbt(ph, "rd", [P, 512], F32)
                bcs = sbt(ph, "bcs", [P, 512], F32)
                ps_s = [pst(ph, f"ps_s{k}", [P, 512], F32) for k in range(NPS)]
                po = [pst(ph, f"po{k}", [P, 512], F32) for k in range(3)]
                pbc = pst(ph, "pbc", [P, 512], F32)
                qT, qTc, kT, Vp = M["qT"], M["qTc"], M["kT"], M["Vp"]
                cnt = [0]
                grp = [0]
                kTz = sbt(ph, "kTz", [P, 2, NT * P], BF16)
                S.op("pool", lambda e: e.memset(kTz[:], 0.0), w=[kTz])
                S.op("dve", lambda e: e.tensor_copy(kTz[0:64, 0, :], kT[0:64, :]), r=M["kT_b"], w=[kTz])
                S.op("act", lambda e: e.activation(kTz[64:128, 1, :], kT[64:128, :], AF.Copy), r=M["kT_b"], w=[kTz])
                cq = convert_thunks(ph) if (l == 0 and stop in (None, "p1")) else []
                cqn = [0]

                def attend(qsrc_fn, qfull, qbuf, n, ktiles):
                    its = [(kt_i, kt, hh) for kt_i, kt in enumerate(ktiles) for hh in range(2)]
                    base = cnt[0]
                    cnt[0] += len(its)
                    pg = [po[(2 * grp[0]) % 3], po[(2 * grp[0] + 1) % 3]]
                    grp[0] += 1

                    def emit_s(m):
                        kt_i, kt, hh = its[m]
                        k = (base + m) % NPS
                        kp = (base + m) % NPT
                        S.op("pe", lambda e: e.matmul(ps_s[k][:, 0:n], lhsT=kTz[:, hh, kt * P:(kt + 1) * P], rhs=qfull,
                                                      start=True, stop=True), r=[kTz, qbuf], w=[ps_s[k]])
                        S.op("act", lambda e: e.activation(pT[kp][:, 0:n], ps_s[k][:, 0:n], AF.Exp, scale=0.125),
                             r=[ps_s[k]], w=[pT[kp]])

                    def emit_pv(m):
                        kt_i, kt, hh = its[m]
                        kp = (base + m) % NPT
                        S.op("pe", lambda e: e.matmul(pg[hh][:, 0:n], lhsT=Vp[:, kt, hh * 64:hh * 64 + 128], rhs=pT[kp][:, 0:n],
                                                      start=(kt_i == 0), stop=(kt_i == len(ktiles) - 1)), r=[M["Vp_b"][kt], pT[kp]], w=[pg[hh]])

                    npair = len(its) // 2
                    for j in range(npair + LA):
                        if j < npair:
                            emit_s(2 * j)
                            emit_s(2 * j + 1)
                        if j - LA >= 0:
                            emit_pv(2 * (j - LA))
                            emit_pv(2 * (j - LA) + 1)
                        cqn[0] += 1
                        if cqn[0] % 4 != 3:
                            emit(cq, 1)
                    for hh in range(2):
                        dp = 64 if hh == 0 else 0
                        S.op("dve", lambda e, hh=hh, dp=dp: e.reciprocal(rd[dp:dp + 1, 0:n], pg[hh][dp:dp + 1, 0:n]),
                             r=[pg[hh]], w=[rd])
                        S.op("pe", lambda e, dp=dp: e.matmul(pbc[0:64, 0:n], lhsT=ones_f[dp:dp + 1, 0:64], rhs=rd[dp:dp + 1, 0:n],
                                                            start=True, stop=True), r=[ones_f, rd], w=[pbc])
                        S.op("act", lambda e, hh=hh: e.activation(bcs[hh * 64:(hh + 1) * 64, 0:n], pbc[0:64, 0:n], AF.Copy),
                             r=[pbc], w=[bcs])
                        S.op("dve", lambda e, hh=hh: e.tensor_tensor(qsrc_fn(hh), pg[hh][hh * 64:(hh + 1) * 64, 0:n],
                                                                     bcs[hh * 64:(hh + 1) * 64, 0:n], op=ALU.mult),
                             r=[pg[hh], bcs], w=[qbuf])

                for c in range(8):
                    for pr in range(4):
                        attend(lambda hh, c=c, pr=pr: qT[hh * 64:(hh + 1) * 64, pr, c * 512:(c + 1) * 512],
                               qT[:, pr, c * 512:(c + 1) * 512], M["qT_b"][pr][c], 512, list(range(NT)))
                if not last:
                    for pr in range(4):
                        attend(lambda hh, pr=pr: qTc[hh * 64:(hh + 1) * 64, pr, :], qTc[:, pr, :], qTc.b, S_CTX, [NTL, NTL + 1])
                emit(cq, len(cq))
                S.barrier()

        def phase_c(l, M, tiles, src_aps, r_, modbc):
            with ExitStack() as ph:
                wcol = sbt(ph, "wcol", [P, 4, 31], F32)
                DG = sbt(ph, "DG", [P, 4, 31, P], BF16)
                wo = sbt(ph, "wo", [P, 8, D], BF16)
                stg = [sbt(ph, f"stgC{k}", [P, D], F32) for k in range(2)]
                stg_slot = [DmaSlot(S, f"stgC{k}") for k in range(2)]
                betaA = sbt(ph, "betaA", [P, 8], F32)
                betac = sbt(ph, "betac", [P, 8], F32)
                cb_bc = sbt(ph, "cb_bc", [P, 512], F32)
                lg_bc = sbt(ph, "lg_bc", [P, 512], F32)
                lb_bc = sbt(ph, "lb_bc", [P, 512], F32)
                xt = [sbt(ph, f"xtC{k}", [P, D], F32) for k in range(2)]
                xt_slot = [DmaSlot(S, f"xtC{k}") for k in range(2)]
                y = sbt(ph, "yC", [P, 512], F32)
                junk = sbt(ph, "junkC", [P, 512], F32)
                st4 = sbt(ph, "st4", [P, 4], F32)
                rln = sbt(ph, "rln", [P, 1], F32)
                msq = sbt(ph, "msq", [P, 1], F32)
                z = sbt(ph, "zC", [P, 512], F32)
                oc = sbt(ph, "oc", [P, 512], F32)
                ocb = sbt(ph, "ocb", [P, 512], BF16)
                ocT = sbt(ph, "ocT", [P, 4, P], BF16)
                sqo = sbt(ph, "sqo", [P, 4, P], F32)
                ssc = sbt(ph, "ssc", [P, 2], F32)
                rsc = sbt(ph, "rsc", [P, 2], F32)
                t1 = sbt(ph, "t1", [P, D], F32)
                x1 = [sbt(ph, f"x1_{k}", [P, D], F32) for k in range(2)]
                x1_slot = [DmaSlot(S, f"x1s{k}") for k in range(2)]
                py2 = [pst(ph, f"py{k}", [P, 512], F32) for k in range(2)]
                ptc = pst(ph, "ptc", [P, 4, P], BF16)
                pss = pst(ph, "pss", [P, 16], F32)
                pa = [pst(ph, f"pa{k}", [P, 512], F32) for k in range(2)]
                pc = [pst(ph, f"pc{k}", [P, 512], F32) for k in range(2)]
                qT, qTc, gT, gTc = M["qT"], M["qTc"], M["gT"], M["gTc"]

                vs = DmaSlot(S, "vecC", group=True)
                for c in range(4):
                    S.dma("sp", lambda e, c=c: e.dma_start(out=wcol[:, c, :], in_=conv_w_d[l, :, c * P:(c + 1) * P].rearrange("j p -> p j"),
                                                           allow_slow_non_contiguous=True), vs, w=[wcol])
                for half in range(2):
                    S.dma("sp", lambda e, half=half: e.dma_start(out=betaA[half * 64:(half + 1) * 64, :],
                                                                 in_=beta_attn_d[l].rearrange("(h d) -> d h", d=64),
                                                                 allow_slow_non_contiguous=True), vs, w=[betaA])
                S.dma("sp", lambda e: e.dma_start(out=betac[:, 4:8], in_=beta_conv_d[l].rearrange("(c p) -> p c", p=P),
                                                  allow_slow_non_contiguous=True), vs, w=[betac])
                S.dma("sp", lambda e: e.dma_start(out=cb_bc[:], in_=conv_b_d[l].partition_broadcast(P)), vs, w=[cb_bc])
                S.dma("sp", lambda e: e.dma_start(out=lg_bc[:], in_=ln_g_d[l].partition_broadcast(P)), vs, w=[lg_bc])
                S.dma("sp", lambda e: e.dma_start(out=lb_bc[:], in_=ln_b_d[l].partition_broadcast(P)), vs, w=[lb_bc])
                vs.close()
                S.op("dve", lambda e: e.tensor_copy(betac[0:64, 0:4], betaA[0:64, 0:4]), r=[betaA], w=[betac])
                S.op("dve", lambda e: e.tensor_copy(betac[64:128, 0:4], betaA[64:128, 4:8]), r=[betaA], w=[betac])
                for kk in range(8):
                    t = stg[kk % 2]
                    sl = stg_slot[kk % 2]
                    if kk < 4:
                        S.dma("sp", lambda e, t=t, kk=kk: e.dma_start(out=t[0:64, :], in_=w_o_d[l, kk * 64:(kk + 1) * 64, :]), sl, w=[t])
                        S.dma("sp", lambda e, t=t, kk=kk: e.dma_start(out=t[64:128, :], in_=w_o_d[l, (kk + 4) * 64:(kk + 5) * 64, :]), sl, w=[t])
                    else:
                        S.dma("sp", lambda e, t=t, kk=kk: e.dma_start(out=t[:], in_=w_o_d[l, 512 + (kk - 4) * P:512 + (kk - 3) * P, :]), sl, w=[t])
                    S.op("act", lambda e, t=t, kk=kk: e.activation(wo[:, kk, :], t[:], AF.Identity, scale=betac[:, kk:kk + 1]),
                         r=[t, betac], w=[wo])
                for c in range(4):
                    for j in range(31):
                        S.op("pool", lambda e, c=c, j=j: e.tensor_scalar(DG[:, c, j, :], ident_f[:], wcol[:, c, j:j + 1], None, op0=ALU.mult),
                             r=[ident_f, wcol], w=[DG])

                def load(n_):
                    i = tiles[n_]
                    k = n_ % 2
                    S.dma("sp", lambda e: e.dma_start(out=xt[k][:], in_=src_aps[i]), xt_slot[k], r=[xs_buf[i]], w=[xt[k]])

                def conv(n_):
                    i = tiles[n_]
                    pyc = py2[n_ % 2]
                    if i < NTL:
                        gsrc, gb, t0 = gT, M["gT_b"], i * P
                        lo, hi = max(0, i - 1), min(NTL - 1, i + 1)
                    else:
                        gsrc, gb, t0 = gTc, M["gTc_b"], (i - NTL) * P
                        lo, hi = max(0, i - NTL - 1), min(NTC - 1, i - NTL + 1)
                    for c in range(4):
                        for j in range(31):
                            S.op("pe", lambda e, c=c, j=j: e.matmul(pyc[:, c * P:(c + 1) * P],
                                                                  lhsT=gsrc[:, c, t0 + j + GPAD - 15:t0 + j + GPAD - 15 + P],
                                                                  rhs=DG[:, c, j, :], start=(j == 0), stop=(j == 30)),
                                 r=[gb[q] for q in range(lo, hi + 1)] + [DG], w=[pyc])

                load(0)
                conv(0)
                for n_, i in enumerate(tiles):
                    k = n_ % 2
                    is_ctx = i >= NTL
                    if i == NTL:
                        load_modbc(1, modbc)
                    if n_ + 1 < len(tiles):
                        load(n_ + 1)
                    if not is_ctx:
                        osrc = lambda pr: qT[:, pr, i * P:(i + 1) * P]
                        obufs = [M["qT_b"][pr][i // 4] for pr in range(4)]
                    else:
                        osrc = lambda pr: qTc[:, pr, (i - NTL) * P:(i - NTL + 1) * P]
                        obufs = [qTc.b]
                    if n_ + 1 < len(tiles):
                        conv(n_ + 1)
                    py = py2[n_ % 2]
                    S.op("dve", lambda e: e.tensor_tensor(y[:], py[:], cb_bc[:], op=ALU.add), r=[py, cb_bc], w=[y])
                    S.op("act", lambda e: e.activation(junk[:], y[:], AF.Identity, accum_out=st4[:, 0:1]), r=[y], w=[junk, st4])
                    S.op("act", lambda e: e.activation(junk[:], y[:], AF.Square, accum_out=st4[:, 1:2]), r=[y], w=[junk, st4])
                    S.op("dve", lambda e: e.tensor_scalar(st4[:, 2:4], st4[:, 0:2], 1.0 / 512, None, op0=ALU.mult), r=[st4], w=[st4])
                    S.op("dve", lambda e: e.tensor_tensor(msq[:], st4[:, 2:3], st4[:, 2:3], op=ALU.mult), r=[st4], w=[msq])
                    S.op("dve", lambda e: e.tensor_tensor(msq[:], st4[:, 3:4], msq[:], op=ALU.subtract), r=[st4, msq], w=[msq])
                    rstd_from_ss(msq, rln, 1.0, eps5)
                    S.op("dve", lambda e: e.tensor_scalar(z[:], y[:], st4[:, 2:3], rln[:, 0:1], op0=ALU.subtract, op1=ALU.mult),
                         r=[y, st4, rln], w=[z])
                    S.op("pool", lambda e: e.tensor_tensor(z[:], z[:], lg_bc[:], op=ALU.mult), r=[z, lg_bc], w=[z])
                    S.op("pool", lambda e: e.tensor_tensor(z[:], z[:], lb_bc[:], op=ALU.add), r=[z, lb_bc], w=[z])
                    S.op("act", lambda e: e.activation(oc[:], z[:], AF.Silu), r=[z], w=[oc])
                    S.op("act", lambda e: e.activation(junk[:], oc[:], AF.Square, accum_out=ssc[:, 0:1]), r=[oc], w=[junk, ssc])
                    S.op("pool", lambda e: e.tensor_copy(ocb[:], oc[:]), r=[oc], w=[ocb])
                    for c in range(4):
                        S.op("pe", lambda e, c=c: e.transpose(ptc[:, c, :], ocb[:, c * P:(c + 1) * P], ident_b[:]),
                             r=[ocb, ident_b], w=[ptc])
                    S.op("act", lambda e: e.activation(ocT[:], ptc[:], AF.Copy), r=[ptc], w=[ocT])
                    for pr in range(4):
                        S.op("pool", lambda e, pr=pr: e.tensor_tensor(sqo[:, pr, :], osrc(pr), osrc(pr), op=ALU.mult), r=obufs, w=[sqo])
                    for pr in range(4):
                        S.op("pe", lambda e, pr=pr: e.matmul(pss[:, 0:2], lhsT=sqo[:, pr, :], rhs=ones_f[:, 0:2],
                                                            start=(pr == 0), stop=(pr == 3)), r=[sqo, ones_f], w=[pss])
                    S.op("dve", lambda e: e.tensor_copy(ssc[:, 1:2], pss[:, 0:1]), r=[pss], w=[ssc])
                    rstd_from_ss(ssc, rsc, 1.0 / 512, eps6)
                    for cc in range(2):
                        for pr in range(4):
                            S.op("pe", lambda e, cc=cc, pr=pr: e.matmul(pa[cc][:], lhsT=osrc(pr), rhs=wo[:, pr, cc * 512:(cc + 1) * 512],
                                                                      start=(pr == 0), stop=(pr == 3)), r=obufs + [wo], w=[pa[cc]])
                        for c in range(4):
                            S.op("pe", lambda e, cc=cc, c=c: e.matmul(pc[cc][:], lhsT=ocT[:, c, :], rhs=wo[:, 4 + c, cc * 512:(cc + 1) * 512],
                                                                    start=(c == 0), stop=(c == 3)), r=[ocT, wo], w=[pc[cc]])
                        sl = slice(cc * 512, (cc + 1) * 512)
                        S.op("dve", lambda e, cc=cc, sl=sl: e.tensor_scalar(t1[:, sl], pa[cc][:], rsc[:, 1:2], None, op0=ALU.mult),
                             r=[pa[cc], rsc], w=[t1])
                        S.op("dve", lambda e, cc=cc, sl=sl: e.scalar_tensor_tensor(out=t1[:, sl], in0=pc[cc][:], scalar=rsc[:, 0:1],
                                                                                   in1=t1[:, sl], op0=ALU.mult, op1=ALU.add),
                             r=[pc[cc], rsc, t1], w=[t1])
                    S.op("pool", lambda e: e.tensor_tensor(t1[:], t1[:], modbc[:, 0:1024], op=ALU.mult), r=[t1, modbc], w=[t1])
                    S.op("pool", lambda e: e.tensor_tensor(x1[k][:], t1[:], xt[k][:], op=ALU.add), r=[t1, xt[k]], w=[x1[k]])
                    S.dma("sp", lambda e: e.dma_start(out=xs_d[i * P:(i + 1) * P, :], in_=x1[k][:]), x1_slot[k], r=[x1[k]], w=[xs_buf[i]])
                    if l == 0:
                        dbg_store("d_x1", slice(i * P, (i + 1) * P), x1[k][:], [x1[k]])
                S.barrier()

        def phase_p(l, tiles, modbc, last):
            with ExitStack() as ph:
                wq = sbt(ph, "wq", [P, 8, D], F32)
                keysTz = sbt(ph, "keysTz", [P, 8, 2, P], F32)
                iota16 = sbt(ph, "iota16", [P, 16], F32)
                xt = [sbt(ph, f"xtP{k}", [P, D], F32) for k in range(2)]
                xt_slot = [DmaSlot(S, f"xtP{k}") for k in range(2)]
                junk = sbt(ph, "junkP", [P, D], BF16)
                junkr = sbt(ph, "junkR", [P, D], BF16)
                prod = [sbt(ph, f"prod{k}", [P, D], BF16) for k in range(3)]
                hb = [sbt(ph, f"hb{k}", [P, D], BF16) for k in range(2)]
                pi = [0]
                ss = sbt(ph, "ssP", [P, 1], F32)
                rstd = sbt(ph, "rstdP", [P, 1], F32)
                ssf = sbt(ph, "ssF", [P, 1], F32)
                rstdf = sbt(ph, "rstdF", [P, 1], F32)
                h = [sbt(ph, "hP", [P, D], F32)] * 2
                hT = sbt(ph, "hT", [P, 8, P], F32)
                qpT = hT
                qp = sbt(ph, "qp", [P, D], F32)
                sc = sbt(ph, "scP", [P, 16, 128], F32)
                wk = sbt(ph, "wkP", [P, 16, 128], F32)
                eq_ap = wk[:].rearrange("p a b -> p (a b)").rearrange("p (x c) -> p x c", c=16)
                topv = sbt(ph, "topv", [P, 16, 16], F32)
                idx = sbt(ph, "idxP", [P, 16, 16], U32)
                idxf = sbt(ph, "idxf", [P, 16, 16], F32)
                cand = sbt(ph, "cand", [P, 8, 256], F32)
                wk2_ap = sc[:].rearrange("p a b -> p (a b)").rearrange("p (h x) -> p h x", h=8)
                best = sbt(ph, "best", [P, 8, 16], F32)
                pos = sbt(ph, "pos", [P, 8, 16], U32)
                pab = sbt(ph, "pab", [P, 2, 128], U32)
                abf = sbt(ph, "abf", [P, 2, 128], F32)
                isel = sbt(ph, "isel", [P, 2, 128], F32)
                ef = [sbt(ph, f"ef{k}", [P, 128], F32) for k in range(2)]
                eidx = [sbt(ph, f"eidx{k}", [P, 128], I32) for k in range(2)]
                ew = sbt(ph, "ew", [P, 128], F32)
                se = sbt(ph, "se", [P, 8], F32)
                wgt = [sbt(ph, f"wgt{k}", [P, 128], F32) for k in range(2)]
                pre = sbt(ph, "pre", [P, 128], F32)
                aw = sbt(ph, "aw", [P, 128], F32)
                gbuf = [sbt(ph, f"gbuf{k}", [P, 2 * D], BF16) for k in range(NSLOT)]
                diag = [sbt(ph, f"diag{k}", [P, P], BF16) for k in range(6)]
                pre_b = [Buf(f"pre_b{k}") for k in range(4)]
                pre_d = [Buf(f"pre_d{k}") for k in range(4)]
                di = [0]
                gslot = [DmaSlot(S, f"g{k}") for k in range(NSLOT)]
                x2 = [sbt(ph, "x2_0", [P, D], F32)] * 2
                x2_slot = [DmaSlot(S, f"x2s{k}") for k in range(2)]
                fg_bc = sbt(ph, "fg_bc", [P, D], F32)
                pT2 = pst(ph, "pT2", [P, 8, P], F32)
                pqp = [pst(ph, f"pqp{k}", [P, 512], F32) for k in range(2)]
                pcs = [pst(ph, f"pcs{k}", [P, 512], F32) for k in range(2)]
                pva = [pst(ph, f"pva{k}", [P, 512], F32) for k in range(2)]
                psc = pqp + pcs
                print(f"[build] phase_p sbuf remaining {nc.sbuf_bytes_remaining}")

                S.op("pool", lambda e: e.iota(iota16[:], pattern=[[1, 16]], base=0, channel_multiplier=0,
                                              allow_small_or_imprecise_dtypes=True), w=[iota16])
                ws = DmaSlot(S, "wqload", group=True)
                for j in range(8):
                    S.dma("sp", lambda e, j=j: e.dma_start(out=wq[:, j, :], in_=peer_wq_d[l, j * P:(j + 1) * P, :]), ws, w=[wq])
                kraw_ap = sc[:].rearrange("p a b -> p (a b)")[:, 0:1024].rearrange("p (a d) -> p a d", d=64)
                keysT_ap = cand[:].rearrange("p a b -> p (a b)")[:, 0:1024].rearrange("p (a k) -> p a k", k=P)
                S.dma("sp", lambda e: e.dma_start(out=kraw_ap, in_=peer_keys_d[l].rearrange("h c k d -> k (h c) d")), ws, w=[sc])
                if last:
                    S.dma("sp", lambda e: e.dma_start(out=fg_bc[:], in_=final_g_d[0].partition_broadcast(P)), ws, w=[fg_bc])
                ws.close()
                for hh in range(8):
                    S.op("pe", lambda e, hh=hh: e.transpose(pT2[:, hh, :], kraw_ap[:, 2 * hh:2 * hh + 2, :].rearrange("p c d -> p (c d)"), ident_f[:]),
                         r=[sc, ident_f], w=[pT2])
                S.op("act", lambda e: e.activation(keysT_ap, pT2[:], AF.Copy), r=[pT2], w=[cand])
                S.op("pool", lambda e: e.memset(keysTz[:], 0.0), w=[keysTz])
                S.op("dve", lambda e: e.tensor_copy(keysTz[0:64, :, 0, :], keysT_ap[0:64, :, :]), r=[cand], w=[keysTz])
                S.op("dve", lambda e: e.tensor_copy(keysTz[64:128, :, 1, :], keysT_ap[64:128, :, :]), r=[cand], w=[keysTz])

                gi = [0]

                def routing(n_):
                    rq = []
                    i = tiles[n_]
                    k = n_ % 2
                    xk, hk, efk, eik, wgk = xt[k], h[k], ef[k], eidx[k], wgt[k]
                    hbk = hb[k]

                    def op(eng, fn, r=(), w=()):
                        rq.append(("op", eng, fn, r, w))

                    rq.append(("dma", "sp", lambda e: e.dma_start(out=xk[:], in_=xs_d[i * P:(i + 1) * P, :]), xt_slot[k], [xs_buf[i]], [xk]))
                    op("act", lambda e: e.activation(junkr[:], xk[:], AF.Square, accum_out=ss[:]), r=[xk], w=[junkr, ss])
                    op("act", lambda e: e.activation(rstd[:], ss[:], AF.Sqrt, scale=1.0 / D, bias=eps6[:, 0:1]), r=[ss, eps6], w=[rstd])
                    op("dve", lambda e: e.reciprocal(rstd[:], rstd[:]), r=[rstd], w=[rstd])
                    op("dve", lambda e: e.scalar_tensor_tensor(out=hk[:], in0=xk[:], scalar=rstd[:, 0:1], in1=modbc[:, 2048:3072],
                                                               op0=ALU.mult, op1=ALU.mult), r=[xk, rstd, modbc], w=[hk])
                    op("dve", lambda e: e.tensor_tensor(hk[:], hk[:], modbc[:, 1024:2048], op=ALU.add), r=[hk, modbc], w=[hk])
                    op("act", lambda e: e.activation(hbk[:], hk[:], AF.Copy), r=[hk], w=[hbk])
                    for j in range(8):
                        op("pe", lambda e, j=j: e.transpose(pT2[:, j, :], hk[:, j * P:(j + 1) * P], ident_f[:]), r=[hk, ident_f], w=[pT2])
                    op("act", lambda e: e.activation(hT[:], pT2[:], AF.Copy), r=[pT2], w=[hT])
                    for cc in range(2):
                        for j in range(8):
                            op("pe", lambda e, cc=cc, j=j: e.matmul(pqp[cc][:], lhsT=hT[:, j, :], rhs=wq[:, j, cc * 512:(cc + 1) * 512],
                                                                  start=(j == 0), stop=(j == 7)), r=[hT, wq], w=[pqp[cc]])
                        op("act", lambda e, cc=cc: e.activation(qp[:, cc * 512:(cc + 1) * 512], pqp[cc][:], AF.Copy), r=[pqp[cc]], w=[qp])
                    for hh in range(8):
                        op("pe", lambda e, hh=hh: e.transpose(pT2[:, hh, :], qp[:, hh * P:(hh + 1) * P], ident_f[:]), r=[qp, ident_f], w=[pT2])
                    op("act", lambda e: e.activation(qpT[:], pT2[:], AF.Copy), r=[pT2], w=[qpT])
                    for hh in range(8):
                        pv = psc[hh // 2][:].rearrange("p (a k) -> p a k", k=P)
                        op("pe", lambda e, hh=hh, pv=pv: e.matmul(pv[:, (hh % 2) * 2:(hh % 2) * 2 + 2, :], lhsT=qpT[:, hh, :],
                                                                 rhs=keysTz[:, hh, :, :], start=True, stop=True),
                           r=[qpT, keysTz], w=[psc[hh // 2]])
                    for q in range(4):
                        op("act", lambda e, q=q: e.activation(sc[:, 4 * q:4 * q + 4, :], psc[q][:].rearrange("p (a k) -> p a k", k=P), AF.Copy),
                           r=[psc[q]], w=[sc])
                    for g in range(16):
                        op("dve", lambda e, g=g: e.max(out=topv[:, g, 0:8], in_=sc[:, g, :]), r=[sc], w=[topv])
                        op("dve", lambda e, g=g: e.max_index(out=idx[:, g, 0:8], in_max=topv[:, g, 0:8], in_values=sc[:, g, :]),
                           r=[sc, topv], w=[idx])
                        op("dve", lambda e, g=g: e.match_replace(out=wk[:, g, :], in_to_replace=topv[:, g, 0:8], in_values=sc[:, g, :],
                                                                 imm_value=-1e30), r=[sc, topv], w=[wk])
                        op("dve", lambda e, g=g: e.max(out=topv[:, g, 8:16], in_=wk[:, g, :]), r=[wk], w=[topv])
                        op("dve", lambda e, g=g: e.max_index(out=idx[:, g, 8:16], in_max=topv[:, g, 8:16], in_values=wk[:, g, :]),
                           r=[wk, topv], w=[idx])
                    op("dve", lambda e: e.tensor_copy(idxf[:], idx[:]), r=[idx], w=[idxf])
                    tv4 = topv[:].rearrange("p (h c) a -> p h c a", c=2)
                    if4 = idxf[:].rearrange("p (h c) a -> p h c a", c=2)
                    op("dve", lambda e: e.tensor_tensor(cand[:].rearrange("p h (a b) -> p h a b", a=16),
                                                        tv4[:, :, 0, :].unsqueeze(3).to_broadcast([P, 8, 16, 16]),
                                                        tv4[:, :, 1, :].unsqueeze(2).to_broadcast([P, 8, 16, 16]), op=ALU.add),
                       r=[topv], w=[cand])
                    for hh in range(8):
                        op("dve", lambda e, hh=hh: e.max(out=best[:, hh, 0:8], in_=cand[:, hh, :]), r=[cand], w=[best])
                        op("dve", lambda e, hh=hh: e.max_index(out=pos[:, hh, 0:8], in_max=best[:, hh, 0:8], in_values=cand[:, hh, :]),
                           r=[cand, best], w=[pos])
                        op("dve", lambda e, hh=hh: e.match_replace(out=wk2_ap[:, hh, :], in_to_replace=best[:, hh, 0:8],
                                                                   in_values=cand[:, hh, :], imm_value=-1e30), r=[cand, best], w=[sc])
                        op("dve", lambda e, hh=hh: e.max(out=best[:, hh, 8:16], in_=wk2_ap[:, hh, :]), r=[sc], w=[best])
                        op("dve", lambda e, hh=hh: e.max_index(out=pos[:, hh, 8:16], in_max=best[:, hh, 8:16], in_values=wk2_ap[:, hh, :]),
                           r=[sc, best], w=[pos])
                    posf = pos[:].rearrange("p h s -> p (h s)")
                    op("dve", lambda e: e.tensor_single_scalar(pab[:, 0, :], posf, 4, op=ALU.logical_shift_right), r=[pos], w=[pab])
                    op("dve", lambda e: e.tensor_single_scalar(pab[:, 1, :], posf, 15, op=ALU.bitwise_and), r=[pos], w=[pab])
                    op("dve", lambda e: e.tensor_copy(abf[:], pab[:]), r=[pab], w=[abf])
                    for c in range(2):
                        op("dve", lambda e, c=c: e.tensor_tensor(eq_ap, iota16[:].unsqueeze(1).to_broadcast([P, 128, 16]), abf[:, c, :].unsqueeze(2).to_broadcast([P, 128, 16]),
                                                                 op=ALU.is_equal), r=[iota16, abf], w=[wk])
                        op("dve", lambda e, c=c: e.tensor_tensor(eq_ap.rearrange("p (h s) a -> p h s a", h=8),
                                                                 eq_ap.rearrange("p (h s) a -> p h s a", h=8),
                                                                 if4[:, :, c, :].unsqueeze(2).to_broadcast([P, 8, 16, 16]), op=ALU.mult),
                           r=[wk, idxf], w=[wk])
                        op("dve", lambda e, c=c: e.tensor_reduce(out=isel[:, c, :], in_=eq_ap, axis=AX.X, op=ALU.add), r=[wk], w=[isel])
                    op("dve", lambda e: e.scalar_tensor_tensor(out=efk[:], in0=isel[:, 0, :], scalar=128.0, in1=isel[:, 1, :],
                                                               op0=ALU.mult, op1=ALU.add), r=[isel], w=[efk])
                    op("dve", lambda e: e.tensor_scalar(efk[:], efk[:], float(l * NPEER), None, op0=ALU.add), r=[efk], w=[efk])
                    op("dve", lambda e: e.tensor_copy(eik[:], efk[:]), r=[efk], w=[eik])
                    op("dve", lambda e: e.tensor_tensor(ew[:].rearrange("p (h s) -> p h s", h=8), best[:],
                                                        best[:, :, 0:1].to_broadcast([P, 8, 16]), op=ALU.subtract), r=[best], w=[ew])
                    op("act", lambda e: e.activation(ew[:], ew[:], AF.Exp), r=[ew], w=[ew])
                    op("dve", lambda e: e.tensor_reduce(out=se[:], in_=ew[:].rearrange("p (h s) -> p h s", h=8), axis=AX.X, op=ALU.add),
                       r=[ew], w=[se])
                    op("dve", lambda e: e.reciprocal(se[:], se[:]), r=[se], w=[se])
                    op("dve", lambda e: e.tensor_tensor(wgk[:].rearrange("p (h s) -> p h s", h=8), ew[:].rearrange("p (h s) -> p h s", h=8),
                                                        se[:].unsqueeze(2).to_broadcast([P, 8, 16]), op=ALU.mult), r=[ew, se], w=[wgk])
                    return rq

                rq = routing(0)
                emit(rq, len(rq))
                for n_, i in enumerate(tiles):
                    k = n_ % 2
                    hbk, efk, eik, wgk = hb[k], ef[k], eidx[k], wgt[k]
                    rq = routing(n_ + 1) if n_ + 1 < len(tiles) else []
                    per_slot = -(-len(rq) // 112)
                    NG = 128 // GRP
                    gb = {}
                    for g_ in range(NG + 1):
                        if g_ < NG:
                            pb_ = pre_b[g_ % 4]
                            pd_ = pre_d[g_ % 4]
                            for s_ in range(g_ * GRP, (g_ + 1) * GRP):
                                b_ = gi[0] % NSLOT
                                gi[0] += 1
                                gb[s_] = b_
                                S.dma("pool", lambda e, s_=s_, b_=b_: e.indirect_dma_start(
                                    out=gbuf[b_][:], out_offset=None, in_=uv_d,
                                    in_offset=bass.IndirectOffsetOnAxis(ap=eik[:, s_:s_ + 1], axis=0)), gslot[b_], r=[eik], w=[gbuf[b_]])
                                pr_ = prod[pi[0] % len(prod)]
                                pi[0] += 1
                                S.op("dve", lambda e, b_=b_, pr_=pr_: e.tensor_tensor(pr_[:], hbk[:], gbuf[b_][:, 0:D], op=ALU.mult),
                                     r=[hbk, gbuf[b_]], w=[pr_])
                                S.op("act", lambda e, s_=s_, pr_=pr_: e.activation(junkr[:], pr_[:], AF.Identity, accum_out=pre[:, s_:s_ + 1]),
                                     r=[pr_], w=[junkr, pb_])
                                emit(rq, per_slot)
                            gs = slice(g_ * GRP, (g_ + 1) * GRP)
                            S.op("act", lambda e, gs=gs: e.activation(aw[:, gs], pre[:, gs], AF.Gelu), r=[pb_, pd_], w=[pb_])
                        if g_ >= 1:
                            gq = g_ - 1
                            pbq = pre_b[gq % 4]
                            for s_ in range(gq * GRP, (gq + 1) * GRP):
                                b_ = gb[s_]
                                dg = diag[di[0] % len(diag)]
                                di[0] += 1
                                S.op("dve", lambda e, s_=s_, dg=dg: e.tensor_scalar(dg[:], ident_b[:], aw[:, s_:s_ + 1], wgk[:, s_:s_ + 1],
                                                                                   op0=ALU.mult, op1=ALU.mult),
                                     r=[ident_b, pbq, wgk], w=[dg])
                                for cc in range(2):
                                    S.op("pe", lambda e, s_=s_, b_=b_, dg=dg, cc=cc: e.matmul(
                                        pva[cc][:], lhsT=dg[:], rhs=gbuf[b_][:, D + cc * 512:D + (cc + 1) * 512],
                                        start=(s_ == 0), stop=(s_ == 127)), r=[dg, gbuf[b_]], w=[pva[cc]])
                    if l == 0:
                        dbg_store("d_pre", slice(i * P, (i + 1) * P), pre[:], pre_b + pre_d)
                        dbg_store("d_eidx", slice(i * P, (i + 1) * P), efk[:], [efk])
                        dbg_store("d_w", slice(i * P, (i + 1) * P), wgk[:], [wgk])
                    for cc in range(2):
                        sl = slice(cc * 512, (cc + 1) * 512)
                        S.op("dve", lambda e, cc=cc, sl=sl: e.tensor_tensor(x2[k][:, sl], pva[cc][:], modbc[:, 3072 + cc * 512:3072 + (cc + 1) * 512],
                                                                           op=ALU.mult), r=[pva[cc], modbc], w=[x2[k]])
                    S.op("dve", lambda e: e.tensor_tensor(x2[k][:], x2[k][:], xt[k][:], op=ALU.add), r=[x2[k], xt[k]], w=[x2[k]])
                    if l == 0:
                        dbg_store("d_x2", slice(i * P, (i + 1) * P), x2[k][:], [x2[k]])
                    if last:
                        S.op("act", lambda e: e.activation(junk[:], x2[k][:], AF.Square, accum_out=ssf[:]), r=[x2[k]], w=[junk, ssf])
                        rstd_from_ss(ssf, rstdf, 1.0 / D, eps6)
                        S.op("dve", lambda e: e.scalar_tensor_tensor(out=x2[k][:], in0=x2[k][:], scalar=rstdf[:, 0:1], in1=fg_bc[:],
                                                                     op0=ALU.mult, op1=ALU.mult), r=[x2[k], rstdf, fg_bc], w=[x2[k]])
                        S.dma("sp", lambda e: e.dma_start(out=out_d[i * P:(i + 1) * P, :], in_=x2[k][:]), x2_slot[k], r=[x2[k]])
                    else:
                        S.dma("sp", lambda e: e.dma_start(out=xs_d[i * P:(i + 1) * P, :], in_=x2[k][:]), x2_slot[k], r=[x2[k]], w=[xs_buf[i]])
                    emit(rq, len(rq))
                S.barrier()

        def load_modbc(r_, modbc):
            S.dma("sp", lambda e: e.dma_start(out=modbc[:], in_=modscr_d[r_]), DmaSlot(S, "modld"), w=[modbc])

        lat_tiles = list(range(NTL))
        ctx_tiles = list(range(NTL, NT))
        for l in range(depth):
            last = (l == DEPTH - 1)
            if l == 0:
                src_aps = [x_d[i * P:(i + 1) * P, :] for i in range(NTL)] + [ctx_d[i * P:(i + 1) * P, :] for i in range(NTC)]
            else:
                src_aps = [xs_d[i * P:(i + 1) * P, :] for i in range(NT)]
            mod_cols(l)
            if l == 0:
                dbg_store("d_mod", (slice(0, P), slice(0, 32)), modcol[:].rearrange("p r c -> p (r c)"), [modcol])
                dbg_store("d_mod", (slice(0, P), slice(32, 48)), scT[:].rearrange("p j r -> p (j r)"), [scT])
            if stop == "m":
                break
            with ExitStack() as mix:
                M = {}
                M["qT"] = sbt(mix, "qT", [P, 4, S_LAT], BF16)
                M["qT_b"] = [[Buf(f"qT{pr}_{c}") for c in range(8)] for pr in range(4)]
                M["qTc"] = sbt(mix, "qTc", [P, 4, S_CTX], BF16)
                M["gT"] = sbt(mix, "gT", [P, 4, S_LAT + 2 * GPAD], BF16)
                M["gT_b"] = [Buf(f"gT{i}") for i in range(NTL)]
                M["gTc"] = sbt(mix, "gTc", [P, 4, S_CTX + 2 * GPAD], BF16)
                M["gTc_b"] = [Buf(f"gTc{i}") for i in range(NTC)]
                S.op("pool", lambda e: e.memset(M["gT"][:], 0.0), w=M["gT_b"])
                S.op("pool", lambda e: e.memset(M["gTc"][:], 0.0), w=M["gTc_b"])
                with ExitStack() as ab:
                    M["kT"] = sbt(ab, "kT", [P, NT * P], BF16)
                    M["kT_b"] = [Buf(f"kT{i}") for i in range(NT)]
                    M["Vp"] = sbt(ab, "Vp", [P, NT, 192], BF16)
                    M["Vp_b"] = [Buf(f"Vp{i}") for i in range(NT)]
                    S.op("pool", lambda e: e.memset(M["Vp"][:], 1.0), w=M["Vp_b"])
                    phase_a(l, M, src_aps, last)
                    if stop in ("a", "a0", "a1"):
                        break
                    phase_b(l, M, last)
                if stop == "b":
                    break
                with ExitStack() as cs:
                    modbc = sbt(cs, "modbcC", [P, 4096], F32)
                    for r_ in ((0,) if last else (0, 1)):
                        mod_bc(l, r_, modbc)
                        S.dma("sp", lambda e, r_=r_: e.dma_start(out=modscr_d[r_], in_=modbc[:]), DmaSlot(S, "modst"), r=[modbc])
                        S.barrier()
                    load_modbc(0, modbc)
                    phase_c(l, M, lat_tiles + ([] if last else ctx_tiles), src_aps, 0, modbc)
            if stop == "c":
                break
            with ExitStack() as pp:
                modbc = sbt(pp, "modbcP", [P, 4096], F32)
                load_modbc(0, modbc)
                phase_p(l, lat_tiles if stop not in ("p1", "p0") else lat_tiles[:2], modbc, last)
                if not last and stop not in ("p1", "p0"):
                    load_modbc(1, modbc)
                    phase_p(l, ctx_tiles, modbc, last)
            if stop in ("p1", "p0"):
                break
        out_slot.close()
        if out_slot.sem is not None:
            for en in ("sp", "act", "pool"):
                S.E[en].eng.wait_ge(out_slot.sem, out_slot.val)
        S.barrier()
        print(f"[build] instr={S.ninstr} waits={S.nwait} sems={S.nsem}")
    return nc


_NC_CACHE = {}


def _rope_tables():
    n = 16
    inv = (10000.0 ** (-np.arange(n, dtype=np.float32) / n)).astype(np.float32)
    row = np.repeat(np.arange(64), 64).astype(np.float32)
    col = np.tile(np.arange(64), 64).astype(np.float32)
    ang = np.concatenate([row[:, None] * inv, col[:, None] * inv], axis=-1).astype(np.float32)
    cos = np.cos(ang).astype(np.float32)
    sin = np.sin(ang).astype(np.float32)
    cos2 = np.concatenate([cos, cos], axis=-1)
    sin2 = np.concatenate([-sin, sin], axis=-1)
    return np.ascontiguousarray(cos2), np.ascontiguousarray(sin2)


def make_in_maps(inputs, cores):
    f = lambda a: np.ascontiguousarray(np.asarray(a, dtype=np.float32))
    cos2, sin2 = _rope_tables()
    shared = {k: f(inputs[k]) for k in ["w_mod", "b_mod", "w_in", "q_gain", "k_gain", "conv_w", "conv_b", "ln_g", "ln_b",
                                        "beta_attn", "beta_conv", "w_o", "peer_wq", "peer_keys", "peer_u", "peer_v"]}
    shared["final_g"] = f(inputs["final_g"]).reshape(1, D)
    shared["cos2"] = cos2
    shared["sin2"] = sin2
    shared["cctxT"] = np.ascontiguousarray(f(inputs["c_ctx"]).reshape(8, P).T)
    x = f(inputs["x"])
    ctx = f(inputs["ctx"])
    c = f(inputs["c"])
    maps = []
    for b in cores:
        m = dict(shared)
        m["x"] = x[b]
        m["ctx"] = ctx[b]
        m["cT"] = np.ascontiguousarray(c[b].reshape(8, P).T)
        maps.append(m)
    return maps


def kernel(**inputs):
    if "nc" not in _NC_CACHE:
        _NC_CACHE["nc"] = build()
    nc = _NC_CACHE["nc"]
    B = np.asarray(inputs["x"]).shape[0]
    maps = make_in_maps(inputs, list(range(B)))
    res = run_bass_kernel_spmd(nc, maps, core_ids=list(range(B)))
    out = np.stack([np.asarray(r["out"], dtype=np.float32) for r in res.results], axis=0)
    return out
```

```python
import numpy as np
from contextlib import ExitStack
import concourse.bass as bass
import concourse.mybir as mybir
from concourse.bass_utils import run_bass_kernel_spmd

F32 = mybir.dt.float32
BF16 = mybir.dt.bfloat16
I32 = mybir.dt.int32
U32 = mybir.dt.uint32
ALU = mybir.AluOpType
AF = mybir.ActivationFunctionType
AX = mybir.AxisListType

import os
CUT = int(os.environ.get("KCUT", "99"))
SEM_LIMIT = 30000


class Buf:
    __slots__ = ("name", "w", "r")

    def __init__(self, name):
        self.name = name
        self.w = None
        self.r = {}


class Eng:
    def __init__(self, sync, name, eng):
        self.sync = sync
        self.name = name
        self.eng = eng
        self.sem = None
        self.count = 0
        self.known = {}
        self.last = None

    def new_event(self):
        if self.sem is None or self.count >= SEM_LIMIT:
            self.sem = self.sync.new_sem(self.name)
            self.count = 0
        self.count += 1
        self.last = [self.sem, self.count, self.name]
        return self.last


def DmaSlot(sync, name, group=False):
    if name not in sync.slot_by_name:
        sync.slot_by_name[name] = _DmaSlot(sync, name, group)
    return sync.slot_by_name[name]


class _DmaSlot:
    def __init__(self, sync, name, group=False):
        self.name = name
        self.sem = None
        self.val = 0
        self.group = group
        self.pending = []
        sync.slots.append(self)

    def bump(self, sync):
        if self.sem is None or (self.val >= SEM_LIMIT and not self.pending):
            self.sem = sync.new_sem("d" + self.name)
            self.val = 0
        self.val += 16
        ev = [self.sem, self.val, "dma"]
        if self.group:
            self.pending.append(ev)
        return ev

    def close(self):
        for ev in self.pending:
            ev[1] = self.val
        self.pending = []


class Sync:
    def __init__(self, nc, stack):
        self.nc = nc
        self.stack = stack
        self.nsem = 0
        self.slots = []
        self.slot_by_name = {}
        self.E = {
            "pe": Eng(self, "pe", nc.tensor),
            "dve": Eng(self, "dve", nc.vector),
            "act": Eng(self, "act", nc.scalar),
            "pool": Eng(self, "pool", nc.gpsimd),
            "sp": Eng(self, "sp", nc.sync),
        }
        self.ninstr = 0
        self.nwait = 0

    def new_sem(self, name):
        self.nsem += 1
        return self.stack.enter_context(self.nc.semaphore(f"s{self.nsem}_{name}"))

    def _wait(self, E, evs):
        best = {}
        for ev in evs:
            if ev is None:
                continue
            if E.name == "pe" and ev[2] == "pe":
                continue
            k = id(ev[0])
            if k not in best or best[k][1] < ev[1]:
                best[k] = ev
        for ev in best.values():
            sem, val, src = ev
            if E.known.get(id(sem), 0) >= val:
                continue
            E.eng.wait_ge(sem, val)
            self.nwait += 1
            E.known[id(sem)] = val

    def _deps(self, E, reads, writes):
        evs = []
        for b in reads:
            evs.append(b.w)
        inorder = E.name != "pool"
        for b in writes:
            if b.w is not None and not (inorder and b.w[2] == E.name):
                evs.append(b.w)
            for ev in b.r.values():
                if inorder and ev[2] == E.name:
                    continue
                evs.append(ev)
        return evs

    @staticmethod
    def _bufs(lst):
        return [t.b if hasattr(t, "b") else t for t in lst]

    def op(self, engname, fn, r=(), w=()):
        E = self.E[engname]
        r = self._bufs(r)
        w = self._bufs(w)
        self._wait(E, self._deps(E, r, w))
        ins = fn(E.eng)
        ev = E.new_event()
        ins.then_inc(ev[0], 1)
        self.ninstr += 1
        key = ev[2] if ev[2] != "dma" else id(ev[0])
        for b in r:
            b.r[key] = ev
        for b in w:
            b.w = ev
            b.r = {}
        return ev

    def dma(self, qname, fn, slot, r=(), w=()):
        E = self.E[qname]
        r = self._bufs(r)
        w = self._bufs(w)
        self._wait(E, self._deps(E, r, w))
        ins = fn(E.eng)
        ev = slot.bump(self)
        ins.then_inc(ev[0], 16)
        self.ninstr += 1
        key = ev[2] if ev[2] != "dma" else id(ev[0])
        for b in r:
            b.r[key] = ev
        for b in w:
            b.w = ev
            b.r = {}
        return ev

    def barrier(self):
        for s in self.slots:
            if s.group:
                s.close()
        evs = []
        for E in self.E.values():
            if E.last is not None:
                evs.append(E.last)
        for s in self.slots:
            if s.sem is not None:
                evs.append([s.sem, s.val, "dma"])
        for E in self.E.values():
            self._wait(E, [ev for ev in evs if not (E.name == "pe" and ev[2] == "pe")])


class Tile:
    def __init__(self, h, name):
        self.h = h
        self.b = Buf(name)

    def __getitem__(self, k):
        return self.h[k]


P = 128
D = 1024
S_LAT = 4096
S_CTX = 256
NTL = 32
NTC = 2
NT = NTL + NTC
DEPTH = 2
INC = 1792
EPS = 1e-6
NPEER = 16384
GPAD = 16
NSLOT = 17
GRP = 4


def build(depth=DEPTH, dbg=False, stop=None):
    nc = bass.Bass("TRN2", target_bir_lowering=False)

    def din(name, shape, dt=F32):
        return nc.dram_tensor(name, shape, dt, kind="ExternalInput").ap()

    x_d = din("x", [S_LAT, D])
    ctx_d = din("ctx", [S_CTX, D])
    cT_d = din("cT", [P, 8])
    cctxT_d = din("cctxT", [P, 8])
    w_mod_d = din("w_mod", [DEPTH, D, 6 * D])
    b_mod_d = din("b_mod", [DEPTH, 6 * D])
    w_in_d = din("w_in", [DEPTH, D, INC])
    q_gain_d = din("q_gain", [DEPTH, 64])
    k_gain_d = din("k_gain", [DEPTH, 64])
    conv_w_d = din("conv_w", [DEPTH, 31, 512])
    conv_b_d = din("conv_b", [DEPTH, 512])
    ln_g_d = din("ln_g", [DEPTH, 512])
    ln_b_d = din("ln_b", [DEPTH, 512])
    beta_attn_d = din("beta_attn", [DEPTH, 512])
    beta_conv_d = din("beta_conv", [DEPTH, 512])
    w_o_d = din("w_o", [DEPTH, D, D])
    peer_wq_d = din("peer_wq", [DEPTH, D, D])
    peer_keys_d = din("peer_keys", [DEPTH, 8, 2, 128, 64])
    peer_u_d = din("peer_u", [DEPTH, NPEER, D])
    peer_v_d = din("peer_v", [DEPTH, NPEER, D])
    final_g_d = din("final_g", [1, D])
    cos2_d = din("cos2", [S_LAT, 64])
    sin2_d = din("sin2", [S_LAT, 64])
    peer_u_flat = peer_u_d.rearrange("l n d -> (l n) d")
    peer_v_flat = peer_v_d.rearrange("l n d -> (l n) d")
    out_d = nc.dram_tensor("out", [S_LAT, D], F32, kind="ExternalOutput").ap()
    xs_d = nc.dram_tensor("xs", [S_LAT + S_CTX, D], F32).ap()
    uv_d = nc.dram_tensor("uv", [DEPTH * NPEER, 2 * D], BF16).ap()
    modscr_d = nc.dram_tensor("modscr", [2, P, 4096], F32).ap()
    dbg_d = {}
    if dbg:
        for nm, shp in [("d_qkvu", [NT * P, INC]), ("d_x1", [NT * P, D]), ("d_oT", [P, 4 * S_LAT]),
                        ("d_pre", [NT * P, 128]), ("d_eidx", [NT * P, 128]), ("d_w", [NT * P, 128]),
                        ("d_qT", [P, 4 * S_LAT]), ("d_x2", [NT * P, D]), ("d_mod", [P, 4096])]:
            dbg_d[nm] = nc.dram_tensor(nm, shp, F32, kind="ExternalOutput").ap()

    top = ExitStack()
    with top:
        S = Sync(nc, top)

        uid = [0]

        def sbt(stack, name, shape, dt):
            uid[0] += 1
            name = f"{name}_u{uid[0]}"
            return Tile(stack.enter_context(nc.sbuf_tensor(name, shape, dt)), name)

        def pst(stack, name, shape, dt):
            uid[0] += 1
            name = f"{name}_u{uid[0]}"
            return Tile(stack.enter_context(nc.psum_tensor(name, shape, dt)), name)

        out_slot = DmaSlot(S, "out", group=True)
        dbg_slots = {}
        xs_buf = [Buf(f"xs{i}") for i in range(NT)]

        ident_f = sbt(top, "ident_f", [P, P], F32)
        ident_b = sbt(top, "ident_b", [P, P], BF16)
        ones_f = sbt(top, "ones_f", [P, P], F32)
        io_t = sbt(top, "io_t", [P, P], F32)
        pid_t = sbt(top, "pid_t", [P, 1], F32)
        S.op("pool", lambda e: e.iota(io_t[:], pattern=[[1, P]], base=0, channel_multiplier=0,
                                      allow_small_or_imprecise_dtypes=True), w=[io_t])
        S.op("pool", lambda e: e.iota(pid_t[:], pattern=[[0, 1]], base=0, channel_multiplier=1,
                                      allow_small_or_imprecise_dtypes=True), w=[pid_t])
        S.op("dve", lambda e: e.tensor_scalar(ident_f[:], io_t[:], pid_t[:, 0:1], None, op0=ALU.is_equal),
             r=[io_t, pid_t], w=[ident_f])
        S.op("dve", lambda e: e.tensor_copy(ident_b[:], ident_f[:]), r=[ident_f], w=[ident_b])
        S.op("pool", lambda e: e.memset(ones_f[:], 1.0), w=[ones_f])

        cslot = DmaSlot(S, "const", group=True)
        craw = sbt(top, "craw", [P, 2, 8], F32)
        scT = sbt(top, "scT", [P, 8, 2], F32)
        S.dma("sp", lambda e: e.dma_start(out=craw[:, 0, :], in_=cT_d), cslot, w=[craw])
        S.dma("sp", lambda e: e.dma_start(out=craw[:, 1, :], in_=cctxT_d), cslot, w=[craw])
        cslot.close()
        S.op("act", lambda e: e.activation(scT[:].rearrange("p j r -> p r j"), craw[:], AF.Silu), r=[craw], w=[scT])
        modcol = sbt(top, "modcol", [P, 2, 16], F32)
        eps6 = sbt(top, "eps6", [P, 1], F32)
        eps5 = sbt(top, "eps5", [P, 1], F32)
        S.op("pool", lambda e: e.memset(eps6[:], 1e-6), w=[eps6])
        S.op("pool", lambda e: e.memset(eps5[:], 1e-5), w=[eps5])


        def rstd_from_ss(ss, rs, scale, eps_t):
            S.op("act", lambda e: e.activation(rs[:], ss[:], AF.Sqrt, scale=scale, bias=eps_t[:, 0:1]), r=[ss, eps_t], w=[rs])
            S.op("dve", lambda e: e.reciprocal(rs[:], rs[:]), r=[rs], w=[rs])

        def dbg_store(name, rows, tile_ap, rtiles):
            if dbg and name in dbg_d:
                if name not in dbg_slots:
                    dbg_slots[name] = (DmaSlot(S, name), Buf(name))
                S.dma("sp", lambda e: e.dma_start(out=dbg_d[name][rows], in_=tile_ap), dbg_slots[name][0], r=rtiles, w=[dbg_slots[name][1]])


        def convert_thunks(ph, NB=3):
            stg = [sbt(ph, f"cvi{k}", [P, 4, D], F32) for k in range(NB)]
            obf = [sbt(ph, f"cvo{k}", [P, 4, D], BF16) for k in range(NB)]
            islot = [DmaSlot(S, f"cvi{k}") for k in range(NB)]
            oslot = [DmaSlot(S, f"cvo{k}") for k in range(NB)]
            steps = []
            n = 0
            for l in range(DEPTH):
                for tab, col0 in ((peer_u_d, 0), (peer_v_d, D)):
                    for t in range(NPEER // 512):
                        k = n % NB
                        src = tab[l, t * 512:(t + 1) * 512, :].rearrange("(p r) d -> p r d", r=4)
                        dst = uv_d[l * NPEER + t * 512:l * NPEER + (t + 1) * 512, col0:col0 + D].rearrange("(p r) d -> p r d", r=4)
                        steps.append((
                            ("dma", "sp", lambda e, k=k, src=src: e.dma_start(out=stg[k][:], in_=src), islot[k], [], [stg[k]]),
                            ("op", "dve", lambda e, k=k: e.tensor_copy(obf[k][:], stg[k][:]), [stg[k]], [obf[k]]),
                            ("dma", "pool", lambda e, k=k, dst=dst: e.dma_start(out=dst, in_=obf[k][:]), oslot[k], [obf[k]], []),
                        ))
                        n += 1
            cq = []
            for n in range(len(steps) + 2):
                if n < len(steps):
                    cq.append(steps[n][0])
                if n >= 2:
                    cq.append(steps[n - 2][1])
                    cq.append(steps[n - 2][2])
            return cq

        def emit(rq, n):
            for _ in range(n):
                if not rq:
                    return
                t = rq.pop(0)
                if t[0] == "op":
                    S.op(t[1], t[2], t[3], t[4])
                else:
                    S.dma(t[1], t[2], t[3], t[4], t[5])

        def mod_cols(l):
            with ExitStack() as ph:
                wm = [sbt(ph, f"wm{k}", [P, 8, P], F32) for k in range(2)]
                wslot = [DmaSlot(S, f"wm{k}") for k in range(2)]
                bcol = sbt(ph, "bcol", [P, 16], F32)
                pcol = pst(ph, "pcol", [P, 16, 2], F32)
                S.dma("sp", lambda e: e.dma_start(out=bcol[:], in_=b_mod_d[l, 0:2048].rearrange("(c p) -> p c", p=P),
                                                  allow_slow_non_contiguous=True), DmaSlot(S, "bcol"), w=[bcol])
                for cc in range(16):
                    t = wm[cc % 2]
                    S.dma("sp", lambda e, t=t, cc=cc: e.dma_start(
                        out=t[:], in_=w_mod_d[l, :, cc * P:(cc + 1) * P].rearrange("(j p) m -> p j m", p=P)),
                          wslot[cc % 2], w=[t])
                    for j in range(8):
                        S.op("pe", lambda e, t=t, j=j, cc=cc: e.matmul(pcol[:, cc, :], lhsT=t[:, j, :],
                                                                     rhs=scT[:, j, :], start=(j == 0), stop=(j == 7)),
                             r=[t, scT], w=[pcol])
                S.op("dve", lambda e: e.tensor_tensor(modcol[:].rearrange("p r c -> p c r"), pcol[:],
                                                      bcol[:].unsqueeze(2).to_broadcast([P, 16, 2]), op=ALU.add),
                     r=[pcol, bcol], w=[modcol])
                S.op("dve", lambda e: e.tensor_scalar(modcol[:, :, 8:16], modcol[:, :, 8:16], 1.0, None, op0=ALU.add),
                     r=[modcol], w=[modcol])
                S.barrier()

        def mod_bc(l, r, modbc):
            with ExitStack() as ph:
                NW = 4
                wm = [sbt(ph, f"wmb{k}", [P, 512], F32) for k in range(NW)]
                wslot = [DmaSlot(S, f"wmb{k}") for k in range(NW)]
                bbc = [sbt(ph, f"bbc{k}", [P, 512], F32) for k in range(2)]
                bslot = [DmaSlot(S, f"bbc{k}") for k in range(2)]
                screp = sbt(ph, "screp", [P, 8, P], F32)
                pb = pst(ph, "pbm", [P, 512], F32)
                S.op("dve", lambda e: e.tensor_copy(screp[:], scT[:, :, r].unsqueeze(2).to_broadcast([P, 8, P])),
                     r=[scT], w=[screp])
                n = 0
                for cc in range(8):
                    c0 = 2048 + cc * 512
                    S.dma("sp", lambda e, cc=cc, c0=c0: e.dma_start(out=bbc[cc % 2][:], in_=b_mod_d[l, c0:c0 + 512].partition_broadcast(P)),
                          bslot[cc % 2], w=[bbc[cc % 2]])
                    for j in range(8):
                        t = wm[n % NW]
                        S.dma("sp", lambda e, t=t, j=j, c0=c0: e.dma_start(out=t[:], in_=w_mod_d[l, j * P:(j + 1) * P, c0:c0 + 512]),
                              wslot[n % NW], w=[t])
                        n += 1
                        S.op("pe", lambda e, t=t, j=j: e.matmul(pb[:], lhsT=screp[:, j, :], rhs=t[:], start=(j == 0), stop=(j == 7)),
                             r=[t, screp], w=[pb])
                    S.op("dve", lambda e, cc=cc: e.tensor_tensor(modbc[:, cc * 512:(cc + 1) * 512], pb[:], bbc[cc % 2][:], op=ALU.add),
                         r=[pb, bbc[cc % 2]], w=[modbc])
                S.op("dve", lambda e: e.tensor_scalar(modbc[:, 2048:3072], modbc[:, 2048:3072], 1.0, None, op0=ALU.add),
                     r=[modbc], w=[modbc])
                S.barrier()

        def phase_a(l, M, src_aps, last):
            with ExitStack() as ph:
                win = sbt(ph, "win", [P, 8, INC], BF16)
                stg = sbt(ph, "stg", [P, INC], F32)
                stg_slot = DmaSlot(S, "stg")
                bias_bc = sbt(ph, "bias_bc", [P, INC], F32)
                sh1rep = sbt(ph, "sh1rep", [P, 2, 8, P], BF16)
                gain = sbt(ph, "gain", [P, 640], F32)
                graw = sbt(ph, "graw", [P, 128], F32)
                cos2 = sbt(ph, "cos2", [P, NTL, 64], F32)
                sin2 = sbt(ph, "sin2", [P, NTL, 64], F32)
                xt = [sbt(ph, f"xt{k}", [P, D], F32) for k in range(2)]
                xt_slot = [DmaSlot(S, f"xt{k}") for k in range(2)]
                xb = [sbt(ph, f"xb{k}", [P, D], BF16) for k in range(2)]
                xT = [sbt(ph, f"xT{k}", [P, 8, P], BF16) for k in range(2)]
                junk = sbt(ph, "junkA", [P, D], BF16)
                ss = [sbt(ph, f"ssA{k}", [P, 1], F32) for k in range(2)]
                rstd = [sbt(ph, f"rstdA{k}", [P, 1], F32) for k in range(2)]
                qkvu = sbt(ph, "qkvu", [P, INC], F32)
                sq = sbt(ph, "sq", [P, 640], F32)
                ssq = sbt(ph, "ssq", [P, 10], F32)
                rsq = sbt(ph, "rsq", [P, 10], F32)
                qn = sbt(ph, "qn", [P, 640], F32)
                rb = sbt(ph, "rb", [P, 640], F32)
                qr = sbt(ph, "qr", [P, 4, 2, 64], BF16)
                kr = sbt(ph, "kr", [P, 128], BF16)
                sig = sbt(ph, "sig", [P, 512], F32)
                gg = sbt(ph, "gg", [P, 512], BF16)
                ptb = pst(ph, "ptb", [P, 8, P], BF16)
                pq = [pst(ph, f"pq{k}", [P, 512], F32) for k in range(4)]
                ptq = pst(ph, "ptq", [P, 5, P], BF16)
                ptg = pst(ph, "ptg", [P, 4, P], BF16)
                ra = sq
                qT, qTc, kT, Vp, gT, gTc = M["qT"], M["qTc"], M["kT"], M["Vp"], M["gT"], M["gTc"]

                cslot2 = DmaSlot(S, "rope", group=True)
                S.dma("sp", lambda e: e.dma_start(out=cos2[:], in_=cos2_d.rearrange("(n p) f -> p n f", p=P)), cslot2, w=[cos2])
                S.dma("sp", lambda e: e.dma_start(out=sin2[:], in_=sin2_d.rearrange("(n p) f -> p n f", p=P)), cslot2, w=[sin2])
                S.dma("sp", lambda e: e.dma_start(out=graw[:, 0:64], in_=q_gain_d[l].partition_broadcast(P)), cslot2, w=[graw])
                S.dma("sp", lambda e: e.dma_start(out=graw[:, 64:128], in_=k_gain_d[l].partition_broadcast(P)), cslot2, w=[graw])
                cslot2.close()
                for j in range(8):
                    S.dma("sp", lambda e, j=j: e.dma_start(out=stg[:], in_=w_in_d[l, j * P:(j + 1) * P, :]), stg_slot, w=[stg])
                    S.op("act", lambda e, j=j: e.activation(win[:, j, :], stg[:], AF.Copy), r=[stg], w=[win])
                S.op("dve", lambda e: e.tensor_copy(gain[:, 0:512].rearrange("p (h d) -> p h d", h=8),
                                                    graw[:, 0:64].unsqueeze(1).to_broadcast([P, 8, 64])), r=[graw], w=[gain])
                S.op("dve", lambda e: e.tensor_copy(gain[:, 512:640].rearrange("p (h d) -> p h d", h=2),
                                                    graw[:, 64:128].unsqueeze(1).to_broadcast([P, 2, 64])), r=[graw], w=[gain])
                S.op("dve", lambda e: e.tensor_copy(sh1rep[:], modcol[:, :, 0:8].unsqueeze(3).to_broadcast([P, 2, 8, P])),
                     r=[modcol], w=[sh1rep])

                def make_bias(r_):
                    for cc in range(4):
                        w_ = min(512, INC - cc * 512)
                        for j in range(8):
                            S.op("pe", lambda e, cc=cc, j=j, w_=w_: e.matmul(
                                pq[cc][:, 0:w_], lhsT=sh1rep[:, r_, j, :], rhs=win[:, j, cc * 512:cc * 512 + w_],
                                start=(j == 0), stop=(j == 7)), r=[sh1rep, win], w=[pq[cc]])
                        S.op("act", lambda e, cc=cc, w_=w_: e.activation(
                            bias_bc[:, cc * 512:cc * 512 + w_], pq[cc][:, 0:w_], AF.Copy), r=[pq[cc]], w=[bias_bc])

                def load(i):
                    k = i % 2
                    S.dma("sp", lambda e: e.dma_start(out=xt[k][:], in_=src_aps[i]), xt_slot[k], r=[xs_buf[i]], w=[xt[k]])

                make_bias(0)
                load(0)
                qk2 = [qkvu, stg]

                def stage1(i):
                    k = i % 2
                    r_ = 0 if i < NTL else 1
                    qk_ = qk2[i % 2]
                    S.op("act", lambda e: e.activation(junk[:], xt[k][:], AF.Square, accum_out=ss[k][:]), r=[xt[k]], w=[junk, ss[k]])
                    S.op("act", lambda e: e.activation(xb[k][:], xt[k][:], AF.Copy), r=[xt[k]], w=[xb[k]])
                    rstd_from_ss(ss[k], rstd[k], 1.0 / D, eps6)
                    for j in range(8):
                        S.op("pe", lambda e, j=j: e.transpose(ptb[:, j, :], xb[k][:, j * P:(j + 1) * P], ident_b[:]),
                             r=[xb[k], ident_b], w=[ptb])
                    S.op("dve", lambda e: e.tensor_tensor(xT[k][:], ptb[:],
                                                          modcol[:, r_, 8:16].unsqueeze(2).to_broadcast([P, 8, P]), op=ALU.mult),
                         r=[ptb, modcol], w=[xT[k]])
                    if CUT <= 1:
                        return
                    for cc in range(4):
                        w_ = min(512, INC - cc * 512)
                        for j in range(8):
                            S.op("pe", lambda e, cc=cc, j=j, w_=w_: e.matmul(
                                pq[cc][:, 0:w_], lhsT=xT[k][:, j, :], rhs=win[:, j, cc * 512:cc * 512 + w_],
                                start=(j == 0), stop=(j == 7)), r=[xT[k], win], w=[pq[cc]])
                        S.op("dve", lambda e, cc=cc, w_=w_: e.scalar_tensor_tensor(
                            out=qk_[:, cc * 512:cc * 512 + w_], in0=pq[cc][:, 0:w_], scalar=rstd[k][:, 0:1],
                            in1=bias_bc[:, cc * 512:cc * 512 + w_], op0=ALU.mult, op1=ALU.add),
                             r=[pq[cc], rstd[k], bias_bc], w=[qk_])

                def stage2(i):
                    k = i % 2
                    is_ctx = i >= NTL
                    qk_ = qk2[i % 2]
                    if l == 0:
                        dbg_store("d_qkvu", slice(i * P, (i + 1) * P), qk_[:], [qk_])
                    if CUT <= 2:
                        return
                    S.op("pool", lambda e: e.tensor_tensor(sq[:], qk_[:, 0:640], qk_[:, 0:640], op=ALU.mult), r=[qk_], w=[sq])
                    S.op("dve", lambda e: e.tensor_reduce(out=ssq[:], in_=sq[:].rearrange("p (h d) -> p h d", h=10),
                                                          axis=AX.X, op=ALU.add), r=[sq], w=[ssq])
                    rstd_from_ss(ssq, rsq, 1.0 / 64, eps6)
                    S.op("dve", lambda e: e.tensor_tensor(qn[:].rearrange("p (h d) -> p h d", h=10),
                                                          qk_[:, 0:640].rearrange("p (h d) -> p h d", h=10),
                                                          rsq[:].unsqueeze(2).to_broadcast([P, 10, 64]), op=ALU.mult),
                         r=[qk_, rsq], w=[qn])
                    S.op("pool", lambda e: e.tensor_tensor(qn[:], qn[:], gain[:], op=ALU.mult), r=[qn, gain], w=[qn])
                    if CUT <= 3:
                        return
                    if not is_ctx:
                        qn3 = qn[:].rearrange("p (h d) -> p h d", h=10)
                        rb3 = rb[:].rearrange("p (h d) -> p h d", h=10)
                        S.op("dve", lambda e: e.tensor_tensor(ra[:].rearrange("p (h d) -> p h d", h=10), qn3,
                                                              cos2[:, i, :].unsqueeze(1).to_broadcast([P, 10, 64]), op=ALU.mult),
                             r=[qn, cos2], w=[ra])
                        S.op("pool", lambda e: e.tensor_tensor(rb3[:, :, 0:32], qn3[:, :, 32:64],
                                                               sin2[:, i, 0:32].unsqueeze(1).to_broadcast([P, 10, 32]), op=ALU.mult),
                             r=[qn, sin2], w=[rb])
                        S.op("pool", lambda e: e.tensor_tensor(rb3[:, :, 32:64], qn3[:, :, 0:32],
                                                               sin2[:, i, 32:64].unsqueeze(1).to_broadcast([P, 10, 32]), op=ALU.mult),
                             r=[qn, sin2], w=[rb])
                        S.op("dve", lambda e: e.tensor_tensor(qr[:].rearrange("p pr hh d -> p hh pr d"),
                                                              ra[:, 0:512].rearrange("p (hh pr d) -> p hh pr d", hh=2, pr=4),
                                                              rb[:, 0:512].rearrange("p (hh pr d) -> p hh pr d", hh=2, pr=4), op=ALU.add),
                             r=[ra, rb], w=[qr])
                        S.op("pool", lambda e: e.tensor_tensor(kr[:], ra[:, 512:640], rb[:, 512:640], op=ALU.add), r=[ra, rb], w=[kr])
                    else:
                        S.op("dve", lambda e: e.tensor_copy(qr[:].rearrange("p pr hh d -> p hh pr d"),
                                                            qn[:, 0:512].rearrange("p (hh pr d) -> p hh pr d", hh=2, pr=4)),
                             r=[qn], w=[qr])
                        S.op("pool", lambda e: e.tensor_copy(kr[:], qn[:, 512:640]), r=[qn], w=[kr])
                    if CUT <= 4:
                        return
                    need_q = (not is_ctx) or (not last)
                    if need_q:
                        for pr in range(4):
                            S.op("pe", lambda e, pr=pr: e.transpose(ptq[:, pr, :], qr[:, pr, :, :].rearrange("p hh d -> p (hh d)"), ident_b[:]),
                                 r=[qr, ident_b], w=[ptq])
                    S.op("pe", lambda e: e.transpose(ptq[:, 4, :], kr[:], ident_b[:]), r=[kr, ident_b], w=[ptq])
                    if need_q:
                        if not is_ctx:
                            S.op("act", lambda e: e.activation(qT[:, :, i * P:(i + 1) * P], ptq[:, 0:4, :], AF.Copy),
                                 r=[ptq], w=[M["qT_b"][pr][i // 4] for pr in range(4)])
                        else:
                            S.op("act", lambda e: e.activation(qTc[:, :, (i - NTL) * P:(i - NTL + 1) * P], ptq[:, 0:4, :], AF.Copy),
                                 r=[ptq], w=[qTc])
                    S.op("act", lambda e: e.activation(kT[:, i * P:(i + 1) * P], ptq[:, 4, :], AF.Copy), r=[ptq], w=[M["kT_b"][i]])
                    if CUT <= 5:
                        return
                    S.op("pool", lambda e: e.tensor_copy(Vp[:, i, 0:64], qk_[:, 640:704]), r=[qk_], w=[M["Vp_b"][i]])
                    S.op("pool", lambda e: e.tensor_copy(Vp[:, i, 128:192], qk_[:, 704:768]), r=[qk_], w=[M["Vp_b"][i]])
                    if CUT <= 6:
                        return
                    if need_q:
                        S.op("act", lambda e: e.activation(sig[:], qk_[:, 1280:1792], AF.Sigmoid), r=[qk_], w=[sig])
                        S.op("dve", lambda e: e.tensor_tensor(gg[:], qk_[:, 768:1280], sig[:], op=ALU.mult), r=[qk_, sig], w=[gg])
                        for c in range(4):
                            S.op("pe", lambda e, c=c: e.transpose(ptg[:, c, :], gg[:, c * P:(c + 1) * P], ident_b[:]),
                                 r=[gg, ident_b], w=[ptg])
                        if not is_ctx:
                            S.op("act", lambda e: e.activation(gT[:, :, GPAD + i * P:GPAD + (i + 1) * P], ptg[:], AF.Copy),
                                 r=[ptg], w=[M["gT_b"][i]])
                        else:
                            S.op("act", lambda e: e.activation(gTc[:, :, GPAD + (i - NTL) * P:GPAD + (i - NTL + 1) * P], ptg[:], AF.Copy),
                                 r=[ptg], w=[M["gTc_b"][i - NTL]])


                ntile = NT if stop not in ("a0", "a1") else (0 if stop == "a0" else 1)
                for i in range(ntile):
                    k = i % 2
                    r_ = 0 if i < NTL else 1
                    is_ctx = i >= NTL
                    if i == NTL:
                        make_bias(1)
                    if i + 1 < NT:
                        load(i + 1)
                    stage1(i)
                    if i >= 1:
                        stage2(i - 1)
                if ntile:
                    stage2(ntile - 1)

                S.barrier()

        def phase_b(l, M, last):
            with ExitStack() as ph:
                NPT = 4
                NPS = 4
                LA = 1
                pT = [sbt(ph, f"pT{k}", [P, 512], BF16) for k in range(NPT)]
                rd = sbt(ph, "rd", [P, 512], F32)
                bcs = sbt(ph, "bcs", [P, 512], F32)
                ps_s = [pst(ph, f"ps_s{k}", [P, 512], F32) for k in range(NPS)]
                po = [pst(ph, f"po{k}", [P, 512], F32) for k in range(3)]
                pbc = pst(ph, "pbc", [P, 512], F32)
                qT, qTc, kT, Vp = M["qT"], M["qTc"], M["kT"], M["Vp"]
                cnt = [0]
                grp = [0]
                kTz = sbt(ph, "kTz", [P, 2, NT * P], BF16)
                S.op("pool", lambda e: e.memset(kTz[:], 0.0), w=[kTz])
                S.op("dve", lambda e: e.tensor_copy(kTz[0:64, 0, :], kT[0:64, :]), r=M["kT_b"], w=[kTz])
                S.op("act", lambda e: e.activation(kTz[64:128, 1, :], kT[64:128, :], AF.Copy), r=M["kT_b"], w=[kTz])
                cq = convert_thunks(ph) if (l == 0 and stop in (None, "p1")) else []
                cqn = [0]

                def attend(qsrc_fn, qfull, qbuf, n, ktiles):
                    its = [(kt_i, kt, hh) for kt_i, kt in enumerate(ktiles) for hh in range(2)]
                    base = cnt[0]
                    cnt[0] += len(its)
                    pg = [po[(2 * grp[0]) % 3], po[(2 * grp[0] + 1) % 3]]
                    grp[0] += 1

                    def emit_s(m):
                        kt_i, kt, hh = its[m]
                        k = (base + m) % NPS
                        kp = (base + m) % NPT
                        S.op("pe", lambda e: e.matmul(ps_s[k][:, 0:n], lhsT=kTz[:, hh, kt * P:(kt + 1) * P], rhs=qfull,
                                                      start=True, stop=True), r=[kTz, qbuf], w=[ps_s[k]])
                        S.op("act", lambda e: e.activation(pT[kp][:, 0:n], ps_s[k][:, 0:n], AF.Exp, scale=0.125),
                             r=[ps_s[k]], w=[pT[kp]])

                    def emit_pv(m):
                        kt_i, kt, hh = its[m]
                        kp = (base + m) % NPT
                        S.op("pe", lambda e: e.matmul(pg[hh][:, 0:n], lhsT=Vp[:, kt, hh * 64:hh * 64 + 128], rhs=pT[kp][:, 0:n],
                                                      start=(kt_i == 0), stop=(kt_i == len(ktiles) - 1)), r=[M["Vp_b"][kt], pT[kp]], w=[pg[hh]])

                    npair = len(its) // 2
                    for j in range(npair + LA):
                        if j < npair:
                            emit_s(2 * j)
                            emit_s(2 * j + 1)
                        if j - LA >= 0:
                            emit_pv(2 * (j - LA))
                            emit_pv(2 * (j - LA) + 1)
                        cqn[0] += 1
                        if cqn[0] % 4 != 3:
                            emit(cq, 1)
                    for hh in range(2):
                        dp = 64 if hh == 0 else 0
                        S.op("dve", lambda e, hh=hh, dp=dp: e.reciprocal(rd[dp:dp + 1, 0:n], pg[hh][dp:dp + 1, 0:n]),
                             r=[pg[hh]], w=[rd])
                        S.op("pe", lambda e, dp=dp: e.matmul(pbc[0:64, 0:n], lhsT=ones_f[dp:dp + 1, 0:64], rhs=rd[dp:dp + 1, 0:n],
                                                            start=True, stop=True), r=[ones_f, rd], w=[pbc])
                        S.op("act", lambda e, hh=hh: e.activation(bcs[hh * 64:(hh + 1) * 64, 0:n], pbc[0:64, 0:n], AF.Copy),
                             r=[pbc], w=[bcs])
                        S.op("dve", lambda e, hh=hh: e.tensor_tensor(qsrc_fn(hh), pg[hh][hh * 64:(hh + 1) * 64, 0:n],
                                                                     bcs[hh * 64:(hh + 1) * 64, 0:n], op=ALU.mult),
                             r=[pg[hh], bcs], w=[qbuf])

                for c in range(8):
                    for pr in range(4):
                        attend(lambda hh, c=c, pr=pr: qT[hh * 64:(hh + 1) * 64, pr, c * 512:(c + 1) * 512],
                               qT[:, pr, c * 512:(c + 1) * 512], M["qT_b"][pr][c], 512, list(range(NT)))
                if not last:
                    for pr in range(4):
                        attend(lambda hh, pr=pr: qTc[hh * 64:(hh + 1) * 64, pr, :], qTc[:, pr, :], qTc.b, S_CTX, [NTL, NTL + 1])
                emit(cq, len(cq))
                S.barrier()

        def phase_c(l, M, tiles, src_aps, r_, modbc):
            with ExitStack() as ph:
                wcol = sbt(ph, "wcol", [P, 4, 31], F32)
                DG = sbt(ph, "DG", [P, 4, 31, P], BF16)
                wo = sbt(ph, "wo", [P, 8, D], BF16)
                stg = [sbt(ph, f"stgC{k}", [P, D], F32) for k in range(2)]
                stg_slot = [DmaSlot(S, f"stgC{k}") for k in range(2)]
                betaA = sbt(ph, "betaA", [P, 8], F32)
                betac = sbt(ph, "betac", [P, 8], F32)
                cb_bc = sbt(ph, "cb_bc", [P, 512], F32)
                lg_bc = sbt(ph, "lg_bc", [P, 512], F32)
                lb_bc = sbt(ph, "lb_bc", [P, 512], F32)
                xt = [sbt(ph, f"xtC{k}", [P, D], F32) for k in range(2)]
                xt_slot = [DmaSlot(S, f"xtC{k}") for k in range(2)]
                y = sbt(ph, "yC", [P, 512], F32)
                junk = sbt(ph, "junkC", [P, 512], F32)
                st4 = sbt(ph, "st4", [P, 4], F32)
                rln = sbt(ph, "rln", [P, 1], F32)
                msq = sbt(ph, "msq", [P, 1], F32)
                z = sbt(ph, "zC", [P, 512], F32)
                oc = sbt(ph, "oc", [P, 512], F32)
                ocb = sbt(ph, "ocb", [P, 512], BF16)
                ocT = sbt(ph, "ocT", [P, 4, P], BF16)
                sqo = sbt(ph, "sqo", [P, 4, P], F32)
                ssc = sbt(ph, "ssc", [P, 2], F32)
                rsc = sbt(ph, "rsc", [P, 2], F32)
                t1 = sbt(ph, "t1", [P, D], F32)
                x1 = [sbt(ph, f"x1_{k}", [P, D], F32) for k in range(2)]
                x1_slot = [DmaSlot(S, f"x1s{k}") for k in range(2)]
                py2 = [pst(ph, f"py{k}", [P, 512], F32) for k in range(2)]
                ptc = pst(ph, "ptc", [P, 4, P], BF16)
                pss = pst(ph, "pss", [P, 16], F32)
                pa = [pst(ph, f"pa{k}", [P, 512], F32) for k in range(2)]
                pc = [pst(ph, f"pc{k}", [P, 512], F32) for k in range(2)]
                qT, qTc, gT, gTc = M["qT"], M["qTc"], M["gT"], M["gTc"]

                vs = DmaSlot(S, "vecC", group=True)
                for c in range(4):
                    S.dma("sp", lambda e, c=c: e.dma_start(out=wcol[:, c, :], in_=conv_w_d[l, :, c * P:(c + 1) * P].rearrange("j p -> p j"),
                                                           allow_slow_non_contiguous=True), vs, w=[wcol])
                for half in range(2):
                    S.dma("sp", lambda e, half=half: e.dma_start(out=betaA[half * 64:(half + 1) * 64, :],
                                                                 in_=beta_attn_d[l].rearrange("(h d) -> d h", d=64),
                                                                 allow_slow_non_contiguous=True), vs, w=[betaA])
                S.dma("sp", lambda e: e.dma_start(out=betac[:, 4:8], in_=beta_conv_d[l].rearrange("(c p) -> p c", p=P),
                                                  allow_slow_non_contiguous=True), vs, w=[betac])
                S.dma("sp", lambda e: e.dma_start(out=cb_bc[:], in_=conv_b_d[l].partition_broadcast(P)), vs, w=[cb_bc])
                S.dma("sp", lambda e: e.dma_start(out=lg_bc[:], in_=ln_g_d[l].partition_broadcast(P)), vs, w=[lg_bc])
                S.dma("sp", lambda e: e.dma_start(out=lb_bc[:], in_=ln_b_d[l].partition_broadcast(P)), vs, w=[lb_bc])
                vs.close()
                S.op("dve", lambda e: e.tensor_copy(betac[0:64, 0:4], betaA[0:64, 0:4]), r=[betaA], w=[betac])
                S.op("dve", lambda e: e.tensor_copy(betac[64:128, 0:4], betaA[64:128, 4:8]), r=[betaA], w=[betac])
                for kk in range(8):
                    t = stg[kk % 2]
                    sl = stg_slot[kk % 2]
                    if kk < 4:
                        S.dma("sp", lambda e, t=t, kk=kk: e.dma_start(out=t[0:64, :], in_=w_o_d[l, kk * 64:(kk + 1) * 64, :]), sl, w=[t])
                        S.dma("sp", lambda e, t=t, kk=kk: e.dma_start(out=t[64:128, :], in_=w_o_d[l, (kk + 4) * 64:(kk + 5) * 64, :]), sl, w=[t])
                    else:
                        S.dma("sp", lambda e, t=t, kk=kk: e.dma_start(out=t[:], in_=w_o_d[l, 512 + (kk - 4) * P:512 + (kk - 3) * P, :]), sl, w=[t])
                    S.op("act", lambda e, t=t, kk=kk: e.activation(wo[:, kk, :], t[:], AF.Identity, scale=betac[:, kk:kk + 1]),
                         r=[t, betac], w=[wo])
                for c in range(4):
                    for j in range(31):
                        S.op("pool", lambda e, c=c, j=j: e.tensor_scalar(DG[:, c, j, :], ident_f[:], wcol[:, c, j:j + 1], None, op0=ALU.mult),
                             r=[ident_f, wcol], w=[DG])

                def load(n_):
                    i = tiles[n_]
                    k = n_ % 2
                    S.dma("sp", lambda e: e.dma_start(out=xt[k][:], in_=src_aps[i]), xt_slot[k], r=[xs_buf[i]], w=[xt[k]])

                def conv(n_):
                    i = tiles[n_]
                    pyc = py2[n_ % 2]
                    if i < NTL:
                        gsrc, gb, t0 = gT, M["gT_b"], i * P
                        lo, hi = max(0, i - 1), min(NTL - 1, i + 1)
                    else:
                        gsrc, gb, t0 = gTc, M["gTc_b"], (i - NTL) * P
                        lo, hi = max(0, i - NTL - 1), min(NTC - 1, i - NTL + 1)
                    for c in range(4):
                        for j in range(31):
                            S.op("pe", lambda e, c=c, j=j: e.matmul(pyc[:, c * P:(c + 1) * P],
                                                                  lhsT=gsrc[:, c, t0 + j + GPAD - 15:t0 + j + GPAD - 15 + P],
                                                                  rhs=DG[:, c, j, :], start=(j == 0), stop=(j == 30)),
                                 r=[gb[q] for q in range(lo, hi + 1)] + [DG], w=[pyc])

                load(0)
                conv(0)
                for n_, i in enumerate(tiles):
                    k = n_ % 2
                    is_ctx = i >= NTL
                    if i == NTL:
                        load_modbc(1, modbc)
                    if n_ + 1 < len(tiles):
                        load(n_ + 1)
                    if not is_ctx:
                        osrc = lambda pr: qT[:, pr, i * P:(i + 1) * P]
                        obufs = [M["qT_b"][pr][i // 4] for pr in range(4)]
                    else:
                        osrc = lambda pr: qTc[:, pr, (i - NTL) * P:(i - NTL + 1) * P]
                        obufs = [qTc.b]
                    if n_ + 1 < len(tiles):
                        conv(n_ + 1)
                    py = py2[n_ % 2]
                    S.op("dve", lambda e: e.tensor_tensor(y[:], py[:], cb_bc[:], op=ALU.add), r=[py, cb_bc], w=[y])
                    S.op("act", lambda e: e.activation(junk[:], y[:], AF.Identity, accum_out=st4[:, 0:1]), r=[y], w=[junk, st4])
                    S.op("act", lambda e: e.activation(junk[:], y[:], AF.Square, accum_out=st4[:, 1:2]), r=[y], w=[junk, st4])
                    S.op("dve", lambda e: e.tensor_scalar(st4[:, 2:4], st4[:, 0:2], 1.0 / 512, None, op0=ALU.mult), r=[st4], w=[st4])
                    S.op("dve", lambda e: e.tensor_tensor(msq[:], st4[:, 2:3], st4[:, 2:3], op=ALU.mult), r=[st4], w=[msq])
                    S.op("dve", lambda e: e.tensor_tensor(msq[:], st4[:, 3:4], msq[:], op=ALU.subtract), r=[st4, msq], w=[msq])
                    rstd_from_ss(msq, rln, 1.0, eps5)
                    S.op("dve", lambda e: e.tensor_scalar(z[:], y[:], st4[:, 2:3], rln[:, 0:1], op0=ALU.subtract, op1=ALU.mult),
                         r=[y, st4, rln], w=[z])
                    S.op("pool", lambda e: e.tensor_tensor(z[:], z[:], lg_bc[:], op=ALU.mult), r=[z, lg_bc], w=[z])
                    S.op("pool", lambda e: e.tensor_tensor(z[:], z[:], lb_bc[:], op=ALU.add), r=[z, lb_bc], w=[z])
                    S.op("act", lambda e: e.activation(oc[:], z[:], AF.Silu), r=[z], w=[oc])
                    S.op("act", lambda e: e.activation(junk[:], oc[:], AF.Square, accum_out=ssc[:, 0:1]), r=[oc], w=[junk, ssc])
                    S.op("pool", lambda e: e.tensor_copy(ocb[:], oc[:]), r=[oc], w=[ocb])
                    for c in range(4):
                        S.op("pe", lambda e, c=c: e.transpose(ptc[:, c, :], ocb[:, c * P:(c + 1) * P], ident_b[:]),
                             r=[ocb, ident_b], w=[ptc])
                    S.op("act", lambda e: e.activation(ocT[:], ptc[:], AF.Copy), r=[ptc], w=[ocT])
                    for pr in range(4):
                        S.op("pool", lambda e, pr=pr: e.tensor_tensor(sqo[:, pr, :], osrc(pr), osrc(pr), op=ALU.mult), r=obufs, w=[sqo])
                    for pr in range(4):
                        S.op("pe", lambda e, pr=pr: e.matmul(pss[:, 0:2], lhsT=sqo[:, pr, :], rhs=ones_f[:, 0:2],
                                                            start=(pr == 0), stop=(pr == 3)), r=[sqo, ones_f], w=[pss])
                    S.op("dve", lambda e: e.tensor_copy(ssc[:, 1:2], pss[:, 0:1]), r=[pss], w=[ssc])
                    rstd_from_ss(ssc, rsc, 1.0 / 512, eps6)
                    for cc in range(2):
                        for pr in range(4):
                            S.op("pe", lambda e, cc=cc, pr=pr: e.matmul(pa[cc][:], lhsT=osrc(pr), rhs=wo[:, pr, cc * 512:(cc + 1) * 512],
                                                                      start=(pr == 0), stop=(pr == 3)), r=obufs + [wo], w=[pa[cc]])
                        for c in range(4):
                            S.op("pe", lambda e, cc=cc, c=c: e.matmul(pc[cc][:], lhsT=ocT[:, c, :], rhs=wo[:, 4 + c, cc * 512:(cc + 1) * 512],
                                                                    start=(c == 0), stop=(c == 3)), r=[ocT, wo], w=[pc[cc]])
                        sl = slice(cc * 512, (cc + 1) * 512)
                        S.op("dve", lambda e, cc=cc, sl=sl: e.tensor_scalar(t1[:, sl], pa[cc][:], rsc[:, 1:2], None, op0=ALU.mult),
                             r=[pa[cc], rsc], w=[t1])
                        S.op("dve", lambda e, cc=cc, sl=sl: e.scalar_tensor_tensor(out=t1[:, sl], in0=pc[cc][:], scalar=rsc[:, 0:1],
                                                                                   in1=t1[:, sl], op0=ALU.mult, op1=ALU.add),
                             r=[pc[cc], rsc, t1], w=[t1])
                    S.op("pool", lambda e: e.tensor_tensor(t1[:], t1[:], modbc[:, 0:1024], op=ALU.mult), r=[t1, modbc], w=[t1])
                    S.op("pool", lambda e: e.tensor_tensor(x1[k][:], t1[:], xt[k][:], op=ALU.add), r=[t1, xt[k]], w=[x1[k]])
                    S.dma("sp", lambda e: e.dma_start(out=xs_d[i * P:(i + 1) * P, :], in_=x1[k][:]), x1_slot[k], r=[x1[k]], w=[xs_buf[i]])
                    if l == 0:
                        dbg_store("d_x1", slice(i * P, (i + 1) * P), x1[k][:], [x1[k]])
                S.barrier()

        def phase_p(l, tiles, modbc, last):
            with ExitStack() as ph:
                wq = sbt(ph, "wq", [P, 8, D], F32)
                keysTz = sbt(ph, "keysTz", [P, 8, 2, P], F32)
                iota16 = sbt(ph, "iota16", [P, 16], F32)
                xt = [sbt(ph, f"xtP{k}", [P, D], F32) for k in range(2)]
                xt_slot = [DmaSlot(S, f"xtP{k}") for k in range(2)]
                junk = sbt(ph, "junkP", [P, D], BF16)
                junkr = sbt(ph, "junkR", [P, D], BF16)
                prod = [sbt(ph, f"prod{k}", [P, D], BF16) for k in range(3)]
                hb = [sbt(ph, f"hb{k}", [P, D], BF16) for k in range(2)]
                pi = [0]
                ss = sbt(ph, "ssP", [P, 1], F32)
                rstd = sbt(ph, "rstdP", [P, 1], F32)
                ssf = sbt(ph, "ssF", [P, 1], F32)
                rstdf = sbt(ph, "rstdF", [P, 1], F32)
                h = [sbt(ph, "hP", [P, D], F32)] * 2
                hT = sbt(ph, "hT", [P, 8, P], F32)
                qpT = hT
                qp = sbt(ph, "qp", [P, D], F32)
                sc = sbt(ph, "scP", [P, 16, 128], F32)
                wk = sbt(ph, "wkP", [P, 16, 128], F32)
                eq_ap = wk[:].rearrange("p a b -> p (a b)").rearrange("p (x c) -> p x c", c=16)
                topv = sbt(ph, "topv", [P, 16, 16], F32)
                idx = sbt(ph, "idxP", [P, 16, 16], U32)
                idxf = sbt(ph, "idxf", [P, 16, 16], F32)
                cand = sbt(ph, "cand", [P, 8, 256], F32)
                wk2_ap = sc[:].rearrange("p a b -> p (a b)").rearrange("p (h x) -> p h x", h=8)
                best = sbt(ph, "best", [P, 8, 16], F32)
                pos = sbt(ph, "pos", [P, 8, 16], U32)
                pab = sbt(ph, "pab", [P, 2, 128], U32)
                abf = sbt(ph, "abf", [P, 2, 128], F32)
                isel = sbt(ph, "isel", [P, 2, 128], F32)
                ef = [sbt(ph, f"ef{k}", [P, 128], F32) for k in range(2)]
                eidx = [sbt(ph, f"eidx{k}", [P, 128], I32) for k in range(2)]
                ew = sbt(ph, "ew", [P, 128], F32)
                se = sbt(ph, "se", [P, 8], F32)
                wgt = [sbt(ph, f"wgt{k}", [P, 128], F32) for k in range(2)]
                pre = sbt(ph, "pre", [P, 128], F32)
                aw = sbt(ph, "aw", [P, 128], F32)
                gbuf = [sbt(ph, f"gbuf{k}", [P, 2 * D], BF16) for k in range(NSLOT)]
                diag = [sbt(ph, f"diag{k}", [P, P], BF16) for k in range(6)]
                pre_b = [Buf(f"pre_b{k}") for k in range(4)]
                pre_d = [Buf(f"pre_d{k}") for k in range(4)]
                di = [0]
                gslot = [DmaSlot(S, f"g{k}") for k in range(NSLOT)]
                x2 = [sbt(ph, "x2_0", [P, D], F32)] * 2
                x2_slot = [DmaSlot(S, f"x2s{k}") for k in range(2)]
                fg_bc = sbt(ph, "fg_bc", [P, D], F32)
                pT2 = pst(ph, "pT2", [P, 8, P], F32)
                pqp = [pst(ph, f"pqp{k}", [P, 512], F32) for k in range(2)]
                pcs = [pst(ph, f"pcs{k}", [P, 512], F32) for k in range(2)]
                pva = [pst(ph, f"pva{k}", [P, 512], F32) for k in range(2)]
                psc = pqp + pcs
                print(f"[build] phase_p sbuf remaining {nc.sbuf_bytes_remaining}")

                S.op("pool", lambda e: e.iota(iota16[:], pattern=[[1, 16]], base=0, channel_multiplier=0,
                                              allow_small_or_imprecise_dtypes=True), w=[iota16])
                ws = DmaSlot(S, "wqload", group=True)
                for j in range(8):
                    S.dma("sp", lambda e, j=j: e.dma_start(out=wq[:, j, :], in_=peer_wq_d[l, j * P:(j + 1) * P, :]), ws, w=[wq])
                kraw_ap = sc[:].rearrange("p a b -> p (a b)")[:, 0:1024].rearrange("p (a d) -> p a d", d=64)
                keysT_ap = cand[:].rearrange("p a b -> p (a b)")[:, 0:1024].rearrange("p (a k) -> p a k", k=P)
                S.dma("sp", lambda e: e.dma_start(out=kraw_ap, in_=peer_keys_d[l].rearrange("h c k d -> k (h c) d")), ws, w=[sc])
                if last:
                    S.dma("sp", lambda e: e.dma_start(out=fg_bc[:], in_=final_g_d[0].partition_broadcast(P)), ws, w=[fg_bc])
                ws.close()
                for hh in range(8):
                    S.op("pe", lambda e, hh=hh: e.transpose(pT2[:, hh, :], kraw_ap[:, 2 * hh:2 * hh + 2, :].rearrange("p c d -> p (c d)"), ident_f[:]),
                         r=[sc, ident_f], w=[pT2])
                S.op("act", lambda e: e.activation(keysT_ap, pT2[:], AF.Copy), r=[pT2], w=[cand])
                S.op("pool", lambda e: e.memset(keysTz[:], 0.0), w=[keysTz])
                S.op("dve", lambda e: e.tensor_copy(keysTz[0:64, :, 0, :], keysT_ap[0:64, :, :]), r=[cand], w=[keysTz])
                S.op("dve", lambda e: e.tensor_copy(keysTz[64:128, :, 1, :], keysT_ap[64:128, :, :]), r=[cand], w=[keysTz])

                gi = [0]

                def routing(n_):
                    rq = []
                    i = tiles[n_]
                    k = n_ % 2
                    xk, hk, efk, eik, wgk = xt[k], h[k], ef[k], eidx[k], wgt[k]
                    hbk = hb[k]

                    def op(eng, fn, r=(), w=()):
                        rq.append(("op", eng, fn, r, w))

                    rq.append(("dma", "sp", lambda e: e.dma_start(out=xk[:], in_=xs_d[i * P:(i + 1) * P, :]), xt_slot[k], [xs_buf[i]], [xk]))
                    op("act", lambda e: e.activation(junkr[:], xk[:], AF.Square, accum_out=ss[:]), r=[xk], w=[junkr, ss])
                    op("act", lambda e: e.activation(rstd[:], ss[:], AF.Sqrt, scale=1.0 / D, bias=eps6[:, 0:1]), r=[ss, eps6], w=[rstd])
                    op("dve", lambda e: e.reciprocal(rstd[:], rstd[:]), r=[rstd], w=[rstd])
                    op("dve", lambda e: e.scalar_tensor_tensor(out=hk[:], in0=xk[:], scalar=rstd[:, 0:1], in1=modbc[:, 2048:3072],
                                                               op0=ALU.mult, op1=ALU.mult), r=[xk, rstd, modbc], w=[hk])
                    op("dve", lambda e: e.tensor_tensor(hk[:], hk[:], modbc[:, 1024:2048], op=ALU.add), r=[hk, modbc], w=[hk])
                    op("act", lambda e: e.activation(hbk[:], hk[:], AF.Copy), r=[hk], w=[hbk])
                    for j in range(8):
                        op("pe", lambda e, j=j: e.transpose(pT2[:, j, :], hk[:, j * P:(j + 1) * P], ident_f[:]), r=[hk, ident_f], w=[pT2])
                    op("act", lambda e: e.activation(hT[:], pT2[:], AF.Copy), r=[pT2], w=[hT])
                    for cc in range(2):
                        for j in range(8):
                            op("pe", lambda e, cc=cc, j=j: e.matmul(pqp[cc][:], lhsT=hT[:, j, :], rhs=wq[:, j, cc * 512:(cc + 1) * 512],
                                                                  start=(j == 0), stop=(j == 7)), r=[hT, wq], w=[pqp[cc]])
                        op("act", lambda e, cc=cc: e.activation(qp[:, cc * 512:(cc + 1) * 512], pqp[cc][:], AF.Copy), r=[pqp[cc]], w=[qp])
                    for hh in range(8):
                        op("pe", lambda e, hh=hh: e.transpose(pT2[:, hh, :], qp[:, hh * P:(hh + 1) * P], ident_f[:]), r=[qp, ident_f], w=[pT2])
                    op("act", lambda e: e.activation(qpT[:], pT2[:], AF.Copy), r=[pT2], w=[qpT])
                    for hh in range(8):
                        pv = psc[hh // 2][:].rearrange("p (a k) -> p a k", k=P)
                        op("pe", lambda e, hh=hh, pv=pv: e.matmul(pv[:, (hh % 2) * 2:(hh % 2) * 2 + 2, :], lhsT=qpT[:, hh, :],
                                                                 rhs=keysTz[:, hh, :, :], start=True, stop=True),
                           r=[qpT, keysTz], w=[psc[hh // 2]])
                    for q in range(4):
                        op("act", lambda e, q=q: e.activation(sc[:, 4 * q:4 * q + 4, :], psc[q][:].rearrange("p (a k) -> p a k", k=P), AF.Copy),
                           r=[psc[q]], w=[sc])
                    for g in range(16):
                        op("dve", lambda e, g=g: e.max(out=topv[:, g, 0:8], in_=sc[:, g, :]), r=[sc], w=[topv])
                        op("dve", lambda e, g=g: e.max_index(out=idx[:, g, 0:8], in_max=topv[:, g, 0:8], in_values=sc[:, g, :]),
                           r=[sc, topv], w=[idx])
                        op("dve", lambda e, g=g: e.match_replace(out=wk[:, g, :], in_to_replace=topv[:, g, 0:8], in_values=sc[:, g, :],
                                                                 imm_value=-1e30), r=[sc, topv], w=[wk])
                        op("dve", lambda e, g=g: e.max(out=topv[:, g, 8:16], in_=wk[:, g, :]), r=[wk], w=[topv])
                        op("dve", lambda e, g=g: e.max_index(out=idx[:, g, 8:16], in_max=topv[:, g, 8:16], in_values=wk[:, g, :]),
                           r=[wk, topv], w=[idx])
                    op("dve", lambda e: e.tensor_copy(idxf[:], idx[:]), r=[idx], w=[idxf])
                    tv4 = topv[:].rearrange("p (h c) a -> p h c a", c=2)
                    if4 = idxf[:].rearrange("p (h c) a -> p h c a", c=2)
                    op("dve", lambda e: e.tensor_tensor(cand[:].rearrange("p h (a b) -> p h a b", a=16),
                                                        tv4[:, :, 0, :].unsqueeze(3).to_broadcast([P, 8, 16, 16]),
                                                        tv4[:, :, 1, :].unsqueeze(2).to_broadcast([P, 8, 16, 16]), op=ALU.add),
                       r=[topv], w=[cand])
                    for hh in range(8):
                        op("dve", lambda e, hh=hh: e.max(out=best[:, hh, 0:8], in_=cand[:, hh, :]), r=[cand], w=[best])
                        op("dve", lambda e, hh=hh: e.max_index(out=pos[:, hh, 0:8], in_max=best[:, hh, 0:8], in_values=cand[:, hh, :]),
                           r=[cand, best], w=[pos])
                        op("dve", lambda e, hh=hh: e.match_replace(out=wk2_ap[:, hh, :], in_to_replace=best[:, hh, 0:8],
                                                                   in_values=cand[:, hh, :], imm_value=-1e30), r=[cand, best], w=[sc])
                        op("dve", lambda e, hh=hh: e.max(out=best[:, hh, 8:16], in_=wk2_ap[:, hh, :]), r=[sc], w=[best])
                        op("dve", lambda e, hh=hh: e.max_index(out=pos[:, hh, 8:16], in_max=best[:, hh, 8:16], in_values=wk2_ap[:, hh, :]),
                           r=[sc, best], w=[pos])
                    posf = pos[:].rearrange("p h s -> p (h s)")
                    op("dve", lambda e: e.tensor_single_scalar(pab[:, 0, :], posf, 4, op=ALU.logical_shift_right), r=[pos], w=[pab])
                    op("dve", lambda e: e.tensor_single_scalar(pab[:, 1, :], posf, 15, op=ALU.bitwise_and), r=[pos], w=[pab])
                    op("dve", lambda e: e.tensor_copy(abf[:], pab[:]), r=[pab], w=[abf])
                    for c in range(2):
                        op("dve", lambda e, c=c: e.tensor_tensor(eq_ap, iota16[:].unsqueeze(1).to_broadcast([P, 128, 16]), abf[:, c, :].unsqueeze(2).to_broadcast([P, 128, 16]),
                                                                 op=ALU.is_equal), r=[iota16, abf], w=[wk])
                        op("dve", lambda e, c=c: e.tensor_tensor(eq_ap.rearrange("p (h s) a -> p h s a", h=8),
                                                                 eq_ap.rearrange("p (h s) a -> p h s a", h=8),
                                                                 if4[:, :, c, :].unsqueeze(2).to_broadcast([P, 8, 16, 16]), op=ALU.mult),
                           r=[wk, idxf], w=[wk])
                        op("dve", lambda e, c=c: e.tensor_reduce(out=isel[:, c, :], in_=eq_ap, axis=AX.X, op=ALU.add), r=[wk], w=[isel])
                    op("dve", lambda e: e.scalar_tensor_tensor(out=efk[:], in0=isel[:, 0, :], scalar=128.0, in1=isel[:, 1, :],
                                                               op0=ALU.mult, op1=ALU.add), r=[isel], w=[efk])
                    op("dve", lambda e: e.tensor_scalar(efk[:], efk[:], float(l * NPEER), None, op0=ALU.add), r=[efk], w=[efk])
                    op("dve", lambda e: e.tensor_copy(eik[:], efk[:]), r=[efk], w=[eik])
                    op("dve", lambda e: e.tensor_tensor(ew[:].rearrange("p (h s) -> p h s", h=8), best[:],
                                                        best[:, :, 0:1].to_broadcast([P, 8, 16]), op=ALU.subtract), r=[best], w=[ew])
                    op("act", lambda e: e.activation(ew[:], ew[:], AF.Exp), r=[ew], w=[ew])
                    op("dve", lambda e: e.tensor_reduce(out=se[:], in_=ew[:].rearrange("p (h s) -> p h s", h=8), axis=AX.X, op=ALU.add),
                       r=[ew], w=[se])
                    op("dve", lambda e: e.reciprocal(se[:], se[:]), r=[se], w=[se])
                    op("dve", lambda e: e.tensor_tensor(wgk[:].rearrange("p (h s) -> p h s", h=8), ew[:].rearrange("p (h s) -> p h s", h=8),
                                                        se[:].unsqueeze(2).to_broadcast([P, 8, 16]), op=ALU.mult), r=[ew, se], w=[wgk])
                    return rq

                rq = routing(0)
                emit(rq, len(rq))
                for n_, i in enumerate(tiles):
                    k = n_ % 2
                    hbk, efk, eik, wgk = hb[k], ef[k], eidx[k], wgt[k]
                    rq = routing(n_ + 1) if n_ + 1 < len(tiles) else []
                    per_slot = -(-len(rq) // 112)
                    NG = 128 // GRP
                    gb = {}
                    for g_ in range(NG + 1):
                        if g_ < NG:
                            pb_ = pre_b[g_ % 4]
                            pd_ = pre_d[g_ % 4]
                            for s_ in range(g_ * GRP, (g_ + 1) * GRP):
                                b_ = gi[0] % NSLOT
                                gi[0] += 1
                                gb[s_] = b_
                                S.dma("pool", lambda e, s_=s_, b_=b_: e.indirect_dma_start(
                                    out=gbuf[b_][:], out_offset=None, in_=uv_d,
                                    in_offset=bass.IndirectOffsetOnAxis(ap=eik[:, s_:s_ + 1], axis=0)), gslot[b_], r=[eik], w=[gbuf[b_]])
                                pr_ = prod[pi[0] % len(prod)]
                                pi[0] += 1
                                S.op("dve", lambda e, b_=b_, pr_=pr_: e.tensor_tensor(pr_[:], hbk[:], gbuf[b_][:, 0:D], op=ALU.mult),
                                     r=[hbk, gbuf[b_]], w=[pr_])
                                S.op("act", lambda e, s_=s_, pr_=pr_: e.activation(junkr[:], pr_[:], AF.Identity, accum_out=pre[:, s_:s_ + 1]),
                                     r=[pr_], w=[junkr, pb_])
                                emit(rq, per_slot)
                            gs = slice(g_ * GRP, (g_ + 1) * GRP)
                            S.op("act", lambda e, gs=gs: e.activation(aw[:, gs], pre[:, gs], AF.Gelu), r=[pb_, pd_], w=[pb_])
                        if g_ >= 1:
                            gq = g_ - 1
                            pbq = pre_b[gq % 4]
                            for s_ in range(gq * GRP, (gq + 1) * GRP):
                                b_ = gb[s_]
                                dg = diag[di[0] % len(diag)]
                                di[0] += 1
                                S.op("dve", lambda e, s_=s_, dg=dg: e.tensor_scalar(dg[:], ident_b[:], aw[:, s_:s_ + 1], wgk[:, s_:s_ + 1],
                                                                                   op0=ALU.mult, op1=ALU.mult),
                                     r=[ident_b, pbq, wgk], w=[dg])
                                for cc in range(2):
                                    S.op("pe", lambda e, s_=s_, b_=b_, dg=dg, cc=cc: e.matmul(
                                        pva[cc][:], lhsT=dg[:], rhs=gbuf[b_][:, D + cc * 512:D + (cc + 1) * 512],
                                        start=(s_ == 0), stop=(s_ == 127)), r=[dg, gbuf[b_]], w=[pva[cc]])
                    if l == 0:
                        dbg_store("d_pre", slice(i * P, (i + 1) * P), pre[:], pre_b + pre_d)
                        dbg_store("d_eidx", slice(i * P, (i + 1) * P), efk[:], [efk])
                        dbg_store("d_w", slice(i * P, (i + 1) * P), wgk[:], [wgk])
                    for cc in range(2):
                        sl = slice(cc * 512, (cc + 1) * 512)
                        S.op("dve", lambda e, cc=cc, sl=sl: e.tensor_tensor(x2[k][:, sl], pva[cc][:], modbc[:, 3072 + cc * 512:3072 + (cc + 1) * 512],
                                                                           op=ALU.mult), r=[pva[cc], modbc], w=[x2[k]])
                    S.op("dve", lambda e: e.tensor_tensor(x2[k][:], x2[k][:], xt[k][:], op=ALU.add), r=[x2[k], xt[k]], w=[x2[k]])
                    if l == 0:
                        dbg_store("d_x2", slice(i * P, (i + 1) * P), x2[k][:], [x2[k]])
                    if last:
                        S.op("act", lambda e: e.activation(junk[:], x2[k][:], AF.Square, accum_out=ssf[:]), r=[x2[k]], w=[junk, ssf])
                        rstd_from_ss(ssf, rstdf, 1.0 / D, eps6)
                        S.op("dve", lambda e: e.scalar_tensor_tensor(out=x2[k][:], in0=x2[k][:], scalar=rstdf[:, 0:1], in1=fg_bc[:],
                                                                     op0=ALU.mult, op1=ALU.mult), r=[x2[k], rstdf, fg_bc], w=[x2[k]])
                        S.dma("sp", lambda e: e.dma_start(out=out_d[i * P:(i + 1) * P, :], in_=x2[k][:]), x2_slot[k], r=[x2[k]])
                    else:
                        S.dma("sp", lambda e: e.dma_start(out=xs_d[i * P:(i + 1) * P, :], in_=x2[k][:]), x2_slot[k], r=[x2[k]], w=[xs_buf[i]])
                    emit(rq, len(rq))
                S.barrier()

        def load_modbc(r_, modbc):
            S.dma("sp", lambda e: e.dma_start(out=modbc[:], in_=modscr_d[r_]), DmaSlot(S, "modld"), w=[modbc])

        lat_tiles = list(range(NTL))
        ctx_tiles = list(range(NTL, NT))
        for l in range(depth):
            last = (l == DEPTH - 1)
            if l == 0:
                src_aps = [x_d[i * P:(i + 1) * P, :] for i in range(NTL)] + [ctx_d[i * P:(i + 1) * P, :] for i in range(NTC)]
            else:
                src_aps = [xs_d[i * P:(i + 1) * P, :] for i in range(NT)]
            mod_cols(l)
            if l == 0:
                dbg_store("d_mod", (slice(0, P), slice(0, 32)), modcol[:].rearrange("p r c -> p (r c)"), [modcol])
                dbg_store("d_mod", (slice(0, P), slice(32, 48)), scT[:].rearrange("p j r -> p (j r)"), [scT])
            if stop == "m":
                break
            with ExitStack() as mix:
                M = {}
                M["qT"] = sbt(mix, "qT", [P, 4, S_LAT], BF16)
                M["qT_b"] = [[Buf(f"qT{pr}_{c}") for c in range(8)] for pr in range(4)]
                M["qTc"] = sbt(mix, "qTc", [P, 4, S_CTX], BF16)
                M["gT"] = sbt(mix, "gT", [P, 4, S_LAT + 2 * GPAD], BF16)
                M["gT_b"] = [Buf(f"gT{i}") for i in range(NTL)]
                M["gTc"] = sbt(mix, "gTc", [P, 4, S_CTX + 2 * GPAD], BF16)
                M["gTc_b"] = [Buf(f"gTc{i}") for i in range(NTC)]
                S.op("pool", lambda e: e.memset(M["gT"][:], 0.0), w=M["gT_b"])
                S.op("pool", lambda e: e.memset(M["gTc"][:], 0.0), w=M["gTc_b"])
                with ExitStack() as ab:
                    M["kT"] = sbt(ab, "kT", [P, NT * P], BF16)
                    M["kT_b"] = [Buf(f"kT{i}") for i in range(NT)]
                    M["Vp"] = sbt(ab, "Vp", [P, NT, 192], BF16)
                    M["Vp_b"] = [Buf(f"Vp{i}") for i in range(NT)]
                    S.op("pool", lambda e: e.memset(M["Vp"][:], 1.0), w=M["Vp_b"])
                    phase_a(l, M, src_aps, last)
                    if stop in ("a", "a0", "a1"):
                        break
                    phase_b(l, M, last)
                if stop == "b":
                    break
                with ExitStack() as cs:
                    modbc = sbt(cs, "modbcC", [P, 4096], F32)
                    for r_ in ((0,) if last else (0, 1)):
                        mod_bc(l, r_, modbc)
                        S.dma("sp", lambda e, r_=r_: e.dma_start(out=modscr_d[r_], in_=modbc[:]), DmaSlot(S, "modst"), r=[modbc])
                        S.barrier()
                    load_modbc(0, modbc)
                    phase_c(l, M, lat_tiles + ([] if last else ctx_tiles), src_aps, 0, modbc)
            if stop == "c":
                break
            with ExitStack() as pp:
                modbc = sbt(pp, "modbcP", [P, 4096], F32)
                load_modbc(0, modbc)
                phase_p(l, lat_tiles if stop not in ("p1", "p0") else lat_tiles[:2], modbc, last)
                if not last and stop not in ("p1", "p0"):
                    load_modbc(1, modbc)
                    phase_p(l, ctx_tiles, modbc, last)
            if stop in ("p1", "p0"):
                break
        out_slot.close()
        if out_slot.sem is not None:
            for en in ("sp", "act", "pool"):
                S.E[en].eng.wait_ge(out_slot.sem, out_slot.val)
        S.barrier()
        print(f"[build] instr={S.ninstr} waits={S.nwait} sems={S.nsem}")
    return nc


_NC_CACHE = {}


def _rope_tables():
    n = 16
    inv = (10000.0 ** (-np.arange(n, dtype=np.float32) / n)).astype(np.float32)
    row = np.repeat(np.arange(64), 64).astype(np.float32)
    col = np.tile(np.arange(64), 64).astype(np.float32)
    ang = np.concatenate([row[:, None] * inv, col[:, None] * inv], axis=-1).astype(np.float32)
    cos = np.cos(ang).astype(np.float32)
    sin = np.sin(ang).astype(np.float32)
    cos2 = np.concatenate([cos, cos], axis=-1)
    sin2 = np.concatenate([-sin, sin], axis=-1)
    return np.ascontiguousarray(cos2), np.ascontiguousarray(sin2)


def make_in_maps(inputs, cores):
    f = lambda a: np.ascontiguousarray(np.asarray(a, dtype=np.float32))
    cos2, sin2 = _rope_tables()
    shared = {k: f(inputs[k]) for k in ["w_mod", "b_mod", "w_in", "q_gain", "k_gain", "conv_w", "conv_b", "ln_g", "ln_b",
                                        "beta_attn", "beta_conv", "w_o", "peer_wq", "peer_keys", "peer_u", "peer_v"]}
    shared["final_g"] = f(inputs["final_g"]).reshape(1, D)
    shared["cos2"] = cos2
    shared["sin2"] = sin2
    shared["cctxT"] = np.ascontiguousarray(f(inputs["c_ctx"]).reshape(8, P).T)
    x = f(inputs["x"])
    ctx = f(inputs["ctx"])
    c = f(inputs["c"])
    maps = []
    for b in cores:
        m = dict(shared)
        m["x"] = x[b]
        m["ctx"] = ctx[b]
        m["cT"] = np.ascontiguousarray(c[b].reshape(8, P).T)
        maps.append(m)
    return maps


def kernel(**inputs):
    if "nc" not in _NC_CACHE:
        _NC_CACHE["nc"] = build()
    nc = _NC_CACHE["nc"]
    B = np.asarray(inputs["x"]).shape[0]
    maps = make_in_maps(inputs, list(range(B)))
    res = run_bass_kernel_spmd(nc, maps, core_ids=list(range(B)))
    out = np.stack([np.asarray(r["out"], dtype=np.float32) for r in res.results], axis=0)
    return out
```
